# Optimizing a Trainium2 kernel written in Bass

```python
import jax, jax.numpy as jnp
from jax import lax
import numpy as np

D_MODEL = 1024
BATCH = 32
SEQ = 2048
DEPTH = 2

HEAD_DIM = 64
D_MIX = D_MODEL
GROUP_W = D_MIX // 4
GROUP_HEADS = GROUP_W // HEAD_DIM
N_MIX_HEADS = D_MIX // HEAD_DIM
NSA_KV_DIM = HEAD_DIM
ATTN_SCALE = HEAD_DIM ** -0.5
ROPE_THETA = 500000.0
ROT_DIM = HEAD_DIM // 4
HG_CHUNK = 64
HG_SUB = 16
Q_BLOCK = 128
NSA_CMP_LEN = 32
NSA_CMP_STRIDE = 16
NSA_CMP_HIDDEN = HEAD_DIM
NSA_SEL_BLOCK = 64
NSA_TOP_N = 16
NSA_WINDOW = 512
NSA_SEL_Q_BLOCK = 32
NSA_FORCE = 1.0e4
NEG_BIG = -1.0e30
EPS = 1e-6

IN_WIDTHS = (
    GROUP_W, GROUP_W, GROUP_W,
    GROUP_W, GROUP_W, GROUP_W, GROUP_HEADS,
    GROUP_W, GROUP_W, GROUP_W,
    GROUP_W,
    NSA_KV_DIM, NSA_KV_DIM,
    NSA_KV_DIM, NSA_KV_DIM,
    NSA_KV_DIM, NSA_KV_DIM,
    3 * GROUP_HEADS,
    D_MIX,
)
N_IN = sum(IN_WIDTHS)

kernel_name = "hymba_hgrn2_fox_stickbreak_nsa_trunk"


def _rmsnorm(x, g):
    xf = x.astype(jnp.float32)
    y = xf * lax.rsqrt(jnp.mean(xf * xf, axis=-1, keepdims=True) + EPS)
    return (y * g.astype(jnp.float32)).astype(x.dtype)


def _to_heads(a, n_heads):
    B, T, _ = a.shape
    return a.reshape(B, T, n_heads, -1).transpose(0, 2, 1, 3)


def _from_heads(a):
    B, H, T, d = a.shape
    return a.transpose(0, 2, 1, 3).reshape(B, T, H * d)


def _partial_rope(x, pos):
    half = ROT_DIM // 2
    inv_freq = ROPE_THETA ** (-(jnp.arange(half, dtype=jnp.float32) * 2.0 / ROT_DIM))
    ang = pos.astype(jnp.float32)[:, None] * inv_freq[None, :]
    cos, sin = jnp.cos(ang), jnp.sin(ang)
    xr = x[..., :ROT_DIM].astype(jnp.float32)
    x1, x2 = xr[..., :half], xr[..., half:]
    rot = jnp.concatenate([x1 * cos - x2 * sin, x2 * cos + x1 * sin], axis=-1).astype(x.dtype)
    return jnp.concatenate([rot, x[..., ROT_DIM:]], axis=-1)


def _masked_softmax(s, mask):
    s = jnp.where(mask, s, NEG_BIG)
    m = jnp.max(s, axis=-1, keepdims=True)
    p = jnp.where(mask, jnp.exp(s - m), 0.0)
    denom = jnp.sum(p, axis=-1, keepdims=True)
    return p / jnp.where(denom > 0, denom, 1.0)


def hgrn2_mixer(q, f_logit, i, lb):
    B, T, _ = q.shape
    H, dk, C, c = GROUP_HEADS, HEAD_DIM, HG_CHUNK, HG_SUB
    n_sub = C // c
    q = jax.nn.silu(q.astype(jnp.float32))
    f_logit = f_logit.astype(jnp.float32)
    log_f = jnp.log(lb + (1.0 - lb) * jax.nn.sigmoid(f_logit))
    k = (1.0 - lb) * jax.nn.sigmoid(-f_logit)
    v = i.astype(jnp.float32)

    def chunks(a):
        return a.reshape(B, T // C, C, H, dk).transpose(1, 0, 3, 2, 4)

    sub_before = jnp.tril(jnp.ones((n_sub, n_sub), dtype=bool), -1)
    causal = jnp.tril(jnp.ones((c, c), dtype=bool))

    def step(S, xs):
        qc, kc, vc, gc = xs
        b = jnp.cumsum(gc, axis=2)
        o_inter = jnp.einsum('bhck,bhkv->bhcv', qc * jnp.exp(b), S)
        qs, ks, vs, bs = (a.reshape(B, H, n_sub, c, dk) for a in (qc, kc, vc, b))
        b_ref = jnp.concatenate([jnp.zeros_like(bs[:, :, :1, 0]), bs[:, :, :-1, -1]], axis=2)
        q_off = qs * jnp.exp(bs - b_ref[:, :, :, None])
        e_off = jnp.where(sub_before[:, :, None, None],
                          b_ref[:, :, :, None, None] - bs[:, :, None], NEG_BIG)
        a_off = jnp.einsum('bhitd,bhijsd->bhijts', q_off, ks[:, :, None] * jnp.exp(e_off))
        e_diag = jnp.where(causal[:, :, None],
                           bs[:, :, :, :, None] - bs[:, :, :, None], NEG_BIG)
        a_diag = jnp.einsum('bhntd,bhntsd,bhnsd->bhnts', qs, jnp.exp(e_diag), ks)
        o_intra = (jnp.einsum('bhijts,bhjsv->bhitv', a_off, vs)
                   + jnp.einsum('bhnts,bhnsv->bhntv', a_diag, vs))
        o = o_inter + o_intra.reshape(B, H, C, dk)
        b_last = b[:, :, -1]
        S = (jnp.exp(b_last)[..., None] * S
             + jnp.einsum('bhck,bhcv->bhkv', kc * jnp.exp(b_last[:, :, None] - b), vc))
        return S, o

    S0 = jnp.zeros((B, H, dk, dk), jnp.float32)
    _, o = lax.scan(step, S0, (chunks(q), chunks(k), chunks(v), chunks(log_f)))
    return o.transpose(1, 0, 3, 2, 4).reshape(B, T, H * dk)


def fox_mixer(q, k, v, f_logit, fb):
    B, T, _ = q.shape
    q, k, v = (_to_heads(a, GROUP_HEADS) for a in (q, k, v))
    log_f = jax.nn.log_sigmoid(f_logit.astype(jnp.float32) + fb.astype(jnp.float32))
    cum = jnp.cumsum(log_f, axis=1).transpose(0, 2, 1)
    outs = []
    for blk in range(T // Q_BLOCK):
        t0, t1 = blk * Q_BLOCK, (blk + 1) * Q_BLOCK
        s = jnp.einsum('bhqd,bhkd->bhqk', q[:, :, t0:t1], k[:, :, :t1]).astype(jnp.float32) * ATTN_SCALE
        s = s + cum[:, :, t0:t1, None] - cum[:, :, None, :t1]
        mask = jnp.arange(t0, t1)[:, None] >= jnp.arange(t1)[None, :]
        p = _masked_softmax(s, mask)
        outs.append(jnp.einsum('bhqk,bhkd->bhqd', p.astype(v.dtype), v[:, :, :t1]))
    return _from_heads(jnp.concatenate(outs, axis=2))


def stick_breaking_mixer(q, k, v):
    B, T, _ = q.shape
    q, k, v = (_to_heads(a, GROUP_HEADS) for a in (q, k, v))
    outs = []
    for blk in range(T // Q_BLOCK):
        t0, t1 = blk * Q_BLOCK, (blk + 1) * Q_BLOCK
        z = jnp.einsum('bhqd,bhkd->bhqk', q[:, :, t0:t1], k[:, :, :t1]).astype(jnp.float32) * ATTN_SCALE
        mask = jnp.arange(t1)[None, :] < jnp.arange(t0, t1)[:, None]
        log_om = jnp.where(mask, jax.nn.log_sigmoid(-z), 0.0)
        suffix = lax.cumsum(log_om, axis=3, reverse=True) - log_om
        a = jnp.where(mask, jnp.exp(jnp.where(mask, jax.nn.log_sigmoid(z) + suffix, 0.0)), 0.0)
        outs.append(jnp.einsum('bhqk,bhkd->bhqd', a.astype(v.dtype), v[:, :, :t1]))
    return _from_heads(jnp.concatenate(outs, axis=2))


def _nsa_compress(a, pe, w1, w2):
    B, T, d = a.shape
    n_cmp = (T - NSA_CMP_LEN) // NSA_CMP_STRIDE + 1
    idx = jnp.arange(n_cmp)[:, None] * NSA_CMP_STRIDE + jnp.arange(NSA_CMP_LEN)[None, :]
    blocks = a[:, idx] + pe
    return jax.nn.silu(blocks.reshape(B, n_cmp, NSA_CMP_LEN * d) @ w1) @ w2


def nsa_mixer(q, kc, vc, ks, vs, kw, vw, gate_logits,
              pe_k, w1_k, w2_k, pe_v, w1_v, w2_v):
    B, T, _ = q.shape
    H, d = GROUP_HEADS, HEAD_DIM
    pos = jnp.arange(T)
    q = _partial_rope(_to_heads(q, H), pos)

    n_cmp = (T - NSA_CMP_LEN) // NSA_CMP_STRIDE + 1
    cmp_end = jnp.arange(n_cmp) * NSA_CMP_STRIDE + NSA_CMP_LEN - 1
    k_cmp = _partial_rope(_nsa_compress(kc, pe_k, w1_k, w2_k), cmp_end)
    v_cmp = _nsa_compress(vc, pe_v, w1_v, w2_v)
    s_cmp = jnp.einsum('bhtd,bnd->bhtn', q, k_cmp).astype(jnp.float32) * ATTN_SCALE
    p_cmp = _masked_softmax(s_cmp, cmp_end[None, :] <= pos[:, None])
    o_cmp = jnp.einsum('bhtn,bnd->bhtd', p_cmp, v_cmp)

    n_sel = T // NSA_SEL_BLOCK
    top_n = min(NSA_TOP_N, n_sel)
    c_start = jnp.arange(n_cmp)[:, None] * NSA_CMP_STRIDE
    s_start = jnp.arange(n_sel)[None, :] * NSA_SEL_BLOCK
    overlap = jnp.clip(jnp.minimum(c_start + NSA_CMP_LEN, s_start + NSA_SEL_BLOCK)
                       - jnp.maximum(c_start, s_start), 0, None).astype(jnp.float32) / NSA_CMP_LEN
    importance = jnp.einsum('bhtn,nj->btj', p_cmp, overlap)
    q_blk = pos // NSA_SEL_BLOCK
    blk_ids = jnp.arange(n_sel)
    forced = ((blk_ids[None, :] == 0) | (blk_ids[None, :] == q_blk[:, None])
              | (blk_ids[None, :] == q_blk[:, None] - 1))
    score = jnp.where(forced, NSA_FORCE, importance)
    score = jnp.where(blk_ids[None, :] <= q_blk[:, None], score, -NSA_FORCE)
    _, sel_idx = lax.top_k(score, top_n)

    ks_r = _partial_rope(ks, pos)
    k_blocks = ks_r.reshape(B, n_sel, NSA_SEL_BLOCK, d)
    v_blocks = vs.reshape(B, n_sel, NSA_SEL_BLOCK, d)
    qb_n = NSA_SEL_Q_BLOCK
    n_qb = T // qb_n
    bidx = jnp.arange(B)[:, None, None]

    def sel_block(args):
        qb, idxb, posb = args
        kg = k_blocks[bidx, idxb]
        vg = v_blocks[bidx, idxb]
        s = jnp.einsum('bhqd,bqnkd->bhqnk', qb, kg).astype(jnp.float32) * ATTN_SCALE
        key_pos = idxb[..., None] * NSA_SEL_BLOCK + jnp.arange(NSA_SEL_BLOCK)
        mask = key_pos <= posb[None, :, None, None]
        p = _masked_softmax(s.reshape(B, H, qb_n, top_n * NSA_SEL_BLOCK),
                            mask.reshape(B, 1, qb_n, top_n * NSA_SEL_BLOCK))
        return jnp.einsum('bhqm,bqmd->bhqd', p, vg.reshape(B, qb_n, top_n * NSA_SEL_BLOCK, d))

    o_sel = lax.map(sel_block, (q.reshape(B, H, n_qb, qb_n, d).transpose(2, 0, 1, 3, 4),
                                sel_idx.reshape(B, n_qb, qb_n, top_n).transpose(1, 0, 2, 3),
                                pos.reshape(n_qb, qb_n)))
    o_sel = o_sel.transpose(1, 2, 0, 3, 4).reshape(B, H, T, d)

    W = NSA_WINDOW
    kw_pad = jnp.pad(_partial_rope(kw, pos), ((0, 0), (W, 0), (0, 0)))
    vw_pad = jnp.pad(vw, ((0, 0), (W, 0), (0, 0)))
    n_wb = T // Q_BLOCK

    def win_block(args):
        qb, t0 = args
        kb = lax.dynamic_slice_in_dim(kw_pad, t0, W + Q_BLOCK, axis=1)
        vb = lax.dynamic_slice_in_dim(vw_pad, t0, W + Q_BLOCK, axis=1)
        s = jnp.einsum('bhqd,bkd->bhqk', qb, kb).astype(jnp.float32) * ATTN_SCALE
        qpos = t0 + jnp.arange(Q_BLOCK)
        kpos = t0 - W + jnp.arange(W + Q_BLOCK)
        mask = ((kpos[None, :] <= qpos[:, None]) & (kpos[None, :] > qpos[:, None] - W)
                & (kpos[None, :] >= 0))
        p = _masked_softmax(s, mask)
        return jnp.einsum('bhqk,bkd->bhqd', p, vb)

    o_win = lax.map(win_block, (q.reshape(B, H, n_wb, Q_BLOCK, d).transpose(2, 0, 1, 3, 4),
                                jnp.arange(n_wb) * Q_BLOCK))
    o_win = o_win.transpose(1, 2, 0, 3, 4).reshape(B, H, T, d)

    g = jax.nn.sigmoid(gate_logits.astype(jnp.float32)).reshape(B, T, 3, H).transpose(2, 0, 3, 1)
    o = g[0][..., None] * o_cmp + g[1][..., None] * o_sel + g[2][..., None] * o_win
    return _from_heads(o)


def setup_inputs(seed: int = 0) -> dict:
    key = jax.random.key(seed)
    ks = jax.random.split(key, 14)
    f32 = jnp.float32
    nrm = jax.random.normal
    cmp_in = NSA_CMP_LEN * HEAD_DIM
    return {
        "x": nrm(ks[0], (BATCH, SEQ, D_MODEL), f32),
        "norm_g": 1.0 + 0.02 * nrm(ks[1], (DEPTH, D_MODEL), f32),
        "w_in": nrm(ks[2], (DEPTH, D_MODEL, N_IN), f32) * D_MODEL ** -0.5,
        "hgrn_lb_logits": 0.5 * nrm(ks[3], (DEPTH, GROUP_W), f32),
        "fox_fb": 2.0 + 0.5 * nrm(ks[4], (DEPTH, GROUP_HEADS), f32),
        "nsa_cmp_pe_k": 0.1 * nrm(ks[5], (DEPTH, NSA_CMP_LEN, HEAD_DIM), f32),
        "nsa_cmp_w1_k": nrm(ks[6], (DEPTH, cmp_in, NSA_CMP_HIDDEN), f32) * cmp_in ** -0.5,
        "nsa_cmp_w2_k": nrm(ks[7], (DEPTH, NSA_CMP_HIDDEN, HEAD_DIM), f32) * NSA_CMP_HIDDEN ** -0.5,
        "nsa_cmp_pe_v": 0.1 * nrm(ks[8], (DEPTH, NSA_CMP_LEN, HEAD_DIM), f32),
        "nsa_cmp_w1_v": nrm(ks[9], (DEPTH, cmp_in, NSA_CMP_HIDDEN), f32) * cmp_in ** -0.5,
        "nsa_cmp_w2_v": nrm(ks[10], (DEPTH, NSA_CMP_HIDDEN, HEAD_DIM), f32) * NSA_CMP_HIDDEN ** -0.5,
        "out_norm_g": 1.0 + 0.02 * nrm(ks[11], (DEPTH, D_MIX), f32),
        "w_out": nrm(ks[12], (DEPTH, D_MIX, D_MODEL), f32) * D_MIX ** -0.5,
        "final_norm_g": 1.0 + 0.02 * nrm(ks[13], (D_MODEL,), f32),
    }


def reference(x, norm_g, w_in, hgrn_lb_logits, fox_fb, nsa_cmp_pe_k, nsa_cmp_w1_k, nsa_cmp_w2_k,
              nsa_cmp_pe_v, nsa_cmp_w1_v, nsa_cmp_w2_v, out_norm_g, w_out, final_norm_g):
    B, T, _ = x.shape
    lb_sm = jax.nn.softmax(hgrn_lb_logits.astype(jnp.float32), axis=0)
    lb_all = jnp.cumsum(lb_sm, axis=0) - lb_sm[0:1]
    split_at = np.cumsum(IN_WIDTHS)[:-1].tolist()
    for l in range(DEPTH):
        h = _rmsnorm(x, norm_g[l])
        (hg_q, hg_f, hg_i, fx_q, fx_k, fx_v, fx_f, sb_q, sb_k, sb_v,
         ns_q, ns_kc, ns_vc, ns_ks, ns_vs, ns_kw, ns_vw, ns_g, gate) = jnp.split(h @ w_in[l], split_at, axis=-1)
        y = jnp.concatenate([
            hgrn2_mixer(hg_q, hg_f, hg_i, lb_all[l]).astype(x.dtype),
            fox_mixer(fx_q, fx_k, fx_v, fx_f, fox_fb[l]).astype(x.dtype),
            stick_breaking_mixer(sb_q, sb_k, sb_v).astype(x.dtype),
            nsa_mixer(ns_q, ns_kc, ns_vc, ns_ks, ns_vs, ns_kw, ns_vw, ns_g,
                      nsa_cmp_pe_k[l], nsa_cmp_w1_k[l], nsa_cmp_w2_k[l],
                      nsa_cmp_pe_v[l], nsa_cmp_w1_v[l], nsa_cmp_w2_v[l]).astype(x.dtype),
        ], axis=-1)
        yh = y.reshape(B, T, N_MIX_HEADS, HEAD_DIM).astype(jnp.float32)
        yh = yh * lax.rsqrt(jnp.mean(yh * yh, axis=-1, keepdims=True) + EPS)
        y = (yh.reshape(B, T, D_MIX) * out_norm_g[l].astype(jnp.float32)
             * jax.nn.silu(gate.astype(jnp.float32)))
        x = x + y.astype(x.dtype) @ w_out[l]
    return _rmsnorm(x, final_norm_g)
```

```python
import os
import numpy as np
import ml_dtypes
from contextlib import ExitStack
import concourse.bass as bass
import concourse.mybir as mybir
from concourse.bass_utils import run_bass_kernel_spmd

F32 = mybir.dt.float32
BF16 = mybir.dt.bfloat16
AF = mybir.ActivationFunctionType
ALU = mybir.AluOpType
AX = mybir.AxisListType

T_ = 2048
D_ = 1024
NIN = 3984
NEG = -30000.0
EPS = 1e-6
NCORES = 8
SEQ_PER_CORE = 4

C_HGQ, C_HGF, C_HGI = 0, 256, 512
C_FXQ, C_FXK, C_FXV, C_FXF = 768, 1024, 1280, 1536
C_SBQ, C_SBK, C_SBV = 1540, 1796, 2052
C_NSQ = 2308
C_NKC, C_NVC, C_NKS, C_NVS, C_NKW, C_NVW = 2564, 2628, 2692, 2756, 2820, 2884
C_NSG = 2948
C_GATE = 2960


class Tok:
    __slots__ = ("w", "r", "name")

    def __init__(self, name=""):
        self.w = None
        self.r = {}
        self.name = name


class T:
    def __init__(self, h, name, excl=False):
        self.h = h
        self.tok = Tok(name)
        self.name = name
        self.excl = excl

    def __getitem__(self, k):
        return self.h[k]


class KB:
    NDQ = 8
    EPOCH_LIMIT = 30000

    def __init__(self, nc):
        self.nc = nc
        self.stack = [ExitStack()]
        self.E = {"pe": nc.tensor, "act": nc.scalar, "dve": nc.vector,
                  "pool": nc.gpsimd, "sp": nc.sync}
        self.semh = {}
        self.cur = {}
        self.cnt = {}
        self.epoch = {}
        for e in ["pe", "act", "dve", "pool"]:
            self.epoch[e] = -1
            self._new_epoch(e)
        self.dq = {}
        for q in ["sp", "pool", "act"]:
            sems = [self.stack[0].enter_context(nc.semaphore(f"d_{q}{i}")) for i in range(self.NDQ)]
            self.dq[q] = dict(n=0)
            for i, s in enumerate(sems):
                self.semh[("dma", q, i)] = s
        self.seen = {}
        self.nins = {e: 0 for e in self.E}
        self.uid = 0

    def _new_epoch(self, e):
        self.epoch[e] += 1
        key = (e, self.epoch[e])
        self.semh[key] = self.stack[0].enter_context(self.nc.semaphore(f"s_{e}{self.epoch[e]}"))
        self.cur[e] = key
        self.cnt[e] = 0

    def scope(self):
        kb = self

        class _S:
            def __enter__(s):
                kb.stack.append(ExitStack())

            def __exit__(s, *a):
                kb.barrier()
                kb.stack.pop().close()
                return False
        return _S()

    def _nm(self, name):
        self.uid += 1
        return f"{name}_{self.uid}"

    def sb(self, name, shape, dt=F32):
        h = self.stack[-1].enter_context(self.nc.sbuf_tensor(self._nm(name), list(shape), dt))
        return T(h, name)

    def ps(self, name, shape, dt=F32):
        h = self.stack[-1].enter_context(self.nc.psum_tensor(self._nm(name), list(shape), dt))
        return T(h, name, excl=True)

    def dram(self, name, shape, dt, kind):
        h = self.nc.dram_tensor(name, list(shape), dt, kind=kind)
        return T(h, name)

    def _wait(self, eng, deps):
        for key, val in deps.items():
            if key[0] == "pe" and eng == "pe":
                continue
            sk = (eng, key)
            if self.seen.get(sk, 0) >= val:
                continue
            self.seen[sk] = val
            self.E[eng].wait_ge(self.semh[key], val)
            self.nins[eng] += 1

    @staticmethod
    def _tok(x):
        return x.tok if isinstance(x, T) else x

    def _deps(self, reads, writes):
        deps = {}
        for t in reads:
            t = self._tok(t)
            if t.w and deps.get(t.w[0], 0) < t.w[1]:
                deps[t.w[0]] = t.w[1]
        for t in writes:
            t = self._tok(t)
            if t.w and deps.get(t.w[0], 0) < t.w[1]:
                deps[t.w[0]] = t.w[1]
            for kk, v in t.r.items():
                if deps.get(kk, 0) < v:
                    deps[kk] = v
        return deps

    def _mark(self, ev, reads, writes):
        kk, v = ev
        for t in reads:
            t = self._tok(t)
            if t.r.get(kk, 0) < v:
                t.r[kk] = v
        for t in writes:
            t = self._tok(t)
            t.w = ev
            t.r = {}

    def op(self, eng, fn, reads=(), writes=()):
        ex = [t for t in reads if isinstance(t, T) and t.excl]
        if ex:
            reads = [t for t in reads if not (isinstance(t, T) and t.excl)]
            writes = list(writes) + ex
        self._wait(eng, self._deps(reads, writes))
        if self.cnt[eng] >= self.EPOCH_LIMIT:
            self._new_epoch(eng)
        ins = fn()
        self.cnt[eng] += 1
        ins.then_inc(self.semh[self.cur[eng]], 1)
        self.nins[eng] += 1
        self._mark((self.cur[eng], self.cnt[eng]), reads, writes)
        return ins

    def pe_selfwait(self):
        key, val = self.cur["pe"], self.cnt["pe"]
        if val > 0 and self.seen.get(("pe", key), 0) < val:
            self.seen[("pe", key)] = val
            self.E["pe"].wait_ge(self.semh[key], val)
            self.nins["pe"] += 1

    def dma(self, q, out, in_, reads=(), writes=(), **kw):
        self._wait(q, self._deps(reads, writes))
        d = self.dq[q]
        slot = d["n"] % self.NDQ
        val = 16 * (d["n"] // self.NDQ + 1)
        d["n"] += 1
        ins = self.E[q].dma_start(out=out, in_=in_, **kw)
        ins.then_inc(self.semh[("dma", q, slot)], 16)
        self.nins[q] += 1
        self._mark((("dma", q, slot), val), reads, writes)
        return ins

    def _all_events(self):
        ev = {}
        for e in ["pe", "act", "dve", "pool"]:
            if self.cnt[e] > 0:
                ev[self.cur[e]] = self.cnt[e]
        for q, d in self.dq.items():
            n = d["n"]
            for slot in range(self.NDQ):
                c = (n - slot + self.NDQ - 1) // self.NDQ
                if c > 0:
                    ev[("dma", q, slot)] = 16 * c
        return ev

    def barrier(self):
        ev = self._all_events()
        for e in ["pe", "act", "dve", "pool", "sp"]:
            self._wait(e, {kk: v for kk, v in ev.items() if not (kk[0] == e)})

    def finish(self, toks, eng="sp"):
        deps = {}
        for t in toks:
            t = self._tok(t)
            if t.w and deps.get(t.w[0], 0) < t.w[1]:
                deps[t.w[0]] = t.w[1]
        self._wait(eng, deps)

    def close(self):
        while self.stack:
            self.stack.pop().close()


def _bf(a):
    return np.asarray(a, dtype=np.float32).astype(ml_dtypes.bfloat16)


def make_consts():
    c = {}
    i = np.arange(128)[:, None]
    j = np.arange(128)[None, :]
    c["identF"] = np.eye(128, dtype=np.float32)
    c["triF"] = (i <= j).astype(np.float32)
    c["onesF"] = np.ones((128, 128), np.float32)
    c["tribdF"] = ((i // 32 == j // 32) & (i <= j)).astype(np.float32)
    c["trisufF"] = ((i // 32 == j // 32) & (i > j)).astype(np.float32)
    c["maskbdF"] = ((i // 32 == j // 32) & (i <= j)).astype(np.float32)
    perm = np.zeros((64, 16), np.float32)
    for a in range(16):
        perm[a + 8 if a < 8 else a - 8, a] = 1.0
    c["permF"] = perm
    cf = np.concatenate([c["identF"], c["triF"], c["onesF"], c["tribdF"], c["trisufF"], c["maskbdF"]], axis=1)
    c["constF"] = cf
    negtri = -(i >= j).astype(np.float32)
    neglow = -(i < j).astype(np.float32)
    c["constB"] = _bf(np.concatenate([np.eye(128), np.ones((128, 128)), negtri, neglow], axis=1))
    jj = np.arange(896)[None, :] - 384
    caus = np.where(jj < 0, NEG, np.where(jj >= 128, 0.0, np.where(jj >= i, 0.0, NEG)))
    strict = np.where(jj < 0, NEG, np.where(jj >= 128, 0.0, np.where(jj > i, 0.0, NEG)))
    win = np.where(jj < 0, 0.0, np.where(jj >= 128, NEG, np.where(jj < i, 0.0, NEG)))
    c["strips"] = _bf(np.stack([caus, strict, win], axis=1))
    n = np.arange(128)[:, None]
    t = np.arange(T_)[None, :]
    c["cmask"] = _bf(np.where((16 * n + 31 <= t) & (n < 127), 0.0, NEG))
    cs = (np.arange(128) * 16)[:, None]
    ss = (np.arange(32) * 64)[None, :]
    ov = np.clip(np.minimum(cs + 32, ss + 64) - np.maximum(cs, ss), 0, None).astype(np.float32) / 32.0
    ovl = np.concatenate([np.ones((128, 1), np.float32), ov], axis=1)
    ovl[127] = 0.0
    c["ovl"] = _bf(ovl)
    c["eall"] = _bf((np.arange(T_)[None, :] // 64 == np.arange(32)[:, None]).astype(np.float32))
    half = 8
    inv = (500000.0 ** (-(np.arange(half, dtype=np.float32) * 2.0 / 16.0))).astype(np.float32)

    def tabs(pos):
        ang = pos.astype(np.float32)[None, :] * inv[:, None]
        cos, sin = np.cos(ang), np.sin(ang)
        return (np.concatenate([cos, cos], 0).astype(np.float32),
                np.concatenate([-sin, sin], 0).astype(np.float32))
    C, S = tabs(np.arange(T_))
    c["ropeC"], c["ropeS"] = C, S
    pc = np.arange(128) * 16 + 31
    Cc, Sc = tabs(pc)
    c["ropeCc"], c["ropeSc"] = Cc, Sc
    tt_ = np.arange(T_)
    qblk = tt_ // 64
    blk = np.arange(32)[None, :]
    forced = (blk == 0) | (blk == qblk[:, None]) | (blk == qblk[:, None] - 1)
    valid = blk <= qblk[:, None]
    A = (~forced & valid).astype(np.float32)
    Bc = np.where(forced, 1.0e4, np.where(valid, 0.0, -1.0e4)).astype(np.float32)
    c["tkA"] = A.reshape(16, 128, 32).transpose(1, 0, 2).copy()
    c["tkB"] = Bc.reshape(16, 128, 32).transpose(1, 0, 2).copy()
    return c


def build(nseq=SEQ_PER_CORE, nlayer=2, mixers=("hg", "fx", "sb", "ns", "out"), debug=None):
    nc = bass.Bass("TRN2", target_bir_lowering=False)
    k = KB(nc)
    dbg_outs = {}

    def din(name, shape, dt=F32):
        return k.dram(name, shape, dt, "ExternalInput")

    xT_d = din("xT", [nseq, D_, T_])
    w_in_d = din("w_in", [2, D_, NIN])
    w_out_d = din("w_out", [2, D_, D_])
    normg_d = din("norm_gT", [2, 128, 8])
    fnormg_d = din("fnorm_gT", [128, 8])
    ong_d = din("out_norm_g", [2, D_])
    lbl_d = din("lb_logits", [2, 256])
    lblT_d = din("lb_logitsT", [128, 2, 2])
    fb_d = din("fox_fb", [2, 4])
    peT_d = {kv: din(f"peT_{kv}", [2, 64, 32]) for kv in "kv"}
    w1_d = {kv: din(f"w1_{kv}", [2, 2048, 64]) for kv in "kv"}
    w2_d = {kv: din(f"w2_{kv}", [2, 64, 64]) for kv in "kv"}
    constF_d = din("constF", [128, 768])
    constB_d = din("constB", [128, 512], BF16)
    strips_d = din("strips", [128, 3, 896], BF16)
    cmask_d = din("cmask", [128, T_], BF16)
    ovl_d = din("ovl", [128, 33], BF16)
    eall_d = din("eall", [32, T_], BF16)
    ropeC_d = din("ropeC", [16, T_])
    ropeS_d = din("ropeS", [16, T_])
    ropeCc_d = din("ropeCc", [16, 128])
    ropeSc_d = din("ropeSc", [16, 128])
    permF_d = din("permF", [64, 16])
    tkA_d = din("tkA", [128, 16, 32])
    tkB_d = din("tkB", [128, 16, 32])
    outT_d = k.dram("outT", [nseq, D_, T_], F32, "ExternalOutput")
    xres_d = k.dram("xres", [nseq, D_, T_], F32, "Internal")

    def dbg_out(name, shape, dt=F32):
        d = k.dram(name, shape, dt, "ExternalOutput")
        dbg_outs[name] = d
        return d

    constF = k.sb("constF", [128, 768])
    constB = k.sb("constB", [128, 512], BF16)
    strips = k.sb("strips", [128, 3, 896], BF16)
    k.dma("sp", constF[:], constF_d[:], writes=[constF])
    k.dma("sp", constB[:], constB_d[:], writes=[constB])
    k.dma("sp", strips[:], strips_d[:], writes=[strips])
    identF = constF[:, 0:128]
    triF = constF[:, 128:256]
    onesF = constF[:, 256:384]
    tribdF = constF[:, 384:512]
    trisufF = constF[:, 512:640]
    maskbdF = constF[:, 640:768]
    identB = constB[:, 0:128]
    onesB = constB[:, 128:256]
    negtriB = constB[:, 256:384]
    neglowB = constB[:, 384:512]

    hT = k.sb("hT", [128, 8, T_], BF16)
    ytok = k.sb("ytok", [128, 16, D_], BF16)
    normg = k.sb("normg", [128, 2, 8])
    fnormg = k.sb("fnormg", [128, 8])
    for l in range(2):
        k.dma("sp", normg[:, l, :], normg_d[l], writes=[normg])
    k.dma("sp", fnormg[:], fnormg_d[:], writes=[fnormg])

    PS = [k.ps(f"ps{i}", [128, 512]) for i in range(7)]
    PST = k.ps("pst", [128, 1024], BF16)
    ps_rr = {"s": [0, 1], "a": [2, 3], "o": [4, 5], "m": [6]}
    ps_ctr = {kk: 0 for kk in ps_rr}

    def psb(role):
        lst = ps_rr[role]
        i = lst[ps_ctr[role] % len(lst)]
        ps_ctr[role] += 1
        return PS[i]

    def mm(out, lhsT, rhs, start, stop, reads, writes, tp=None, ser=False):
        kw = {}
        if ser:
            k.pe_selfwait()
        if tp is not None:
            kw["tile_position"] = tp
        return k.op("pe", lambda: nc.tensor.matmul(out, lhsT=lhsT, rhs=rhs, start=start, stop=stop, **kw),
                    reads=reads, writes=writes)

    def act(out, in_, func, reads, writes, bias=None, scale=None):
        kw = {}
        if bias is not None:
            kw["bias"] = bias
        if scale is not None:
            kw["scale"] = scale
        return k.op("act", lambda: nc.scalar.activation(out=out, in_=in_, func=func, **kw),
                    reads=reads, writes=writes)

    def tt(eng, out, in0, in1, op, reads, writes):
        e = k.E[eng]
        return k.op(eng, lambda: e.tensor_tensor(out=out, in0=in0, in1=in1, op=op), reads=reads, writes=writes)

    def ts(eng, out, in0, s1, s2, op0, op1, reads, writes):
        e = k.E[eng]
        if s2 is None:
            return k.op(eng, lambda: e.tensor_scalar(out=out, in0=in0, scalar1=s1, scalar2=None, op0=op0),
                        reads=reads, writes=writes)
        return k.op(eng, lambda: e.tensor_scalar(out=out, in0=in0, scalar1=s1, scalar2=s2, op0=op0, op1=op1),
                    reads=reads, writes=writes)

    def stt(eng, out, in0, scalar, in1, op0, op1, reads, writes):
        e = k.E[eng]
        return k.op(eng, lambda: e.scalar_tensor_tensor(out=out, in0=in0, scalar=scalar, in1=in1, op0=op0, op1=op1),
                    reads=reads, writes=writes)

    def cp(eng, out, in_, reads, writes):
        if eng == "act":
            return k.op("act", lambda: nc.scalar.copy(out=out, in_=in_), reads=reads, writes=writes)
        e = k.E[eng]
        return k.op(eng, lambda: e.tensor_copy(out=out, in_=in_), reads=reads, writes=writes)

    def memset(eng, out, val, writes):
        e = k.E[eng]
        return k.op(eng, lambda: e.memset(out, val), writes=writes)

    def recip(out, in_, reads, writes):
        return k.op("dve", lambda: nc.vector.reciprocal(out=out, in_=in_), reads=reads, writes=writes)

    def load_w(l, c0, ncols, name="wblk"):
        w = k.sb(name, [128, 8, ncols], BF16)
        src = w_in_d[l][:, c0:c0 + ncols].rearrange("(j p) c -> p j c", p=128)
        half = 4
        k.dma("pool", w[:, 0:half, :], src[:, 0:half, :], writes=[w])
        k.dma("pool", w[:, half:8, :], src[:, half:8, :], writes=[w])
        return w

    def proj_fm(w, off, M, tb, ps, extra_reads=()):
        for j in range(8):
            mm(ps[0:M, :], w[:, j, off:off + M], hT[:, j, tb * 512:(tb + 1) * 512], j == 0, j == 7,
               reads=[w, hT, *extra_reads], writes=[ps])

    def proj_tm(w, off, N, tB, ps):
        for j in range(8):
            mm(ps[:, 0:N], hT[:, j, tB * 128:(tB + 1) * 128], w[:, j, off:off + N], j == 0, j == 7,
               reads=[w, hT], writes=[ps])

    def finalize(o, o_tok, out_ap, tmp, n=4):
        sq, ss, sd = tmp
        tt("dve", sq[:, 0:n, :], o, o, ALU.mult, reads=[o_tok], writes=[sq])
        k.op("dve", lambda: nc.vector.tensor_reduce(out=ss[:, 0:n], in_=sq[:, 0:n, :], axis=AX.X, op=ALU.add),
             reads=[sq], writes=[ss])
        act(sd[:, 0:n], ss[:, 0:n], AF.Sqrt, reads=[ss], writes=[sd], bias=EPS, scale=1.0 / 64.0)
        recip(sd[:, 0:n], sd[:, 0:n], reads=[sd], writes=[sd])
        tt("dve", out_ap, o, sd[:, 0:n].unsqueeze(2).to_broadcast([128, n, 64]), ALU.mult,
           reads=[o_tok, sd], writes=[ytok])

    def fin_tmp(n=4):
        return (k.sb("sq", [128, n, 64]), k.sb("ss", [128, n]), k.sb("sd", [128, n]))


    def run_streams(gens):
        gens = list(gens)
        while gens:
            for g in list(gens):
                try:
                    next(g)
                except StopIteration:
                    gens.remove(g)

    def attn_stream(jobs, sbank, obank, pTs, K_ap, Q_ap, kq_toks, masks, bias_ap, bias_toks, V_ap, v_toks,
                    kb_range, qs_range, ncol, fin):
        tiles = []
        for (h, qb) in jobs:
            kbs = list(kb_range(qb))
            for kb in kbs:
                tiles.append((h, qb, kb, kb == kbs[0], kb == kbs[-1]))
        nsb = len(sbank)

        def emit_qk(i):
            h, qb, kb, _, _ = tiles[i]
            ps = sbank[i % nsb]
            ml = masks(h, qb, kb)
            mm(ps[:], K_ap(h, kb), Q_ap(h, qb), True, len(ml) == 0, reads=kq_toks, writes=[ps])
            for mi, (lt, rh, rd) in enumerate(ml):
                mm(ps[:], lt, rh, False, mi == len(ml) - 1, reads=rd, writes=[ps])

        emit_qk(0)
        first = True
        accv = obank[:, 0:4 * ncol].rearrange("p (q d) -> p q d", q=4)
        for i, (h, qb, kb, isfirst, islast) in enumerate(tiles):
            if nsb > 1 and i + 1 < len(tiles):
                emit_qk(i + 1)
            yield
            ps = sbank[i % nsb]
            p = pTs[i % len(pTs)]
            if isfirst:
                first = True
            b = bias_ap(h, kb) if bias_ap is not None else None
            act(p[:], ps[:], AF.Exp, reads=[ps, *bias_toks], writes=[p], bias=b)
            for qs in range(4):
                lo, hi = qs_range(qb, qs)
                if kb < lo or kb > hi:
                    continue
                mm(accv[:, qs, :], p[:, qs * 128:(qs + 1) * 128], V_ap(h, kb), first, kb == hi,
                   reads=[p, *v_toks], writes=[obank])
                first = False
            if islast:
                fin(h, qb, obank, accv)
            if nsb == 1 and i + 1 < len(tiles):
                emit_qk(i + 1)
            yield

    def phase_norm(s, l):
        xsrc = xT_d if l == 0 else xres_d
        with k.scope():
            xin = [k.sb(f"xin{i}", [128, 8, 512]) for i in range(2)]
            sq = [k.sb(f"sqn{i}", [128, 8, 512], BF16) for i in range(2)]
            rs = [k.sb(f"rs{i}", [128, 512]) for i in range(2)]
            for tb in range(4):
                xi, sqi, rsi = xin[tb % 2], sq[tb % 2], rs[tb % 2]
                src = xsrc[s].rearrange("(j p) t -> p j t", p=128)[:, :, tb * 512:(tb + 1) * 512]
                k.dma("sp", xi[:], src, reads=[xsrc], writes=[xi])
                act(sqi[:], xi[:], AF.Square, reads=[xi], writes=[sqi])
                ps = psb("m")
                for j in range(8):
                    mm(ps[:], onesB, sqi[:, j, :], j == 0, j == 7, reads=[constB, sqi], writes=[ps])
                act(rsi[:], ps[:], AF.Sqrt, reads=[ps], writes=[rsi], bias=EPS, scale=1.0 / D_)
                recip(rsi[:], rsi[:], reads=[rsi], writes=[rsi])
                for j in range(8):
                    stt("dve", hT[:, j, tb * 512:(tb + 1) * 512], xi[:, j, :], normg[:, l, j:j + 1], rsi[:],
                        ALU.mult, ALU.mult, reads=[xi, rsi, normg], writes=[hT])

    def phase_fox(s, l):
        with k.scope():
            QT = k.sb("fxQT", [128, 4, T_], BF16)
            KT = k.sb("fxKT", [128, 4, T_], BF16)
            V = k.sb("fxV", [128, 16, 4, 65], BF16)
            ltok = k.sb("fxl", [128, 16, 4])
            negcum = k.sb("fxnc", [128, 16, 4])
            fbb = k.sb("fbb", [128, 4])
            k.dma("sp", fbb[:], fb_d[l:l + 1, :].to_broadcast([128, 4]), writes=[fbb])
            memset("pool", V[:, :, :, 64:65], 1.0, writes=[V])
            memset("pool", KT[64:67, :, :], 1.0, writes=[KT])
            for (c0, dst, scale) in ((C_FXQ, QT, 0.125), (C_FXK, KT, None)):
                w = load_w(l, c0, 256)
                for pr in range(2):
                    for tb in range(4):
                        ps = psb("m")
                        proj_fm(w, pr * 128, 128, tb, ps)
                        sl = slice(tb * 512, (tb + 1) * 512)
                        for hh in range(2):
                            if scale is None:
                                cp("act", dst[0:64, 2 * pr + hh, sl], ps[64 * hh:64 * hh + 64, :], reads=[ps], writes=[dst])
                            else:
                                k.op("act", lambda: nc.scalar.mul(out=dst[0:64, 2 * pr + hh, sl],
                                                                  in_=ps[64 * hh:64 * hh + 64, :], mul=scale),
                                     reads=[ps], writes=[dst])
            if os.environ.get('FX_STOP') == '1':
                return
            w = load_w(l, C_FXV, 260)
            _skip = os.environ.get("FX_SKIP", "")
            for tB in range(int(os.environ.get("FX_NTB", "16"))):
                ps = psb("m")
                if "mm" not in _skip:
                    proj_tm(w, 0, 260, tB, ps)
                if "cp" not in _skip:
                    cp("act", V[:, tB, :, 0:64], ps[:, 0:256].rearrange("p (h d) -> p h d", h=4), reads=[ps], writes=[V])
                if "tt" not in _skip:
                    tt("dve", ltok[:, tB, :], ps[:, 256:260], fbb[:], ALU.add, reads=[ps, fbb], writes=[ltok])
            if os.environ.get('FX_STOP') == '2':
                return
            act(ltok[:], ltok[:], AF.Exp, reads=[ltok], writes=[ltok], scale=-1.0)
            act(ltok[:], ltok[:], AF.Ln, reads=[ltok], writes=[ltok], bias=1.0)
            if os.environ.get('FX_STOP') == '3':
                return
            ps = psb("m")
            for Bp in range(16):
                for B in range(Bp + 1):
                    mm(ps[:, 4 * Bp:4 * Bp + 4], triF if B == Bp else onesF, ltok[:, B, :], B == 0, B == Bp,
                       reads=[constF, ltok], writes=[ps])
            cp("dve", negcum[:], ps[:, 0:64].rearrange("p (b h) -> p b h", h=4), reads=[ps], writes=[negcum])
            if os.environ.get('FX_STOP') == '4':
                return
            cumf = k.sb("cumf", [4, T_])
            c1f = k.sb("c1f", [4, T_])
            cb = [k.sb(f"cb{i}", [4, T_], BF16) for i in range(3)]
            for g in range(4):
                ps = psb("m")
                for bb in range(4):
                    B = 4 * g + bb
                    mm(ps[0:4, 128 * bb:128 * bb + 128], negcum[:, B, :], identF, True, True,
                       reads=[negcum, constF], writes=[ps])
                k.op("act", lambda: nc.scalar.mul(out=cumf[:, 512 * g:512 * g + 512], in_=ps[0:4, :], mul=-1.0),
                     reads=[ps], writes=[cumf])
            cp("dve", cb[0][:], cumf[:], reads=[cumf], writes=[cb[0]])
            cp("dve", c1f[:], cb[0][:], reads=[cb[0]], writes=[c1f])
            tt("dve", cumf[:], cumf[:], c1f[:], ALU.subtract, reads=[cumf, c1f], writes=[cumf])
            cp("dve", cb[1][:], cumf[:], reads=[cumf], writes=[cb[1]])
            cp("dve", c1f[:], cb[1][:], reads=[cb[1]], writes=[c1f])
            tt("dve", cumf[:], cumf[:], c1f[:], ALU.subtract, reads=[cumf, c1f], writes=[cumf])
            cp("dve", cb[2][:], cumf[:], reads=[cumf], writes=[cb[2]])
            for i in range(3):
                k.dma("sp", QT[64 + i:65 + i, :, :], cb[i][:], reads=[cb[i]], writes=[QT])
            if os.environ.get('FX_STOP') == '5':
                return
            def mk_stream(si, heads):
                pTs = [k.sb(f"fxpT{si}_{i}", [128, 512], BF16) for i in range(2)]
                o_sb = k.sb(f"fxo{si}", [128, 4, 64])
                rd = k.sb(f"fxrd{si}", [128, 4])
                tmp = fin_tmp()

                def masks(h, qb, kb):
                    if kb >= 4 * qb:
                        r = kb - 4 * qb
                        return [(identB, strips[:, 0, 384 - 128 * r:384 - 128 * r + 512], [constB, strips])]
                    return []

                def fin(h, qb, acc, accv):
                    recip(rd[:], accv[:, :, 64], reads=[acc], writes=[rd])
                    tt("dve", o_sb[:], accv[:, :, 0:64], rd[:].unsqueeze(2).to_broadcast([128, 4, 64]), ALU.mult,
                       reads=[acc, rd], writes=[o_sb])
                    finalize(o_sb[:], o_sb, ytok[:, 4 * qb:4 * qb + 4, 256 + 64 * h:256 + 64 * h + 64], tmp)

                return attn_stream(
                    [(h, qb) for h in heads for qb in range(4)],
                    [PS[2 * si], PS[2 * si + 1]], PS[4 + si], pTs,
                    lambda h, kb: KT[0:67, h, kb * 128:(kb + 1) * 128],
                    lambda h, qb: QT[0:67, h, qb * 512:(qb + 1) * 512], [KT, QT],
                    masks, lambda h, kb: negcum[:, kb, h:h + 1], [negcum],
                    lambda h, kb: V[:, kb, h, :], [V],
                    lambda qb: range(0, 4 * qb + 4), lambda qb, qs: (0, 4 * qb + qs), 65, fin)

            run_streams([mk_stream(0, (0, 1)), mk_stream(1, (2, 3))])


    def phase_sb(s, l):
        with k.scope():
            QT = k.sb("sbQT", [64, 4, T_], BF16)
            KT = k.sb("sbKT", [64, 4, T_], BF16)
            V = k.sb("sbV", [128, 16, 256], BF16)
            for (c0, dst, scale) in ((C_SBQ, QT, 0.125), (C_SBK, KT, None)):
                w = load_w(l, c0, 256)
                for pr in range(2):
                    for tb in range(4):
                        ps = psb("m")
                        proj_fm(w, pr * 128, 128, tb, ps)
                        sl = slice(tb * 512, (tb + 1) * 512)
                        for hh in range(2):
                            if scale is None:
                                cp("act", dst[0:64, 2 * pr + hh, sl], ps[64 * hh:64 * hh + 64, :], reads=[ps], writes=[dst])
                            else:
                                k.op("act", lambda: nc.scalar.mul(out=dst[0:64, 2 * pr + hh, sl],
                                                                  in_=ps[64 * hh:64 * hh + 64, :], mul=scale),
                                     reads=[ps], writes=[dst])
            w = load_w(l, C_SBV, 256)
            for tB in range(16):
                ps = psb("m")
                proj_tm(w, 0, 256, tB, ps)
                cp("act", V[:, tB, :], ps[:, 0:256], reads=[ps], writes=[V])
            def sb_stream(si, heads):
                e = k.sb(f"sbe{si}", [128, 512])
                sp_ = k.sb(f"sbsp{si}", [128, 512], BF16)
                er_ = k.sb(f"sber{si}", [128, 512])
                a_ = k.sb(f"sbaT{si}", [128, 512], BF16)
                o = k.sb(f"sbo{si}", [128, 4, 64])
                tmp = fin_tmp()
                ps, psR, acc = PS[2 * si], PS[2 * si + 1], PS[4 + si]
                accv = acc[:, 0:256].rearrange("p (q d) -> p q d", q=4)
                for h in heads:
                    for qb in range(4):
                        nkb = 4 * qb + 4
                        first_acc = True
                        for idx, kb in enumerate(reversed(range(nkb))):
                            diag = kb >= 4 * qb
                            mm(ps[:], KT[0:64, h, kb * 128:(kb + 1) * 128], QT[0:64, h, qb * 512:(qb + 1) * 512],
                               True, not diag, reads=[KT, QT], writes=[ps])
                            if diag:
                                r = kb - 4 * qb
                                mm(ps[:], identB, strips[:, 1, 384 - 128 * r:384 - 128 * r + 512], False, True,
                                   reads=[constB, strips], writes=[ps])
                            yield
                            act(e[:], ps[:], AF.Exp, reads=[ps], writes=[e])
                            act(sp_[:], e[:], AF.Ln, reads=[e], writes=[sp_], bias=1.0)
                            mm(psR[:], negtriB, sp_[:], idx == 0, False, reads=[constB, sp_], writes=[psR])
                            yield
                            act(er_[:], psR[:], AF.Exp, reads=[psR], writes=[er_])
                            tt("dve", a_[:], e[:], er_[:], ALU.mult, reads=[e, er_], writes=[a_])
                            mm(psR[:], neglowB, sp_[:], False, idx == nkb - 1, reads=[constB, sp_], writes=[psR])
                            yield
                            for qs in range(4):
                                if kb > 4 * qb + qs:
                                    continue
                                mm(accv[:, qs, :], a_[:, qs * 128:(qs + 1) * 128], V[:, kb, 64 * h:64 * h + 64],
                                   first_acc, kb == 0, reads=[a_, V], writes=[acc])
                                first_acc = False
                        cp("dve", o[:], accv, reads=[acc], writes=[o])
                        finalize(o[:], o, ytok[:, 4 * qb:4 * qb + 4, 512 + 64 * h:512 + 64 * h + 64], tmp)

            run_streams([sb_stream(0, (0, 1)), sb_stream(1, (2, 3))])


    def phase_hgrn(s, l):
        with k.scope():
            lbb = k.sb("lbb", [128, 2, 256])
            lblT = k.sb("lblT", [128, 2, 2])
            omlb = k.sb("omlb", [128, 256])
            omlT = k.sb("omlT", [128, 2])
            if l == 0:
                memset("pool", omlb[:], 1.0, writes=[omlb])
                memset("pool", omlT[:], 1.0, writes=[omlT])
            else:
                k.dma("sp", lbb[:], lbl_d[:].unsqueeze(0).to_broadcast([128, 2, 256]), writes=[lbb])
                k.dma("sp", lblT[:], lblT_d[:], writes=[lblT])
                tt("dve", omlb[:], lbb[:, 0, :], lbb[:, 1, :], ALU.subtract, reads=[lbb], writes=[omlb])
                act(omlb[:], omlb[:], AF.Sigmoid, reads=[omlb], writes=[omlb])
                tt("dve", omlT[:], lblT[:, :, 0], lblT[:, :, 1], ALU.subtract, reads=[lblT], writes=[omlT])
                act(omlT[:], omlT[:], AF.Sigmoid, reads=[omlT], writes=[omlT])
            big1 = k.sb("hgbig1", [128, 4096])
            gtok = k.sb("hggtok", [128, 16, 256])
            vtok = k.sb("hgvtok", [128, 16, 256], BF16)
            khat = k.sb("hgkhat", [128, 16, 256], BF16)
            ktok = big1[:].rearrange("p (b c) -> p b c", c=256)
            w = load_w(l, C_HGF, 512)
            for tB in range(16):
                ps = psb("m")
                proj_tm(w, 0, 512, tB, ps)
                act(ktok[:, tB, :], ps[:, 0:256], AF.Sigmoid, reads=[ps], writes=[big1], scale=-1.0)
                cp("dve", vtok[:, tB, :], ps[:, 256:512], reads=[ps], writes=[vtok])
            for tB in range(16):
                tt("dve", ktok[:, tB, :], ktok[:, tB, :], omlb[:], ALU.mult, reads=[big1, omlb], writes=[big1])
            act(gtok[:], ktok, AF.Ln, reads=[big1], writes=[gtok], bias=1.0, scale=-1.0)
            ebs = [k.sb(f"hgebs{i}", [128, 256]) for i in range(2)]
            for tB in range(16):
                ps = psb("m")
                mm(ps[:, 0:256], trisufF, gtok[:, tB, :], True, True, reads=[constF, gtok], writes=[ps])
                eb = ebs[tB % 2]
                act(eb[:], ps[:, 0:256], AF.Exp, reads=[ps], writes=[eb])
                tt("dve", khat[:, tB, :], ktok[:, tB, :], eb[:], ALU.mult, reads=[big1, eb], writes=[khat])
            if os.environ.get('HG_STOP') == '1':
                return
            qsT = k.sb("hgqsT", [128, 2, T_])
            kT = big1[:].rearrange("p (a t) -> p a t", a=2)
            w2 = load_w(l, C_HGQ, 512)
            for pr in range(2):
                for tb in range(4):
                    sl = slice(tb * 512, (tb + 1) * 512)
                    ps = psb("m")
                    proj_fm(w2, pr * 128, 128, tb, ps)
                    act(qsT[:, pr, sl], ps[:], AF.Silu, reads=[ps], writes=[qsT])
                    ps = psb("m")
                    proj_fm(w2, 256 + pr * 128, 128, tb, ps, extra_reads=[khat])
                    act(kT[:, pr, sl], ps[:], AF.Sigmoid, reads=[ps, khat], writes=[big1], scale=-1.0)
                    ts("dve", kT[:, pr, sl], kT[:, pr, sl], omlT[:, pr:pr + 1], None, ALU.mult, None,
                       reads=[big1, omlT], writes=[big1])
            qtT = k.sb("hgqtT", [128, 2, T_], BF16)
            ktT = k.sb("hgktT", [128, 2, T_], BF16)
            dl = k.sb("hgdl", [128, 2, 64])
            e1s = [k.sb(f"hge1{i}", [128, 512]) for i in range(2)]
            e2s = [k.sb(f"hge2{i}", [128, 512]) for i in range(2)]
            it = 0
            for pr in range(2):
                for g4 in range(4):
                    sl = slice(g4 * 512, (g4 + 1) * 512)
                    ps = psb("a")
                    for bb in range(4):
                        tB = 4 * g4 + bb
                        mm(ps[:, 128 * bb:128 * bb + 128], gtok[:, tB, pr * 128:(pr + 1) * 128], tribdF, True, True,
                           reads=[gtok, constF], writes=[ps])
                    e1, e2 = e1s[it % 2], e2s[it % 2]
                    it += 1
                    act(e1[:], ps[:], AF.Exp, reads=[ps], writes=[e1])
                    act(e2[:], ps[:], AF.Exp, reads=[ps], writes=[e2], scale=-1.0)
                    tt("dve", qtT[:, pr, sl], qsT[:, pr, sl], e1[:], ALU.mult, reads=[qsT, e1], writes=[qtT])
                    tt("dve", ktT[:, pr, sl], kT[:, pr, sl], e2[:], ALU.mult, reads=[big1, e2], writes=[ktT])
                    cp("pool", dl[:, pr, 16 * g4:16 * g4 + 16], e1[:, 31:512:32], reads=[e1], writes=[dl])
            if os.environ.get('HG_STOP') == '2':
                return
            Srun = [k.sb(f"hgS{i}", [128, 5, 2, 64]) for i in range(2)]
            Sbf = [k.sb(f"hgSb{i}", [128, 4, 2, 64], BF16) for i in range(2)]
            Abd = [k.sb(f"hgA{i}", [128, 4, 128], BF16) for i in range(2)]
            o_sb = [k.sb(f"hgo{i}", [128, 4, 64]) for i in range(2)]
            tmp = fin_tmp()
            memset("pool", Srun[1][:, 4, :, :], 0.0, writes=[Srun[1]])
            for B in range(16):
                cur, prev = Srun[B % 2], Srun[(B + 1) % 2]
                bsl = slice(B * 128, (B + 1) * 128)
                psA = psb("s")
                for h in range(4):
                    pr, r0 = h // 2, 64 * (h % 2)
                    mm(psA[:, 128 * h:128 * h + 128], ktT[r0:r0 + 64, pr, bsl], qtT[r0:r0 + 64, pr, bsl], True, True,
                       reads=[ktT, qtT], writes=[psA], ser=True)
                A = Abd[B % 2]
                tt("dve", A[:], psA[:].rearrange("p (h t) -> p h t", h=4),
                   maskbdF.unsqueeze(1).to_broadcast([128, 4, 128]), ALU.mult, reads=[psA, constF], writes=[A])
                psD = psb("a")
                for c in range(4):
                    for h in range(4):
                        pr, r0 = h // 2, 64 * (h % 2)
                        col = (c * 2 + pr) * 64
                        mm(psD[r0:r0 + 64, col:col + 64], khat[32 * c:32 * c + 32, B, 64 * h:64 * h + 64],
                           vtok[32 * c:32 * c + 32, B, 64 * h:64 * h + 64], True, True,
                           reads=[khat, vtok], writes=[psD], tp=(32 * c, r0), ser=True)
                cp("pool", cur[:, 0, :, :], prev[:, 4, :, :], reads=[prev], writes=[cur])
                for c in range(4):
                    for pr in range(2):
                        col = (c * 2 + pr) * 64
                        stt("dve", cur[:, c + 1, pr, :], cur[:, c, pr, :], dl[:, pr, 4 * B + c:4 * B + c + 1],
                            psD[:, col:col + 64], ALU.mult, ALU.add, reads=[cur, dl, psD], writes=[cur])
                Sb = Sbf[B % 2]
                cp("act", Sb[:], cur[:, 0:4, :, :], reads=[cur], writes=[Sb])
                psO = psb("o")
                for h in range(4):
                    mm(psO[:, 64 * h:64 * h + 64], A[:, h, :], vtok[:, B, 64 * h:64 * h + 64], h == 0, False,
                       reads=[A, vtok], writes=[psO], ser=(h == 0))
                for h in range(4):
                    pr, r0 = h // 2, 64 * (h % 2)
                    for c in range(4):
                        mm(psO[32 * c:32 * c + 32, 64 * h:64 * h + 64],
                           qtT[r0:r0 + 64, pr, B * 128 + 32 * c:B * 128 + 32 * c + 32], Sb[r0:r0 + 64, c, pr, :],
                           False, h == 3 and c == 3, reads=[qtT, Sb], writes=[psO], tp=(r0, 32 * c), ser=True)
                o = o_sb[B % 2]
                cp("dve", o[:], psO[:, 0:256].rearrange("p (h d) -> p h d", h=4), reads=[psO], writes=[o])
                finalize(o[:], o, ytok[:, B, 0:256].rearrange("p (h d) -> p h d", h=4), tmp)

    def phase_nsa(s, l):
        with k.scope():
            cmask = k.sb("cmask", [128, T_], BF16)
            ovl = k.sb("ovl", [128, 33], BF16)
            eall = k.sb("eall", [32, T_], BF16)
            ropeC = k.sb("ropeC", [16, T_])
            ropeS = k.sb("ropeS", [16, T_])
            ropeCc = k.sb("ropeCc", [16, 128])
            ropeSc = k.sb("ropeSc", [16, 128])
            permF = k.sb("permF", [64, 16])
            tkA = k.sb("tkA", [128, 16, 32])
            tkB = k.sb("tkB", [128, 16, 32])
            for dst, src in ((cmask, cmask_d), (ovl, ovl_d), (eall, eall_d), (ropeC, ropeC_d), (ropeS, ropeS_d),
                             (ropeCc, ropeCc_d), (ropeSc, ropeSc_d), (permF, permF_d), (tkA, tkA_d), (tkB, tkB_d)):
                k.dma("sp", dst[:], src[:], writes=[dst])
            qT = k.sb("nsqT", [64, 4, T_], BF16)
            ksT = k.sb("nsksT", [64, T_], BF16)
            kwT = k.sb("nskwT", [64, T_], BF16)
            kcT = k.sb("nskcT", [64, T_], BF16)
            vcT = k.sb("nsvcT", [64, T_], BF16)
            Vs = k.sb("nsVs", [128, 16, 65], BF16)
            Vw = k.sb("nsVw", [128, 16, 65], BF16)
            gtok = k.sb("nsg", [128, 16, 12])
            nacc = k.sb("nsacc", [128, 16, 256])
            imp = k.sb("nsimp", [128, 16, 32])
            negT = k.sb("nsnegT", [32, T_], BF16)
            memset("pool", Vs[:, :, 64:65], 1.0, writes=[Vs])
            memset("pool", Vw[:, :, 64:65], 1.0, writes=[Vw])
            kcmpT = k.sb("nskcmpT", [64, 128], BF16)
            rhsc = k.sb("nsrhsc", [128, 97], BF16)
            with k.scope():
                q32s = [k.sb(f"nsq32{i}", [64, 512]) for i in range(2)]
                t1s = [k.sb(f"nst1{i}", [16, 512]) for i in range(2)]
                t2s = [k.sb(f"nst2{i}", [16, 512]) for i in range(2)]
                rc = [0]

                def rope_evac(src, dst, n, scale, Ct, St, extra_tok):
                    i = rc[0] % 2
                    rc[0] += 1
                    q32, t1, t2 = q32s[i], t1s[i], t2s[i]
                    k.op("act", lambda: nc.scalar.mul(out=q32[:, 0:n], in_=src, mul=scale),
                         reads=[extra_tok["src"]], writes=[q32])
                    cp("pool", dst, q32[:, 0:n], reads=[q32], writes=[extra_tok["dst"]])
                    psw = psb("a")
                    mm(psw[0:16, 0:n], permF[:, :], q32[:, 0:n], True, True, reads=[permF, q32], writes=[psw])
                    tt("dve", t1[:, 0:n], q32[0:16, 0:n], Ct, ALU.mult, reads=[q32, extra_tok["tab"]], writes=[t1])
                    tt("dve", t2[:, 0:n], psw[0:16, 0:n], St, ALU.mult, reads=[psw, extra_tok["tab"]], writes=[t2])
                    tt("dve", extra_tok["dst16"], t1[:, 0:n], t2[:, 0:n], ALU.add,
                       reads=[t1, t2], writes=[extra_tok["dst"]])

                w = load_w(l, C_NSQ, 652)
                for pr in range(2):
                    for tb in range(4):
                        sl = slice(tb * 512, (tb + 1) * 512)
                        ps = psb("m")
                        proj_fm(w, pr * 128, 128, tb, ps)
                        for hh in range(2):
                            h = 2 * pr + hh
                            rope_evac(ps[64 * hh:64 * hh + 64, :], qT[0:64, h, sl], 512, 0.125, ropeC[:, sl], ropeS[:, sl],
                                      dict(src=ps, dst=qT, tab=ropeC, dst16=qT[0:16, h, sl]))
                for tb in range(4):
                    sl = slice(tb * 512, (tb + 1) * 512)
                    ps = psb("m")
                    proj_fm(w, 256, 128, tb, ps)
                    cp("act", kcT[:, sl], ps[0:64, :], reads=[ps], writes=[kcT])
                    cp("act", vcT[:, sl], ps[64:128, :], reads=[ps], writes=[vcT])
                for off, dst in ((384, ksT), (512, kwT)):
                    for tb in range(4):
                        sl = slice(tb * 512, (tb + 1) * 512)
                        ps = psb("m")
                        proj_fm(w, off, 64, tb, ps)
                        rope_evac(ps[0:64, :], dst[0:64, sl], 512, 1.0, ropeC[:, sl], ropeS[:, sl],
                                  dict(src=ps, dst=dst, tab=ropeC, dst16=dst[0:16, sl]))
                for tB in range(16):
                    ps = psb("m")
                    proj_tm(w, 448, 204, tB, ps)
                    cp("act", Vs[:, tB, 0:64], ps[:, 0:64], reads=[ps], writes=[Vs])
                    cp("act", Vw[:, tB, 0:64], ps[:, 128:192], reads=[ps], writes=[Vw])
                    act(gtok[:, tB, :], ps[:, 192:204], AF.Sigmoid, reads=[ps], writes=[gtok])
                memset("pool", kcmpT[:], 0.0, writes=[kcmpT])
                memset("pool", rhsc[:], 0.0, writes=[rhsc])
                cp("pool", rhsc[:, 64:97], ovl[:], reads=[ovl], writes=[rhsc])
                for kv, srcT in (("k", kcT), ("v", vcT)):
                    W1 = k.sb(f"nsW1{kv}", [64, 32, 64], BF16)
                    W2 = k.sb(f"nsW2{kv}", [64, 64], BF16)
                    peT = k.sb(f"nspeT{kv}", [64, 32], BF16)
                    k.dma("pool", W1[:], w1_d[kv][l].rearrange("(lp d) j -> d lp j", d=64), writes=[W1])
                    k.dma("pool", W2[:], w2_d[kv][l], writes=[W2])
                    k.dma("pool", peT[:], peT_d[kv][l], writes=[peT])
                    psH = psb("a")
                    for lp in range(32):
                        mm(psH[0:64, 0:127], W1[:, lp, :], srcT[:, lp:lp + 16 * 126 + 1:16], lp == 0, lp == 31,
                           reads=[W1, srcT], writes=[psH])
                    for lp in range(32):
                        mm(psH[0:64, 127:128], W1[:, lp, :], peT[:, lp:lp + 1], lp == 0, lp == 31,
                           reads=[W1, peT], writes=[psH])
                    bias = k.sb(f"nsbias{kv}", [64, 1])
                    hid = k.sb(f"nshid{kv}", [64, 128], BF16)
                    cp("dve", bias[:], psH[0:64, 127:128], reads=[psH], writes=[bias])
                    act(hid[:, 0:127], psH[0:64, 0:127], AF.Silu, reads=[psH, bias], writes=[hid], bias=bias[:, 0:1])
                    ps2 = psb("m")
                    if kv == "k":
                        mm(ps2[0:64, 0:127], W2[:, :], hid[:, 0:127], True, True, reads=[W2, hid], writes=[ps2])
                        rope_evac(ps2[0:64, 0:127], kcmpT[0:64, 0:127], 127, 1.0, ropeCc[:, 0:127], ropeSc[:, 0:127],
                                  dict(src=ps2, dst=kcmpT, tab=ropeCc, dst16=kcmpT[0:16, 0:127]))
                    else:
                        mm(ps2[0:127, 0:64], hid[:, 0:127], W2[:, :], True, True, reads=[W2, hid], writes=[ps2])
                        cp("act", rhsc[0:127, 0:64], ps2[0:127, 0:64], reads=[ps2], writes=[rhsc])
            pT = [k.sb(f"nspT{i}", [128, 512], BF16) for i in range(3)]
            rden = [k.sb(f"nsrd{i}", [128, 4]) for i in range(2)]
            cf = [k.sb(f"nscf{i}", [128, 4]) for i in range(2)]
            tmpo = [k.sb(f"nstmpo{i}", [128, 4, 64]) for i in range(2)]
            tmpi = [k.sb(f"nstmpi{i}", [128, 4, 32]) for i in range(2)]
            it = 0
            fi = 0
            for h in range(4):
                for qb in range(4):
                    qsl = slice(qb * 512, (qb + 1) * 512)
                    ps = psb("s")
                    mm(ps[:], kcmpT[:, :], qT[0:64, h, qsl], True, False, reads=[kcmpT, qT], writes=[ps])
                    mm(ps[:], identB, cmask[:, qsl], False, True, reads=[constB, cmask], writes=[ps])
                    p = pT[it % 3]
                    it += 1
                    act(p[:], ps[:], AF.Exp, reads=[ps], writes=[p])
                    acc = psb("o")
                    accv = acc[:, 0:388].rearrange("p (q d) -> p q d", q=4)
                    for qs in range(4):
                        mm(accv[:, qs, :], p[:, qs * 128:(qs + 1) * 128], rhsc[:, :], qs == 0, True,
                           reads=[p, rhsc], writes=[acc])
                    rd, c_ = rden[fi % 2], cf[fi % 2]
                    to, ti = tmpo[fi % 2], tmpi[fi % 2]
                    fi += 1
                    ts("dve", rd[:], accv[:, :, 64], 1e-30, None, ALU.max, None, reads=[acc], writes=[rd])
                    recip(rd[:], rd[:], reads=[rd], writes=[rd])
                    tt("dve", c_[:], rd[:], gtok[:, 4 * qb:4 * qb + 4, h], ALU.mult, reads=[rd, gtok], writes=[c_])
                    tt("dve", nacc[:, 4 * qb:4 * qb + 4, 64 * h:64 * h + 64], accv[:, :, 0:64],
                       c_[:].unsqueeze(2).to_broadcast([128, 4, 64]), ALU.mult, reads=[acc, c_], writes=[nacc])
                    if h == 0:
                        tt("dve", imp[:, 4 * qb:4 * qb + 4, :], accv[:, :, 65:97],
                           rd[:].unsqueeze(2).to_broadcast([128, 4, 32]), ALU.mult, reads=[acc, rd], writes=[imp])
                    else:
                        tt("dve", ti[:], accv[:, :, 65:97], rd[:].unsqueeze(2).to_broadcast([128, 4, 32]), ALU.mult,
                           reads=[acc, rd], writes=[ti])
                        tt("pool", imp[:, 4 * qb:4 * qb + 4, :], imp[:, 4 * qb:4 * qb + 4, :], ti[:], ALU.add,
                           reads=[imp, ti], writes=[imp])
            score = k.sb("nsscore", [128, 16, 32])
            negm = k.sb("nsnegm", [128, 16, 32])
            mx = [k.sb(f"nsmx{i}", [128, 8]) for i in range(2)]
            sc2 = k.sb("nssc2", [128, 32])
            tt("dve", score[:], imp[:], tkA[:], ALU.mult, reads=[imp, tkA], writes=[score])
            tt("dve", score[:], score[:], tkB[:], ALU.add, reads=[score, tkB], writes=[score])
            for tB in range(16):
                k.op("dve", lambda: nc.vector.max(out=mx[0][:], in_=score[:, tB, :]), reads=[score], writes=[mx[0]])
                k.op("dve", lambda: nc.vector.match_replace(out=sc2[:], in_to_replace=mx[0][:], in_values=score[:, tB, :],
                                                            imm_value=-1.0e9), reads=[score, mx[0]], writes=[sc2])
                k.op("dve", lambda: nc.vector.max(out=mx[1][:], in_=sc2[:]), reads=[sc2], writes=[mx[1]])
                ts("dve", negm[:, tB, :], score[:, tB, :], mx[1][:, 7:8], NEG, ALU.is_lt, ALU.mult,
                   reads=[score, mx[1]], writes=[negm])
            for g4 in range(4):
                ps = psb("m")
                for bb in range(4):
                    mm(ps[0:32, 128 * bb:128 * bb + 128], negm[:, 4 * g4 + bb, :], identF, True, True,
                       reads=[negm, constF], writes=[ps])
                cp("act", negT[:, 512 * g4:512 * g4 + 512], ps[0:32, :], reads=[ps], writes=[negT])

            def caus_mask(qb, kb):
                if kb >= 4 * qb:
                    r = kb - 4 * qb
                    return strips[:, 0, 384 - 128 * r:384 - 128 * r + 512]
                return None

            def win_mask(qb, kb):
                r = kb - 4 * qb
                if r >= 0:
                    return strips[:, 0, 384 - 128 * r:384 - 128 * r + 512]
                return strips[:, 2, 384 - 128 * (r + 4):384 - 128 * (r + 4) + 512]

            def br_stream(si, bi, KTt, Vt, kb_range, mask_fn, use_sel, qs_range):
                pTs = [k.sb(f"nsbp{si}_{i}", [128, 512], BF16) for i in range(2)]
                rd = k.sb(f"nsbrd{si}", [128, 4])
                c_ = k.sb(f"nsbcf{si}", [128, 4])
                to = k.sb(f"nsbto{si}", [128, 4, 64])

                def masks(h, qb, kb):
                    ml = []
                    ksl = slice(kb * 128, (kb + 1) * 128)
                    if use_sel and qb >= 2:
                        ml.append((eall[0:32, ksl], negT[0:32, qb * 512:(qb + 1) * 512], [eall, negT]))
                    mk = mask_fn(qb, kb)
                    if mk is not None:
                        ml.append((identB, mk, [constB, strips]))
                    return ml

                def fin(h, qb, acc, accv):
                    recip(rd[:], accv[:, :, 64], reads=[acc], writes=[rd])
                    tt("dve", c_[:], rd[:], gtok[:, 4 * qb:4 * qb + 4, 4 * bi + h], ALU.mult, reads=[rd, gtok], writes=[c_])
                    tt("dve", to[:], accv[:, :, 0:64], c_[:].unsqueeze(2).to_broadcast([128, 4, 64]), ALU.mult,
                       reads=[acc, c_], writes=[to])
                    tt("pool", nacc[:, 4 * qb:4 * qb + 4, 64 * h:64 * h + 64],
                       nacc[:, 4 * qb:4 * qb + 4, 64 * h:64 * h + 64], to[:], ALU.add, reads=[nacc, to], writes=[nacc])

                return attn_stream(
                    [(h, qb) for h in range(4) for qb in range(4)],
                    [PS[2 * si], PS[2 * si + 1]], PS[4 + si], pTs,
                    lambda h, kb: KTt[0:64, kb * 128:(kb + 1) * 128],
                    lambda h, qb: qT[0:64, h, qb * 512:(qb + 1) * 512], [KTt, qT],
                    masks, None, [],
                    lambda h, kb: Vt[:, kb, :], [Vt],
                    kb_range, qs_range, 65, fin)

            run_streams([
                br_stream(0, 1, ksT, Vs, lambda qb: range(0, 4 * qb + 4), caus_mask, True,
                          lambda qb, qs: (0, 4 * qb + qs)),
                br_stream(1, 2, kwT, Vw, lambda qb: range(max(0, 4 * qb - 4), 4 * qb + 4), win_mask, False,
                          lambda qb, qs: (max(0, 4 * qb + qs - 4), 4 * qb + qs)),
            ])
            tmp = fin_tmp(16)
            for g4 in range(4):
                for bb in range(4):
                    finalize(nacc[:, 4 * g4 + bb, :].rearrange("p (h d) -> p h d", h=4), nacc,
                             ytok[:, 4 * g4 + bb, 768:1024].rearrange("p (h d) -> p h d", h=4), tmp, n=4)

    def phase_out(s, l, last):
        xsrc = xT_d if l == 0 else xres_d
        with k.scope():
            wg = load_w(l, C_GATE, 1024, "wg")
            ong = k.sb("ong", [128, D_])
            k.dma("sp", ong[:], ong_d[l:l + 1, :].to_broadcast([128, D_]), writes=[ong])
            yT = k.sb("yT", [128, 8, T_], BF16)
            wo = k.sb("wo", [128, 8, D_], BF16)
            wsrc = w_out_d[l].rearrange("(j p) c -> p j c", p=128)
            k.dma("pool", wo[:, 0:4, :], wsrc[:, 0:4, :], writes=[wo])
            k.dma("pool", wo[:, 4:8, :], wsrc[:, 4:8, :], writes=[wo])
            sg = [k.sb(f"sg{i}", [128, D_]) for i in range(2)]
            yg = [k.sb(f"yg{i}", [128, D_], BF16) for i in range(2)]
            for tB in range(16):
                sgi, ygi = sg[tB % 2], yg[tB % 2]
                for half in range(2):
                    ps = psb("m") if half == 0 else psb("a")
                    proj_tm(wg, half * 512, 512, tB, ps)
                    act(sgi[:, half * 512:(half + 1) * 512], ps[:], AF.Silu, reads=[ps], writes=[sgi])
                tt("pool", sgi[:], sgi[:], ong[:], ALU.mult, reads=[sgi, ong], writes=[sgi])
                tt("dve", ygi[:], sgi[:], ytok[:, tB, :], ALU.mult, reads=[sgi, ytok], writes=[ygi])
                for cc in range(8):
                    k.op("pe", lambda: nc.tensor.transpose(PST[:, cc * 128:(cc + 1) * 128],
                                                           ygi[:, cc * 128:(cc + 1) * 128], identB),
                         reads=[ygi, constB], writes=[PST])
                cp("act", yT[:, :, tB * 128:(tB + 1) * 128], PST[:].rearrange("p (c t) -> p c t", c=8),
                   reads=[PST], writes=[yT])
            xin = [k.sb(f"xo{i}", [128, 8, 512]) for i in range(2)]
            if last:
                sqf = k.sb("sqf", [128, 8, 512], BF16)
                rsf = k.sb("rsf", [128, 512])
            for tb in range(4):
                sl = slice(tb * 512, (tb + 1) * 512)
                xi = xin[tb % 2]
                src = xsrc[s].rearrange("(j p) t -> p j t", p=128)[:, :, sl]
                k.dma("sp", xi[:], src, reads=[xsrc], writes=[xi])
                for dj in range(8):
                    ps = psb("s")
                    for cc in range(8):
                        mm(ps[:], wo[:, cc, dj * 128:(dj + 1) * 128], yT[:, cc, sl], cc == 0, cc == 7,
                           reads=[wo, yT], writes=[ps])
                    tt("dve", xi[:, dj, :], xi[:, dj, :], ps[:], ALU.add, reads=[xi, ps], writes=[xi])
                if not last:
                    dst = xres_d[s].rearrange("(j p) t -> p j t", p=128)[:, :, sl]
                    k.dma("sp", dst, xi[:], reads=[xi], writes=[xres_d])
                else:
                    act(sqf[:], xi[:], AF.Square, reads=[xi], writes=[sqf])
                    ps = psb("m")
                    for j in range(8):
                        mm(ps[:], onesB, sqf[:, j, :], j == 0, j == 7, reads=[constB, sqf], writes=[ps])
                    act(rsf[:], ps[:], AF.Sqrt, reads=[ps], writes=[rsf], bias=EPS, scale=1.0 / D_)
                    recip(rsf[:], rsf[:], reads=[rsf], writes=[rsf])
                    for j in range(8):
                        stt("dve", xi[:, j, :], xi[:, j, :], fnormg[:, j:j + 1], rsf[:], ALU.mult, ALU.mult,
                            reads=[xi, rsf, fnormg], writes=[xi])
                    dst = outT_d[s].rearrange("(j p) t -> p j t", p=128)[:, :, sl]
                    k.dma("sp", dst, xi[:], reads=[xi], writes=[outT_d])

    for s in range(nseq):
        for l in range(nlayer):
            phase_norm(s, l)
            if debug and s == 0 and l == 0 and "hT" in debug:
                d = dbg_out("dbg_hT", [128, 8, T_], BF16)
                k.dma("sp", d[:], hT[:], reads=[hT], writes=[d])
            if "hg" in mixers:
                phase_hgrn(s, l)
            if "fx" in mixers:
                phase_fox(s, l)
            if "sb" in mixers:
                phase_sb(s, l)
            if "ns" in mixers:
                phase_nsa(s, l)
            if debug and s == 0 and l == nlayer - 1 and "ytok" in debug:
                d = dbg_out("dbg_ytok", [128, 16, D_], BF16)
                k.dma("sp", d[:], ytok[:], reads=[ytok], writes=[d])
            if "out" in mixers:
                phase_out(s, l, l == nlayer - 1)

    k.finish(list(dbg_outs.values()) + [outT_d])
    k.barrier()
    k.close()
    return nc, k, list(dbg_outs.keys())


def host_inputs(inputs, core, consts, nseq=SEQ_PER_CORE):
    f32 = np.float32
    x = inputs["x"]
    b0 = core * nseq
    m = {}
    m["xT"] = np.ascontiguousarray(np.transpose(x[b0:b0 + nseq], (0, 2, 1))).astype(f32)
    m["w_in"] = np.ascontiguousarray(inputs["w_in"], dtype=f32)
    m["w_out"] = np.ascontiguousarray(inputs["w_out"], dtype=f32)
    m["norm_gT"] = np.ascontiguousarray(inputs["norm_g"].reshape(2, 8, 128).transpose(0, 2, 1), dtype=f32)
    m["fnorm_gT"] = np.ascontiguousarray(inputs["final_norm_g"].reshape(8, 128).T, dtype=f32)
    m["out_norm_g"] = np.ascontiguousarray(inputs["out_norm_g"], dtype=f32)
    lbl = np.asarray(inputs["hgrn_lb_logits"], dtype=f32)
    m["lb_logits"] = np.ascontiguousarray(lbl)
    m["lb_logitsT"] = np.ascontiguousarray(lbl.reshape(2, 2, 128).transpose(2, 1, 0))
    m["fox_fb"] = np.ascontiguousarray(inputs["fox_fb"], dtype=f32)
    for kv in "kv":
        m[f"peT_{kv}"] = np.ascontiguousarray(np.transpose(inputs[f"nsa_cmp_pe_{kv}"], (0, 2, 1)), dtype=f32)
        m[f"w1_{kv}"] = np.ascontiguousarray(inputs[f"nsa_cmp_w1_{kv}"], dtype=f32)
        m[f"w2_{kv}"] = np.ascontiguousarray(inputs[f"nsa_cmp_w2_{kv}"], dtype=f32)
    for nm in ["constF", "constB", "strips", "cmask", "ovl", "eall", "ropeC", "ropeS", "ropeCc", "ropeSc",
               "permF", "tkA", "tkB"]:
        m[nm] = consts[nm]
    return m


def kernel(**inputs):
    inputs = {kk: np.asarray(v) for kk, v in inputs.items()}
    consts = make_consts()
    nc, kb, _ = build()
    in_maps = [host_inputs(inputs, c, consts) for c in range(NCORES)]
    res = run_bass_kernel_spmd(nc, in_maps, core_ids=list(range(NCORES)))
    outs = [np.asarray(r["outT"]) for r in res.results]
    full = np.concatenate(outs, axis=0)
    return np.ascontiguousarray(np.transpose(full, (0, 2, 1))).astype(np.float32)
```

```python
import os
import numpy as np
import ml_dtypes
from contextlib import ExitStack
import concourse.bass as bass
import concourse.mybir as mybir
from concourse.bass_utils import run_bass_kernel_spmd

F32 = mybir.dt.float32
BF16 = mybir.dt.bfloat16
AF = mybir.ActivationFunctionType
ALU = mybir.AluOpType
AX = mybir.AxisListType

T_ = 2048
D_ = 1024
NIN = 3984
NEG = -30000.0
EPS = 1e-6
NCORES = 8
SEQ_PER_CORE = 4

C_HGQ, C_HGF, C_HGI = 0, 256, 512
C_FXQ, C_FXK, C_FXV, C_FXF = 768, 1024, 1280, 1536
C_SBQ, C_SBK, C_SBV = 1540, 1796, 2052
C_NSQ = 2308
C_NKC, C_NVC, C_NKS, C_NVS, C_NKW, C_NVW = 2564, 2628, 2692, 2756, 2820, 2884
C_NSG = 2948
C_GATE = 2960


class Tok:
    __slots__ = ("w", "r", "name")

    def __init__(self, name=""):
        self.w = None
        self.r = {}
        self.name = name


class T:
    def __init__(self, h, name, excl=False):
        self.h = h
        self.tok = Tok(name)
        self.name = name
        self.excl = excl

    def __getitem__(self, k):
        return self.h[k]


class KB:
    NDQ = 8
    EPOCH_LIMIT = 30000

    def __init__(self, nc):
        self.nc = nc
        self.stack = [ExitStack()]
        self.E = {"pe": nc.tensor, "act": nc.scalar, "dve": nc.vector,
                  "pool": nc.gpsimd, "sp": nc.sync}
        self.semh = {}
        self.cur = {}
        self.cnt = {}
        self.epoch = {}
        for e in ["pe", "act", "dve", "pool"]:
            self.epoch[e] = -1
            self._new_epoch(e)
        self.dq = {}
        for q in ["sp", "pool", "act"]:
            sems = [self.stack[0].enter_context(nc.semaphore(f"d_{q}{i}")) for i in range(self.NDQ)]
            self.dq[q] = dict(n=0)
            for i, s in enumerate(sems):
                self.semh[("dma", q, i)] = s
        self.seen = {}
        self.nins = {e: 0 for e in self.E}
        self.uid = 0

    def _new_epoch(self, e):
        self.epoch[e] += 1
        key = (e, self.epoch[e])
        self.semh[key] = self.stack[0].enter_context(self.nc.semaphore(f"s_{e}{self.epoch[e]}"))
        self.cur[e] = key
        self.cnt[e] = 0

    def scope(self):
        kb = self

        class _S:
            def __enter__(s):
                kb.stack.append(ExitStack())

            def __exit__(s, *a):
                kb.barrier()
                kb.stack.pop().close()
                return False
        return _S()

    def _nm(self, name):
        self.uid += 1
        return f"{name}_{self.uid}"

    def sb(self, name, shape, dt=F32):
        h = self.stack[-1].enter_context(self.nc.sbuf_tensor(self._nm(name), list(shape), dt))
        return T(h, name)

    def ps(self, name, shape, dt=F32):
        h = self.stack[-1].enter_context(self.nc.psum_tensor(self._nm(name), list(shape), dt))
        return T(h, name, excl=True)

    def dram(self, name, shape, dt, kind):
        h = self.nc.dram_tensor(name, list(shape), dt, kind=kind)
        return T(h, name)

    def _wait(self, eng, deps):
        for key, val in deps.items():
            if key[0] == "pe" and eng == "pe":
                continue
            sk = (eng, key)
            if self.seen.get(sk, 0) >= val:
                continue
            self.seen[sk] = val
            self.E[eng].wait_ge(self.semh[key], val)
            self.nins[eng] += 1

    @staticmethod
    def _tok(x):
        return x.tok if isinstance(x, T) else x

    def _deps(self, reads, writes):
        deps = {}
        for t in reads:
            t = self._tok(t)
            if t.w and deps.get(t.w[0], 0) < t.w[1]:
                deps[t.w[0]] = t.w[1]
        for t in writes:
            t = self._tok(t)
            if t.w and deps.get(t.w[0], 0) < t.w[1]:
                deps[t.w[0]] = t.w[1]
            for kk, v in t.r.items():
                if deps.get(kk, 0) < v:
                    deps[kk] = v
        return deps

    def _mark(self, ev, reads, writes):
        kk, v = ev
        for t in reads:
            t = self._tok(t)
            if t.r.get(kk, 0) < v:
                t.r[kk] = v
        for t in writes:
            t = self._tok(t)
            t.w = ev
            t.r = {}

    def op(self, eng, fn, reads=(), writes=()):
        ex = [t for t in reads if isinstance(t, T) and t.excl]
        if ex:
            reads = [t for t in reads if not (isinstance(t, T) and t.excl)]
            writes = list(writes) + ex
        self._wait(eng, self._deps(reads, writes))
        if self.cnt[eng] >= self.EPOCH_LIMIT:
            self._new_epoch(eng)
        ins = fn()
        self.cnt[eng] += 1
        ins.then_inc(self.semh[self.cur[eng]], 1)
        self.nins[eng] += 1
        self._mark((self.cur[eng], self.cnt[eng]), reads, writes)
        return ins

    def pe_selfwait(self):
        key, val = self.cur["pe"], self.cnt["pe"]
        if val > 0 and self.seen.get(("pe", key), 0) < val:
            self.seen[("pe", key)] = val
            self.E["pe"].wait_ge(self.semh[key], val)
            self.nins["pe"] += 1

    def dma(self, q, out, in_, reads=(), writes=(), **kw):
        self._wait(q, self._deps(reads, writes))
        d = self.dq[q]
        slot = d["n"] % self.NDQ
        val = 16 * (d["n"] // self.NDQ + 1)
        d["n"] += 1
        ins = self.E[q].dma_start(out=out, in_=in_, **kw)
        ins.then_inc(self.semh[("dma", q, slot)], 16)
        self.nins[q] += 1
        self._mark((("dma", q, slot), val), reads, writes)
        return ins

    def _all_events(self):
        ev = {}
        for e in ["pe", "act", "dve", "pool"]:
            if self.cnt[e] > 0:
                ev[self.cur[e]] = self.cnt[e]
        for q, d in self.dq.items():
            n = d["n"]
            for slot in range(self.NDQ):
                c = (n - slot + self.NDQ - 1) // self.NDQ
                if c > 0:
                    ev[("dma", q, slot)] = 16 * c
        return ev

    def barrier(self):
        ev = self._all_events()
        for e in ["pe", "act", "dve", "pool", "sp"]:
            self._wait(e, {kk: v for kk, v in ev.items() if not (kk[0] == e)})

    def finish(self, toks, eng="sp"):
        deps = {}
        for t in toks:
            t = self._tok(t)
            if t.w and deps.get(t.w[0], 0) < t.w[1]:
                deps[t.w[0]] = t.w[1]
        self._wait(eng, deps)

    def close(self):
        while self.stack:
            self.stack.pop().close()


def _bf(a):
    return np.asarray(a, dtype=np.float32).astype(ml_dtypes.bfloat16)


def make_consts():
    c = {}
    i = np.arange(128)[:, None]
    j = np.arange(128)[None, :]
    c["identF"] = np.eye(128, dtype=np.float32)
    c["triF"] = (i <= j).astype(np.float32)
    c["onesF"] = np.ones((128, 128), np.float32)
    c["tribdF"] = ((i // 32 == j // 32) & (i <= j)).astype(np.float32)
    c["trisufF"] = ((i // 32 == j // 32) & (i > j)).astype(np.float32)
    c["maskbdF"] = ((i // 32 == j // 32) & (i <= j)).astype(np.float32)
    perm = np.zeros((64, 16), np.float32)
    for a in range(16):
        perm[a + 8 if a < 8 else a - 8, a] = 1.0
    c["permF"] = perm
    cf = np.concatenate([c["identF"], c["triF"], c["onesF"], c["tribdF"], c["trisufF"], c["maskbdF"]], axis=1)
    c["constF"] = cf
    negtri = -(i >= j).astype(np.float32)
    neglow = -(i < j).astype(np.float32)
    c["constB"] = _bf(np.concatenate([np.eye(128), np.ones((128, 128)), negtri, neglow], axis=1))
    jj = np.arange(896)[None, :] - 384
    caus = np.where(jj < 0, NEG, np.where(jj >= 128, 0.0, np.where(jj >= i, 0.0, NEG)))
    strict = np.where(jj < 0, NEG, np.where(jj >= 128, 0.0, np.where(jj > i, 0.0, NEG)))
    win = np.where(jj < 0, 0.0, np.where(jj >= 128, NEG, np.where(jj < i, 0.0, NEG)))
    c["strips"] = _bf(np.stack([caus, strict, win], axis=1))
    n = np.arange(128)[:, None]
    t = np.arange(T_)[None, :]
    c["cmask"] = _bf(np.where((16 * n + 31 <= t) & (n < 127), 0.0, NEG))
    cs = (np.arange(128) * 16)[:, None]
    ss = (np.arange(32) * 64)[None, :]
    ov = np.clip(np.minimum(cs + 32, ss + 64) - np.maximum(cs, ss), 0, None).astype(np.float32) / 32.0
    ovl = np.concatenate([np.ones((128, 1), np.float32), ov], axis=1)
    ovl[127] = 0.0
    c["ovl"] = _bf(ovl)
    c["eall"] = _bf((np.arange(T_)[None, :] // 64 == np.arange(32)[:, None]).astype(np.float32))
    half = 8
    inv = (500000.0 ** (-(np.arange(half, dtype=np.float32) * 2.0 / 16.0))).astype(np.float32)

    def tabs(pos):
        ang = pos.astype(np.float32)[None, :] * inv[:, None]
        cos, sin = np.cos(ang), np.sin(ang)
        return (np.concatenate([cos, cos], 0).astype(np.float32),
                np.concatenate([-sin, sin], 0).astype(np.float32))
    C, S = tabs(np.arange(T_))
    c["ropeC"], c["ropeS"] = C, S
    pc = np.arange(128) * 16 + 31
    Cc, Sc = tabs(pc)
    c["ropeCc"], c["ropeSc"] = Cc, Sc
    tt_ = np.arange(T_)
    qblk = tt_ // 64
    blk = np.arange(32)[None, :]
    forced = (blk == 0) | (blk == qblk[:, None]) | (blk == qblk[:, None] - 1)
    valid = blk <= qblk[:, None]
    A = (~forced & valid).astype(np.float32)
    Bc = np.where(forced, 1.0e4, np.where(valid, 0.0, -1.0e4)).astype(np.float32)
    c["tkA"] = A.reshape(16, 128, 32).transpose(1, 0, 2).copy()
    c["tkB"] = Bc.reshape(16, 128, 32).transpose(1, 0, 2).copy()
    return c


def build(nseq=SEQ_PER_CORE, nlayer=2, mixers=("hg", "fx", "sb", "ns", "out"), debug=None):
    nc = bass.Bass("TRN2", target_bir_lowering=False)
    k = KB(nc)
    dbg_outs = {}

    def din(name, shape, dt=F32):
        return k.dram(name, shape, dt, "ExternalInput")

    xT_d = din("xT", [nseq, D_, T_])
    w_in_d = din("w_in", [2, D_, NIN])
    w_out_d = din("w_out", [2, D_, D_])
    normg_d = din("norm_gT", [2, 128, 8])
    fnormg_d = din("fnorm_gT", [128, 8])
    ong_d = din("out_norm_g", [2, D_])
    lbl_d = din("lb_logits", [2, 256])
    lblT_d = din("lb_logitsT", [128, 2, 2])
    fb_d = din("fox_fb", [2, 4])
    peT_d = {kv: din(f"peT_{kv}", [2, 64, 32]) for kv in "kv"}
    w1_d = {kv: din(f"w1_{kv}", [2, 2048, 64]) for kv in "kv"}
    w2_d = {kv: din(f"w2_{kv}", [2, 64, 64]) for kv in "kv"}
    constF_d = din("constF", [128, 768])
    constB_d = din("constB", [128, 512], BF16)
    strips_d = din("strips", [128, 3, 896], BF16)
    cmask_d = din("cmask", [128, T_], BF16)
    ovl_d = din("ovl", [128, 33], BF16)
    eall_d = din("eall", [32, T_], BF16)
    ropeC_d = din("ropeC", [16, T_])
    ropeS_d = din("ropeS", [16, T_])
    ropeCc_d = din("ropeCc", [16, 128])
    ropeSc_d = din("ropeSc", [16, 128])
    permF_d = din("permF", [64, 16])
    tkA_d = din("tkA", [128, 16, 32])
    tkB_d = din("tkB", [128, 16, 32])
    outT_d = k.dram("outT", [nseq, D_, T_], F32, "ExternalOutput")
    xres_d = k.dram("xres", [nseq, D_, T_], F32, "Internal")

    def dbg_out(name, shape, dt=F32):
        d = k.dram(name, shape, dt, "ExternalOutput")
        dbg_outs[name] = d
        return d

    constF = k.sb("constF", [128, 768])
    constB = k.sb("constB", [128, 512], BF16)
    strips = k.sb("strips", [128, 3, 896], BF16)
    k.dma("sp", constF[:], constF_d[:], writes=[constF])
    k.dma("sp", constB[:], constB_d[:], writes=[constB])
    k.dma("sp", strips[:], strips_d[:], writes=[strips])
    identF = constF[:, 0:128]
    triF = constF[:, 128:256]
    onesF = constF[:, 256:384]
    tribdF = constF[:, 384:512]
    trisufF = constF[:, 512:640]
    maskbdF = constF[:, 640:768]
    identB = constB[:, 0:128]
    onesB = constB[:, 128:256]
    negtriB = constB[:, 256:384]
    neglowB = constB[:, 384:512]

    hT = k.sb("hT", [128, 8, T_], BF16)
    ytok = k.sb("ytok", [128, 16, D_], BF16)
    normg = k.sb("normg", [128, 2, 8])
    fnormg = k.sb("fnormg", [128, 8])
    for l in range(2):
        k.dma("sp", normg[:, l, :], normg_d[l], writes=[normg])
    k.dma("sp", fnormg[:], fnormg_d[:], writes=[fnormg])

    PS = [k.ps(f"ps{i}", [128, 512]) for i in range(7)]
    PST = k.ps("pst", [128, 1024], BF16)
    ps_rr = {"s": [0, 1], "a": [2, 3], "o": [4, 5], "m": [6]}
    ps_ctr = {kk: 0 for kk in ps_rr}

    def psb(role):
        lst = ps_rr[role]
        i = lst[ps_ctr[role] % len(lst)]
        ps_ctr[role] += 1
        return PS[i]

    def mm(out, lhsT, rhs, start, stop, reads, writes, tp=None, ser=False):
        kw = {}
        if ser:
            k.pe_selfwait()
        if tp is not None:
            kw["tile_position"] = tp
        return k.op("pe", lambda: nc.tensor.matmul(out, lhsT=lhsT, rhs=rhs, start=start, stop=stop, **kw),
                    reads=reads, writes=writes)

    def act(out, in_, func, reads, writes, bias=None, scale=None):
        kw = {}
        if bias is not None:
            kw["bias"] = bias
        if scale is not None:
            kw["scale"] = scale
        return k.op("act", lambda: nc.scalar.activation(out=out, in_=in_, func=func, **kw),
                    reads=reads, writes=writes)

    def tt(eng, out, in0, in1, op, reads, writes):
        e = k.E[eng]
        return k.op(eng, lambda: e.tensor_tensor(out=out, in0=in0, in1=in1, op=op), reads=reads, writes=writes)

    def ts(eng, out, in0, s1, s2, op0, op1, reads, writes):
        e = k.E[eng]
        if s2 is None:
            return k.op(eng, lambda: e.tensor_scalar(out=out, in0=in0, scalar1=s1, scalar2=None, op0=op0),
                        reads=reads, writes=writes)
        return k.op(eng, lambda: e.tensor_scalar(out=out, in0=in0, scalar1=s1, scalar2=s2, op0=op0, op1=op1),
                    reads=reads, writes=writes)

    def stt(eng, out, in0, scalar, in1, op0, op1, reads, writes):
        e = k.E[eng]
        return k.op(eng, lambda: e.scalar_tensor_tensor(out=out, in0=in0, scalar=scalar, in1=in1, op0=op0, op1=op1),
                    reads=reads, writes=writes)

    def cp(eng, out, in_, reads, writes):
        if eng == "act":
            return k.op("act", lambda: nc.scalar.copy(out=out, in_=in_), reads=reads, writes=writes)
        e = k.E[eng]
        return k.op(eng, lambda: e.tensor_copy(out=out, in_=in_), reads=reads, writes=writes)

    def memset(eng, out, val, writes):
        e = k.E[eng]
        return k.op(eng, lambda: e.memset(out, val), writes=writes)

    def recip(out, in_, reads, writes):
        return k.op("dve", lambda: nc.vector.reciprocal(out=out, in_=in_), reads=reads, writes=writes)

    def load_w(l, c0, ncols, name="wblk"):
        w = k.sb(name, [128, 8, ncols], BF16)
        src = w_in_d[l][:, c0:c0 + ncols].rearrange("(j p) c -> p j c", p=128)
        half = 4
        k.dma("pool", w[:, 0:half, :], src[:, 0:half, :], writes=[w])
        k.dma("pool", w[:, half:8, :], src[:, half:8, :], writes=[w])
        return w

    def proj_fm(w, off, M, tb, ps, extra_reads=()):
        for j in range(8):
            mm(ps[0:M, :], w[:, j, off:off + M], hT[:, j, tb * 512:(tb + 1) * 512], j == 0, j == 7,
               reads=[w, hT, *extra_reads], writes=[ps])

    def proj_tm(w, off, N, tB, ps):
        for j in range(8):
            mm(ps[:, 0:N], hT[:, j, tB * 128:(tB + 1) * 128], w[:, j, off:off + N], j == 0, j == 7,
               reads=[w, hT], writes=[ps])

    def finalize(o, o_tok, out_ap, tmp, n=4):
        sq, ss, sd = tmp
        tt("dve", sq[:, 0:n, :], o, o, ALU.mult, reads=[o_tok], writes=[sq])
        k.op("dve", lambda: nc.vector.tensor_reduce(out=ss[:, 0:n], in_=sq[:, 0:n, :], axis=AX.X, op=ALU.add),
             reads=[sq], writes=[ss])
        act(sd[:, 0:n], ss[:, 0:n], AF.Ln, reads=[ss], writes=[sd], bias=EPS, scale=1.0 / 64.0)
        act(sd[:, 0:n], sd[:, 0:n], AF.Exp, reads=[sd], writes=[sd], scale=-0.5)
        tt("dve", out_ap, o, sd[:, 0:n].unsqueeze(2).to_broadcast([128, n, 64]), ALU.mult,
           reads=[o_tok, sd], writes=[ytok])

    def fin_tmp(n=4):
        return (k.sb("sq", [128, n, 64]), k.sb("ss", [128, n]), k.sb("sd", [128, n]))


    def run_streams(gens):
        gens = list(gens)
        while gens:
            for g in list(gens):
                try:
                    next(g)
                except StopIteration:
                    gens.remove(g)

    def attn_stream(jobs, sbank, obank, pTs, K_ap, Q_ap, kq_toks, masks, bias_ap, bias_toks, V_ap, v_toks,
                    kb_range, qs_range, ncol, fin):
        tiles = []
        for (h, qb) in jobs:
            kbs = list(kb_range(qb))
            for kb in kbs:
                tiles.append((h, qb, kb, kb == kbs[0], kb == kbs[-1]))
        nsb = len(sbank)

        def emit_qk(i):
            h, qb, kb, _, _ = tiles[i]
            ps = sbank[i % nsb]
            ml = masks(h, qb, kb)
            mm(ps[:], K_ap(h, kb), Q_ap(h, qb), True, len(ml) == 0, reads=kq_toks, writes=[ps])
            for mi, (lt, rh, rd) in enumerate(ml):
                mm(ps[:], lt, rh, False, mi == len(ml) - 1, reads=rd, writes=[ps])

        emit_qk(0)
        first = True
        accv = obank[:, 0:4 * ncol].rearrange("p (q d) -> p q d", q=4)
        for i, (h, qb, kb, isfirst, islast) in enumerate(tiles):
            if nsb > 1 and i + 1 < len(tiles):
                emit_qk(i + 1)
            yield
            ps = sbank[i % nsb]
            p = pTs[i % len(pTs)]
            if isfirst:
                first = True
            b = bias_ap(h, kb) if bias_ap is not None else None
            act(p[:], ps[:], AF.Exp, reads=[ps, *bias_toks], writes=[p], bias=b)
            for qs in range(4):
                lo, hi = qs_range(qb, qs)
                if kb < lo or kb > hi:
                    continue
                mm(accv[:, qs, :], p[:, qs * 128:(qs + 1) * 128], V_ap(h, kb), first, kb == hi,
                   reads=[p, *v_toks], writes=[obank])
                first = False
            if islast:
                fin(h, qb, obank, accv)
            if nsb == 1 and i + 1 < len(tiles):
                emit_qk(i + 1)
            yield

    def phase_norm(s, l):
        xsrc = xT_d if l == 0 else xres_d
        with k.scope():
            xin = [k.sb(f"xin{i}", [128, 8, 512]) for i in range(2)]
            sq = [k.sb(f"sqn{i}", [128, 8, 512], BF16) for i in range(2)]
            rs = [k.sb(f"rs{i}", [128, 512]) for i in range(2)]
            for tb in range(4):
                xi, sqi, rsi = xin[tb % 2], sq[tb % 2], rs[tb % 2]
                src = xsrc[s].rearrange("(j p) t -> p j t", p=128)[:, :, tb * 512:(tb + 1) * 512]
                k.dma("sp", xi[:], src, reads=[xsrc], writes=[xi])
                act(sqi[:], xi[:], AF.Square, reads=[xi], writes=[sqi])
                ps = psb("m")
                for j in range(8):
                    mm(ps[:], onesB, sqi[:, j, :], j == 0, j == 7, reads=[constB, sqi], writes=[ps])
                act(rsi[:], ps[:], AF.Ln, reads=[ps], writes=[rsi], bias=EPS, scale=1.0 / D_)
                act(rsi[:], rsi[:], AF.Exp, reads=[rsi], writes=[rsi], scale=-0.5)
                for j in range(8):
                    stt("dve", hT[:, j, tb * 512:(tb + 1) * 512], xi[:, j, :], normg[:, l, j:j + 1], rsi[:],
                        ALU.mult, ALU.mult, reads=[xi, rsi, normg], writes=[hT])

    def phase_fox(s, l):
        with k.scope():
            QT = k.sb("fxQT", [128, 4, T_], BF16)
            KT = k.sb("fxKT", [128, 4, T_], BF16)
            V = k.sb("fxV", [128, 16, 4, 65], BF16)
            ltok = k.sb("fxl", [128, 16, 4])
            negcum = k.sb("fxnc", [128, 16, 4])
            fbb = k.sb("fbb", [128, 4])
            k.dma("sp", fbb[:], fb_d[l:l + 1, :].to_broadcast([128, 4]), writes=[fbb])
            memset("pool", V[:, :, :, 64:65], 1.0, writes=[V])
            memset("pool", KT[64:67, :, :], 1.0, writes=[KT])
            wq_, wk_, wv_ = load_w(l, C_FXQ, 256), load_w(l, C_FXK, 256), load_w(l, C_FXV, 260)
            for (w, dst, scale) in ((wq_, QT, 0.125), (wk_, KT, None)):
                for pr in range(2):
                    for tb in range(4):
                        ps = psb("m")
                        proj_fm(w, pr * 128, 128, tb, ps)
                        sl = slice(tb * 512, (tb + 1) * 512)
                        for hh in range(2):
                            if scale is None:
                                cp("act", dst[0:64, 2 * pr + hh, sl], ps[64 * hh:64 * hh + 64, :], reads=[ps], writes=[dst])
                            else:
                                k.op("act", lambda: nc.scalar.mul(out=dst[0:64, 2 * pr + hh, sl],
                                                                  in_=ps[64 * hh:64 * hh + 64, :], mul=scale),
                                     reads=[ps], writes=[dst])
            if os.environ.get('FX_STOP') == '1':
                return
            w = wv_
            _skip = os.environ.get("FX_SKIP", "")
            for tB in range(int(os.environ.get("FX_NTB", "16"))):
                ps = psb("m")
                if "mm" not in _skip:
                    proj_tm(w, 0, 260, tB, ps)
                if "cp" not in _skip:
                    cp("act", V[:, tB, :, 0:64], ps[:, 0:256].rearrange("p (h d) -> p h d", h=4), reads=[ps], writes=[V])
                if "tt" not in _skip:
                    tt("dve", ltok[:, tB, :], ps[:, 256:260], fbb[:], ALU.add, reads=[ps, fbb], writes=[ltok])
            if os.environ.get('FX_STOP') == '2':
                return
            act(ltok[:], ltok[:], AF.Exp, reads=[ltok], writes=[ltok], scale=-1.0)
            act(ltok[:], ltok[:], AF.Ln, reads=[ltok], writes=[ltok], bias=1.0)
            if os.environ.get('FX_STOP') == '3':
                return
            ps = psb("m")
            for Bp in range(16):
                for B in range(Bp + 1):
                    mm(ps[:, 4 * Bp:4 * Bp + 4], triF if B == Bp else onesF, ltok[:, B, :], B == 0, B == Bp,
                       reads=[constF, ltok], writes=[ps])
            cp("dve", negcum[:], ps[:, 0:64].rearrange("p (b h) -> p b h", h=4), reads=[ps], writes=[negcum])
            if os.environ.get('FX_STOP') == '4':
                return
            cumf = k.sb("cumf", [4, T_])
            c1f = k.sb("c1f", [4, T_])
            cb = [k.sb(f"cb{i}", [4, T_], BF16) for i in range(3)]
            for g in range(4):
                ps = psb("m")
                for bb in range(4):
                    B = 4 * g + bb
                    mm(ps[0:4, 128 * bb:128 * bb + 128], negcum[:, B, :], identF, True, True,
                       reads=[negcum, constF], writes=[ps])
                k.op("act", lambda: nc.scalar.mul(out=cumf[:, 512 * g:512 * g + 512], in_=ps[0:4, :], mul=-1.0),
                     reads=[ps], writes=[cumf])
            cp("dve", cb[0][:], cumf[:], reads=[cumf], writes=[cb[0]])
            cp("dve", c1f[:], cb[0][:], reads=[cb[0]], writes=[c1f])
            tt("dve", cumf[:], cumf[:], c1f[:], ALU.subtract, reads=[cumf, c1f], writes=[cumf])
            cp("dve", cb[1][:], cumf[:], reads=[cumf], writes=[cb[1]])
            cp("dve", c1f[:], cb[1][:], reads=[cb[1]], writes=[c1f])
            tt("dve", cumf[:], cumf[:], c1f[:], ALU.subtract, reads=[cumf, c1f], writes=[cumf])
            cp("dve", cb[2][:], cumf[:], reads=[cumf], writes=[cb[2]])
            for i in range(3):
                k.dma("sp", QT[64 + i:65 + i, :, :], cb[i][:], reads=[cb[i]], writes=[QT])
            if os.environ.get('FX_STOP') == '5':
                return
            def mk_stream(si, heads):
                pTs = [k.sb(f"fxpT{si}_{i}", [128, 512], BF16) for i in range(2)]
                o_sb = k.sb(f"fxo{si}", [128, 4, 64])
                rd = k.sb(f"fxrd{si}", [128, 4])
                tmp = fin_tmp()

                def masks(h, qb, kb):
                    if kb >= 4 * qb:
                        r = kb - 4 * qb
                        return [(identB, strips[:, 0, 384 - 128 * r:384 - 128 * r + 512], [constB, strips])]
                    return []

                def fin(h, qb, acc, accv):
                    recip(rd[:], accv[:, :, 64], reads=[acc], writes=[rd])
                    tt("dve", o_sb[:], accv[:, :, 0:64], rd[:].unsqueeze(2).to_broadcast([128, 4, 64]), ALU.mult,
                       reads=[acc, rd], writes=[o_sb])
                    finalize(o_sb[:], o_sb, ytok[:, 4 * qb:4 * qb + 4, 256 + 64 * h:256 + 64 * h + 64], tmp)

                return attn_stream(
                    [(h, qb) for h in heads for qb in range(4)],
                    [PS[2 * si], PS[2 * si + 1]], PS[4 + si], pTs,
                    lambda h, kb: KT[0:67, h, kb * 128:(kb + 1) * 128],
                    lambda h, qb: QT[0:67, h, qb * 512:(qb + 1) * 512], [KT, QT],
                    masks, lambda h, kb: negcum[:, kb, h:h + 1], [negcum],
                    lambda h, kb: V[:, kb, h, :], [V],
                    lambda qb: range(0, 4 * qb + 4), lambda qb, qs: (0, 4 * qb + qs), 65, fin)

            run_streams([mk_stream(0, (0, 1)), mk_stream(1, (2, 3))])


    def phase_sb(s, l):
        with k.scope():
            QT = k.sb("sbQT", [64, 4, T_], BF16)
            KT = k.sb("sbKT", [64, 4, T_], BF16)
            V = k.sb("sbV", [128, 16, 256], BF16)
            wq_, wk_, wv_ = load_w(l, C_SBQ, 256), load_w(l, C_SBK, 256), load_w(l, C_SBV, 256)
            for (w, dst, scale) in ((wq_, QT, 0.125), (wk_, KT, None)):
                for pr in range(2):
                    for tb in range(4):
                        ps = psb("m")
                        proj_fm(w, pr * 128, 128, tb, ps)
                        sl = slice(tb * 512, (tb + 1) * 512)
                        for hh in range(2):
                            if scale is None:
                                cp("act", dst[0:64, 2 * pr + hh, sl], ps[64 * hh:64 * hh + 64, :], reads=[ps], writes=[dst])
                            else:
                                k.op("act", lambda: nc.scalar.mul(out=dst[0:64, 2 * pr + hh, sl],
                                                                  in_=ps[64 * hh:64 * hh + 64, :], mul=scale),
                                     reads=[ps], writes=[dst])
            w = wv_
            for tB in range(16):
                ps = psb("m")
                proj_tm(w, 0, 256, tB, ps)
                cp("act", V[:, tB, :], ps[:, 0:256], reads=[ps], writes=[V])
            def sb_stream(si, heads):
                e = k.sb(f"sbe{si}", [128, 512])
                sp_ = k.sb(f"sbsp{si}", [128, 512], BF16)
                er_ = k.sb(f"sber{si}", [128, 512])
                a_ = k.sb(f"sbaT{si}", [128, 512], BF16)
                o = k.sb(f"sbo{si}", [128, 4, 64])
                tmp = fin_tmp()
                ps, psR, acc = PS[2 * si], PS[2 * si + 1], PS[4 + si]
                accv = acc[:, 0:256].rearrange("p (q d) -> p q d", q=4)
                for h in heads:
                    for qb in range(4):
                        nkb = 4 * qb + 4
                        first_acc = True
                        for idx, kb in enumerate(reversed(range(nkb))):
                            diag = kb >= 4 * qb
                            mm(ps[:], KT[0:64, h, kb * 128:(kb + 1) * 128], QT[0:64, h, qb * 512:(qb + 1) * 512],
                               True, not diag, reads=[KT, QT], writes=[ps])
                            if diag:
                                r = kb - 4 * qb
                                mm(ps[:], identB, strips[:, 1, 384 - 128 * r:384 - 128 * r + 512], False, True,
                                   reads=[constB, strips], writes=[ps])
                            yield
                            act(e[:], ps[:], AF.Exp, reads=[ps], writes=[e])
                            act(sp_[:], e[:], AF.Ln, reads=[e], writes=[sp_], bias=1.0)
                            mm(psR[:], negtriB, sp_[:], idx == 0, False, reads=[constB, sp_], writes=[psR])
                            yield
                            act(er_[:], psR[:], AF.Exp, reads=[psR], writes=[er_])
                            tt("dve", a_[:], e[:], er_[:], ALU.mult, reads=[e, er_], writes=[a_])
                            mm(psR[:], neglowB, sp_[:], False, idx == nkb - 1, reads=[constB, sp_], writes=[psR])
                            yield
                            for qs in range(4):
                                if kb > 4 * qb + qs:
                                    continue
                                mm(accv[:, qs, :], a_[:, qs * 128:(qs + 1) * 128], V[:, kb, 64 * h:64 * h + 64],
                                   first_acc, kb == 0, reads=[a_, V], writes=[acc])
                                first_acc = False
                        cp("dve", o[:], accv, reads=[acc], writes=[o])
                        finalize(o[:], o, ytok[:, 4 * qb:4 * qb + 4, 512 + 64 * h:512 + 64 * h + 64], tmp)

            run_streams([sb_stream(0, (0, 1)), sb_stream(1, (2, 3))])


    def phase_hgrn(s, l):
        with k.scope():
            lbb = k.sb("lbb", [128, 2, 256])
            lblT = k.sb("lblT", [128, 2, 2])
            omlb = k.sb("omlb", [128, 256])
            omlT = k.sb("omlT", [128, 2])
            if l == 0:
                memset("pool", omlb[:], 1.0, writes=[omlb])
                memset("pool", omlT[:], 1.0, writes=[omlT])
            else:
                k.dma("sp", lbb[:], lbl_d[:].unsqueeze(0).to_broadcast([128, 2, 256]), writes=[lbb])
                k.dma("sp", lblT[:], lblT_d[:], writes=[lblT])
                tt("dve", omlb[:], lbb[:, 0, :], lbb[:, 1, :], ALU.subtract, reads=[lbb], writes=[omlb])
                act(omlb[:], omlb[:], AF.Sigmoid, reads=[omlb], writes=[omlb])
                tt("dve", omlT[:], lblT[:, :, 0], lblT[:, :, 1], ALU.subtract, reads=[lblT], writes=[omlT])
                act(omlT[:], omlT[:], AF.Sigmoid, reads=[omlT], writes=[omlT])
            big1 = k.sb("hgbig1", [128, 4096])
            gtok = k.sb("hggtok", [128, 16, 256])
            vtok = k.sb("hgvtok", [128, 16, 256], BF16)
            khat = k.sb("hgkhat", [128, 16, 256], BF16)
            ktok = big1[:].rearrange("p (b c) -> p b c", c=256)
            w = load_w(l, C_HGF, 512)
            w2 = load_w(l, C_HGQ, 512)
            for tB in range(16):
                ps = psb("m")
                proj_tm(w, 0, 512, tB, ps)
                act(ktok[:, tB, :], ps[:, 0:256], AF.Sigmoid, reads=[ps], writes=[big1], scale=-1.0)
                cp("dve", vtok[:, tB, :], ps[:, 256:512], reads=[ps], writes=[vtok])
            for tB in range(16):
                tt("dve", ktok[:, tB, :], ktok[:, tB, :], omlb[:], ALU.mult, reads=[big1, omlb], writes=[big1])
            act(gtok[:], ktok, AF.Ln, reads=[big1], writes=[gtok], bias=1.0, scale=-1.0)
            ebs = [k.sb(f"hgebs{i}", [128, 256]) for i in range(2)]
            for tB in range(16):
                ps = psb("m")
                mm(ps[:, 0:256], trisufF, gtok[:, tB, :], True, True, reads=[constF, gtok], writes=[ps])
                eb = ebs[tB % 2]
                act(eb[:], ps[:, 0:256], AF.Exp, reads=[ps], writes=[eb])
                tt("dve", khat[:, tB, :], ktok[:, tB, :], eb[:], ALU.mult, reads=[big1, eb], writes=[khat])
            if os.environ.get('HG_STOP') == '1':
                return
            qsT = k.sb("hgqsT", [128, 2, T_])
            kT = big1[:].rearrange("p (a t) -> p a t", a=2)
            for pr in range(2):
                for tb in range(4):
                    sl = slice(tb * 512, (tb + 1) * 512)
                    ps = psb("m")
                    proj_fm(w2, pr * 128, 128, tb, ps)
                    act(qsT[:, pr, sl], ps[:], AF.Silu, reads=[ps], writes=[qsT])
                    ps = psb("m")
                    proj_fm(w2, 256 + pr * 128, 128, tb, ps, extra_reads=[khat])
                    act(kT[:, pr, sl], ps[:], AF.Sigmoid, reads=[ps, khat], writes=[big1], scale=-1.0)
                    ts("dve", kT[:, pr, sl], kT[:, pr, sl], omlT[:, pr:pr + 1], None, ALU.mult, None,
                       reads=[big1, omlT], writes=[big1])
            qtT = k.sb("hgqtT", [128, 2, T_], BF16)
            ktT = k.sb("hgktT", [128, 2, T_], BF16)
            dl = k.sb("hgdl", [128, 2, 64])
            e1s = [k.sb(f"hge1{i}", [128, 512]) for i in range(2)]
            e2s = [k.sb(f"hge2{i}", [128, 512]) for i in range(2)]
            it = 0
            for pr in range(2):
                for g4 in range(4):
                    sl = slice(g4 * 512, (g4 + 1) * 512)
                    ps = psb("a")
                    for bb in range(4):
                        tB = 4 * g4 + bb
                        mm(ps[:, 128 * bb:128 * bb + 128], gtok[:, tB, pr * 128:(pr + 1) * 128], tribdF, True, True,
                           reads=[gtok, constF], writes=[ps])
                    e1, e2 = e1s[it % 2], e2s[it % 2]
                    it += 1
                    act(e1[:], ps[:], AF.Exp, reads=[ps], writes=[e1])
                    act(e2[:], ps[:], AF.Exp, reads=[ps], writes=[e2], scale=-1.0)
                    tt("dve", qtT[:, pr, sl], qsT[:, pr, sl], e1[:], ALU.mult, reads=[qsT, e1], writes=[qtT])
                    tt("dve", ktT[:, pr, sl], kT[:, pr, sl], e2[:], ALU.mult, reads=[big1, e2], writes=[ktT])
                    cp("pool", dl[:, pr, 16 * g4:16 * g4 + 16], e1[:, 31:512:32], reads=[e1], writes=[dl])
            if os.environ.get('HG_STOP') == '2':
                return
            Srun = [k.sb(f"hgS{i}", [128, 5, 2, 64]) for i in range(2)]
            Sbf = [k.sb(f"hgSb{i}", [128, 4, 2, 64], BF16) for i in range(2)]
            Abd = [k.sb(f"hgA{i}", [128, 4, 128], BF16) for i in range(2)]
            o_sb = [k.sb(f"hgo{i}", [128, 4, 64]) for i in range(2)]
            tmp = fin_tmp()
            memset("pool", Srun[1][:, 4, :, :], 0.0, writes=[Srun[1]])
            for B in range(16):
                cur, prev = Srun[B % 2], Srun[(B + 1) % 2]
                bsl = slice(B * 128, (B + 1) * 128)
                psA = psb("s")
                for h in range(4):
                    pr, r0 = h // 2, 64 * (h % 2)
                    mm(psA[:, 128 * h:128 * h + 128], ktT[r0:r0 + 64, pr, bsl], qtT[r0:r0 + 64, pr, bsl], True, True,
                       reads=[ktT, qtT], writes=[psA], ser=True)
                A = Abd[B % 2]
                tt("dve", A[:], psA[:].rearrange("p (h t) -> p h t", h=4),
                   maskbdF.unsqueeze(1).to_broadcast([128, 4, 128]), ALU.mult, reads=[psA, constF], writes=[A])
                psD = psb("a")
                for c in range(4):
                    for h in range(4):
                        pr, r0 = h // 2, 64 * (h % 2)
                        col = (c * 2 + pr) * 64
                        mm(psD[r0:r0 + 64, col:col + 64], khat[32 * c:32 * c + 32, B, 64 * h:64 * h + 64],
                           vtok[32 * c:32 * c + 32, B, 64 * h:64 * h + 64], True, True,
                           reads=[khat, vtok], writes=[psD], tp=(32 * c, r0), ser=True)
                cp("pool", cur[:, 0, :, :], prev[:, 4, :, :], reads=[prev], writes=[cur])
                for c in range(4):
                    for pr in range(2):
                        col = (c * 2 + pr) * 64
                        stt("dve", cur[:, c + 1, pr, :], cur[:, c, pr, :], dl[:, pr, 4 * B + c:4 * B + c + 1],
                            psD[:, col:col + 64], ALU.mult, ALU.add, reads=[cur, dl, psD], writes=[cur])
                Sb = Sbf[B % 2]
                cp("act", Sb[:], cur[:, 0:4, :, :], reads=[cur], writes=[Sb])
                psO = psb("o")
                for h in range(4):
                    mm(psO[:, 64 * h:64 * h + 64], A[:, h, :], vtok[:, B, 64 * h:64 * h + 64], h == 0, False,
                       reads=[A, vtok], writes=[psO], ser=(h == 0))
                for h in range(4):
                    pr, r0 = h // 2, 64 * (h % 2)
                    for c in range(4):
                        mm(psO[32 * c:32 * c + 32, 64 * h:64 * h + 64],
                           qtT[r0:r0 + 64, pr, B * 128 + 32 * c:B * 128 + 32 * c + 32], Sb[r0:r0 + 64, c, pr, :],
                           False, h == 3 and c == 3, reads=[qtT, Sb], writes=[psO], tp=(r0, 32 * c), ser=True)
                o = o_sb[B % 2]
                cp("dve", o[:], psO[:, 0:256].rearrange("p (h d) -> p h d", h=4), reads=[psO], writes=[o])
                finalize(o[:], o, ytok[:, B, 0:256].rearrange("p (h d) -> p h d", h=4), tmp)

    def phase_nsa(s, l):
        with k.scope():
            cmask = k.sb("cmask", [128, T_], BF16)
            ovl = k.sb("ovl", [128, 33], BF16)
            eall = k.sb("eall", [32, T_], BF16)
            ropeC = k.sb("ropeC", [16, T_])
            ropeS = k.sb("ropeS", [16, T_])
            ropeCc = k.sb("ropeCc", [16, 128])
            ropeSc = k.sb("ropeSc", [16, 128])
            permF = k.sb("permF", [64, 16])
            tkA = k.sb("tkA", [128, 16, 32])
            tkB = k.sb("tkB", [128, 16, 32])
            for dst, src in ((cmask, cmask_d), (ovl, ovl_d), (eall, eall_d), (ropeC, ropeC_d), (ropeS, ropeS_d),
                             (ropeCc, ropeCc_d), (ropeSc, ropeSc_d), (permF, permF_d), (tkA, tkA_d), (tkB, tkB_d)):
                k.dma("sp", dst[:], src[:], writes=[dst])
            qT = k.sb("nsqT", [64, 4, T_], BF16)
            ksT = k.sb("nsksT", [64, T_], BF16)
            kwT = k.sb("nskwT", [64, T_], BF16)
            kcT = k.sb("nskcT", [64, T_], BF16)
            vcT = k.sb("nsvcT", [64, T_], BF16)
            Vs = k.sb("nsVs", [128, 16, 65], BF16)
            Vw = k.sb("nsVw", [128, 16, 65], BF16)
            gtok = k.sb("nsg", [128, 16, 12])
            nacc = k.sb("nsacc", [128, 16, 256])
            imp = k.sb("nsimp", [128, 16, 32])
            negT = k.sb("nsnegT", [32, T_], BF16)
            memset("pool", Vs[:, :, 64:65], 1.0, writes=[Vs])
            memset("pool", Vw[:, :, 64:65], 1.0, writes=[Vw])
            kcmpT = k.sb("nskcmpT", [64, 128], BF16)
            rhsc = k.sb("nsrhsc", [128, 97], BF16)
            with k.scope():
                q32s = [k.sb(f"nsq32{i}", [64, 512]) for i in range(2)]
                t1s = [k.sb(f"nst1{i}", [16, 512]) for i in range(2)]
                t2s = [k.sb(f"nst2{i}", [16, 512]) for i in range(2)]
                rc = [0]

                def rope_evac(src, dst, n, scale, Ct, St, extra_tok):
                    i = rc[0] % 2
                    rc[0] += 1
                    q32, t1, t2 = q32s[i], t1s[i], t2s[i]
                    k.op("act", lambda: nc.scalar.mul(out=q32[:, 0:n], in_=src, mul=scale),
                         reads=[extra_tok["src"]], writes=[q32])
                    cp("pool", dst, q32[:, 0:n], reads=[q32], writes=[extra_tok["dst"]])
                    psw = psb("a")
                    mm(psw[0:16, 0:n], permF[:, :], q32[:, 0:n], True, True, reads=[permF, q32], writes=[psw])
                    tt("dve", t1[:, 0:n], q32[0:16, 0:n], Ct, ALU.mult, reads=[q32, extra_tok["tab"]], writes=[t1])
                    tt("dve", t2[:, 0:n], psw[0:16, 0:n], St, ALU.mult, reads=[psw, extra_tok["tab"]], writes=[t2])
                    tt("dve", extra_tok["dst16"], t1[:, 0:n], t2[:, 0:n], ALU.add,
                       reads=[t1, t2], writes=[extra_tok["dst"]])

                w = load_w(l, C_NSQ, 652)
                cmpW = {}
                for kv in "kv":
                    W1 = k.sb(f"nsW1{kv}", [64, 32, 64], BF16)
                    W2 = k.sb(f"nsW2{kv}", [64, 64], BF16)
                    peT = k.sb(f"nspeT{kv}", [64, 32], BF16)
                    k.dma("pool", W1[:], w1_d[kv][l].rearrange("(lp d) j -> d lp j", d=64), writes=[W1])
                    k.dma("pool", W2[:], w2_d[kv][l], writes=[W2])
                    k.dma("pool", peT[:], peT_d[kv][l], writes=[peT])
                    cmpW[kv] = (W1, W2, peT)
                for pr in range(2):
                    for tb in range(4):
                        sl = slice(tb * 512, (tb + 1) * 512)
                        ps = psb("m")
                        proj_fm(w, pr * 128, 128, tb, ps)
                        for hh in range(2):
                            h = 2 * pr + hh
                            rope_evac(ps[64 * hh:64 * hh + 64, :], qT[0:64, h, sl], 512, 0.125, ropeC[:, sl], ropeS[:, sl],
                                      dict(src=ps, dst=qT, tab=ropeC, dst16=qT[0:16, h, sl]))
                for tb in range(4):
                    sl = slice(tb * 512, (tb + 1) * 512)
                    ps = psb("m")
                    proj_fm(w, 256, 128, tb, ps)
                    cp("act", kcT[:, sl], ps[0:64, :], reads=[ps], writes=[kcT])
                    cp("act", vcT[:, sl], ps[64:128, :], reads=[ps], writes=[vcT])
                for off, dst in ((384, ksT), (512, kwT)):
                    for tb in range(4):
                        sl = slice(tb * 512, (tb + 1) * 512)
                        ps = psb("m")
                        proj_fm(w, off, 64, tb, ps)
                        rope_evac(ps[0:64, :], dst[0:64, sl], 512, 1.0, ropeC[:, sl], ropeS[:, sl],
                                  dict(src=ps, dst=dst, tab=ropeC, dst16=dst[0:16, sl]))
                for tB in range(16):
                    ps = psb("m")
                    proj_tm(w, 448, 204, tB, ps)
                    cp("act", Vs[:, tB, 0:64], ps[:, 0:64], reads=[ps], writes=[Vs])
                    cp("act", Vw[:, tB, 0:64], ps[:, 128:192], reads=[ps], writes=[Vw])
                    act(gtok[:, tB, :], ps[:, 192:204], AF.Sigmoid, reads=[ps], writes=[gtok])
                memset("pool", kcmpT[:], 0.0, writes=[kcmpT])
                memset("pool", rhsc[:], 0.0, writes=[rhsc])
                cp("pool", rhsc[:, 64:97], ovl[:], reads=[ovl], writes=[rhsc])
                for kv, srcT in (("k", kcT), ("v", vcT)):
                    W1, W2, peT = cmpW[kv]
                    psH = psb("a")
                    for lp in range(32):
                        mm(psH[0:64, 0:127], W1[:, lp, :], srcT[:, lp:lp + 16 * 126 + 1:16], lp == 0, lp == 31,
                           reads=[W1, srcT], writes=[psH])
                    for lp in range(32):
                        mm(psH[0:64, 127:128], W1[:, lp, :], peT[:, lp:lp + 1], lp == 0, lp == 31,
                           reads=[W1, peT], writes=[psH])
                    bias = k.sb(f"nsbias{kv}", [64, 1])
                    hid = k.sb(f"nshid{kv}", [64, 128], BF16)
                    cp("dve", bias[:], psH[0:64, 127:128], reads=[psH], writes=[bias])
                    act(hid[:, 0:127], psH[0:64, 0:127], AF.Silu, reads=[psH, bias], writes=[hid], bias=bias[:, 0:1])
                    ps2 = psb("m")
                    if kv == "k":
                        mm(ps2[0:64, 0:127], W2[:, :], hid[:, 0:127], True, True, reads=[W2, hid], writes=[ps2])
                        rope_evac(ps2[0:64, 0:127], kcmpT[0:64, 0:127], 127, 1.0, ropeCc[:, 0:127], ropeSc[:, 0:127],
                                  dict(src=ps2, dst=kcmpT, tab=ropeCc, dst16=kcmpT[0:16, 0:127]))
                    else:
                        mm(ps2[0:127, 0:64], hid[:, 0:127], W2[:, :], True, True, reads=[W2, hid], writes=[ps2])
                        cp("act", rhsc[0:127, 0:64], ps2[0:127, 0:64], reads=[ps2], writes=[rhsc])
            pT = [k.sb(f"nspT{i}", [128, 512], BF16) for i in range(3)]
            rden = [k.sb(f"nsrd{i}", [128, 4]) for i in range(2)]
            cf = [k.sb(f"nscf{i}", [128, 4]) for i in range(2)]
            tmpo = [k.sb(f"nstmpo{i}", [128, 4, 64]) for i in range(2)]
            tmpi = [k.sb(f"nstmpi{i}", [128, 4, 32]) for i in range(2)]
            it = 0
            fi = 0
            for h in range(4):
                for qb in range(4):
                    qsl = slice(qb * 512, (qb + 1) * 512)
                    ps = psb("s")
                    mm(ps[:], kcmpT[:, :], qT[0:64, h, qsl], True, False, reads=[kcmpT, qT], writes=[ps])
                    mm(ps[:], identB, cmask[:, qsl], False, True, reads=[constB, cmask], writes=[ps])
                    p = pT[it % 3]
                    it += 1
                    act(p[:], ps[:], AF.Exp, reads=[ps], writes=[p])
                    acc = psb("o")
                    accv = acc[:, 0:388].rearrange("p (q d) -> p q d", q=4)
                    for qs in range(4):
                        mm(accv[:, qs, :], p[:, qs * 128:(qs + 1) * 128], rhsc[:, :], qs == 0, True,
                           reads=[p, rhsc], writes=[acc])
                    rd, c_ = rden[fi % 2], cf[fi % 2]
                    to, ti = tmpo[fi % 2], tmpi[fi % 2]
                    fi += 1
                    ts("dve", rd[:], accv[:, :, 64], 1e-30, None, ALU.max, None, reads=[acc], writes=[rd])
                    recip(rd[:], rd[:], reads=[rd], writes=[rd])
                    tt("dve", c_[:], rd[:], gtok[:, 4 * qb:4 * qb + 4, h], ALU.mult, reads=[rd, gtok], writes=[c_])
                    tt("dve", nacc[:, 4 * qb:4 * qb + 4, 64 * h:64 * h + 64], accv[:, :, 0:64],
                       c_[:].unsqueeze(2).to_broadcast([128, 4, 64]), ALU.mult, reads=[acc, c_], writes=[nacc])
                    if h == 0:
                        tt("dve", imp[:, 4 * qb:4 * qb + 4, :], accv[:, :, 65:97],
                           rd[:].unsqueeze(2).to_broadcast([128, 4, 32]), ALU.mult, reads=[acc, rd], writes=[imp])
                    else:
                        tt("dve", ti[:], accv[:, :, 65:97], rd[:].unsqueeze(2).to_broadcast([128, 4, 32]), ALU.mult,
                           reads=[acc, rd], writes=[ti])
                        tt("pool", imp[:, 4 * qb:4 * qb + 4, :], imp[:, 4 * qb:4 * qb + 4, :], ti[:], ALU.add,
                           reads=[imp, ti], writes=[imp])
            score = k.sb("nsscore", [128, 16, 32])
            negm = k.sb("nsnegm", [128, 16, 32])
            mx = [k.sb(f"nsmx{i}", [128, 8]) for i in range(2)]
            sc2 = k.sb("nssc2", [128, 32])
            tt("dve", score[:], imp[:], tkA[:], ALU.mult, reads=[imp, tkA], writes=[score])
            tt("dve", score[:], score[:], tkB[:], ALU.add, reads=[score, tkB], writes=[score])
            for tB in range(16):
                k.op("dve", lambda: nc.vector.max(out=mx[0][:], in_=score[:, tB, :]), reads=[score], writes=[mx[0]])
                k.op("dve", lambda: nc.vector.match_replace(out=sc2[:], in_to_replace=mx[0][:], in_values=score[:, tB, :],
                                                            imm_value=-1.0e9), reads=[score, mx[0]], writes=[sc2])
                k.op("dve", lambda: nc.vector.max(out=mx[1][:], in_=sc2[:]), reads=[sc2], writes=[mx[1]])
                ts("dve", negm[:, tB, :], score[:, tB, :], mx[1][:, 7:8], NEG, ALU.is_lt, ALU.mult,
                   reads=[score, mx[1]], writes=[negm])
            for g4 in range(4):
                ps = psb("m")
                for bb in range(4):
                    mm(ps[0:32, 128 * bb:128 * bb + 128], negm[:, 4 * g4 + bb, :], identF, True, True,
                       reads=[negm, constF], writes=[ps])
                cp("act", negT[:, 512 * g4:512 * g4 + 512], ps[0:32, :], reads=[ps], writes=[negT])

            def caus_mask(qb, kb):
                if kb >= 4 * qb:
                    r = kb - 4 * qb
                    return strips[:, 0, 384 - 128 * r:384 - 128 * r + 512]
                return None

            def win_mask(qb, kb):
                r = kb - 4 * qb
                if r >= 0:
                    return strips[:, 0, 384 - 128 * r:384 - 128 * r + 512]
                return strips[:, 2, 384 - 128 * (r + 4):384 - 128 * (r + 4) + 512]

            def br_stream(si, bi, KTt, Vt, kb_range, mask_fn, use_sel, qs_range):
                pTs = [k.sb(f"nsbp{si}_{i}", [128, 512], BF16) for i in range(2)]
                rd = k.sb(f"nsbrd{si}", [128, 4])
                c_ = k.sb(f"nsbcf{si}", [128, 4])
                to = k.sb(f"nsbto{si}", [128, 4, 64])

                def masks(h, qb, kb):
                    ml = []
                    ksl = slice(kb * 128, (kb + 1) * 128)
                    if use_sel and qb >= 2:
                        ml.append((eall[0:32, ksl], negT[0:32, qb * 512:(qb + 1) * 512], [eall, negT]))
                    mk = mask_fn(qb, kb)
                    if mk is not None:
                        ml.append((identB, mk, [constB, strips]))
                    return ml

                def fin(h, qb, acc, accv):
                    recip(rd[:], accv[:, :, 64], reads=[acc], writes=[rd])
                    tt("dve", c_[:], rd[:], gtok[:, 4 * qb:4 * qb + 4, 4 * bi + h], ALU.mult, reads=[rd, gtok], writes=[c_])
                    tt("dve", to[:], accv[:, :, 0:64], c_[:].unsqueeze(2).to_broadcast([128, 4, 64]), ALU.mult,
                       reads=[acc, c_], writes=[to])
                    tt("pool", nacc[:, 4 * qb:4 * qb + 4, 64 * h:64 * h + 64],
                       nacc[:, 4 * qb:4 * qb + 4, 64 * h:64 * h + 64], to[:], ALU.add, reads=[nacc, to], writes=[nacc])

                return attn_stream(
                    [(h, qb) for h in range(4) for qb in range(4)],
                    [PS[2 * si], PS[2 * si + 1]], PS[4 + si], pTs,
                    lambda h, kb: KTt[0:64, kb * 128:(kb + 1) * 128],
                    lambda h, qb: qT[0:64, h, qb * 512:(qb + 1) * 512], [KTt, qT],
                    masks, None, [],
                    lambda h, kb: Vt[:, kb, :], [Vt],
                    kb_range, qs_range, 65, fin)

            run_streams([
                br_stream(0, 1, ksT, Vs, lambda qb: range(0, 4 * qb + 4), caus_mask, True,
                          lambda qb, qs: (0, 4 * qb + qs)),
                br_stream(1, 2, kwT, Vw, lambda qb: range(max(0, 4 * qb - 4), 4 * qb + 4), win_mask, False,
                          lambda qb, qs: (max(0, 4 * qb + qs - 4), 4 * qb + qs)),
            ])
            tmp = fin_tmp(16)
            for g4 in range(4):
                for bb in range(4):
                    finalize(nacc[:, 4 * g4 + bb, :].rearrange("p (h d) -> p h d", h=4), nacc,
                             ytok[:, 4 * g4 + bb, 768:1024].rearrange("p (h d) -> p h d", h=4), tmp, n=4)

    def phase_out(s, l, last):
        xsrc = xT_d if l == 0 else xres_d
        with k.scope():
            wg = load_w(l, C_GATE, 1024, "wg")
            ong = k.sb("ong", [128, D_])
            k.dma("sp", ong[:], ong_d[l:l + 1, :].to_broadcast([128, D_]), writes=[ong])
            yT = k.sb("yT", [128, 8, T_], BF16)
            wo = k.sb("wo", [128, 8, D_], BF16)
            wsrc = w_out_d[l].rearrange("(j p) c -> p j c", p=128)
            k.dma("pool", wo[:, 0:4, :], wsrc[:, 0:4, :], writes=[wo])
            k.dma("pool", wo[:, 4:8, :], wsrc[:, 4:8, :], writes=[wo])
            def gate_stream(si, tBs):
                sgi = k.sb(f"sg{si}", [128, D_])
                ygi = k.sb(f"yg{si}", [128, D_], BF16)
                pa, pb = PS[2 * si], PS[2 * si + 1]
                for tB in tBs:
                    proj_tm(wg, 0, 512, tB, pa)
                    proj_tm(wg, 512, 512, tB, pb)
                    yield
                    act(sgi[:, 0:512], pa[:], AF.Silu, reads=[pa], writes=[sgi])
                    act(sgi[:, 512:1024], pb[:], AF.Silu, reads=[pb], writes=[sgi])
                    tt("pool", sgi[:], sgi[:], ong[:], ALU.mult, reads=[sgi, ong], writes=[sgi])
                    yield
                    tt("dve", ygi[:], sgi[:], ytok[:, tB, :], ALU.mult, reads=[sgi, ytok], writes=[ygi])
                    for cc in range(8):
                        k.op("pe", lambda: nc.tensor.transpose(PST[:, cc * 128:(cc + 1) * 128],
                                                               ygi[:, cc * 128:(cc + 1) * 128], identB),
                             reads=[ygi, constB], writes=[PST])
                    cp("act", yT[:, :, tB * 128:(tB + 1) * 128], PST[:].rearrange("p (c t) -> p c t", c=8),
                       reads=[PST], writes=[yT])

            run_streams([gate_stream(0, range(0, 16, 3)), gate_stream(1, range(1, 16, 3)),
                         gate_stream(2, range(2, 16, 3))])
            xin = [k.sb(f"xo{i}", [128, 8, 512]) for i in range(2)]
            if last:
                sqf = k.sb("sqf", [128, 8, 512], BF16)
                rsf = k.sb("rsf", [128, 512])
            for tb in range(4):
                sl = slice(tb * 512, (tb + 1) * 512)
                xi = xin[tb % 2]
                src = xsrc[s].rearrange("(j p) t -> p j t", p=128)[:, :, sl]
                k.dma("sp", xi[:], src, reads=[xsrc], writes=[xi])
                for dj in range(8):
                    ps = PS[(tb * 8 + dj) % 6]
                    for cc in range(8):
                        mm(ps[:], wo[:, cc, dj * 128:(dj + 1) * 128], yT[:, cc, sl], cc == 0, cc == 7,
                           reads=[wo, yT], writes=[ps])
                    tt("dve", xi[:, dj, :], xi[:, dj, :], ps[:], ALU.add, reads=[xi, ps], writes=[xi])
                if not last:
                    dst = xres_d[s].rearrange("(j p) t -> p j t", p=128)[:, :, sl]
                    k.dma("sp", dst, xi[:], reads=[xi], writes=[xres_d])
                else:
                    act(sqf[:], xi[:], AF.Square, reads=[xi], writes=[sqf])
                    ps = psb("m")
                    for j in range(8):
                        mm(ps[:], onesB, sqf[:, j, :], j == 0, j == 7, reads=[constB, sqf], writes=[ps])
                    act(rsf[:], ps[:], AF.Ln, reads=[ps], writes=[rsf], bias=EPS, scale=1.0 / D_)
                    act(rsf[:], rsf[:], AF.Exp, reads=[rsf], writes=[rsf], scale=-0.5)
                    for j in range(8):
                        stt("dve", xi[:, j, :], xi[:, j, :], fnormg[:, j:j + 1], rsf[:], ALU.mult, ALU.mult,
                            reads=[xi, rsf, fnormg], writes=[xi])
                    dst = outT_d[s].rearrange("(j p) t -> p j t", p=128)[:, :, sl]
                    k.dma("sp", dst, xi[:], reads=[xi], writes=[outT_d])

    for s in range(nseq):
        for l in range(nlayer):
            phase_norm(s, l)
            if debug and s == 0 and l == 0 and "hT" in debug:
                d = dbg_out("dbg_hT", [128, 8, T_], BF16)
                k.dma("sp", d[:], hT[:], reads=[hT], writes=[d])
            if "hg" in mixers:
                phase_hgrn(s, l)
            if "fx" in mixers:
                phase_fox(s, l)
            if "sb" in mixers:
                phase_sb(s, l)
            if "ns" in mixers:
                phase_nsa(s, l)
            if debug and s == 0 and l == nlayer - 1 and "ytok" in debug:
                d = dbg_out("dbg_ytok", [128, 16, D_], BF16)
                k.dma("sp", d[:], ytok[:], reads=[ytok], writes=[d])
            if "out" in mixers:
                phase_out(s, l, l == nlayer - 1)

    k.finish(list(dbg_outs.values()) + [outT_d])
    k.barrier()
    k.close()
    return nc, k, list(dbg_outs.keys())


def host_inputs(inputs, core, consts, nseq=SEQ_PER_CORE):
    f32 = np.float32
    x = inputs["x"]
    b0 = core * nseq
    m = {}
    m["xT"] = np.ascontiguousarray(np.transpose(x[b0:b0 + nseq], (0, 2, 1))).astype(f32)
    m["w_in"] = np.ascontiguousarray(inputs["w_in"], dtype=f32)
    m["w_out"] = np.ascontiguousarray(inputs["w_out"], dtype=f32)
    m["norm_gT"] = np.ascontiguousarray(inputs["norm_g"].reshape(2, 8, 128).transpose(0, 2, 1), dtype=f32)
    m["fnorm_gT"] = np.ascontiguousarray(inputs["final_norm_g"].reshape(8, 128).T, dtype=f32)
    m["out_norm_g"] = np.ascontiguousarray(inputs["out_norm_g"], dtype=f32)
    lbl = np.asarray(inputs["hgrn_lb_logits"], dtype=f32)
    m["lb_logits"] = np.ascontiguousarray(lbl)
    m["lb_logitsT"] = np.ascontiguousarray(lbl.reshape(2, 2, 128).transpose(2, 1, 0))
    m["fox_fb"] = np.ascontiguousarray(inputs["fox_fb"], dtype=f32)
    for kv in "kv":
        m[f"peT_{kv}"] = np.ascontiguousarray(np.transpose(inputs[f"nsa_cmp_pe_{kv}"], (0, 2, 1)), dtype=f32)
        m[f"w1_{kv}"] = np.ascontiguousarray(inputs[f"nsa_cmp_w1_{kv}"], dtype=f32)
        m[f"w2_{kv}"] = np.ascontiguousarray(inputs[f"nsa_cmp_w2_{kv}"], dtype=f32)
    for nm in ["constF", "constB", "strips", "cmask", "ovl", "eall", "ropeC", "ropeS", "ropeCc", "ropeSc",
               "permF", "tkA", "tkB"]:
        m[nm] = consts[nm]
    return m


def kernel(**inputs):
    inputs = {kk: np.asarray(v) for kk, v in inputs.items()}
    consts = make_consts()
    nc, kb, _ = build()
    in_maps = [host_inputs(inputs, c, consts) for c in range(NCORES)]
    res = run_bass_kernel_spmd(nc, in_maps, core_ids=list(range(NCORES)))
    outs = [np.asarray(r["outT"]) for r in res.results]
    full = np.concatenate(outs, axis=0)
    return np.ascontiguousarray(np.transpose(full, (0, 2, 1))).astype(np.float32)
```

```python
import os
import numpy as np
import ml_dtypes
from contextlib import ExitStack
import concourse.bass as bass
import concourse.mybir as mybir
from concourse.bass_utils import run_bass_kernel_spmd

F32 = mybir.dt.float32
BF16 = mybir.dt.bfloat16
AF = mybir.ActivationFunctionType
ALU = mybir.AluOpType
AX = mybir.AxisListType

T_ = 2048
D_ = 1024
NIN = 3984
NEG = -30000.0
EPS = 1e-6
NCORES = 8
SEQ_PER_CORE = 4

C_HGQ, C_HGF, C_HGI = 0, 256, 512
C_FXQ, C_FXK, C_FXV, C_FXF = 768, 1024, 1280, 1536
C_SBQ, C_SBK, C_SBV = 1540, 1796, 2052
C_NSQ = 2308
C_NKC, C_NVC, C_NKS, C_NVS, C_NKW, C_NVW = 2564, 2628, 2692, 2756, 2820, 2884
C_NSG = 2948
C_GATE = 2960


class Tok:
    __slots__ = ("w", "r", "name")

    def __init__(self, name=""):
        self.w = None
        self.r = {}
        self.name = name


class T:
    def __init__(self, h, name, excl=False):
        self.h = h
        self.tok = Tok(name)
        self.name = name
        self.excl = excl

    def __getitem__(self, k):
        return self.h[k]


class WV:
    def __init__(self, t, base):
        self.t = t
        self.base = base
        self.tok = t.tok

    def __getitem__(self, key):
        p, j, sl = key
        return self.t.h[p, j, self.base + sl.start:self.base + sl.stop]


class KB:
    NDQ = 8
    EPOCH_LIMIT = 30000

    def __init__(self, nc):
        self.nc = nc
        self.stack = [ExitStack()]
        self.E = {"pe": nc.tensor, "act": nc.scalar, "dve": nc.vector,
                  "pool": nc.gpsimd, "sp": nc.sync}
        self.semh = {}
        self.cur = {}
        self.cnt = {}
        self.epoch = {}
        for e in ["pe", "act", "dve", "pool"]:
            self.epoch[e] = -1
            self._new_epoch(e)
        self.dq = {}
        for q in ["sp", "pool", "act"]:
            sems = [self.stack[0].enter_context(nc.semaphore(f"d_{q}{i}")) for i in range(self.NDQ)]
            self.dq[q] = dict(n=0)
            for i, s in enumerate(sems):
                self.semh[("dma", q, i)] = s
        self.seen = {}
        self.nins = {e: 0 for e in self.E}
        self.uid = 0

    def _new_epoch(self, e):
        self.epoch[e] += 1
        key = (e, self.epoch[e])
        self.semh[key] = self.stack[0].enter_context(self.nc.semaphore(f"s_{e}{self.epoch[e]}"))
        self.cur[e] = key
        self.cnt[e] = 0

    def scope(self):
        kb = self

        class _S:
            def __enter__(s):
                kb.stack.append(ExitStack())

            def __exit__(s, *a):
                kb.barrier()
                kb.stack.pop().close()
                return False
        return _S()

    def _nm(self, name):
        self.uid += 1
        return f"{name}_{self.uid}"

    def sb(self, name, shape, dt=F32):
        h = self.stack[-1].enter_context(self.nc.sbuf_tensor(self._nm(name), list(shape), dt))
        return T(h, name)

    def ps(self, name, shape, dt=F32):
        h = self.stack[-1].enter_context(self.nc.psum_tensor(self._nm(name), list(shape), dt))
        return T(h, name, excl=True)

    def dram(self, name, shape, dt, kind):
        h = self.nc.dram_tensor(name, list(shape), dt, kind=kind)
        return T(h, name)

    def _wait(self, eng, deps):
        for key, val in deps.items():
            if key[0] == "pe" and eng == "pe":
                continue
            sk = (eng, key)
            if self.seen.get(sk, 0) >= val:
                continue
            self.seen[sk] = val
            self.E[eng].wait_ge(self.semh[key], val)
            self.nins[eng] += 1

    @staticmethod
    def _tok(x):
        return getattr(x, "tok", x)

    def _deps(self, reads, writes):
        deps = {}
        for t in reads:
            t = self._tok(t)
            if t.w and deps.get(t.w[0], 0) < t.w[1]:
                deps[t.w[0]] = t.w[1]
        for t in writes:
            t = self._tok(t)
            if t.w and deps.get(t.w[0], 0) < t.w[1]:
                deps[t.w[0]] = t.w[1]
            for kk, v in t.r.items():
                if deps.get(kk, 0) < v:
                    deps[kk] = v
        return deps

    def _mark(self, ev, reads, writes):
        kk, v = ev
        for t in reads:
            t = self._tok(t)
            if t.r.get(kk, 0) < v:
                t.r[kk] = v
        for t in writes:
            t = self._tok(t)
            t.w = ev
            t.r = {}

    def op(self, eng, fn, reads=(), writes=()):
        ex = [t for t in reads if isinstance(t, T) and t.excl]
        if ex:
            reads = [t for t in reads if not (isinstance(t, T) and t.excl)]
            writes = list(writes) + ex
        self._wait(eng, self._deps(reads, writes))
        if self.cnt[eng] >= self.EPOCH_LIMIT:
            self._new_epoch(eng)
        ins = fn()
        self.cnt[eng] += 1
        ins.then_inc(self.semh[self.cur[eng]], 1)
        self.nins[eng] += 1
        self._mark((self.cur[eng], self.cnt[eng]), reads, writes)
        return ins

    def pe_selfwait(self):
        key, val = self.cur["pe"], self.cnt["pe"]
        if val > 0 and self.seen.get(("pe", key), 0) < val:
            self.seen[("pe", key)] = val
            self.E["pe"].wait_ge(self.semh[key], val)
            self.nins["pe"] += 1

    def dma(self, q, out, in_, reads=(), writes=(), **kw):
        self._wait(q, self._deps(reads, writes))
        d = self.dq[q]
        slot = d["n"] % self.NDQ
        val = 16 * (d["n"] // self.NDQ + 1)
        d["n"] += 1
        ins = self.E[q].dma_start(out=out, in_=in_, **kw)
        ins.then_inc(self.semh[("dma", q, slot)], 16)
        self.nins[q] += 1
        self._mark((("dma", q, slot), val), reads, writes)
        return ins

    def _all_events(self):
        ev = {}
        for e in ["pe", "act", "dve", "pool"]:
            if self.cnt[e] > 0:
                ev[self.cur[e]] = self.cnt[e]
        for q, d in self.dq.items():
            n = d["n"]
            for slot in range(self.NDQ):
                c = (n - slot + self.NDQ - 1) // self.NDQ
                if c > 0:
                    ev[("dma", q, slot)] = 16 * c
        return ev

    def barrier(self):
        ev = self._all_events()
        for e in ["pe", "act", "dve", "pool", "sp"]:
            self._wait(e, {kk: v for kk, v in ev.items() if not (kk[0] == e)})

    def finish(self, toks, eng="sp"):
        deps = {}
        for t in toks:
            t = self._tok(t)
            if t.w and deps.get(t.w[0], 0) < t.w[1]:
                deps[t.w[0]] = t.w[1]
        self._wait(eng, deps)

    def close(self):
        while self.stack:
            self.stack.pop().close()


def _bf(a):
    return np.asarray(a, dtype=np.float32).astype(ml_dtypes.bfloat16)


def make_consts():
    c = {}
    i = np.arange(128)[:, None]
    j = np.arange(128)[None, :]
    c["identF"] = np.eye(128, dtype=np.float32)
    c["triF"] = (i <= j).astype(np.float32)
    c["onesF"] = np.ones((128, 128), np.float32)
    c["tribdF"] = ((i // 32 == j // 32) & (i <= j)).astype(np.float32)
    c["trisufF"] = ((i // 32 == j // 32) & (i > j)).astype(np.float32)
    c["maskbdF"] = ((i // 32 == j // 32) & (i <= j)).astype(np.float32)
    perm = np.zeros((64, 16), np.float32)
    for a in range(16):
        perm[a + 8 if a < 8 else a - 8, a] = 1.0
    c["permF"] = perm
    cf = np.concatenate([c["identF"], c["triF"], c["onesF"], c["tribdF"], c["trisufF"], c["maskbdF"]], axis=1)
    c["constF"] = cf
    negtri = -(i >= j).astype(np.float32)
    neglow = -(i < j).astype(np.float32)
    c["constB"] = _bf(np.concatenate([np.eye(128), np.ones((128, 128)), negtri, neglow], axis=1))
    jj = np.arange(896)[None, :] - 384
    caus = np.where(jj < 0, NEG, np.where(jj >= 128, 0.0, np.where(jj >= i, 0.0, NEG)))
    strict = np.where(jj < 0, NEG, np.where(jj >= 128, 0.0, np.where(jj > i, 0.0, NEG)))
    win = np.where(jj < 0, 0.0, np.where(jj >= 128, NEG, np.where(jj < i, 0.0, NEG)))
    c["strips"] = _bf(np.stack([caus, strict, win], axis=1))
    n = np.arange(128)[:, None]
    t = np.arange(T_)[None, :]
    c["cmask"] = _bf(np.where((16 * n + 31 <= t) & (n < 127), 0.0, NEG))
    cs = (np.arange(128) * 16)[:, None]
    ss = (np.arange(32) * 64)[None, :]
    ov = np.clip(np.minimum(cs + 32, ss + 64) - np.maximum(cs, ss), 0, None).astype(np.float32) / 32.0
    ovl = np.concatenate([np.ones((128, 1), np.float32), ov], axis=1)
    ovl[127] = 0.0
    c["ovl"] = _bf(ovl)
    c["eall"] = _bf((np.arange(T_)[None, :] // 64 == np.arange(32)[:, None]).astype(np.float32))
    half = 8
    inv = (500000.0 ** (-(np.arange(half, dtype=np.float32) * 2.0 / 16.0))).astype(np.float32)

    def tabs(pos):
        ang = pos.astype(np.float32)[None, :] * inv[:, None]
        cos, sin = np.cos(ang), np.sin(ang)
        return (np.concatenate([cos, cos], 0).astype(np.float32),
                np.concatenate([-sin, sin], 0).astype(np.float32))
    C, S = tabs(np.arange(T_))
    c["ropeC"], c["ropeS"] = C, S
    pc = np.arange(128) * 16 + 31
    Cc, Sc = tabs(pc)
    c["ropeCc"], c["ropeSc"] = Cc, Sc
    tt_ = np.arange(T_)
    qblk = tt_ // 64
    blk = np.arange(32)[None, :]
    forced = (blk == 0) | (blk == qblk[:, None]) | (blk == qblk[:, None] - 1)
    valid = blk <= qblk[:, None]
    A = (~forced & valid).astype(np.float32)
    Bc = np.where(forced, 1.0e4, np.where(valid, 0.0, -1.0e4)).astype(np.float32)
    c["tkA"] = A.reshape(16, 128, 32).transpose(1, 0, 2).copy()
    c["tkB"] = Bc.reshape(16, 128, 32).transpose(1, 0, 2).copy()
    return c


def build(nseq=SEQ_PER_CORE, nlayer=2, mixers=("hg", "fx", "sb", "ns", "out"), debug=None):
    nc = bass.Bass("TRN2", target_bir_lowering=False)
    k = KB(nc)
    dbg_outs = {}

    def din(name, shape, dt=F32):
        return k.dram(name, shape, dt, "ExternalInput")

    xT_d = din("xT", [nseq, D_, T_])
    w_in_d = din("w_in", [2, D_, NIN])
    w_out_d = din("w_out", [2, D_, D_])
    normg_d = din("norm_gT", [2, 128, 8])
    fnormg_d = din("fnorm_gT", [128, 8])
    ong_d = din("out_norm_g", [2, D_])
    lbl_d = din("lb_logits", [2, 256])
    lblT_d = din("lb_logitsT", [128, 2, 2])
    fb_d = din("fox_fb", [2, 4])
    peT_d = {kv: din(f"peT_{kv}", [2, 64, 32]) for kv in "kv"}
    w1_d = {kv: din(f"w1_{kv}", [2, 2048, 64]) for kv in "kv"}
    w2_d = {kv: din(f"w2_{kv}", [2, 64, 64]) for kv in "kv"}
    constF_d = din("constF", [128, 768])
    constB_d = din("constB", [128, 512], BF16)
    strips_d = din("strips", [128, 3, 896], BF16)
    cmask_d = din("cmask", [128, T_], BF16)
    ovl_d = din("ovl", [128, 33], BF16)
    eall_d = din("eall", [32, T_], BF16)
    ropeC_d = din("ropeC", [16, T_])
    ropeS_d = din("ropeS", [16, T_])
    ropeCc_d = din("ropeCc", [16, 128])
    ropeSc_d = din("ropeSc", [16, 128])
    permF_d = din("permF", [64, 16])
    tkA_d = din("tkA", [128, 16, 32])
    tkB_d = din("tkB", [128, 16, 32])
    outT_d = k.dram("outT", [nseq, D_, T_], F32, "ExternalOutput")
    xres_d = k.dram("xres", [nseq, D_, T_], F32, "Internal")

    def dbg_out(name, shape, dt=F32):
        d = k.dram(name, shape, dt, "ExternalOutput")
        dbg_outs[name] = d
        return d

    constF = k.sb("constF", [128, 768])
    constB = k.sb("constB", [128, 512], BF16)
    strips = k.sb("strips", [128, 3, 896], BF16)
    k.dma("sp", constF[:], constF_d[:], writes=[constF])
    k.dma("sp", constB[:], constB_d[:], writes=[constB])
    k.dma("sp", strips[:], strips_d[:], writes=[strips])
    identF = constF[:, 0:128]
    triF = constF[:, 128:256]
    onesF = constF[:, 256:384]
    tribdF = constF[:, 384:512]
    trisufF = constF[:, 512:640]
    maskbdF = constF[:, 640:768]
    identB = constB[:, 0:128]
    onesB = constB[:, 128:256]
    negtriB = constB[:, 256:384]
    neglowB = constB[:, 384:512]

    hT = k.sb("hT", [128, 8, T_], BF16)
    ytok = k.sb("ytok", [128, 16, D_], BF16)
    normg = k.sb("normg", [128, 2, 8])
    fnormg = k.sb("fnormg", [128, 8])
    for l in range(2):
        k.dma("sp", normg[:, l, :], normg_d[l], writes=[normg])
    k.dma("sp", fnormg[:], fnormg_d[:], writes=[fnormg])

    PS = [k.ps(f"ps{i}", [128, 512]) for i in range(7)]
    PST = k.ps("pst", [128, 1024], BF16)
    ps_rr = {"s": [0, 1], "a": [2, 3], "o": [4, 5], "m": [6]}
    ps_ctr = {kk: 0 for kk in ps_rr}

    def psb(role):
        lst = ps_rr[role]
        i = lst[ps_ctr[role] % len(lst)]
        ps_ctr[role] += 1
        return PS[i]

    def mm(out, lhsT, rhs, start, stop, reads, writes, tp=None, ser=False):
        kw = {}
        if ser:
            k.pe_selfwait()
        if tp is not None:
            kw["tile_position"] = tp
        return k.op("pe", lambda: nc.tensor.matmul(out, lhsT=lhsT, rhs=rhs, start=start, stop=stop, **kw),
                    reads=reads, writes=writes)

    def act(out, in_, func, reads, writes, bias=None, scale=None):
        kw = {}
        if bias is not None:
            kw["bias"] = bias
        if scale is not None:
            kw["scale"] = scale
        return k.op("act", lambda: nc.scalar.activation(out=out, in_=in_, func=func, **kw),
                    reads=reads, writes=writes)

    def tt(eng, out, in0, in1, op, reads, writes):
        e = k.E[eng]
        return k.op(eng, lambda: e.tensor_tensor(out=out, in0=in0, in1=in1, op=op), reads=reads, writes=writes)

    def ts(eng, out, in0, s1, s2, op0, op1, reads, writes):
        e = k.E[eng]
        if s2 is None:
            return k.op(eng, lambda: e.tensor_scalar(out=out, in0=in0, scalar1=s1, scalar2=None, op0=op0),
                        reads=reads, writes=writes)
        return k.op(eng, lambda: e.tensor_scalar(out=out, in0=in0, scalar1=s1, scalar2=s2, op0=op0, op1=op1),
                    reads=reads, writes=writes)

    def stt(eng, out, in0, scalar, in1, op0, op1, reads, writes):
        e = k.E[eng]
        return k.op(eng, lambda: e.scalar_tensor_tensor(out=out, in0=in0, scalar=scalar, in1=in1, op0=op0, op1=op1),
                    reads=reads, writes=writes)

    def cp(eng, out, in_, reads, writes):
        if eng == "act":
            return k.op("act", lambda: nc.scalar.copy(out=out, in_=in_), reads=reads, writes=writes)
        e = k.E[eng]
        return k.op(eng, lambda: e.tensor_copy(out=out, in_=in_), reads=reads, writes=writes)

    def memset(eng, out, val, writes):
        e = k.E[eng]
        return k.op(eng, lambda: e.memset(out, val), writes=writes)

    def recip(out, in_, reads, writes):
        return k.op("dve", lambda: nc.vector.reciprocal(out=out, in_=in_), reads=reads, writes=writes)

    wslots = [k.sb("wslot0", [128, 8, 1024], BF16), k.sb("wslot1", [128, 8, 1024], BF16)]
    wcols = {"hg": [(C_HGF, 512), (C_HGQ, 512)], "fx": [(C_FXQ, 256), (C_FXK, 256), (C_FXV, 260)],
             "sb": [(C_SBQ, 256), (C_SBK, 256), (C_SBV, 256)], "ns": [(C_NSQ, 652)], "out": [(C_GATE, 1024)]}
    wplan = [(s_, l_, ph) for s_ in range(nseq) for l_ in range(nlayer)
             for ph in ("hg", "fx", "sb", "ns", "out") if ph in mixers]
    wissued = set()

    def w_issue(idx):
        if idx >= len(wplan) or idx in wissued:
            return
        wissued.add(idx)
        _, l_, ph = wplan[idx]
        slot = wslots[idx % 2]
        base = 0
        for (c0, n) in wcols[ph]:
            src = w_in_d[l_][:, c0:c0 + n].rearrange("(j p) c -> p j c", p=128)
            k.dma("pool", slot[:, 0:4, base:base + n], src[:, 0:4, :], writes=[slot])
            k.dma("pool", slot[:, 4:8, base:base + n], src[:, 4:8, :], writes=[slot])
            base += n

    def get_w(s_, l_, ph):
        idx = wplan.index((s_, l_, ph))
        w_issue(idx)
        views, base = [], 0
        for (c0, n) in wcols[ph]:
            views.append(WV(wslots[idx % 2], base))
            base += n
        w_issue(idx + 1)
        return views

    def load_w(l, c0, ncols, name="wblk"):
        w = k.sb(name, [128, 8, ncols], BF16)
        src = w_in_d[l][:, c0:c0 + ncols].rearrange("(j p) c -> p j c", p=128)
        half = 4
        k.dma("pool", w[:, 0:half, :], src[:, 0:half, :], writes=[w])
        k.dma("pool", w[:, half:8, :], src[:, half:8, :], writes=[w])
        return w

    def proj_fm(w, off, M, tb, ps, extra_reads=()):
        for j in range(8):
            mm(ps[0:M, :], w[:, j, off:off + M], hT[:, j, tb * 512:(tb + 1) * 512], j == 0, j == 7,
               reads=[w, hT, *extra_reads], writes=[ps])

    def proj_tm(w, off, N, tB, ps):
        for j in range(8):
            mm(ps[:, 0:N], hT[:, j, tB * 128:(tB + 1) * 128], w[:, j, off:off + N], j == 0, j == 7,
               reads=[w, hT], writes=[ps])

    def finalize(o, o_tok, out_ap, tmp, n=4):
        sq, ss, sd = tmp
        tt("dve", sq[:, 0:n, :], o, o, ALU.mult, reads=[o_tok], writes=[sq])
        k.op("dve", lambda: nc.vector.tensor_reduce(out=ss[:, 0:n], in_=sq[:, 0:n, :], axis=AX.X, op=ALU.add),
             reads=[sq], writes=[ss])
        act(sd[:, 0:n], ss[:, 0:n], AF.Ln, reads=[ss], writes=[sd], bias=EPS, scale=1.0 / 64.0)
        act(sd[:, 0:n], sd[:, 0:n], AF.Exp, reads=[sd], writes=[sd], scale=-0.5)
        tt("dve", out_ap, o, sd[:, 0:n].unsqueeze(2).to_broadcast([128, n, 64]), ALU.mult,
           reads=[o_tok, sd], writes=[ytok])

    def fin_tmp(n=4):
        return (k.sb("sq", [128, n, 64]), k.sb("ss", [128, n]), k.sb("sd", [128, n]))


    def run_streams(gens):
        gens = list(gens)
        while gens:
            for g in list(gens):
                try:
                    next(g)
                except StopIteration:
                    gens.remove(g)

    def attn_stream(jobs, sbank, obanks, pTs, K_ap, Q_ap, kq_toks, masks, bias_ap, bias_toks, V_ap, v_toks,
                    kb_range, qs_range, ncol, fin):
        if not isinstance(obanks, (list, tuple)):
            obanks = [obanks]
        tiles = []
        for ji, (h, qb) in enumerate(jobs):
            kbs = list(kb_range(qb))
            for kb in kbs:
                tiles.append((h, qb, kb, kb == kbs[0], kb == kbs[-1], ji))
        nsb = len(sbank)
        L = max(nsb - 1, 1)

        def emit_qk(i):
            h, qb, kb = tiles[i][0:3]
            ps = sbank[i % nsb]
            ml = masks(h, qb, kb)
            mm(ps[:], K_ap(h, kb), Q_ap(h, qb), True, len(ml) == 0, reads=kq_toks, writes=[ps])
            for mi, (lt, rh, rd) in enumerate(ml):
                mm(ps[:], lt, rh, False, mi == len(ml) - 1, reads=rd, writes=[ps])

        if nsb > 1:
            for i in range(min(L, len(tiles))):
                emit_qk(i)
        else:
            emit_qk(0)
        first = True
        for i, (h, qb, kb, isfirst, islast, ji) in enumerate(tiles):
            if nsb > 1 and i + L < len(tiles):
                emit_qk(i + L)
            yield
            obank = obanks[ji % len(obanks)]
            accv = obank[:, 0:4 * ncol].rearrange("p (q d) -> p q d", q=4)
            ps = sbank[i % nsb]
            p = pTs[i % len(pTs)]
            if isfirst:
                first = True
            b = bias_ap(h, kb) if bias_ap is not None else None
            act(p[:], ps[:], AF.Exp, reads=[ps, *bias_toks], writes=[p], bias=b)
            for qs in range(4):
                lo, hi = qs_range(qb, qs)
                if kb < lo or kb > hi:
                    continue
                mm(accv[:, qs, :], p[:, qs * 128:(qs + 1) * 128], V_ap(h, kb), first, kb == hi,
                   reads=[p, *v_toks], writes=[obank])
                first = False
            if islast:
                fin(h, qb, obank, accv)
            if nsb == 1 and i + 1 < len(tiles):
                emit_qk(i + 1)
            yield

    def phase_norm(s, l):
        xsrc = xT_d if l == 0 else xres_d
        with k.scope():
            xin = [k.sb(f"xin{i}", [128, 8, 512]) for i in range(2)]
            sq = [k.sb(f"sqn{i}", [128, 8, 512], BF16) for i in range(2)]
            rs = [k.sb(f"rs{i}", [128, 512]) for i in range(2)]
            for tb in range(4):
                xi, sqi, rsi = xin[tb % 2], sq[tb % 2], rs[tb % 2]
                src = xsrc[s].rearrange("(j p) t -> p j t", p=128)[:, :, tb * 512:(tb + 1) * 512]
                k.dma("sp", xi[:], src, reads=[xsrc], writes=[xi])
                act(sqi[:], xi[:], AF.Square, reads=[xi], writes=[sqi])
                ps = psb("m")
                for j in range(8):
                    mm(ps[:], onesB, sqi[:, j, :], j == 0, j == 7, reads=[constB, sqi], writes=[ps])
                act(rsi[:], ps[:], AF.Ln, reads=[ps], writes=[rsi], bias=EPS, scale=1.0 / D_)
                act(rsi[:], rsi[:], AF.Exp, reads=[rsi], writes=[rsi], scale=-0.5)
                for j in range(8):
                    stt("dve", hT[:, j, tb * 512:(tb + 1) * 512], xi[:, j, :], normg[:, l, j:j + 1], rsi[:],
                        ALU.mult, ALU.mult, reads=[xi, rsi, normg], writes=[hT])

    def phase_fox(s, l):
        with k.scope():
            QT = k.sb("fxQT", [128, 4, T_], BF16)
            KT = k.sb("fxKT", [128, 4, T_], BF16)
            V = k.sb("fxV", [128, 16, 4, 65], BF16)
            ltok = k.sb("fxl", [128, 16, 4])
            negcum = k.sb("fxnc", [128, 16, 4])
            fbb = k.sb("fbb", [128, 4])
            k.dma("sp", fbb[:], fb_d[l:l + 1, :].to_broadcast([128, 4]), writes=[fbb])
            memset("pool", V[:, :, :, 64:65], 1.0, writes=[V])
            memset("pool", KT[64:67, :, :], 1.0, writes=[KT])
            wq_, wk_, wv_ = get_w(s, l, "fx")
            for (w, dst, scale) in ((wq_, QT, 0.125), (wk_, KT, None)):
                for pr in range(2):
                    for tb in range(4):
                        ps = psb("m")
                        proj_fm(w, pr * 128, 128, tb, ps)
                        sl = slice(tb * 512, (tb + 1) * 512)
                        for hh in range(2):
                            if scale is None:
                                cp("act", dst[0:64, 2 * pr + hh, sl], ps[64 * hh:64 * hh + 64, :], reads=[ps], writes=[dst])
                            else:
                                k.op("act", lambda: nc.scalar.mul(out=dst[0:64, 2 * pr + hh, sl],
                                                                  in_=ps[64 * hh:64 * hh + 64, :], mul=scale),
                                     reads=[ps], writes=[dst])
            if os.environ.get('FX_STOP') == '1':
                return
            w = wv_
            _skip = os.environ.get("FX_SKIP", "")
            for tB in range(int(os.environ.get("FX_NTB", "16"))):
                ps = psb("m")
                if "mm" not in _skip:
                    proj_tm(w, 0, 260, tB, ps)
                if "cp" not in _skip:
                    cp("act", V[:, tB, :, 0:64], ps[:, 0:256].rearrange("p (h d) -> p h d", h=4), reads=[ps], writes=[V])
                if "tt" not in _skip:
                    tt("dve", ltok[:, tB, :], ps[:, 256:260], fbb[:], ALU.add, reads=[ps, fbb], writes=[ltok])
            if os.environ.get('FX_STOP') == '2':
                return
            act(ltok[:], ltok[:], AF.Exp, reads=[ltok], writes=[ltok], scale=-1.0)
            act(ltok[:], ltok[:], AF.Ln, reads=[ltok], writes=[ltok], bias=1.0)
            if os.environ.get('FX_STOP') == '3':
                return
            ps = psb("m")
            for Bp in range(16):
                for B in range(Bp + 1):
                    mm(ps[:, 4 * Bp:4 * Bp + 4], triF if B == Bp else onesF, ltok[:, B, :], B == 0, B == Bp,
                       reads=[constF, ltok], writes=[ps])
            cp("dve", negcum[:], ps[:, 0:64].rearrange("p (b h) -> p b h", h=4), reads=[ps], writes=[negcum])
            if os.environ.get('FX_STOP') == '4':
                return
            cumf = k.sb("cumf", [4, T_])
            c1f = k.sb("c1f", [4, T_])
            cb = [k.sb(f"cb{i}", [4, T_], BF16) for i in range(3)]
            for g in range(4):
                ps = psb("m")
                for bb in range(4):
                    B = 4 * g + bb
                    mm(ps[0:4, 128 * bb:128 * bb + 128], negcum[:, B, :], identF, True, True,
                       reads=[negcum, constF], writes=[ps])
                k.op("act", lambda: nc.scalar.mul(out=cumf[:, 512 * g:512 * g + 512], in_=ps[0:4, :], mul=-1.0),
                     reads=[ps], writes=[cumf])
            cp("dve", cb[0][:], cumf[:], reads=[cumf], writes=[cb[0]])
            cp("dve", c1f[:], cb[0][:], reads=[cb[0]], writes=[c1f])
            tt("dve", cumf[:], cumf[:], c1f[:], ALU.subtract, reads=[cumf, c1f], writes=[cumf])
            cp("dve", cb[1][:], cumf[:], reads=[cumf], writes=[cb[1]])
            cp("dve", c1f[:], cb[1][:], reads=[cb[1]], writes=[c1f])
            tt("dve", cumf[:], cumf[:], c1f[:], ALU.subtract, reads=[cumf, c1f], writes=[cumf])
            cp("dve", cb[2][:], cumf[:], reads=[cumf], writes=[cb[2]])
            for i in range(3):
                k.dma("sp", QT[64 + i:65 + i, :, :], cb[i][:], reads=[cb[i]], writes=[QT])
            if os.environ.get('FX_STOP') == '5':
                return
            def mk_stream(si, heads):
                pTs = [k.sb(f"fxpT{si}_{i}", [128, 512], BF16) for i in range(4)]
                o_sbs = [k.sb(f"fxo{si}_{i}", [128, 4, 64]) for i in range(2)]
                rds = [k.sb(f"fxrd{si}_{i}", [128, 4]) for i in range(2)]
                tmps = [fin_tmp() for i in range(2)]
                fc = [0]

                def masks(h, qb, kb):
                    if kb >= 4 * qb:
                        r = kb - 4 * qb
                        return [(identB, strips[:, 0, 384 - 128 * r:384 - 128 * r + 512], [constB, strips])]
                    return []

                def fin(h, qb, acc, accv):
                    o_sb, rd, tmp = o_sbs[fc[0] % 2], rds[fc[0] % 2], tmps[fc[0] % 2]
                    fc[0] += 1
                    recip(rd[:], accv[:, :, 64], reads=[acc], writes=[rd])
                    tt("dve", o_sb[:], accv[:, :, 0:64], rd[:].unsqueeze(2).to_broadcast([128, 4, 64]), ALU.mult,
                       reads=[acc, rd], writes=[o_sb])
                    finalize(o_sb[:], o_sb, ytok[:, 4 * qb:4 * qb + 4, 256 + 64 * h:256 + 64 * h + 64], tmp)

                return attn_stream(
                    [(h, qb) for h in heads for qb in range(4)],
                    [PS[0], PS[1], PS[2], PS[3]], [PS[4], PS[5]], pTs,
                    lambda h, kb: KT[0:67, h, kb * 128:(kb + 1) * 128],
                    lambda h, qb: QT[0:67, h, qb * 512:(qb + 1) * 512], [KT, QT],
                    masks, lambda h, kb: negcum[:, kb, h:h + 1], [negcum],
                    lambda h, kb: V[:, kb, h, :], [V],
                    lambda qb: range(0, 4 * qb + 4), lambda qb, qs: (0, 4 * qb + qs), 65, fin)

            run_streams([mk_stream(0, (0, 1, 2, 3))])


    def phase_sb(s, l):
        with k.scope():
            QT = k.sb("sbQT", [64, 4, T_], BF16)
            KT = k.sb("sbKT", [64, 4, T_], BF16)
            V = k.sb("sbV", [128, 16, 256], BF16)
            wq_, wk_, wv_ = get_w(s, l, "sb")
            for (w, dst, scale) in ((wq_, QT, 0.125), (wk_, KT, None)):
                for pr in range(2):
                    for tb in range(4):
                        ps = psb("m")
                        proj_fm(w, pr * 128, 128, tb, ps)
                        sl = slice(tb * 512, (tb + 1) * 512)
                        for hh in range(2):
                            if scale is None:
                                cp("act", dst[0:64, 2 * pr + hh, sl], ps[64 * hh:64 * hh + 64, :], reads=[ps], writes=[dst])
                            else:
                                k.op("act", lambda: nc.scalar.mul(out=dst[0:64, 2 * pr + hh, sl],
                                                                  in_=ps[64 * hh:64 * hh + 64, :], mul=scale),
                                     reads=[ps], writes=[dst])
            w = wv_
            for tB in range(16):
                ps = psb("m")
                proj_tm(w, 0, 256, tB, ps)
                cp("act", V[:, tB, :], ps[:, 0:256], reads=[ps], writes=[V])
            def sb_stream(si, heads):
                es = [k.sb(f"sbe{si}_{i}", [128, 512]) for i in range(2)]
                sps = [k.sb(f"sbsp{si}_{i}", [128, 512], BF16) for i in range(2)]
                er_ = k.sb(f"sber{si}", [128, 512])
                as_ = [k.sb(f"sbaT{si}_{i}", [128, 512], BF16) for i in range(2)]
                o = k.sb(f"sbo{si}", [128, 4, 64])
                tmp = fin_tmp()
                zb = [PS[3 * si], PS[3 * si + 1]]
                psR = PS[3 * si + 2]
                acc = PS[6]
                accv = acc[:, 256 * si:256 * si + 256].rearrange("p (q d) -> p q d", q=4)
                tiles = []
                for h in heads:
                    for qb in range(4):
                        nkb = 4 * qb + 4
                        for idx, kb in enumerate(reversed(range(nkb))):
                            tiles.append((h, qb, kb, idx, nkb))
                n = len(tiles)

                def stage1(i):
                    h, qb, kb, idx, nkb = tiles[i]
                    ps = zb[i % 2]
                    diag = kb >= 4 * qb
                    mm(ps[:], KT[0:64, h, kb * 128:(kb + 1) * 128], QT[0:64, h, qb * 512:(qb + 1) * 512],
                       True, not diag, reads=[KT, QT], writes=[ps])
                    if diag:
                        r = kb - 4 * qb
                        mm(ps[:], identB, strips[:, 1, 384 - 128 * r:384 - 128 * r + 512], False, True,
                           reads=[constB, strips], writes=[ps])

                def stage2(i):
                    ps, e, sp_ = zb[i % 2], es[i % 2], sps[i % 2]
                    act(e[:], ps[:], AF.Exp, reads=[ps], writes=[e])
                    act(sp_[:], e[:], AF.Ln, reads=[e], writes=[sp_], bias=1.0)

                stage1(0)
                stage2(0)
                for i, (h, qb, kb, idx, nkb) in enumerate(tiles):
                    e, sp_, a_ = es[i % 2], sps[i % 2], as_[i % 2]
                    if i + 1 < n:
                        stage1(i + 1)
                    mm(psR[:], negtriB, sp_[:], idx == 0, False, reads=[constB, sp_], writes=[psR])
                    if idx == 0:
                        memset("dve", accv, 0.0, writes=[acc])
                    yield
                    act(er_[:], psR[:], AF.Exp, reads=[psR], writes=[er_])
                    if i + 1 < n:
                        stage2(i + 1)
                    tt("dve", a_[:], e[:], er_[:], ALU.mult, reads=[e, er_], writes=[a_])
                    mm(psR[:], neglowB, sp_[:], False, idx == nkb - 1, reads=[constB, sp_], writes=[psR])
                    yield
                    for qs in range(4):
                        if kb > 4 * qb + qs:
                            continue
                        mm(accv[:, qs, :], a_[:, qs * 128:(qs + 1) * 128], V[:, kb, 64 * h:64 * h + 64],
                           False, kb == 0, reads=[a_, V], writes=[acc])
                    if idx == nkb - 1:
                        cp("dve", o[:], accv, reads=[acc], writes=[o])
                        finalize(o[:], o, ytok[:, 4 * qb:4 * qb + 4, 512 + 64 * h:512 + 64 * h + 64], tmp)
                    yield

            run_streams([sb_stream(0, (0, 1)), sb_stream(1, (2, 3))])


    def phase_hgrn(s, l):
        with k.scope():
            lbb = k.sb("lbb", [128, 2, 256])
            lblT = k.sb("lblT", [128, 2, 2])
            omlb = k.sb("omlb", [128, 256])
            omlT = k.sb("omlT", [128, 2])
            if l == 0:
                memset("pool", omlb[:], 1.0, writes=[omlb])
                memset("pool", omlT[:], 1.0, writes=[omlT])
            else:
                k.dma("sp", lbb[:], lbl_d[:].unsqueeze(0).to_broadcast([128, 2, 256]), writes=[lbb])
                k.dma("sp", lblT[:], lblT_d[:], writes=[lblT])
                tt("dve", omlb[:], lbb[:, 0, :], lbb[:, 1, :], ALU.subtract, reads=[lbb], writes=[omlb])
                act(omlb[:], omlb[:], AF.Sigmoid, reads=[omlb], writes=[omlb])
                tt("dve", omlT[:], lblT[:, :, 0], lblT[:, :, 1], ALU.subtract, reads=[lblT], writes=[omlT])
                act(omlT[:], omlT[:], AF.Sigmoid, reads=[omlT], writes=[omlT])
            big1 = k.sb("hgbig1", [128, 4096])
            gtok = k.sb("hggtok", [128, 16, 256])
            vtok = k.sb("hgvtok", [128, 16, 256], BF16)
            khat = k.sb("hgkhat", [128, 16, 256], BF16)
            ktok = big1[:].rearrange("p (b c) -> p b c", c=256)
            w, w2 = get_w(s, l, "hg")
            for tB in range(16):
                ps = psb("m")
                proj_tm(w, 0, 512, tB, ps)
                act(ktok[:, tB, :], ps[:, 0:256], AF.Sigmoid, reads=[ps], writes=[big1], scale=-1.0)
                cp("dve", vtok[:, tB, :], ps[:, 256:512], reads=[ps], writes=[vtok])
            for tB in range(16):
                tt("dve", ktok[:, tB, :], ktok[:, tB, :], omlb[:], ALU.mult, reads=[big1, omlb], writes=[big1])
            act(gtok[:], ktok, AF.Ln, reads=[big1], writes=[gtok], bias=1.0, scale=-1.0)
            ebs = [k.sb(f"hgebs{i}", [128, 256]) for i in range(2)]
            for tB in range(16):
                ps = psb("m")
                mm(ps[:, 0:256], trisufF, gtok[:, tB, :], True, True, reads=[constF, gtok], writes=[ps])
                eb = ebs[tB % 2]
                act(eb[:], ps[:, 0:256], AF.Exp, reads=[ps], writes=[eb])
                tt("dve", khat[:, tB, :], ktok[:, tB, :], eb[:], ALU.mult, reads=[big1, eb], writes=[khat])
            if os.environ.get('HG_STOP') == '1':
                return
            qsT = k.sb("hgqsT", [128, 2, T_])
            kT = big1[:].rearrange("p (a t) -> p a t", a=2)
            for pr in range(2):
                for tb in range(4):
                    sl = slice(tb * 512, (tb + 1) * 512)
                    ps = psb("m")
                    proj_fm(w2, pr * 128, 128, tb, ps)
                    act(qsT[:, pr, sl], ps[:], AF.Silu, reads=[ps], writes=[qsT])
                    ps = psb("m")
                    proj_fm(w2, 256 + pr * 128, 128, tb, ps, extra_reads=[khat])
                    act(kT[:, pr, sl], ps[:], AF.Sigmoid, reads=[ps, khat], writes=[big1], scale=-1.0)
                    ts("dve", kT[:, pr, sl], kT[:, pr, sl], omlT[:, pr:pr + 1], None, ALU.mult, None,
                       reads=[big1, omlT], writes=[big1])
            qtT = k.sb("hgqtT", [128, 2, T_], BF16)
            ktT = k.sb("hgktT", [128, 2, T_], BF16)
            dl = k.sb("hgdl", [128, 2, 64])
            e1s = [k.sb(f"hge1{i}", [128, 512]) for i in range(1)]
            e2s = [k.sb(f"hge2{i}", [128, 512]) for i in range(1)]
            it = 0
            for pr in range(2):
                for g4 in range(4):
                    sl = slice(g4 * 512, (g4 + 1) * 512)
                    ps = psb("a")
                    for bb in range(4):
                        tB = 4 * g4 + bb
                        mm(ps[:, 128 * bb:128 * bb + 128], gtok[:, tB, pr * 128:(pr + 1) * 128], tribdF, True, True,
                           reads=[gtok, constF], writes=[ps])
                    e1, e2 = e1s[0], e2s[0]
                    it += 1
                    act(e1[:], ps[:], AF.Exp, reads=[ps], writes=[e1])
                    act(e2[:], ps[:], AF.Exp, reads=[ps], writes=[e2], scale=-1.0)
                    tt("dve", qtT[:, pr, sl], qsT[:, pr, sl], e1[:], ALU.mult, reads=[qsT, e1], writes=[qtT])
                    tt("dve", ktT[:, pr, sl], kT[:, pr, sl], e2[:], ALU.mult, reads=[big1, e2], writes=[ktT])
                    cp("pool", dl[:, pr, 16 * g4:16 * g4 + 16], e1[:, 31:512:32], reads=[e1], writes=[dl])
            if os.environ.get('HG_STOP') == '2':
                return
            Srun = [k.sb(f"hgS{i}", [128, 5, 2, 64]) for i in range(2)]
            Sbf = [k.sb(f"hgSb{i}", [128, 4, 2, 64], BF16) for i in range(2)]
            Abd = [k.sb(f"hgA{i}", [128, 4, 128], BF16) for i in range(2)]
            o_sb = [k.sb(f"hgo{i}", [128, 4, 64]) for i in range(2)]
            tmp = fin_tmp()
            memset("pool", Srun[1][:, 4, :, :], 0.0, writes=[Srun[1]])
            for B in range(16):
                cur, prev = Srun[B % 2], Srun[(B + 1) % 2]
                bsl = slice(B * 128, (B + 1) * 128)
                psA = psb("s")
                for h in range(4):
                    pr, r0 = h // 2, 64 * (h % 2)
                    mm(psA[:, 128 * h:128 * h + 128], ktT[r0:r0 + 64, pr, bsl], qtT[r0:r0 + 64, pr, bsl], True, True,
                       reads=[ktT, qtT], writes=[psA], ser=True)
                A = Abd[B % 2]
                tt("dve", A[:], psA[:].rearrange("p (h t) -> p h t", h=4),
                   maskbdF.unsqueeze(1).to_broadcast([128, 4, 128]), ALU.mult, reads=[psA, constF], writes=[A])
                psD = psb("a")
                for c in range(4):
                    for h in range(4):
                        pr, r0 = h // 2, 64 * (h % 2)
                        col = (c * 2 + pr) * 64
                        mm(psD[r0:r0 + 64, col:col + 64], khat[32 * c:32 * c + 32, B, 64 * h:64 * h + 64],
                           vtok[32 * c:32 * c + 32, B, 64 * h:64 * h + 64], True, True,
                           reads=[khat, vtok], writes=[psD], tp=(32 * c, r0), ser=True)
                cp("pool", cur[:, 0, :, :], prev[:, 4, :, :], reads=[prev], writes=[cur])
                for c in range(4):
                    for pr in range(2):
                        col = (c * 2 + pr) * 64
                        stt("dve", cur[:, c + 1, pr, :], cur[:, c, pr, :], dl[:, pr, 4 * B + c:4 * B + c + 1],
                            psD[:, col:col + 64], ALU.mult, ALU.add, reads=[cur, dl, psD], writes=[cur])
                Sb = Sbf[B % 2]
                cp("act", Sb[:], cur[:, 0:4, :, :], reads=[cur], writes=[Sb])
                psO = psb("o")
                for h in range(4):
                    mm(psO[:, 64 * h:64 * h + 64], A[:, h, :], vtok[:, B, 64 * h:64 * h + 64], h == 0, False,
                       reads=[A, vtok], writes=[psO], ser=(h == 0))
                for h in range(4):
                    pr, r0 = h // 2, 64 * (h % 2)
                    for c in range(4):
                        mm(psO[32 * c:32 * c + 32, 64 * h:64 * h + 64],
                           qtT[r0:r0 + 64, pr, B * 128 + 32 * c:B * 128 + 32 * c + 32], Sb[r0:r0 + 64, c, pr, :],
                           False, h == 3 and c == 3, reads=[qtT, Sb], writes=[psO], tp=(r0, 32 * c), ser=True)
                o = o_sb[B % 2]
                cp("dve", o[:], psO[:, 0:256].rearrange("p (h d) -> p h d", h=4), reads=[psO], writes=[o])
                finalize(o[:], o, ytok[:, B, 0:256].rearrange("p (h d) -> p h d", h=4), tmp)

    def phase_nsa(s, l):
        with k.scope():
            ovl = k.sb("ovl", [128, 33], BF16)
            ropeC = k.sb("ropeC", [16, T_])
            ropeS = k.sb("ropeS", [16, T_])
            ropeCc = k.sb("ropeCc", [16, 128])
            ropeSc = k.sb("ropeSc", [16, 128])
            permF = k.sb("permF", [64, 16])
            for dst, src in ((ovl, ovl_d), (ropeC, ropeC_d), (ropeS, ropeS_d),
                             (ropeCc, ropeCc_d), (ropeSc, ropeSc_d), (permF, permF_d)):
                k.dma("sp", dst[:], src[:], writes=[dst])
            qT = k.sb("nsqT", [64, 4, T_], BF16)
            ksT = k.sb("nsksT", [64, T_], BF16)
            kwT = k.sb("nskwT", [64, T_], BF16)
            kcT = k.sb("nskcT", [64, T_], BF16)
            vcT = k.sb("nsvcT", [64, T_], BF16)
            Vs = k.sb("nsVs", [128, 16, 65], BF16)
            Vw = k.sb("nsVw", [128, 16, 65], BF16)
            gtok = k.sb("nsg", [128, 16, 12])
            memset("pool", Vs[:, :, 64:65], 1.0, writes=[Vs])
            memset("pool", Vw[:, :, 64:65], 1.0, writes=[Vw])
            kcmpT = k.sb("nskcmpT", [64, 128], BF16)
            rhsc = k.sb("nsrhsc", [128, 97], BF16)
            with k.scope():
                q32s = [k.sb(f"nsq32{i}", [64, 512]) for i in range(2)]
                t1s = [k.sb(f"nst1{i}", [16, 512]) for i in range(2)]
                t2s = [k.sb(f"nst2{i}", [16, 512]) for i in range(2)]
                rc = [0]

                def rope_evac(src, dst, n, scale, Ct, St, extra_tok):
                    i = rc[0] % 2
                    rc[0] += 1
                    q32, t1, t2 = q32s[i], t1s[i], t2s[i]
                    k.op("act", lambda: nc.scalar.mul(out=q32[:, 0:n], in_=src, mul=scale),
                         reads=[extra_tok["src"]], writes=[q32])
                    cp("pool", dst, q32[:, 0:n], reads=[q32], writes=[extra_tok["dst"]])
                    psw = psb("a")
                    mm(psw[0:16, 0:n], permF[:, :], q32[:, 0:n], True, True, reads=[permF, q32], writes=[psw])
                    tt("dve", t1[:, 0:n], q32[0:16, 0:n], Ct, ALU.mult, reads=[q32, extra_tok["tab"]], writes=[t1])
                    tt("dve", t2[:, 0:n], psw[0:16, 0:n], St, ALU.mult, reads=[psw, extra_tok["tab"]], writes=[t2])
                    tt("dve", extra_tok["dst16"], t1[:, 0:n], t2[:, 0:n], ALU.add,
                       reads=[t1, t2], writes=[extra_tok["dst"]])

                (w,) = get_w(s, l, "ns")
                cmpW = {}
                for kv in "kv":
                    W1 = k.sb(f"nsW1{kv}", [64, 32, 64], BF16)
                    W2 = k.sb(f"nsW2{kv}", [64, 64], BF16)
                    peT = k.sb(f"nspeT{kv}", [64, 32], BF16)
                    k.dma("pool", W1[:], w1_d[kv][l].rearrange("(lp d) j -> d lp j", d=64), writes=[W1])
                    k.dma("pool", W2[:], w2_d[kv][l], writes=[W2])
                    k.dma("pool", peT[:], peT_d[kv][l], writes=[peT])
                    cmpW[kv] = (W1, W2, peT)
                for pr in range(2):
                    for tb in range(4):
                        sl = slice(tb * 512, (tb + 1) * 512)
                        ps = psb("m")
                        proj_fm(w, pr * 128, 128, tb, ps)
                        for hh in range(2):
                            h = 2 * pr + hh
                            rope_evac(ps[64 * hh:64 * hh + 64, :], qT[0:64, h, sl], 512, 0.125, ropeC[:, sl], ropeS[:, sl],
                                      dict(src=ps, dst=qT, tab=ropeC, dst16=qT[0:16, h, sl]))
                for tb in range(4):
                    sl = slice(tb * 512, (tb + 1) * 512)
                    ps = psb("m")
                    proj_fm(w, 256, 128, tb, ps)
                    cp("act", kcT[:, sl], ps[0:64, :], reads=[ps], writes=[kcT])
                    cp("act", vcT[:, sl], ps[64:128, :], reads=[ps], writes=[vcT])
                for off, dst in ((384, ksT), (512, kwT)):
                    for tb in range(4):
                        sl = slice(tb * 512, (tb + 1) * 512)
                        ps = psb("m")
                        proj_fm(w, off, 64, tb, ps)
                        rope_evac(ps[0:64, :], dst[0:64, sl], 512, 1.0, ropeC[:, sl], ropeS[:, sl],
                                  dict(src=ps, dst=dst, tab=ropeC, dst16=dst[0:16, sl]))
                for tB in range(16):
                    ps = psb("m")
                    proj_tm(w, 448, 204, tB, ps)
                    cp("act", Vs[:, tB, 0:64], ps[:, 0:64], reads=[ps], writes=[Vs])
                    cp("act", Vw[:, tB, 0:64], ps[:, 128:192], reads=[ps], writes=[Vw])
                    act(gtok[:, tB, :], ps[:, 192:204], AF.Sigmoid, reads=[ps], writes=[gtok])
                memset("pool", kcmpT[:], 0.0, writes=[kcmpT])
                memset("pool", rhsc[:], 0.0, writes=[rhsc])
                cp("pool", rhsc[:, 64:97], ovl[:], reads=[ovl], writes=[rhsc])
                for kv, srcT in (("k", kcT), ("v", vcT)):
                    W1, W2, peT = cmpW[kv]
                    psH = psb("a")
                    for lp in range(32):
                        mm(psH[0:64, 0:127], W1[:, lp, :], srcT[:, lp:lp + 16 * 126 + 1:16], lp == 0, lp == 31,
                           reads=[W1, srcT], writes=[psH])
                    for lp in range(32):
                        mm(psH[0:64, 127:128], W1[:, lp, :], peT[:, lp:lp + 1], lp == 0, lp == 31,
                           reads=[W1, peT], writes=[psH])
                    bias = k.sb(f"nsbias{kv}", [64, 1])
                    hid = k.sb(f"nshid{kv}", [64, 128], BF16)
                    cp("dve", bias[:], psH[0:64, 127:128], reads=[psH], writes=[bias])
                    act(hid[:, 0:127], psH[0:64, 0:127], AF.Silu, reads=[psH, bias], writes=[hid], bias=bias[:, 0:1])
                    ps2 = psb("m")
                    if kv == "k":
                        mm(ps2[0:64, 0:127], W2[:, :], hid[:, 0:127], True, True, reads=[W2, hid], writes=[ps2])
                        rope_evac(ps2[0:64, 0:127], kcmpT[0:64, 0:127], 127, 1.0, ropeCc[:, 0:127], ropeSc[:, 0:127],
                                  dict(src=ps2, dst=kcmpT, tab=ropeCc, dst16=kcmpT[0:16, 0:127]))
                    else:
                        mm(ps2[0:127, 0:64], hid[:, 0:127], W2[:, :], True, True, reads=[W2, hid], writes=[ps2])
                        cp("act", rhsc[0:127, 0:64], ps2[0:127, 0:64], reads=[ps2], writes=[rhsc])
            cmask = k.sb("cmask", [128, T_], BF16)
            eall = k.sb("eall", [32, T_], BF16)
            tkA = k.sb("tkA", [128, 16, 32])
            tkB = k.sb("tkB", [128, 16, 32])
            nacc = k.sb("nsacc", [128, 16, 256])
            imp = k.sb("nsimp", [128, 16, 32])
            negT = k.sb("nsnegT", [32, T_], BF16)
            for dst, src in ((cmask, cmask_d), (eall, eall_d), (tkA, tkA_d), (tkB, tkB_d)):
                k.dma("sp", dst[:], src[:], writes=[dst])
            pT = [k.sb(f"nspT{i}", [128, 512], BF16) for i in range(3)]
            rden = [k.sb(f"nsrd{i}", [128, 4]) for i in range(2)]
            cf = [k.sb(f"nscf{i}", [128, 4]) for i in range(2)]
            tmpo = [k.sb(f"nstmpo{i}", [128, 4, 64]) for i in range(2)]
            tmpi = [k.sb(f"nstmpi{i}", [128, 4, 32]) for i in range(2)]
            it = 0
            fi = 0
            for h in range(4):
                for qb in range(4):
                    qsl = slice(qb * 512, (qb + 1) * 512)
                    ps = psb("s")
                    mm(ps[:], kcmpT[:, :], qT[0:64, h, qsl], True, False, reads=[kcmpT, qT], writes=[ps])
                    mm(ps[:], identB, cmask[:, qsl], False, True, reads=[constB, cmask], writes=[ps])
                    p = pT[it % 3]
                    it += 1
                    act(p[:], ps[:], AF.Exp, reads=[ps], writes=[p])
                    acc = psb("o")
                    accv = acc[:, 0:388].rearrange("p (q d) -> p q d", q=4)
                    for qs in range(4):
                        mm(accv[:, qs, :], p[:, qs * 128:(qs + 1) * 128], rhsc[:, :], qs == 0, True,
                           reads=[p, rhsc], writes=[acc])
                    rd, c_ = rden[fi % 2], cf[fi % 2]
                    to, ti = tmpo[fi % 2], tmpi[fi % 2]
                    fi += 1
                    ts("dve", rd[:], accv[:, :, 64], 1e-30, None, ALU.max, None, reads=[acc], writes=[rd])
                    recip(rd[:], rd[:], reads=[rd], writes=[rd])
                    tt("dve", c_[:], rd[:], gtok[:, 4 * qb:4 * qb + 4, h], ALU.mult, reads=[rd, gtok], writes=[c_])
                    tt("dve", nacc[:, 4 * qb:4 * qb + 4, 64 * h:64 * h + 64], accv[:, :, 0:64],
                       c_[:].unsqueeze(2).to_broadcast([128, 4, 64]), ALU.mult, reads=[acc, c_], writes=[nacc])
                    if h == 0:
                        tt("dve", imp[:, 4 * qb:4 * qb + 4, :], accv[:, :, 65:97],
                           rd[:].unsqueeze(2).to_broadcast([128, 4, 32]), ALU.mult, reads=[acc, rd], writes=[imp])
                    else:
                        tt("dve", ti[:], accv[:, :, 65:97], rd[:].unsqueeze(2).to_broadcast([128, 4, 32]), ALU.mult,
                           reads=[acc, rd], writes=[ti])
                        tt("pool", imp[:, 4 * qb:4 * qb + 4, :], imp[:, 4 * qb:4 * qb + 4, :], ti[:], ALU.add,
                           reads=[imp, ti], writes=[imp])
            score = k.sb("nsscore", [128, 16, 32])
            negm = k.sb("nsnegm", [128, 16, 32])
            mx = [k.sb(f"nsmx{i}", [128, 8]) for i in range(2)]
            sc2 = k.sb("nssc2", [128, 32])
            tt("dve", score[:], imp[:], tkA[:], ALU.mult, reads=[imp, tkA], writes=[score])
            tt("dve", score[:], score[:], tkB[:], ALU.add, reads=[score, tkB], writes=[score])
            for tB in range(16):
                k.op("dve", lambda: nc.vector.max(out=mx[0][:], in_=score[:, tB, :]), reads=[score], writes=[mx[0]])
                k.op("dve", lambda: nc.vector.match_replace(out=sc2[:], in_to_replace=mx[0][:], in_values=score[:, tB, :],
                                                            imm_value=-1.0e9), reads=[score, mx[0]], writes=[sc2])
                k.op("dve", lambda: nc.vector.max(out=mx[1][:], in_=sc2[:]), reads=[sc2], writes=[mx[1]])
                ts("dve", negm[:, tB, :], score[:, tB, :], mx[1][:, 7:8], NEG, ALU.is_lt, ALU.mult,
                   reads=[score, mx[1]], writes=[negm])
            for g4 in range(4):
                ps = psb("m")
                for bb in range(4):
                    mm(ps[0:32, 128 * bb:128 * bb + 128], negm[:, 4 * g4 + bb, :], identF, True, True,
                       reads=[negm, constF], writes=[ps])
                cp("act", negT[:, 512 * g4:512 * g4 + 512], ps[0:32, :], reads=[ps], writes=[negT])

            def caus_mask(qb, kb):
                if kb >= 4 * qb:
                    r = kb - 4 * qb
                    return strips[:, 0, 384 - 128 * r:384 - 128 * r + 512]
                return None

            def win_mask(qb, kb):
                r = kb - 4 * qb
                if r >= 0:
                    return strips[:, 0, 384 - 128 * r:384 - 128 * r + 512]
                return strips[:, 2, 384 - 128 * (r + 4):384 - 128 * (r + 4) + 512]

            pTs4 = pT + [k.sb("nspT3", [128, 512], BF16)]

            def br_stream(si, bi, KTt, Vt, kb_range, mask_fn, use_sel, qs_range):
                pTs = [k.sb(f"nsbp{si}_{i}", [128, 512], BF16) for i in range(2)] if False else pTs4
                fc = [0]

                def masks(h, qb, kb):
                    ml = []
                    ksl = slice(kb * 128, (kb + 1) * 128)
                    if use_sel and qb >= 2:
                        ml.append((eall[0:32, ksl], negT[0:32, qb * 512:(qb + 1) * 512], [eall, negT]))
                    mk = mask_fn(qb, kb)
                    if mk is not None:
                        ml.append((identB, mk, [constB, strips]))
                    return ml

                def fin(h, qb, acc, accv):
                    rd, c_, to = rden[fc[0] % 2], cf[fc[0] % 2], tmpo[fc[0] % 2]
                    fc[0] += 1
                    recip(rd[:], accv[:, :, 64], reads=[acc], writes=[rd])
                    tt("dve", c_[:], rd[:], gtok[:, 4 * qb:4 * qb + 4, 4 * bi + h], ALU.mult, reads=[rd, gtok], writes=[c_])
                    tt("dve", to[:], accv[:, :, 0:64], c_[:].unsqueeze(2).to_broadcast([128, 4, 64]), ALU.mult,
                       reads=[acc, c_], writes=[to])
                    tt("pool", nacc[:, 4 * qb:4 * qb + 4, 64 * h:64 * h + 64],
                       nacc[:, 4 * qb:4 * qb + 4, 64 * h:64 * h + 64], to[:], ALU.add, reads=[nacc, to], writes=[nacc])

                return attn_stream(
                    [(h, qb) for h in range(4) for qb in range(4)],
                    [PS[0], PS[1], PS[2], PS[3]], [PS[4], PS[5]], pTs,
                    lambda h, kb: KTt[0:64, kb * 128:(kb + 1) * 128],
                    lambda h, qb: qT[0:64, h, qb * 512:(qb + 1) * 512], [KTt, qT],
                    masks, None, [],
                    lambda h, kb: Vt[:, kb, :], [Vt],
                    kb_range, qs_range, 65, fin)

            run_streams([
                br_stream(0, 1, ksT, Vs, lambda qb: range(0, 4 * qb + 4), caus_mask, True,
                          lambda qb, qs: (0, 4 * qb + qs))])
            run_streams([
                br_stream(1, 2, kwT, Vw, lambda qb: range(max(0, 4 * qb - 4), 4 * qb + 4), win_mask, False,
                          lambda qb, qs: (max(0, 4 * qb + qs - 4), 4 * qb + qs)),
            ])
            tmp = fin_tmp(4)
            for g4 in range(4):
                for bb in range(4):
                    finalize(nacc[:, 4 * g4 + bb, :].rearrange("p (h d) -> p h d", h=4), nacc,
                             ytok[:, 4 * g4 + bb, 768:1024].rearrange("p (h d) -> p h d", h=4), tmp, n=4)

    def phase_out(s, l, last):
        xsrc = xT_d if l == 0 else xres_d
        with k.scope():
            (wg,) = get_w(s, l, "out")
            yT = k.sb("yT", [128, 8, T_], BF16)
            wo = k.sb("wo", [128, 8, D_], BF16)
            wsrc = w_out_d[l].rearrange("(j p) c -> p j c", p=128)
            k.dma("pool", wo[:, 0:4, :], wsrc[:, 0:4, :], writes=[wo])
            k.dma("pool", wo[:, 4:8, :], wsrc[:, 4:8, :], writes=[wo])
            def gate_stream(si, tBs):
                sgi = k.sb(f"sg{si}", [128, D_])
                ygi = k.sb(f"yg{si}", [128, D_], BF16)
                pa, pb = PS[2 * si], PS[2 * si + 1]
                for tB in tBs:
                    proj_tm(wg, 0, 512, tB, pa)
                    proj_tm(wg, 512, 512, tB, pb)
                    yield
                    act(sgi[:, 0:512], pa[:], AF.Silu, reads=[pa], writes=[sgi])
                    act(sgi[:, 512:1024], pb[:], AF.Silu, reads=[pb], writes=[sgi])
                    tt("pool", sgi[:], sgi[:], ong[:], ALU.mult, reads=[sgi, ong], writes=[sgi])
                    yield
                    tt("dve", ygi[:], sgi[:], ytok[:, tB, :], ALU.mult, reads=[sgi, ytok], writes=[ygi])
                    for cc in range(8):
                        k.op("pe", lambda: nc.tensor.transpose(PST[:, cc * 128:(cc + 1) * 128],
                                                               ygi[:, cc * 128:(cc + 1) * 128], identB),
                             reads=[ygi, constB], writes=[PST])
                    cp("act", yT[:, :, tB * 128:(tB + 1) * 128], PST[:].rearrange("p (c t) -> p c t", c=8),
                       reads=[PST], writes=[yT])

            with k.scope():
                ong = k.sb("ong", [128, D_])
                k.dma("sp", ong[:], ong_d[l:l + 1, :].to_broadcast([128, D_]), writes=[ong])
                run_streams([gate_stream(0, range(0, 16, 3)), gate_stream(1, range(1, 16, 3)),
                             gate_stream(2, range(2, 16, 3))])
            xin = [k.sb(f"xo{i}", [128, 8, 512]) for i in range(2)]
            if last:
                sqf = k.sb("sqf", [128, 8, 512], BF16)
                rsf = k.sb("rsf", [128, 512])
            for tb in range(4):
                sl = slice(tb * 512, (tb + 1) * 512)
                xi = xin[tb % 2]
                src = xsrc[s].rearrange("(j p) t -> p j t", p=128)[:, :, sl]
                k.dma("sp", xi[:], src, reads=[xsrc], writes=[xi])
                for dj in range(8):
                    ps = PS[(tb * 8 + dj) % 6]
                    for cc in range(8):
                        mm(ps[:], wo[:, cc, dj * 128:(dj + 1) * 128], yT[:, cc, sl], cc == 0, cc == 7,
                           reads=[wo, yT], writes=[ps])
                    tt("dve", xi[:, dj, :], xi[:, dj, :], ps[:], ALU.add, reads=[xi, ps], writes=[xi])
                if not last:
                    dst = xres_d[s].rearrange("(j p) t -> p j t", p=128)[:, :, sl]
                    k.dma("sp", dst, xi[:], reads=[xi], writes=[xres_d])
                else:
                    act(sqf[:], xi[:], AF.Square, reads=[xi], writes=[sqf])
                    ps = psb("m")
                    for j in range(8):
                        mm(ps[:], onesB, sqf[:, j, :], j == 0, j == 7, reads=[constB, sqf], writes=[ps])
                    act(rsf[:], ps[:], AF.Ln, reads=[ps], writes=[rsf], bias=EPS, scale=1.0 / D_)
                    act(rsf[:], rsf[:], AF.Exp, reads=[rsf], writes=[rsf], scale=-0.5)
                    for j in range(8):
                        stt("dve", xi[:, j, :], xi[:, j, :], fnormg[:, j:j + 1], rsf[:], ALU.mult, ALU.mult,
                            reads=[xi, rsf, fnormg], writes=[xi])
                    dst = outT_d[s].rearrange("(j p) t -> p j t", p=128)[:, :, sl]
                    k.dma("sp", dst, xi[:], reads=[xi], writes=[outT_d])

    for s in range(nseq):
        for l in range(nlayer):
            phase_norm(s, l)
            if debug and s == 0 and l == 0 and "hT" in debug:
                d = dbg_out("dbg_hT", [128, 8, T_], BF16)
                k.dma("sp", d[:], hT[:], reads=[hT], writes=[d])
            if "hg" in mixers:
                phase_hgrn(s, l)
            if "fx" in mixers:
                phase_fox(s, l)
            if "sb" in mixers:
                phase_sb(s, l)
            if "ns" in mixers:
                phase_nsa(s, l)
            if debug and s == 0 and l == nlayer - 1 and "ytok" in debug:
                d = dbg_out("dbg_ytok", [128, 16, D_], BF16)
                k.dma("sp", d[:], ytok[:], reads=[ytok], writes=[d])
            if "out" in mixers:
                phase_out(s, l, l == nlayer - 1)

    k.finish(list(dbg_outs.values()) + [outT_d])
    k.barrier()
    k.close()
    return nc, k, list(dbg_outs.keys())


def host_inputs(inputs, core, consts, nseq=SEQ_PER_CORE):
    f32 = np.float32
    x = inputs["x"]
    b0 = core * nseq
    m = {}
    m["xT"] = np.ascontiguousarray(np.transpose(x[b0:b0 + nseq], (0, 2, 1))).astype(f32)
    m["w_in"] = np.ascontiguousarray(inputs["w_in"], dtype=f32)
    m["w_out"] = np.ascontiguousarray(inputs["w_out"], dtype=f32)
    m["norm_gT"] = np.ascontiguousarray(inputs["norm_g"].reshape(2, 8, 128).transpose(0, 2, 1), dtype=f32)
    m["fnorm_gT"] = np.ascontiguousarray(inputs["final_norm_g"].reshape(8, 128).T, dtype=f32)
    m["out_norm_g"] = np.ascontiguousarray(inputs["out_norm_g"], dtype=f32)
    lbl = np.asarray(inputs["hgrn_lb_logits"], dtype=f32)
    m["lb_logits"] = np.ascontiguousarray(lbl)
    m["lb_logitsT"] = np.ascontiguousarray(lbl.reshape(2, 2, 128).transpose(2, 1, 0))
    m["fox_fb"] = np.ascontiguousarray(inputs["fox_fb"], dtype=f32)
    for kv in "kv":
        m[f"peT_{kv}"] = np.ascontiguousarray(np.transpose(inputs[f"nsa_cmp_pe_{kv}"], (0, 2, 1)), dtype=f32)
        m[f"w1_{kv}"] = np.ascontiguousarray(inputs[f"nsa_cmp_w1_{kv}"], dtype=f32)
        m[f"w2_{kv}"] = np.ascontiguousarray(inputs[f"nsa_cmp_w2_{kv}"], dtype=f32)
    for nm in ["constF", "constB", "strips", "cmask", "ovl", "eall", "ropeC", "ropeS", "ropeCc", "ropeSc",
               "permF", "tkA", "tkB"]:
        m[nm] = consts[nm]
    return m


def kernel(**inputs):
    inputs = {kk: np.asarray(v) for kk, v in inputs.items()}
    consts = make_consts()
    nc, kb, _ = build()
    in_maps = [host_inputs(inputs, c, consts) for c in range(NCORES)]
    res = run_bass_kernel_spmd(nc, in_maps, core_ids=list(range(NCORES)))
    outs = [np.asarray(r["outT"]) for r in res.results]
    full = np.concatenate(outs, axis=0)
    return np.ascontiguousarray(np.transpose(full, (0, 2, 1))).astype(np.float32)
```

```python
import os
import numpy as np
import ml_dtypes
from contextlib import ExitStack
import concourse.bass as bass
import concourse.mybir as mybir
from concourse.bass_utils import run_bass_kernel_spmd

F32 = mybir.dt.float32
BF16 = mybir.dt.bfloat16
AF = mybir.ActivationFunctionType
ALU = mybir.AluOpType
AX = mybir.AxisListType

T_ = 2048
D_ = 1024
NIN = 3984
NEG = -30000.0
EPS = 1e-6
NCORES = 8
SEQ_PER_CORE = 4

C_HGQ, C_HGF, C_HGI = 0, 256, 512
C_FXQ, C_FXK, C_FXV, C_FXF = 768, 1024, 1280, 1536
C_SBQ, C_SBK, C_SBV = 1540, 1796, 2052
C_NSQ = 2308
C_NKC, C_NVC, C_NKS, C_NVS, C_NKW, C_NVW = 2564, 2628, 2692, 2756, 2820, 2884
C_NSG = 2948
C_GATE = 2960


class Tok:
    __slots__ = ("w", "r", "name")

    def __init__(self, name=""):
        self.w = None
        self.r = {}
        self.name = name


class T:
    def __init__(self, h, name, excl=False):
        self.h = h
        self.tok = Tok(name)
        self.name = name
        self.excl = excl

    def __getitem__(self, k):
        return self.h[k]


class WV:
    def __init__(self, t, base):
        self.t = t
        self.base = base
        self.tok = t.tok

    def __getitem__(self, key):
        p, j, sl = key
        return self.t.h[p, j, self.base + sl.start:self.base + sl.stop]


class KB:
    NDQ = 8
    EPOCH_LIMIT = 30000

    def __init__(self, nc):
        self.nc = nc
        self.stack = [ExitStack()]
        self.E = {"pe": nc.tensor, "act": nc.scalar, "dve": nc.vector,
                  "pool": nc.gpsimd, "sp": nc.sync}
        self.semh = {}
        self.cur = {}
        self.cnt = {}
        self.epoch = {}
        for e in ["pe", "act", "dve", "pool"]:
            self.epoch[e] = -1
            self._new_epoch(e)
        self.dq = {}
        for q in ["sp", "pool", "act"]:
            sems = [self.stack[0].enter_context(nc.semaphore(f"d_{q}{i}")) for i in range(self.NDQ)]
            self.dq[q] = dict(n=0)
            for i, s in enumerate(sems):
                self.semh[("dma", q, i)] = s
        self.seen = {}
        self.nins = {e: 0 for e in self.E}
        self.uid = 0

    def _new_epoch(self, e):
        self.epoch[e] += 1
        key = (e, self.epoch[e])
        self.semh[key] = self.stack[0].enter_context(self.nc.semaphore(f"s_{e}{self.epoch[e]}"))
        self.cur[e] = key
        self.cnt[e] = 0

    def scope(self):
        kb = self

        class _S:
            def __enter__(s):
                kb.stack.append(ExitStack())

            def __exit__(s, *a):
                kb.barrier()
                kb.stack.pop().close()
                return False
        return _S()

    def _nm(self, name):
        self.uid += 1
        return f"{name}_{self.uid}"

    def sb(self, name, shape, dt=F32):
        h = self.stack[-1].enter_context(self.nc.sbuf_tensor(self._nm(name), list(shape), dt))
        return T(h, name)

    def ps(self, name, shape, dt=F32):
        h = self.stack[-1].enter_context(self.nc.psum_tensor(self._nm(name), list(shape), dt))
        return T(h, name, excl=True)

    def dram(self, name, shape, dt, kind):
        h = self.nc.dram_tensor(name, list(shape), dt, kind=kind)
        return T(h, name)

    def _wait(self, eng, deps):
        for key, val in deps.items():
            if key[0] == "pe" and eng == "pe":
                continue
            sk = (eng, key)
            if self.seen.get(sk, 0) >= val:
                continue
            self.seen[sk] = val
            self.E[eng].wait_ge(self.semh[key], val)
            self.nins[eng] += 1

    @staticmethod
    def _tok(x):
        return getattr(x, "tok", x)

    def _deps(self, reads, writes):
        deps = {}
        for t in reads:
            t = self._tok(t)
            if t.w and deps.get(t.w[0], 0) < t.w[1]:
                deps[t.w[0]] = t.w[1]
        for t in writes:
            t = self._tok(t)
            if t.w and deps.get(t.w[0], 0) < t.w[1]:
                deps[t.w[0]] = t.w[1]
            for kk, v in t.r.items():
                if deps.get(kk, 0) < v:
                    deps[kk] = v
        return deps

    def _mark(self, ev, reads, writes):
        kk, v = ev
        for t in reads:
            t = self._tok(t)
            if t.r.get(kk, 0) < v:
                t.r[kk] = v
        for t in writes:
            t = self._tok(t)
            t.w = ev
            t.r = {}

    def op(self, eng, fn, reads=(), writes=()):
        ex = [t for t in reads if isinstance(t, T) and t.excl]
        if ex:
            reads = [t for t in reads if not (isinstance(t, T) and t.excl)]
            writes = list(writes) + ex
        self._wait(eng, self._deps(reads, writes))
        if self.cnt[eng] >= self.EPOCH_LIMIT:
            self._new_epoch(eng)
        ins = fn()
        self.cnt[eng] += 1
        ins.then_inc(self.semh[self.cur[eng]], 1)
        self.nins[eng] += 1
        self._mark((self.cur[eng], self.cnt[eng]), reads, writes)
        return ins

    def pe_selfwait(self):
        key, val = self.cur["pe"], self.cnt["pe"]
        if val > 0 and self.seen.get(("pe", key), 0) < val:
            self.seen[("pe", key)] = val
            self.E["pe"].wait_ge(self.semh[key], val)
            self.nins["pe"] += 1

    def dma(self, q, out, in_, reads=(), writes=(), **kw):
        self._wait(q, self._deps(reads, writes))
        d = self.dq[q]
        slot = d["n"] % self.NDQ
        val = 16 * (d["n"] // self.NDQ + 1)
        d["n"] += 1
        ins = self.E[q].dma_start(out=out, in_=in_, **kw)
        ins.then_inc(self.semh[("dma", q, slot)], 16)
        self.nins[q] += 1
        self._mark((("dma", q, slot), val), reads, writes)
        return ins

    def _all_events(self):
        ev = {}
        for e in ["pe", "act", "dve", "pool"]:
            if self.cnt[e] > 0:
                ev[self.cur[e]] = self.cnt[e]
        for q, d in self.dq.items():
            n = d["n"]
            for slot in range(self.NDQ):
                c = (n - slot + self.NDQ - 1) // self.NDQ
                if c > 0:
                    ev[("dma", q, slot)] = 16 * c
        return ev

    def barrier(self):
        ev = self._all_events()
        for e in ["pe", "act", "dve", "pool", "sp"]:
            self._wait(e, {kk: v for kk, v in ev.items() if not (kk[0] == e)})

    def finish(self, toks, eng="sp"):
        deps = {}
        for t in toks:
            t = self._tok(t)
            if t.w and deps.get(t.w[0], 0) < t.w[1]:
                deps[t.w[0]] = t.w[1]
        self._wait(eng, deps)

    def close(self):
        while self.stack:
            self.stack.pop().close()


def _bf(a):
    return np.asarray(a, dtype=np.float32).astype(ml_dtypes.bfloat16)


def make_consts():
    c = {}
    i = np.arange(128)[:, None]
    j = np.arange(128)[None, :]
    c["identF"] = np.eye(128, dtype=np.float32)
    c["triF"] = (i <= j).astype(np.float32)
    c["onesF"] = np.ones((128, 128), np.float32)
    c["tribdF"] = ((i // 32 == j // 32) & (i <= j)).astype(np.float32)
    c["trisufF"] = ((i // 32 == j // 32) & (i > j)).astype(np.float32)
    c["maskbdF"] = ((i // 32 == j // 32) & (i <= j)).astype(np.float32)
    perm = np.zeros((64, 16), np.float32)
    for a in range(16):
        perm[a + 8 if a < 8 else a - 8, a] = 1.0
    c["permF"] = perm
    cf = np.concatenate([c["identF"], c["triF"], c["onesF"], c["tribdF"], c["trisufF"], c["maskbdF"]], axis=1)
    c["constF"] = cf
    negtri = -(i >= j).astype(np.float32)
    neglow = -(i < j).astype(np.float32)
    c["constB"] = _bf(np.concatenate([np.eye(128), np.ones((128, 128)), negtri, neglow], axis=1))
    jj = np.arange(896)[None, :] - 384
    caus = np.where(jj < 0, NEG, np.where(jj >= 128, 0.0, np.where(jj >= i, 0.0, NEG)))
    strict = np.where(jj < 0, NEG, np.where(jj >= 128, 0.0, np.where(jj > i, 0.0, NEG)))
    win = np.where(jj < 0, 0.0, np.where(jj >= 128, NEG, np.where(jj < i, 0.0, NEG)))
    c["strips"] = _bf(np.stack([caus, strict, win], axis=1))
    c["strips01"] = _bf(np.stack([(caus == 0.0), (win == 0.0)], axis=1).astype(np.float32))
    n = np.arange(128)[:, None]
    t = np.arange(T_)[None, :]
    c["cmask"] = _bf(np.where((16 * n + 31 <= t) & (n < 127), 0.0, NEG))
    cs = (np.arange(128) * 16)[:, None]
    ss = (np.arange(32) * 64)[None, :]
    ov = np.clip(np.minimum(cs + 32, ss + 64) - np.maximum(cs, ss), 0, None).astype(np.float32) / 32.0
    ovl = np.concatenate([np.ones((128, 1), np.float32), ov], axis=1)
    ovl[127] = 0.0
    c["ovl"] = _bf(ovl)
    c["eall"] = _bf((np.arange(T_)[None, :] // 64 == np.arange(32)[:, None]).astype(np.float32))
    half = 8
    inv = (500000.0 ** (-(np.arange(half, dtype=np.float32) * 2.0 / 16.0))).astype(np.float32)

    def tabs(pos):
        ang = pos.astype(np.float32)[None, :] * inv[:, None]
        cos, sin = np.cos(ang), np.sin(ang)
        return (np.concatenate([cos, cos], 0).astype(np.float32),
                np.concatenate([-sin, sin], 0).astype(np.float32))
    C, S = tabs(np.arange(T_))
    c["ropeC"], c["ropeS"] = C, S
    pc = np.arange(128) * 16 + 31
    Cc, Sc = tabs(pc)
    c["ropeCc"], c["ropeSc"] = Cc, Sc
    tt_ = np.arange(T_)
    qblk = tt_ // 64
    blk = np.arange(32)[None, :]
    forced = (blk == 0) | (blk == qblk[:, None]) | (blk == qblk[:, None] - 1)
    valid = blk <= qblk[:, None]
    A = (~forced & valid).astype(np.float32)
    Bc = np.where(forced, 1.0e4, np.where(valid, 0.0, -1.0e4)).astype(np.float32)
    c["tkA"] = A.reshape(16, 128, 32).transpose(1, 0, 2).copy()
    c["tkB"] = Bc.reshape(16, 128, 32).transpose(1, 0, 2).copy()
    return c


def build(nseq=SEQ_PER_CORE, nlayer=2, mixers=("hg", "fx", "sb", "ns", "out"), debug=None):
    nc = bass.Bass("TRN2", target_bir_lowering=False)
    k = KB(nc)
    dbg_outs = {}

    def din(name, shape, dt=F32):
        return k.dram(name, shape, dt, "ExternalInput")

    xT_d = din("xT", [nseq, D_, T_])
    w_in_d = din("w_in", [2, D_, NIN])
    w_out_d = din("w_out", [2, D_, D_])
    normg_d = din("norm_gT", [2, 128, 8])
    fnormg_d = din("fnorm_gT", [128, 8])
    ong_d = din("out_norm_g", [2, D_])
    lbl_d = din("lb_logits", [2, 256])
    lblT_d = din("lb_logitsT", [128, 2, 2])
    fb_d = din("fox_fb", [2, 4])
    peT_d = {kv: din(f"peT_{kv}", [2, 64, 32]) for kv in "kv"}
    w1_d = {kv: din(f"w1_{kv}", [2, 2048, 64]) for kv in "kv"}
    w2_d = {kv: din(f"w2_{kv}", [2, 64, 64]) for kv in "kv"}
    constF_d = din("constF", [128, 768])
    constB_d = din("constB", [128, 512], BF16)
    strips_d = din("strips", [128, 3, 896], BF16)
    strips01_d = din("strips01", [128, 2, 896], BF16)
    cmask_d = din("cmask", [128, T_], BF16)
    ovl_d = din("ovl", [128, 33], BF16)
    eall_d = din("eall", [32, T_], BF16)
    ropeC_d = din("ropeC", [16, T_])
    ropeS_d = din("ropeS", [16, T_])
    ropeCc_d = din("ropeCc", [16, 128])
    ropeSc_d = din("ropeSc", [16, 128])
    permF_d = din("permF", [64, 16])
    tkA_d = din("tkA", [128, 16, 32])
    tkB_d = din("tkB", [128, 16, 32])
    outT_d = k.dram("outT", [nseq, D_, T_], F32, "ExternalOutput")
    xres_d = k.dram("xres", [nseq, D_, T_], F32, "Internal")

    def dbg_out(name, shape, dt=F32):
        d = k.dram(name, shape, dt, "ExternalOutput")
        dbg_outs[name] = d
        return d

    constF = k.sb("constF", [128, 768])
    constB = k.sb("constB", [128, 512], BF16)
    strips = k.sb("strips", [128, 3, 896], BF16)
    k.dma("sp", constF[:], constF_d[:], writes=[constF])
    k.dma("sp", constB[:], constB_d[:], writes=[constB])
    k.dma("sp", strips[:], strips_d[:], writes=[strips])
    identF = constF[:, 0:128]
    triF = constF[:, 128:256]
    onesF = constF[:, 256:384]
    tribdF = constF[:, 384:512]
    trisufF = constF[:, 512:640]
    maskbdF = constF[:, 640:768]
    identB = constB[:, 0:128]
    onesB = constB[:, 128:256]
    negtriB = constB[:, 256:384]
    neglowB = constB[:, 384:512]

    hT = k.sb("hT", [128, 8, T_], BF16)
    ytok = k.sb("ytok", [128, 16, D_], BF16)
    normg = k.sb("normg", [128, 2, 8])
    fnormg = k.sb("fnormg", [128, 8])
    for l in range(2):
        k.dma("sp", normg[:, l, :], normg_d[l], writes=[normg])
    k.dma("sp", fnormg[:], fnormg_d[:], writes=[fnormg])

    PS = [k.ps(f"ps{i}", [128, 512]) for i in range(7)]
    PST = k.ps("pst", [128, 1024], BF16)
    ps_rr = {"s": [0, 1], "a": [2, 3], "o": [4, 5], "m": [6]}
    ps_ctr = {kk: 0 for kk in ps_rr}

    def psb(role):
        lst = ps_rr[role]
        i = lst[ps_ctr[role] % len(lst)]
        ps_ctr[role] += 1
        return PS[i]

    def mm(out, lhsT, rhs, start, stop, reads, writes, tp=None, ser=False):
        kw = {}
        if ser:
            k.pe_selfwait()
        if tp is not None:
            kw["tile_position"] = tp
        return k.op("pe", lambda: nc.tensor.matmul(out, lhsT=lhsT, rhs=rhs, start=start, stop=stop, **kw),
                    reads=reads, writes=writes)

    def act(out, in_, func, reads, writes, bias=None, scale=None):
        kw = {}
        if bias is not None:
            kw["bias"] = bias
        if scale is not None:
            kw["scale"] = scale
        return k.op("act", lambda: nc.scalar.activation(out=out, in_=in_, func=func, **kw),
                    reads=reads, writes=writes)

    def tt(eng, out, in0, in1, op, reads, writes):
        e = k.E[eng]
        return k.op(eng, lambda: e.tensor_tensor(out=out, in0=in0, in1=in1, op=op), reads=reads, writes=writes)

    def ts(eng, out, in0, s1, s2, op0, op1, reads, writes):
        e = k.E[eng]
        if s2 is None:
            return k.op(eng, lambda: e.tensor_scalar(out=out, in0=in0, scalar1=s1, scalar2=None, op0=op0),
                        reads=reads, writes=writes)
        return k.op(eng, lambda: e.tensor_scalar(out=out, in0=in0, scalar1=s1, scalar2=s2, op0=op0, op1=op1),
                    reads=reads, writes=writes)

    def stt(eng, out, in0, scalar, in1, op0, op1, reads, writes):
        e = k.E[eng]
        return k.op(eng, lambda: e.scalar_tensor_tensor(out=out, in0=in0, scalar=scalar, in1=in1, op0=op0, op1=op1),
                    reads=reads, writes=writes)

    def cp(eng, out, in_, reads, writes):
        if eng == "act":
            return k.op("act", lambda: nc.scalar.copy(out=out, in_=in_), reads=reads, writes=writes)
        e = k.E[eng]
        return k.op(eng, lambda: e.tensor_copy(out=out, in_=in_), reads=reads, writes=writes)

    def memset(eng, out, val, writes):
        e = k.E[eng]
        return k.op(eng, lambda: e.memset(out, val), writes=writes)

    def recip(out, in_, reads, writes):
        return k.op("dve", lambda: nc.vector.reciprocal(out=out, in_=in_), reads=reads, writes=writes)

    wslots = [k.sb("wslot0", [128, 8, 1024], BF16), k.sb("wslot1", [128, 8, 1024], BF16)]
    wcols = {"hg": [(C_HGF, 512), (C_HGQ, 512)], "fx": [(C_FXQ, 256), (C_FXK, 256), (C_FXV, 260)],
             "sb": [(C_SBQ, 256), (C_SBK, 256), (C_SBV, 256)], "ns": [(C_NSQ, 652)], "out": [(C_GATE, 1024)]}
    wplan = [(s_, l_, ph) for s_ in range(nseq) for l_ in range(nlayer)
             for ph in ("hg", "fx", "sb", "ns", "out") if ph in mixers]
    wissued = set()

    def w_issue(idx):
        if idx >= len(wplan) or idx in wissued:
            return
        wissued.add(idx)
        _, l_, ph = wplan[idx]
        slot = wslots[idx % 2]
        base = 0
        for (c0, n) in wcols[ph]:
            src = w_in_d[l_][:, c0:c0 + n].rearrange("(j p) c -> p j c", p=128)
            k.dma("pool", slot[:, 0:4, base:base + n], src[:, 0:4, :], writes=[slot])
            k.dma("pool", slot[:, 4:8, base:base + n], src[:, 4:8, :], writes=[slot])
            base += n

    def get_w(s_, l_, ph):
        idx = wplan.index((s_, l_, ph))
        w_issue(idx)
        views, base = [], 0
        for (c0, n) in wcols[ph]:
            views.append(WV(wslots[idx % 2], base))
            base += n
        w_issue(idx + 1)
        return views

    def load_w(l, c0, ncols, name="wblk"):
        w = k.sb(name, [128, 8, ncols], BF16)
        src = w_in_d[l][:, c0:c0 + ncols].rearrange("(j p) c -> p j c", p=128)
        half = 4
        k.dma("pool", w[:, 0:half, :], src[:, 0:half, :], writes=[w])
        k.dma("pool", w[:, half:8, :], src[:, half:8, :], writes=[w])
        return w

    def proj_fm(w, off, M, tb, ps, extra_reads=()):
        for j in range(8):
            mm(ps[0:M, :], w[:, j, off:off + M], hT[:, j, tb * 512:(tb + 1) * 512], j == 0, j == 7,
               reads=[w, hT, *extra_reads], writes=[ps])

    def proj_tm(w, off, N, tB, ps):
        for j in range(8):
            mm(ps[:, 0:N], hT[:, j, tB * 128:(tB + 1) * 128], w[:, j, off:off + N], j == 0, j == 7,
               reads=[w, hT], writes=[ps])

    def finalize(o, o_tok, out_ap, tmp, n=4):
        sq, ss, sd = tmp
        tt("dve", sq[:, 0:n, :], o, o, ALU.mult, reads=[o_tok], writes=[sq])
        k.op("dve", lambda: nc.vector.tensor_reduce(out=ss[:, 0:n], in_=sq[:, 0:n, :], axis=AX.X, op=ALU.add),
             reads=[sq], writes=[ss])
        act(sd[:, 0:n], ss[:, 0:n], AF.Ln, reads=[ss], writes=[sd], bias=EPS, scale=1.0 / 64.0)
        act(sd[:, 0:n], sd[:, 0:n], AF.Exp, reads=[sd], writes=[sd], scale=-0.5)
        tt("dve", out_ap, o, sd[:, 0:n].unsqueeze(2).to_broadcast([128, n, 64]), ALU.mult,
           reads=[o_tok, sd], writes=[ytok])

    def fin_tmp(n=4):
        return (k.sb("sq", [128, n, 64]), k.sb("ss", [128, n]), k.sb("sd", [128, n]))


    def run_streams(gens):
        gens = list(gens)
        while gens:
            for g in list(gens):
                try:
                    next(g)
                except StopIteration:
                    gens.remove(g)

    def attn_stream(jobs, sbank, obanks, pTs, K_ap, Q_ap, kq_toks, masks, bias_ap, bias_toks, V_ap, v_toks,
                    kb_range, qs_range, ncol, fin, post_mask=None):
        if not isinstance(obanks, (list, tuple)):
            obanks = [obanks]
        tiles = []
        for ji, (h, qb) in enumerate(jobs):
            kbs = list(kb_range(qb))
            for kb in kbs:
                tiles.append((h, qb, kb, kb == kbs[0], kb == kbs[-1], ji))
        nsb = len(sbank)
        L = max(nsb - 1, 1)

        def emit_qk(i):
            h, qb, kb = tiles[i][0:3]
            ps = sbank[i % nsb]
            ml = masks(h, qb, kb)
            mm(ps[:], K_ap(h, kb), Q_ap(h, qb), True, len(ml) == 0, reads=kq_toks, writes=[ps])
            for mi, (lt, rh, rd) in enumerate(ml):
                mm(ps[:], lt, rh, False, mi == len(ml) - 1, reads=rd, writes=[ps])

        if nsb > 1:
            for i in range(min(L, len(tiles))):
                emit_qk(i)
        else:
            emit_qk(0)
        first = True
        for i, (h, qb, kb, isfirst, islast, ji) in enumerate(tiles):
            if nsb > 1 and i + L < len(tiles):
                emit_qk(i + L)
            yield
            obank = obanks[ji % len(obanks)]
            accv = obank[:, 0:4 * ncol].rearrange("p (q d) -> p q d", q=4)
            ps = sbank[i % nsb]
            p = pTs[i % len(pTs)]
            if isfirst:
                first = True
            b = bias_ap(h, kb) if bias_ap is not None else None
            act(p[:], ps[:], AF.Exp, reads=[ps, *bias_toks], writes=[p], bias=b)
            pm = post_mask(h, qb, kb) if post_mask is not None else None
            if pm is not None:
                tt("dve", p[:], p[:], pm[0], ALU.mult, reads=[p, *pm[1]], writes=[p])
            for qs in range(4):
                lo, hi = qs_range(qb, qs)
                if kb < lo or kb > hi:
                    continue
                mm(accv[:, qs, :], p[:, qs * 128:(qs + 1) * 128], V_ap(h, kb), first, kb == hi,
                   reads=[p, *v_toks], writes=[obank])
                first = False
            if islast:
                fin(h, qb, obank, accv)
            if nsb == 1 and i + 1 < len(tiles):
                emit_qk(i + 1)
            yield

    def phase_norm(s, l):
        xsrc = xT_d if l == 0 else xres_d
        with k.scope():
            xin = [k.sb(f"xin{i}", [128, 8, 512]) for i in range(2)]
            sq = [k.sb(f"sqn{i}", [128, 8, 512], BF16) for i in range(2)]
            rs = [k.sb(f"rs{i}", [128, 512]) for i in range(2)]
            for tb in range(4):
                xi, sqi, rsi = xin[tb % 2], sq[tb % 2], rs[tb % 2]
                src = xsrc[s].rearrange("(j p) t -> p j t", p=128)[:, :, tb * 512:(tb + 1) * 512]
                k.dma("sp", xi[:], src, reads=[xsrc], writes=[xi])
                act(sqi[:], xi[:], AF.Square, reads=[xi], writes=[sqi])
                ps = psb("m")
                for j in range(8):
                    mm(ps[:], onesB, sqi[:, j, :], j == 0, j == 7, reads=[constB, sqi], writes=[ps])
                act(rsi[:], ps[:], AF.Ln, reads=[ps], writes=[rsi], bias=EPS, scale=1.0 / D_)
                act(rsi[:], rsi[:], AF.Exp, reads=[rsi], writes=[rsi], scale=-0.5)
                for j in range(8):
                    stt("dve", hT[:, j, tb * 512:(tb + 1) * 512], xi[:, j, :], normg[:, l, j:j + 1], rsi[:],
                        ALU.mult, ALU.mult, reads=[xi, rsi, normg], writes=[hT])

    def phase_fox(s, l):
        with k.scope():
            QT = k.sb("fxQT", [128, 4, T_], BF16)
            KT = k.sb("fxKT", [128, 4, T_], BF16)
            V = k.sb("fxV", [128, 16, 4, 65], BF16)
            ltok = k.sb("fxl", [128, 16, 4])
            negcum = k.sb("fxnc", [128, 16, 4])
            fbb = k.sb("fbb", [128, 4])
            k.dma("sp", fbb[:], fb_d[l:l + 1, :].to_broadcast([128, 4]), writes=[fbb])
            memset("pool", V[:, :, :, 64:65], 1.0, writes=[V])
            memset("pool", KT[64:67, :, :], 1.0, writes=[KT])
            wq_, wk_, wv_ = get_w(s, l, "fx")
            for (w, dst, scale) in ((wq_, QT, 0.125), (wk_, KT, None)):
                for pr in range(2):
                    for tb in range(4):
                        ps = psb("m")
                        proj_fm(w, pr * 128, 128, tb, ps)
                        sl = slice(tb * 512, (tb + 1) * 512)
                        for hh in range(2):
                            if scale is None:
                                cp("act", dst[0:64, 2 * pr + hh, sl], ps[64 * hh:64 * hh + 64, :], reads=[ps], writes=[dst])
                            else:
                                k.op("act", lambda: nc.scalar.mul(out=dst[0:64, 2 * pr + hh, sl],
                                                                  in_=ps[64 * hh:64 * hh + 64, :], mul=scale),
                                     reads=[ps], writes=[dst])
            if os.environ.get('FX_STOP') == '1':
                return
            w = wv_
            _skip = os.environ.get("FX_SKIP", "")
            for tB in range(int(os.environ.get("FX_NTB", "16"))):
                ps = psb("m")
                if "mm" not in _skip:
                    proj_tm(w, 0, 260, tB, ps)
                if "cp" not in _skip:
                    cp("act", V[:, tB, :, 0:64], ps[:, 0:256].rearrange("p (h d) -> p h d", h=4), reads=[ps], writes=[V])
                if "tt" not in _skip:
                    tt("dve", ltok[:, tB, :], ps[:, 256:260], fbb[:], ALU.add, reads=[ps, fbb], writes=[ltok])
            if os.environ.get('FX_STOP') == '2':
                return
            act(ltok[:], ltok[:], AF.Exp, reads=[ltok], writes=[ltok], scale=-1.0)
            act(ltok[:], ltok[:], AF.Ln, reads=[ltok], writes=[ltok], bias=1.0)
            if os.environ.get('FX_STOP') == '3':
                return
            ps = psb("m")
            lflat = ltok[:].rearrange("p b h -> p (b h)")
            mm(ps[:, 0:64], triF, lflat, True, True, reads=[constF, ltok], writes=[ps])
            mm(ps[:, 64:128], onesF, lflat, True, True, reads=[constF, ltok], writes=[ps])
            tot = k.sb("fxtot", [128, 16, 4])
            pre = k.sb("fxpre", [128, 16, 4])
            cp("dve", tot[:], ps[:, 64:128].rearrange("p (b h) -> p b h", h=4), reads=[ps], writes=[tot])
            memset("dve", pre[:, 0, :], 0.0, writes=[pre])
            for B in range(1, 16):
                tt("dve", pre[:, B, :], pre[:, B - 1, :], tot[:, B - 1, :], ALU.add, reads=[pre, tot], writes=[pre])
            tt("dve", negcum[:], ps[:, 0:64].rearrange("p (b h) -> p b h", h=4), pre[:], ALU.add,
               reads=[ps, pre], writes=[negcum])
            if os.environ.get('FX_STOP') == '4':
                return
            cumf = k.sb("cumf", [4, T_])
            c1f = k.sb("c1f", [4, T_])
            cb = [k.sb(f"cb{i}", [4, T_], BF16) for i in range(3)]
            for g in range(4):
                ps = psb("m")
                for bb in range(4):
                    B = 4 * g + bb
                    mm(ps[0:4, 128 * bb:128 * bb + 128], negcum[:, B, :], identF, True, True,
                       reads=[negcum, constF], writes=[ps])
                k.op("act", lambda: nc.scalar.mul(out=cumf[:, 512 * g:512 * g + 512], in_=ps[0:4, :], mul=-1.0),
                     reads=[ps], writes=[cumf])
            cp("dve", cb[0][:], cumf[:], reads=[cumf], writes=[cb[0]])
            cp("dve", c1f[:], cb[0][:], reads=[cb[0]], writes=[c1f])
            tt("dve", cumf[:], cumf[:], c1f[:], ALU.subtract, reads=[cumf, c1f], writes=[cumf])
            cp("dve", cb[1][:], cumf[:], reads=[cumf], writes=[cb[1]])
            cp("dve", c1f[:], cb[1][:], reads=[cb[1]], writes=[c1f])
            tt("dve", cumf[:], cumf[:], c1f[:], ALU.subtract, reads=[cumf, c1f], writes=[cumf])
            cp("dve", cb[2][:], cumf[:], reads=[cumf], writes=[cb[2]])
            for i in range(3):
                k.dma("sp", QT[64 + i:65 + i, :, :], cb[i][:], reads=[cb[i]], writes=[QT])
            if os.environ.get('FX_STOP') == '5':
                return
            def mk_stream(si, heads):
                pTs = [k.sb(f"fxpT{si}_{i}", [128, 512], BF16) for i in range(4)]
                o_sbs = [k.sb(f"fxo{si}_{i}", [128, 4, 64]) for i in range(2)]
                rds = [k.sb(f"fxrd{si}_{i}", [128, 4]) for i in range(2)]
                tmps = [fin_tmp() for i in range(2)]
                fc = [0]

                def masks(h, qb, kb):
                    if kb >= 4 * qb:
                        r = kb - 4 * qb
                        return [(identB, strips[:, 0, 384 - 128 * r:384 - 128 * r + 512], [constB, strips])]
                    return []

                def fin(h, qb, acc, accv):
                    o_sb, rd, tmp = o_sbs[fc[0] % 2], rds[fc[0] % 2], tmps[fc[0] % 2]
                    fc[0] += 1
                    recip(rd[:], accv[:, :, 64], reads=[acc], writes=[rd])
                    tt("dve", o_sb[:], accv[:, :, 0:64], rd[:].unsqueeze(2).to_broadcast([128, 4, 64]), ALU.mult,
                       reads=[acc, rd], writes=[o_sb])
                    finalize(o_sb[:], o_sb, ytok[:, 4 * qb:4 * qb + 4, 256 + 64 * h:256 + 64 * h + 64], tmp)

                return attn_stream(
                    [(h, qb) for h in heads for qb in range(4)],
                    [PS[0], PS[1], PS[2], PS[3]], [PS[4], PS[5]], pTs,
                    lambda h, kb: KT[0:67, h, kb * 128:(kb + 1) * 128],
                    lambda h, qb: QT[0:67, h, qb * 512:(qb + 1) * 512], [KT, QT],
                    masks, lambda h, kb: negcum[:, kb, h:h + 1], [negcum],
                    lambda h, kb: V[:, kb, h, :], [V],
                    lambda qb: range(0, 4 * qb + 4), lambda qb, qs: (0, 4 * qb + qs), 65, fin)

            run_streams([mk_stream(0, (0, 1, 2, 3))])


    def phase_sb(s, l):
        with k.scope():
            QT = k.sb("sbQT", [64, 4, T_], BF16)
            KT = k.sb("sbKT", [64, 4, T_], BF16)
            V = k.sb("sbV", [128, 16, 256], BF16)
            wq_, wk_, wv_ = get_w(s, l, "sb")
            for (w, dst, scale) in ((wq_, QT, 0.125), (wk_, KT, None)):
                for pr in range(2):
                    for tb in range(4):
                        ps = psb("m")
                        proj_fm(w, pr * 128, 128, tb, ps)
                        sl = slice(tb * 512, (tb + 1) * 512)
                        for hh in range(2):
                            if scale is None:
                                cp("act", dst[0:64, 2 * pr + hh, sl], ps[64 * hh:64 * hh + 64, :], reads=[ps], writes=[dst])
                            else:
                                k.op("act", lambda: nc.scalar.mul(out=dst[0:64, 2 * pr + hh, sl],
                                                                  in_=ps[64 * hh:64 * hh + 64, :], mul=scale),
                                     reads=[ps], writes=[dst])
            w = wv_
            for tB in range(16):
                ps = psb("m")
                proj_tm(w, 0, 256, tB, ps)
                cp("act", V[:, tB, :], ps[:, 0:256], reads=[ps], writes=[V])
            def sb_stream(si, heads):
                es = [k.sb(f"sbe{si}_{i}", [128, 512]) for i in range(2)]
                sps = [k.sb(f"sbsp{si}_{i}", [128, 512], BF16) for i in range(2)]
                er_ = k.sb(f"sber{si}", [128, 512])
                as_ = [k.sb(f"sbaT{si}_{i}", [128, 512], BF16) for i in range(2)]
                o = k.sb(f"sbo{si}", [128, 4, 64])
                tmp = fin_tmp()
                zb = [PS[3 * si], PS[3 * si + 1]]
                psR = PS[3 * si + 2]
                acc = PS[6]
                accv = acc[:, 256 * si:256 * si + 256].rearrange("p (q d) -> p q d", q=4)
                tiles = []
                for h in heads:
                    for qb in range(4):
                        nkb = 4 * qb + 4
                        for idx, kb in enumerate(reversed(range(nkb))):
                            tiles.append((h, qb, kb, idx, nkb))
                n = len(tiles)

                def stage1(i):
                    h, qb, kb, idx, nkb = tiles[i]
                    ps = zb[i % 2]
                    diag = kb >= 4 * qb
                    mm(ps[:], KT[0:64, h, kb * 128:(kb + 1) * 128], QT[0:64, h, qb * 512:(qb + 1) * 512],
                       True, not diag, reads=[KT, QT], writes=[ps])
                    if diag:
                        r = kb - 4 * qb
                        mm(ps[:], identB, strips[:, 1, 384 - 128 * r:384 - 128 * r + 512], False, True,
                           reads=[constB, strips], writes=[ps])

                def stage2(i):
                    ps, e, sp_ = zb[i % 2], es[i % 2], sps[i % 2]
                    act(e[:], ps[:], AF.Exp, reads=[ps], writes=[e])
                    act(sp_[:], e[:], AF.Ln, reads=[e], writes=[sp_], bias=1.0)

                stage1(0)
                stage2(0)
                for i, (h, qb, kb, idx, nkb) in enumerate(tiles):
                    e, sp_, a_ = es[i % 2], sps[i % 2], as_[i % 2]
                    if i + 1 < n:
                        stage1(i + 1)
                    mm(psR[:], negtriB, sp_[:], idx == 0, False, reads=[constB, sp_], writes=[psR])
                    if idx == 0:
                        memset("dve", accv, 0.0, writes=[acc])
                    yield
                    act(er_[:], psR[:], AF.Exp, reads=[psR], writes=[er_])
                    if i + 1 < n:
                        stage2(i + 1)
                    tt("dve", a_[:], e[:], er_[:], ALU.mult, reads=[e, er_], writes=[a_])
                    mm(psR[:], neglowB, sp_[:], False, idx == nkb - 1, reads=[constB, sp_], writes=[psR])
                    yield
                    for qs in range(4):
                        if kb > 4 * qb + qs:
                            continue
                        mm(accv[:, qs, :], a_[:, qs * 128:(qs + 1) * 128], V[:, kb, 64 * h:64 * h + 64],
                           False, kb == 0, reads=[a_, V], writes=[acc])
                    if idx == nkb - 1:
                        cp("dve", o[:], accv, reads=[acc], writes=[o])
                        finalize(o[:], o, ytok[:, 4 * qb:4 * qb + 4, 512 + 64 * h:512 + 64 * h + 64], tmp)
                    yield

            run_streams([sb_stream(0, (0, 1)), sb_stream(1, (2, 3))])


    def phase_hgrn(s, l):
        with k.scope():
            omlb = k.sb("omlb", [128, 256])
            omlT = k.sb("omlT", [128, 2])
            if l == 0:
                memset("pool", omlb[:], 1.0, writes=[omlb])
                memset("pool", omlT[:], 1.0, writes=[omlT])
            else:
                with k.scope():
                    lbb = k.sb("lbb", [128, 2, 256])
                    lblT = k.sb("lblT", [128, 2, 2])
                    k.dma("sp", lbb[:], lbl_d[:].unsqueeze(0).to_broadcast([128, 2, 256]), writes=[lbb])
                    k.dma("sp", lblT[:], lblT_d[:], writes=[lblT])
                    tt("dve", omlb[:], lbb[:, 0, :], lbb[:, 1, :], ALU.subtract, reads=[lbb], writes=[omlb])
                    act(omlb[:], omlb[:], AF.Sigmoid, reads=[omlb], writes=[omlb])
                    tt("dve", omlT[:], lblT[:, :, 0], lblT[:, :, 1], ALU.subtract, reads=[lblT], writes=[omlT])
                    act(omlT[:], omlT[:], AF.Sigmoid, reads=[omlT], writes=[omlT])
            big1 = k.sb("hgbig1", [128, 4096])
            gtok = k.sb("hggtok", [128, 16, 256])
            vtok = k.sb("hgvtok", [128, 16, 256], BF16)
            khat = k.sb("hgkhat", [128, 16, 256], BF16)
            ktok = big1[:].rearrange("p (b c) -> p b c", c=256)
            w, w2 = get_w(s, l, "hg")
            for tB in range(16):
                ps = psb("m")
                proj_tm(w, 0, 512, tB, ps)
                act(ktok[:, tB, :], ps[:, 0:256], AF.Sigmoid, reads=[ps], writes=[big1], scale=-1.0)
                cp("dve", vtok[:, tB, :], ps[:, 256:512], reads=[ps], writes=[vtok])
            for tB in range(16):
                tt("dve", ktok[:, tB, :], ktok[:, tB, :], omlb[:], ALU.mult, reads=[big1, omlb], writes=[big1])
            act(gtok[:], ktok, AF.Ln, reads=[big1], writes=[gtok], bias=1.0, scale=-1.0)
            ebs = [k.sb(f"hgebs{i}", [128, 256]) for i in range(1)]
            for tB in range(16):
                ps = psb("m")
                mm(ps[:, 0:256], trisufF, gtok[:, tB, :], True, True, reads=[constF, gtok], writes=[ps])
                eb = ebs[0]
                act(eb[:], ps[:, 0:256], AF.Exp, reads=[ps], writes=[eb])
                tt("dve", khat[:, tB, :], ktok[:, tB, :], eb[:], ALU.mult, reads=[big1, eb], writes=[khat])
            if os.environ.get('HG_STOP') == '1':
                return
            qsT = k.sb("hgqsT", [128, 2, T_])
            kT = big1[:].rearrange("p (a t) -> p a t", a=2)
            for pr in range(2):
                for tb in range(4):
                    sl = slice(tb * 512, (tb + 1) * 512)
                    ps = psb("m")
                    proj_fm(w2, pr * 128, 128, tb, ps)
                    act(qsT[:, pr, sl], ps[:], AF.Silu, reads=[ps], writes=[qsT])
                    ps = psb("m")
                    proj_fm(w2, 256 + pr * 128, 128, tb, ps, extra_reads=[khat])
                    act(kT[:, pr, sl], ps[:], AF.Sigmoid, reads=[ps, khat], writes=[big1], scale=-1.0)
                    ts("dve", kT[:, pr, sl], kT[:, pr, sl], omlT[:, pr:pr + 1], None, ALU.mult, None,
                       reads=[big1, omlT], writes=[big1])
            qtT = k.sb("hgqtT", [128, 2, T_], BF16)
            ktT = k.sb("hgktT", [128, 2, T_], BF16)
            dl = k.sb("hgdl", [128, 2, 64])
            e1s = [k.sb(f"hge1{i}", [128, 512]) for i in range(1)]
            e2s = [k.sb(f"hge2{i}", [128, 512]) for i in range(1)]
            it = 0
            for pr in range(2):
                for g4 in range(4):
                    sl = slice(g4 * 512, (g4 + 1) * 512)
                    ps = psb("a")
                    for bb in range(4):
                        tB = 4 * g4 + bb
                        mm(ps[:, 128 * bb:128 * bb + 128], gtok[:, tB, pr * 128:(pr + 1) * 128], tribdF, True, True,
                           reads=[gtok, constF], writes=[ps])
                    e1, e2 = e1s[0], e2s[0]
                    it += 1
                    act(e1[:], ps[:], AF.Exp, reads=[ps], writes=[e1])
                    act(e2[:], ps[:], AF.Exp, reads=[ps], writes=[e2], scale=-1.0)
                    tt("dve", qtT[:, pr, sl], qsT[:, pr, sl], e1[:], ALU.mult, reads=[qsT, e1], writes=[qtT])
                    tt("dve", ktT[:, pr, sl], kT[:, pr, sl], e2[:], ALU.mult, reads=[big1, e2], writes=[ktT])
                    cp("pool", dl[:, pr, 16 * g4:16 * g4 + 16], e1[:, 31:512:32], reads=[e1], writes=[dl])
            if os.environ.get('HG_STOP') == '2':
                return
            Srun = [k.sb(f"hgS{i}", [128, 5, 2, 64]) for i in range(2)]
            Sbf = [k.sb(f"hgSb{i}", [128, 4, 2, 64], BF16) for i in range(2)]
            Abd = [k.sb(f"hgA{i}", [128, 4, 128], BF16) for i in range(2)]
            vbd = [k.sb(f"hgvbd{i}", [128, 4, 4, 64], BF16) for i in range(2)]
            o_sb = [k.sb(f"hgo{i}", [128, 4, 64]) for i in range(1)]
            tmp = fin_tmp()
            memset("pool", Srun[1][:, 4, :, :], 0.0, writes=[Srun[1]])
            def hg_state(B):
                cur, prev = Srun[B % 2], Srun[(B + 1) % 2]
                vb = vbd[B % 2]
                tt("dve", vb[:], vtok[:, B, :].rearrange("p (h d) -> p h d", h=4).unsqueeze(2).to_broadcast([128, 4, 4, 64]),
                   maskbdF[:, 31:128:32].unsqueeze(1).unsqueeze(3).to_broadcast([128, 4, 4, 64]), ALU.mult,
                   reads=[vtok, constF], writes=[vb])
                psD = psb("a")
                for h in range(4):
                    pr, r0 = h // 2, 64 * (h % 2)
                    mm(psD[r0:r0 + 64, pr * 256:pr * 256 + 256], khat[:, B, 64 * h:64 * h + 64],
                       vb[:, h, :, :].rearrange("p c d -> p (c d)"), True, True,
                       reads=[khat, vb], writes=[psD], tp=(0, r0), ser=True)
                cp("pool", cur[:, 0, :, :], prev[:, 4, :, :], reads=[prev], writes=[cur])
                for c in range(4):
                    for pr in range(2):
                        col = (pr * 4 + c) * 64
                        stt("dve", cur[:, c + 1, pr, :], cur[:, c, pr, :], dl[:, pr, 4 * B + c:4 * B + c + 1],
                            psD[:, col:col + 64], ALU.mult, ALU.add, reads=[cur, dl, psD], writes=[cur])
                Sb = Sbf[B % 2]
                cp("act", Sb[:], cur[:, 0:4, :, :], reads=[cur], writes=[Sb])

            def hg_output(B):
                bsl = slice(B * 128, (B + 1) * 128)
                Sb = Sbf[B % 2]
                psA = psb("s")
                for h in (0, 2, 1, 3):
                    pr, r0 = h // 2, 64 * (h % 2)
                    mm(psA[:, 128 * h:128 * h + 128], ktT[r0:r0 + 64, pr, bsl], qtT[r0:r0 + 64, pr, bsl], True, True,
                       reads=[ktT, qtT], writes=[psA], ser=(h in (0, 1)))
                A = Abd[B % 2]
                tt("dve", A[:], psA[:].rearrange("p (h t) -> p h t", h=4),
                   maskbdF.unsqueeze(1).to_broadcast([128, 4, 128]), ALU.mult, reads=[psA, constF], writes=[A])
                psO = psb("o")
                for h in range(4):
                    mm(psO[:, 64 * h:64 * h + 64], A[:, h, :], vtok[:, B, 64 * h:64 * h + 64], h == 0, False,
                       reads=[A, vtok], writes=[psO], ser=(h == 0))
                for h in range(4):
                    pr, r0 = h // 2, 64 * (h % 2)
                    for c in range(4):
                        mm(psO[32 * c:32 * c + 32, 64 * h:64 * h + 64],
                           qtT[r0:r0 + 64, pr, B * 128 + 32 * c:B * 128 + 32 * c + 32], Sb[r0:r0 + 64, c, pr, :],
                           False, h == 3 and c == 3, reads=[qtT, Sb], writes=[psO], tp=(r0, 32 * c), ser=True)
                o = o_sb[0]
                cp("dve", o[:], psO[:, 0:256].rearrange("p (h d) -> p h d", h=4), reads=[psO], writes=[o])
                finalize(o[:], o, ytok[:, B, 0:256].rearrange("p (h d) -> p h d", h=4), tmp)

            hg_state(0)
            for B in range(16):
                if B + 1 < 16:
                    hg_state(B + 1)
                hg_output(B)

    def phase_nsa(s, l):
        with k.scope():
            ovl = k.sb("ovl", [128, 33], BF16)
            for dst, src in ((ovl, ovl_d),):
                k.dma("sp", dst[:], src[:], writes=[dst])
            qT = k.sb("nsqT", [64, 4, T_], BF16)
            ksT = k.sb("nsksT", [64, T_], BF16)
            kwT = k.sb("nskwT", [64, T_], BF16)
            kcT = k.sb("nskcT", [64, T_], BF16)
            vcT = k.sb("nsvcT", [64, T_], BF16)
            Vs = k.sb("nsVs", [128, 16, 65], BF16)
            Vw = k.sb("nsVw", [128, 16, 65], BF16)
            gtok = k.sb("nsg", [128, 16, 12])
            memset("pool", Vs[:, :, 64:65], 1.0, writes=[Vs])
            memset("pool", Vw[:, :, 64:65], 1.0, writes=[Vw])
            kcmpT = k.sb("nskcmpT", [64, 128], BF16)
            rhsc = k.sb("nsrhsc", [128, 97], BF16)
            with k.scope():
                ropeC = k.sb("ropeC", [16, T_])
                ropeS = k.sb("ropeS", [16, T_])
                ropeCc = k.sb("ropeCc", [16, 128])
                ropeSc = k.sb("ropeSc", [16, 128])
                permF = k.sb("permF", [64, 16])
                for dst, src in ((ropeC, ropeC_d), (ropeS, ropeS_d), (ropeCc, ropeCc_d), (ropeSc, ropeSc_d),
                                 (permF, permF_d)):
                    k.dma("sp", dst[:], src[:], writes=[dst])
                q32s = [k.sb(f"nsq32{i}", [64, 512]) for i in range(2)]
                t1s = [k.sb(f"nst1{i}", [16, 512]) for i in range(2)]
                t2s = [k.sb(f"nst2{i}", [16, 512]) for i in range(2)]
                rc = [0]

                def rope_evac(src, dst, n, scale, Ct, St, extra_tok):
                    i = rc[0] % 2
                    rc[0] += 1
                    q32, t1, t2 = q32s[i], t1s[i], t2s[i]
                    k.op("act", lambda: nc.scalar.mul(out=q32[:, 0:n], in_=src, mul=scale),
                         reads=[extra_tok["src"]], writes=[q32])
                    cp("pool", dst, q32[:, 0:n], reads=[q32], writes=[extra_tok["dst"]])
                    psw = psb("a")
                    mm(psw[0:16, 0:n], permF[:, :], q32[:, 0:n], True, True, reads=[permF, q32], writes=[psw])
                    tt("dve", t1[:, 0:n], q32[0:16, 0:n], Ct, ALU.mult, reads=[q32, extra_tok["tab"]], writes=[t1])
                    tt("dve", t2[:, 0:n], psw[0:16, 0:n], St, ALU.mult, reads=[psw, extra_tok["tab"]], writes=[t2])
                    tt("dve", extra_tok["dst16"], t1[:, 0:n], t2[:, 0:n], ALU.add,
                       reads=[t1, t2], writes=[extra_tok["dst"]])

                (w,) = get_w(s, l, "ns")
                cmpW = {}
                for kv in "kv":
                    W1 = k.sb(f"nsW1{kv}", [64, 32, 64], BF16)
                    W2 = k.sb(f"nsW2{kv}", [64, 64], BF16)
                    peT = k.sb(f"nspeT{kv}", [64, 32], BF16)
                    k.dma("pool", W1[:], w1_d[kv][l].rearrange("(lp d) j -> d lp j", d=64), writes=[W1])
                    k.dma("pool", W2[:], w2_d[kv][l], writes=[W2])
                    k.dma("pool", peT[:], peT_d[kv][l], writes=[peT])
                    cmpW[kv] = (W1, W2, peT)
                for pr in range(2):
                    for tb in range(4):
                        sl = slice(tb * 512, (tb + 1) * 512)
                        ps = psb("m")
                        proj_fm(w, pr * 128, 128, tb, ps)
                        for hh in range(2):
                            h = 2 * pr + hh
                            rope_evac(ps[64 * hh:64 * hh + 64, :], qT[0:64, h, sl], 512, 0.125, ropeC[:, sl], ropeS[:, sl],
                                      dict(src=ps, dst=qT, tab=ropeC, dst16=qT[0:16, h, sl]))
                for tb in range(4):
                    sl = slice(tb * 512, (tb + 1) * 512)
                    ps = psb("m")
                    proj_fm(w, 256, 128, tb, ps)
                    cp("act", kcT[:, sl], ps[0:64, :], reads=[ps], writes=[kcT])
                    cp("act", vcT[:, sl], ps[64:128, :], reads=[ps], writes=[vcT])
                for off, dst in ((384, ksT), (512, kwT)):
                    for tb in range(4):
                        sl = slice(tb * 512, (tb + 1) * 512)
                        ps = psb("m")
                        proj_fm(w, off, 64, tb, ps)
                        rope_evac(ps[0:64, :], dst[0:64, sl], 512, 1.0, ropeC[:, sl], ropeS[:, sl],
                                  dict(src=ps, dst=dst, tab=ropeC, dst16=dst[0:16, sl]))
                for tB in range(16):
                    ps = psb("m")
                    proj_tm(w, 448, 204, tB, ps)
                    cp("act", Vs[:, tB, 0:64], ps[:, 0:64], reads=[ps], writes=[Vs])
                    cp("act", Vw[:, tB, 0:64], ps[:, 128:192], reads=[ps], writes=[Vw])
                    act(gtok[:, tB, :], ps[:, 192:204], AF.Sigmoid, reads=[ps], writes=[gtok])
                memset("pool", kcmpT[:], 0.0, writes=[kcmpT])
                memset("pool", rhsc[:], 0.0, writes=[rhsc])
                cp("pool", rhsc[:, 64:97], ovl[:], reads=[ovl], writes=[rhsc])
                for kv, srcT in (("k", kcT), ("v", vcT)):
                    W1, W2, peT = cmpW[kv]
                    psH = psb("a")
                    for lp in range(32):
                        mm(psH[0:64, 0:127], W1[:, lp, :], srcT[:, lp:lp + 16 * 126 + 1:16], lp == 0, lp == 31,
                           reads=[W1, srcT], writes=[psH])
                    for lp in range(32):
                        mm(psH[0:64, 127:128], W1[:, lp, :], peT[:, lp:lp + 1], lp == 0, lp == 31,
                           reads=[W1, peT], writes=[psH])
                    bias = k.sb(f"nsbias{kv}", [64, 1])
                    hid = k.sb(f"nshid{kv}", [64, 128], BF16)
                    cp("dve", bias[:], psH[0:64, 127:128], reads=[psH], writes=[bias])
                    act(hid[:, 0:127], psH[0:64, 0:127], AF.Silu, reads=[psH, bias], writes=[hid], bias=bias[:, 0:1])
                    ps2 = psb("m")
                    if kv == "k":
                        mm(ps2[0:64, 0:127], W2[:, :], hid[:, 0:127], True, True, reads=[W2, hid], writes=[ps2])
                        rope_evac(ps2[0:64, 0:127], kcmpT[0:64, 0:127], 127, 1.0, ropeCc[:, 0:127], ropeSc[:, 0:127],
                                  dict(src=ps2, dst=kcmpT, tab=ropeCc, dst16=kcmpT[0:16, 0:127]))
                    else:
                        mm(ps2[0:127, 0:64], hid[:, 0:127], W2[:, :], True, True, reads=[W2, hid], writes=[ps2])
                        cp("act", rhsc[0:127, 0:64], ps2[0:127, 0:64], reads=[ps2], writes=[rhsc])
            cmask = k.sb("cmask", [128, T_], BF16)
            eall = k.sb("eall", [32, T_], BF16)
            tkA = k.sb("tkA", [128, 16, 32])
            tkB = k.sb("tkB", [128, 16, 32])
            nacc = k.sb("nsacc", [128, 16, 256])
            imp = k.sb("nsimp", [128, 16, 32])
            negT = k.sb("nsnegT", [32, T_], BF16)
            s01 = k.sb("nss01", [128, 2, 896], BF16)
            for dst, src in ((cmask, cmask_d), (eall, eall_d), (tkA, tkA_d), (tkB, tkB_d), (s01, strips01_d)):
                k.dma("sp", dst[:], src[:], writes=[dst])
            pT = [k.sb(f"nspT{i}", [128, 512], BF16) for i in range(3)]
            rden = [k.sb(f"nsrd{i}", [128, 4]) for i in range(2)]
            cf = [k.sb(f"nscf{i}", [128, 4]) for i in range(2)]
            tmpo = [k.sb(f"nstmpo{i}", [128, 4, 64]) for i in range(2)]
            tmpi = [k.sb(f"nstmpi{i}", [128, 4, 32]) for i in range(2)]
            it = 0
            fi = 0
            for h in range(4):
                for qb in range(4):
                    qsl = slice(qb * 512, (qb + 1) * 512)
                    ps = psb("s")
                    mm(ps[:], kcmpT[:, :], qT[0:64, h, qsl], True, False, reads=[kcmpT, qT], writes=[ps])
                    mm(ps[:], identB, cmask[:, qsl], False, True, reads=[constB, cmask], writes=[ps])
                    p = pT[it % 3]
                    it += 1
                    act(p[:], ps[:], AF.Exp, reads=[ps], writes=[p])
                    acc = psb("o")
                    accv = acc[:, 0:388].rearrange("p (q d) -> p q d", q=4)
                    for qs in range(4):
                        mm(accv[:, qs, :], p[:, qs * 128:(qs + 1) * 128], rhsc[:, :], qs == 0, True,
                           reads=[p, rhsc], writes=[acc])
                    rd, c_ = rden[fi % 2], cf[fi % 2]
                    to, ti = tmpo[fi % 2], tmpi[fi % 2]
                    fi += 1
                    ts("dve", rd[:], accv[:, :, 64], 1e-30, None, ALU.max, None, reads=[acc], writes=[rd])
                    recip(rd[:], rd[:], reads=[rd], writes=[rd])
                    tt("dve", c_[:], rd[:], gtok[:, 4 * qb:4 * qb + 4, h], ALU.mult, reads=[rd, gtok], writes=[c_])
                    tt("dve", nacc[:, 4 * qb:4 * qb + 4, 64 * h:64 * h + 64], accv[:, :, 0:64],
                       c_[:].unsqueeze(2).to_broadcast([128, 4, 64]), ALU.mult, reads=[acc, c_], writes=[nacc])
                    if h == 0:
                        tt("dve", imp[:, 4 * qb:4 * qb + 4, :], accv[:, :, 65:97],
                           rd[:].unsqueeze(2).to_broadcast([128, 4, 32]), ALU.mult, reads=[acc, rd], writes=[imp])
                    else:
                        tt("dve", ti[:], accv[:, :, 65:97], rd[:].unsqueeze(2).to_broadcast([128, 4, 32]), ALU.mult,
                           reads=[acc, rd], writes=[ti])
                        tt("pool", imp[:, 4 * qb:4 * qb + 4, :], imp[:, 4 * qb:4 * qb + 4, :], ti[:], ALU.add,
                           reads=[imp, ti], writes=[imp])
            score = k.sb("nsscore", [128, 16, 32])
            negm = k.sb("nsnegm", [128, 16, 32])
            mx = [k.sb(f"nsmx{i}", [128, 8]) for i in range(2)]
            sc2 = k.sb("nssc2", [128, 32])
            tt("dve", score[:], imp[:], tkA[:], ALU.mult, reads=[imp, tkA], writes=[score])
            tt("dve", score[:], score[:], tkB[:], ALU.add, reads=[score, tkB], writes=[score])
            for tB in range(16):
                k.op("dve", lambda: nc.vector.max(out=mx[0][:], in_=score[:, tB, :]), reads=[score], writes=[mx[0]])
                k.op("dve", lambda: nc.vector.match_replace(out=sc2[:], in_to_replace=mx[0][:], in_values=score[:, tB, :],
                                                            imm_value=-1.0e9), reads=[score, mx[0]], writes=[sc2])
                k.op("dve", lambda: nc.vector.max(out=mx[1][:], in_=sc2[:]), reads=[sc2], writes=[mx[1]])
                ts("dve", negm[:, tB, :], score[:, tB, :], mx[1][:, 7:8], NEG, ALU.is_lt, ALU.mult,
                   reads=[score, mx[1]], writes=[negm])
            for g4 in range(4):
                ps = psb("m")
                for bb in range(4):
                    mm(ps[0:32, 128 * bb:128 * bb + 128], negm[:, 4 * g4 + bb, :], identF, True, True,
                       reads=[negm, constF], writes=[ps])
                cp("act", negT[:, 512 * g4:512 * g4 + 512], ps[0:32, :], reads=[ps], writes=[negT])

            def caus_mask(qb, kb):
                if kb >= 4 * qb:
                    r = kb - 4 * qb
                    return s01[:, 0, 384 - 128 * r:384 - 128 * r + 512]
                return None

            def win_mask(qb, kb):
                r = kb - 4 * qb
                if r >= 0:
                    return s01[:, 0, 384 - 128 * r:384 - 128 * r + 512]
                return s01[:, 1, 384 - 128 * (r + 4):384 - 128 * (r + 4) + 512]

            pTs4 = pT + [k.sb("nspT3", [128, 512], BF16)]

            def br_stream(si, bi, KTt, Vt, kb_range, mask_fn, use_sel, qs_range):
                pTs = [k.sb(f"nsbp{si}_{i}", [128, 512], BF16) for i in range(2)] if False else pTs4
                fc = [0]

                def masks(h, qb, kb):
                    ml = []
                    ksl = slice(kb * 128, (kb + 1) * 128)
                    if use_sel and qb >= 2:
                        ml.append((eall[0:32, ksl], negT[0:32, qb * 512:(qb + 1) * 512], [eall, negT]))
                    return ml

                def post_mask(h, qb, kb):
                    mk = mask_fn(qb, kb)
                    return None if mk is None else (mk, [s01])

                def fin(h, qb, acc, accv):
                    rd, c_, to = rden[fc[0] % 2], cf[fc[0] % 2], tmpo[fc[0] % 2]
                    fc[0] += 1
                    recip(rd[:], accv[:, :, 64], reads=[acc], writes=[rd])
                    tt("dve", c_[:], rd[:], gtok[:, 4 * qb:4 * qb + 4, 4 * bi + h], ALU.mult, reads=[rd, gtok], writes=[c_])
                    tt("dve", to[:], accv[:, :, 0:64], c_[:].unsqueeze(2).to_broadcast([128, 4, 64]), ALU.mult,
                       reads=[acc, c_], writes=[to])
                    tt("pool", nacc[:, 4 * qb:4 * qb + 4, 64 * h:64 * h + 64],
                       nacc[:, 4 * qb:4 * qb + 4, 64 * h:64 * h + 64], to[:], ALU.add, reads=[nacc, to], writes=[nacc])

                return attn_stream(
                    [(h, qb) for h in range(4) for qb in range(4)],
                    [PS[0], PS[1], PS[2], PS[3]], [PS[4], PS[5]], pTs,
                    lambda h, kb: KTt[0:64, kb * 128:(kb + 1) * 128],
                    lambda h, qb: qT[0:64, h, qb * 512:(qb + 1) * 512], [KTt, qT],
                    masks, None, [],
                    lambda h, kb: Vt[:, kb, :], [Vt],
                    kb_range, qs_range, 65, fin, post_mask=post_mask)

            run_streams([
                br_stream(0, 1, ksT, Vs, lambda qb: range(0, 4 * qb + 4), caus_mask, True,
                          lambda qb, qs: (0, 4 * qb + qs))])
            run_streams([
                br_stream(1, 2, kwT, Vw, lambda qb: range(max(0, 4 * qb - 4), 4 * qb + 4), win_mask, False,
                          lambda qb, qs: (max(0, 4 * qb + qs - 4), 4 * qb + qs)),
            ])
            tmp = fin_tmp(4)
            for g4 in range(4):
                for bb in range(4):
                    finalize(nacc[:, 4 * g4 + bb, :].rearrange("p (h d) -> p h d", h=4), nacc,
                             ytok[:, 4 * g4 + bb, 768:1024].rearrange("p (h d) -> p h d", h=4), tmp, n=4)

    def phase_out(s, l, last):
        xsrc = xT_d if l == 0 else xres_d
        with k.scope():
            (wg,) = get_w(s, l, "out")
            yT = k.sb("yT", [128, 8, T_], BF16)
            wo = k.sb("wo", [128, 8, D_], BF16)
            wsrc = w_out_d[l].rearrange("(j p) c -> p j c", p=128)
            k.dma("pool", wo[:, 0:4, :], wsrc[:, 0:4, :], writes=[wo])
            k.dma("pool", wo[:, 4:8, :], wsrc[:, 4:8, :], writes=[wo])
            def gate_stream(si, tBs):
                sgi = k.sb(f"sg{si}", [128, D_])
                ygi = k.sb(f"yg{si}", [128, D_], BF16)
                pa, pb = PS[2 * si], PS[2 * si + 1]
                for tB in tBs:
                    proj_tm(wg, 0, 512, tB, pa)
                    proj_tm(wg, 512, 512, tB, pb)
                    yield
                    act(sgi[:, 0:512], pa[:], AF.Silu, reads=[pa], writes=[sgi])
                    act(sgi[:, 512:1024], pb[:], AF.Silu, reads=[pb], writes=[sgi])
                    tt("pool", sgi[:], sgi[:], ong[:], ALU.mult, reads=[sgi, ong], writes=[sgi])
                    yield
                    tt("dve", ygi[:], sgi[:], ytok[:, tB, :], ALU.mult, reads=[sgi, ytok], writes=[ygi])
                    for cc in range(8):
                        k.op("pe", lambda: nc.tensor.transpose(PST[:, cc * 128:(cc + 1) * 128],
                                                               ygi[:, cc * 128:(cc + 1) * 128], identB),
                             reads=[ygi, constB], writes=[PST])
                    cp("act", yT[:, :, tB * 128:(tB + 1) * 128], PST[:].rearrange("p (c t) -> p c t", c=8),
                       reads=[PST], writes=[yT])

            with k.scope():
                ong = k.sb("ong", [128, D_])
                k.dma("sp", ong[:], ong_d[l:l + 1, :].to_broadcast([128, D_]), writes=[ong])
                run_streams([gate_stream(0, range(0, 16, 3)), gate_stream(1, range(1, 16, 3)),
                             gate_stream(2, range(2, 16, 3))])
            xin = [k.sb(f"xo{i}", [128, 8, 512]) for i in range(2)]
            if last:
                sqf = k.sb("sqf", [128, 8, 512], BF16)
                rsf = k.sb("rsf", [128, 512])
            for tb in range(4):
                sl = slice(tb * 512, (tb + 1) * 512)
                xi = xin[tb % 2]
                src = xsrc[s].rearrange("(j p) t -> p j t", p=128)[:, :, sl]
                k.dma("sp", xi[:], src, reads=[xsrc], writes=[xi])
                for dj in range(8):
                    ps = PS[(tb * 8 + dj) % 6]
                    for cc in range(8):
                        mm(ps[:], wo[:, cc, dj * 128:(dj + 1) * 128], yT[:, cc, sl], cc == 0, cc == 7,
                           reads=[wo, yT], writes=[ps])
                    tt("dve", xi[:, dj, :], xi[:, dj, :], ps[:], ALU.add, reads=[xi, ps], writes=[xi])
                if not last:
                    dst = xres_d[s].rearrange("(j p) t -> p j t", p=128)[:, :, sl]
                    k.dma("sp", dst, xi[:], reads=[xi], writes=[xres_d])
                else:
                    act(sqf[:], xi[:], AF.Square, reads=[xi], writes=[sqf])
                    ps = psb("m")
                    for j in range(8):
                        mm(ps[:], onesB, sqf[:, j, :], j == 0, j == 7, reads=[constB, sqf], writes=[ps])
                    act(rsf[:], ps[:], AF.Ln, reads=[ps], writes=[rsf], bias=EPS, scale=1.0 / D_)
                    act(rsf[:], rsf[:], AF.Exp, reads=[rsf], writes=[rsf], scale=-0.5)
                    for j in range(8):
                        stt("dve", xi[:, j, :], xi[:, j, :], fnormg[:, j:j + 1], rsf[:], ALU.mult, ALU.mult,
                            reads=[xi, rsf, fnormg], writes=[xi])
                    dst = outT_d[s].rearrange("(j p) t -> p j t", p=128)[:, :, sl]
                    k.dma("sp", dst, xi[:], reads=[xi], writes=[outT_d])

    for s in range(nseq):
        for l in range(nlayer):
            phase_norm(s, l)
            if debug and s == 0 and l == 0 and "hT" in debug:
                d = dbg_out("dbg_hT", [128, 8, T_], BF16)
                k.dma("sp", d[:], hT[:], reads=[hT], writes=[d])
            if "hg" in mixers:
                phase_hgrn(s, l)
            if "fx" in mixers:
                phase_fox(s, l)
            if "sb" in mixers:
                phase_sb(s, l)
            if "ns" in mixers:
                phase_nsa(s, l)
            if debug and s == 0 and l == nlayer - 1 and "ytok" in debug:
                d = dbg_out("dbg_ytok", [128, 16, D_], BF16)
                k.dma("sp", d[:], ytok[:], reads=[ytok], writes=[d])
            if "out" in mixers:
                phase_out(s, l, l == nlayer - 1)

    k.finish(list(dbg_outs.values()) + [outT_d])
    k.barrier()
    k.close()
    return nc, k, list(dbg_outs.keys())


def host_inputs(inputs, core, consts, nseq=SEQ_PER_CORE):
    f32 = np.float32
    x = inputs["x"]
    b0 = core * nseq
    m = {}
    m["xT"] = np.ascontiguousarray(np.transpose(x[b0:b0 + nseq], (0, 2, 1))).astype(f32)
    m["w_in"] = np.ascontiguousarray(inputs["w_in"], dtype=f32)
    m["w_out"] = np.ascontiguousarray(inputs["w_out"], dtype=f32)
    m["norm_gT"] = np.ascontiguousarray(inputs["norm_g"].reshape(2, 8, 128).transpose(0, 2, 1), dtype=f32)
    m["fnorm_gT"] = np.ascontiguousarray(inputs["final_norm_g"].reshape(8, 128).T, dtype=f32)
    m["out_norm_g"] = np.ascontiguousarray(inputs["out_norm_g"], dtype=f32)
    lbl = np.asarray(inputs["hgrn_lb_logits"], dtype=f32)
    m["lb_logits"] = np.ascontiguousarray(lbl)
    m["lb_logitsT"] = np.ascontiguousarray(lbl.reshape(2, 2, 128).transpose(2, 1, 0))
    m["fox_fb"] = np.ascontiguousarray(inputs["fox_fb"], dtype=f32)
    for kv in "kv":
        m[f"peT_{kv}"] = np.ascontiguousarray(np.transpose(inputs[f"nsa_cmp_pe_{kv}"], (0, 2, 1)), dtype=f32)
        m[f"w1_{kv}"] = np.ascontiguousarray(inputs[f"nsa_cmp_w1_{kv}"], dtype=f32)
        m[f"w2_{kv}"] = np.ascontiguousarray(inputs[f"nsa_cmp_w2_{kv}"], dtype=f32)
    for nm in ["constF", "constB", "strips", "strips01", "cmask", "ovl", "eall", "ropeC", "ropeS", "ropeCc", "ropeSc",
               "permF", "tkA", "tkB"]:
        m[nm] = consts[nm]
    return m


def kernel(**inputs):
    inputs = {kk: np.asarray(v) for kk, v in inputs.items()}
    consts = make_consts()
    nc, kb, _ = build()
    in_maps = [host_inputs(inputs, c, consts) for c in range(NCORES)]
    res = run_bass_kernel_spmd(nc, in_maps, core_ids=list(range(NCORES)))
    outs = [np.asarray(r["outT"]) for r in res.results]
    full = np.concatenate(outs, axis=0)
    return np.ascontiguousarray(np.transpose(full, (0, 2, 1))).astype(np.float32)
```

```python
import os
import numpy as np
import ml_dtypes
from contextlib import ExitStack
import concourse.bass as bass
import concourse.mybir as mybir
from concourse.bass_utils import run_bass_kernel_spmd

F32 = mybir.dt.float32
BF16 = mybir.dt.bfloat16
AF = mybir.ActivationFunctionType
ALU = mybir.AluOpType
AX = mybir.AxisListType

T_ = 2048
D_ = 1024
NIN = 3984
NEG = -30000.0
EPS = 1e-6
NCORES = 8
SEQ_PER_CORE = 4

C_HGQ, C_HGF, C_HGI = 0, 256, 512
C_FXQ, C_FXK, C_FXV, C_FXF = 768, 1024, 1280, 1536
C_SBQ, C_SBK, C_SBV = 1540, 1796, 2052
C_NSQ = 2308
C_NKC, C_NVC, C_NKS, C_NVS, C_NKW, C_NVW = 2564, 2628, 2692, 2756, 2820, 2884
C_NSG = 2948
C_GATE = 2960


class Tok:
    __slots__ = ("w", "r", "name")

    def __init__(self, name=""):
        self.w = None
        self.r = {}
        self.name = name


class T:
    def __init__(self, h, name, excl=False):
        self.h = h
        self.tok = Tok(name)
        self.name = name
        self.excl = excl

    def __getitem__(self, k):
        return self.h[k]


class WV:
    def __init__(self, t, base):
        self.t = t
        self.base = base
        self.tok = t.tok

    def __getitem__(self, key):
        p, j, sl = key
        return self.t.h[p, j, self.base + sl.start:self.base + sl.stop]


class KB:
    NDQ = 8
    EPOCH_LIMIT = 30000

    def __init__(self, nc):
        self.nc = nc
        self.stack = [ExitStack()]
        self.E = {"pe": nc.tensor, "act": nc.scalar, "dve": nc.vector,
                  "pool": nc.gpsimd, "sp": nc.sync}
        self.semh = {}
        self.cur = {}
        self.cnt = {}
        self.epoch = {}
        for e in ["pe", "act", "dve", "pool"]:
            self.epoch[e] = -1
            self._new_epoch(e)
        self.dq = {}
        for q in ["sp", "pool", "act"]:
            sems = [self.stack[0].enter_context(nc.semaphore(f"d_{q}{i}")) for i in range(self.NDQ)]
            self.dq[q] = dict(n=0)
            for i, s in enumerate(sems):
                self.semh[("dma", q, i)] = s
        self.seen = {}
        self.nins = {e: 0 for e in self.E}
        self.uid = 0

    def _new_epoch(self, e):
        self.epoch[e] += 1
        key = (e, self.epoch[e])
        self.semh[key] = self.stack[0].enter_context(self.nc.semaphore(f"s_{e}{self.epoch[e]}"))
        self.cur[e] = key
        self.cnt[e] = 0

    def scope(self):
        kb = self

        class _S:
            def __enter__(s):
                kb.stack.append(ExitStack())

            def __exit__(s, *a):
                kb.barrier()
                kb.stack.pop().close()
                return False
        return _S()

    def _nm(self, name):
        self.uid += 1
        return f"{name}_{self.uid}"

    def sb(self, name, shape, dt=F32):
        h = self.stack[-1].enter_context(self.nc.sbuf_tensor(self._nm(name), list(shape), dt))
        return T(h, name)

    def ps(self, name, shape, dt=F32):
        h = self.stack[-1].enter_context(self.nc.psum_tensor(self._nm(name), list(shape), dt))
        return T(h, name, excl=True)

    def dram(self, name, shape, dt, kind):
        h = self.nc.dram_tensor(name, list(shape), dt, kind=kind)
        return T(h, name)

    def _wait(self, eng, deps):
        for key, val in deps.items():
            if key[0] == "pe" and eng == "pe":
                continue
            sk = (eng, key)
            if self.seen.get(sk, 0) >= val:
                continue
            self.seen[sk] = val
            self.E[eng].wait_ge(self.semh[key], val)
            self.nins[eng] += 1

    @staticmethod
    def _tok(x):
        return getattr(x, "tok", x)

    def _deps(self, reads, writes):
        deps = {}
        for t in reads:
            t = self._tok(t)
            if t.w and deps.get(t.w[0], 0) < t.w[1]:
                deps[t.w[0]] = t.w[1]
        for t in writes:
            t = self._tok(t)
            if t.w and deps.get(t.w[0], 0) < t.w[1]:
                deps[t.w[0]] = t.w[1]
            for kk, v in t.r.items():
                if deps.get(kk, 0) < v:
                    deps[kk] = v
        return deps

    def _mark(self, ev, reads, writes):
        kk, v = ev
        for t in reads:
            t = self._tok(t)
            if t.r.get(kk, 0) < v:
                t.r[kk] = v
        for t in writes:
            t = self._tok(t)
            t.w = ev
            t.r = {}

    def op(self, eng, fn, reads=(), writes=()):
        ex = [t for t in reads if isinstance(t, T) and t.excl]
        if ex:
            reads = [t for t in reads if not (isinstance(t, T) and t.excl)]
            writes = list(writes) + ex
        self._wait(eng, self._deps(reads, writes))
        if self.cnt[eng] >= self.EPOCH_LIMIT:
            self._new_epoch(eng)
        ins = fn()
        self.cnt[eng] += 1
        ins.then_inc(self.semh[self.cur[eng]], 1)
        self.nins[eng] += 1
        self._mark((self.cur[eng], self.cnt[eng]), reads, writes)
        return ins

    def pe_selfwait(self):
        key, val = self.cur["pe"], self.cnt["pe"]
        if val > 0 and self.seen.get(("pe", key), 0) < val:
            self.seen[("pe", key)] = val
            self.E["pe"].wait_ge(self.semh[key], val)
            self.nins["pe"] += 1

    def dma(self, q, out, in_, reads=(), writes=(), **kw):
        self._wait(q, self._deps(reads, writes))
        d = self.dq[q]
        slot = d["n"] % self.NDQ
        val = 16 * (d["n"] // self.NDQ + 1)
        d["n"] += 1
        ins = self.E[q].dma_start(out=out, in_=in_, **kw)
        ins.then_inc(self.semh[("dma", q, slot)], 16)
        self.nins[q] += 1
        self._mark((("dma", q, slot), val), reads, writes)
        return ins

    def _all_events(self):
        ev = {}
        for e in ["pe", "act", "dve", "pool"]:
            if self.cnt[e] > 0:
                ev[self.cur[e]] = self.cnt[e]
        for q, d in self.dq.items():
            n = d["n"]
            for slot in range(self.NDQ):
                c = (n - slot + self.NDQ - 1) // self.NDQ
                if c > 0:
                    ev[("dma", q, slot)] = 16 * c
        return ev

    def barrier(self):
        ev = self._all_events()
        for e in ["pe", "act", "dve", "pool", "sp"]:
            self._wait(e, {kk: v for kk, v in ev.items() if not (kk[0] == e)})

    def finish(self, toks, eng="sp"):
        deps = {}
        for t in toks:
            t = self._tok(t)
            if t.w and deps.get(t.w[0], 0) < t.w[1]:
                deps[t.w[0]] = t.w[1]
        self._wait(eng, deps)

    def close(self):
        while self.stack:
            self.stack.pop().close()


def _bf(a):
    return np.asarray(a, dtype=np.float32).astype(ml_dtypes.bfloat16)


def make_consts():
    c = {}
    i = np.arange(128)[:, None]
    j = np.arange(128)[None, :]
    c["identF"] = np.eye(128, dtype=np.float32)
    c["triF"] = (i <= j).astype(np.float32)
    c["onesF"] = np.ones((128, 128), np.float32)
    c["tribdF"] = ((i // 32 == j // 32) & (i <= j)).astype(np.float32)
    c["trisufF"] = ((i // 32 == j // 32) & (i > j)).astype(np.float32)
    c["maskbdF"] = ((i // 32 == j // 32) & (i <= j)).astype(np.float32)
    perm = np.zeros((64, 16), np.float32)
    for a in range(16):
        perm[a + 8 if a < 8 else a - 8, a] = 1.0
    c["permF"] = perm
    cf = np.concatenate([c["identF"], c["triF"], c["onesF"], c["tribdF"], c["trisufF"], c["maskbdF"]], axis=1)
    c["constF"] = cf
    negtri = -(i >= j).astype(np.float32)
    neglow = -(i < j).astype(np.float32)
    c["constB"] = _bf(np.concatenate([np.eye(128), np.ones((128, 128)), negtri, neglow], axis=1))
    jj = np.arange(896)[None, :] - 384
    caus = np.where(jj < 0, NEG, np.where(jj >= 128, 0.0, np.where(jj >= i, 0.0, NEG)))
    strict = np.where(jj < 0, NEG, np.where(jj >= 128, 0.0, np.where(jj > i, 0.0, NEG)))
    win = np.where(jj < 0, 0.0, np.where(jj >= 128, NEG, np.where(jj < i, 0.0, NEG)))
    c["strips"] = _bf(np.stack([caus, strict, win], axis=1))
    c["strips01"] = _bf(np.stack([(caus == 0.0), (win == 0.0)], axis=1).astype(np.float32))
    n = np.arange(128)[:, None]
    t = np.arange(T_)[None, :]
    c["cmask"] = _bf(np.where((16 * n + 31 <= t) & (n < 127), 0.0, NEG))
    cs = (np.arange(128) * 16)[:, None]
    ss = (np.arange(32) * 64)[None, :]
    ov = np.clip(np.minimum(cs + 32, ss + 64) - np.maximum(cs, ss), 0, None).astype(np.float32) / 32.0
    ovl = np.concatenate([np.ones((128, 1), np.float32), ov], axis=1)
    ovl[127] = 0.0
    c["ovl"] = _bf(ovl)
    c["eall"] = _bf((np.arange(T_)[None, :] // 64 == np.arange(32)[:, None]).astype(np.float32))
    half = 8
    inv = (500000.0 ** (-(np.arange(half, dtype=np.float32) * 2.0 / 16.0))).astype(np.float32)

    def tabs(pos):
        ang = pos.astype(np.float32)[None, :] * inv[:, None]
        cos, sin = np.cos(ang), np.sin(ang)
        return (np.concatenate([cos, cos], 0).astype(np.float32),
                np.concatenate([-sin, sin], 0).astype(np.float32))
    C, S = tabs(np.arange(T_))
    c["ropeC"], c["ropeS"] = C, S
    pc = np.arange(128) * 16 + 31
    Cc, Sc = tabs(pc)
    c["ropeCc"], c["ropeSc"] = Cc, Sc
    tt_ = np.arange(T_)
    qblk = tt_ // 64
    blk = np.arange(32)[None, :]
    forced = (blk == 0) | (blk == qblk[:, None]) | (blk == qblk[:, None] - 1)
    valid = blk <= qblk[:, None]
    A = (~forced & valid).astype(np.float32)
    Bc = np.where(forced, 1.0e4, np.where(valid, 0.0, -1.0e4)).astype(np.float32)
    c["tkA"] = A.reshape(16, 128, 32).transpose(1, 0, 2).copy()
    c["tkB"] = Bc.reshape(16, 128, 32).transpose(1, 0, 2).copy()
    return c


def build(nseq=SEQ_PER_CORE, nlayer=2, mixers=("hg", "fx", "sb", "ns", "out"), debug=None):
    nc = bass.Bass("TRN2", target_bir_lowering=False)
    k = KB(nc)
    dbg_outs = {}

    def din(name, shape, dt=F32):
        return k.dram(name, shape, dt, "ExternalInput")

    xT_d = din("xT", [nseq, D_, T_])
    w_in_d = din("w_in", [2, D_, NIN])
    w_out_d = din("w_out", [2, D_, D_])
    normg_d = din("norm_gT", [2, 128, 8])
    fnormg_d = din("fnorm_gT", [128, 8])
    ong_d = din("out_norm_g", [2, D_])
    lbl_d = din("lb_logits", [2, 256])
    lblT_d = din("lb_logitsT", [128, 2, 2])
    fb_d = din("fox_fb", [2, 4])
    peT_d = {kv: din(f"peT_{kv}", [2, 64, 32]) for kv in "kv"}
    w1_d = {kv: din(f"w1_{kv}", [2, 2048, 64]) for kv in "kv"}
    w2_d = {kv: din(f"w2_{kv}", [2, 64, 64]) for kv in "kv"}
    constF_d = din("constF", [128, 768])
    constB_d = din("constB", [128, 512], BF16)
    strips_d = din("strips", [128, 3, 896], BF16)
    strips01_d = din("strips01", [128, 2, 896], BF16)
    cmask_d = din("cmask", [128, T_], BF16)
    ovl_d = din("ovl", [128, 33], BF16)
    eall_d = din("eall", [32, T_], BF16)
    ropeC_d = din("ropeC", [16, T_])
    ropeS_d = din("ropeS", [16, T_])
    ropeCc_d = din("ropeCc", [16, 128])
    ropeSc_d = din("ropeSc", [16, 128])
    permF_d = din("permF", [64, 16])
    tkA_d = din("tkA", [128, 16, 32])
    tkB_d = din("tkB", [128, 16, 32])
    outT_d = k.dram("outT", [nseq, D_, T_], F32, "ExternalOutput")
    xres_d = k.dram("xres", [nseq, D_, T_], F32, "Internal")

    def dbg_out(name, shape, dt=F32):
        d = k.dram(name, shape, dt, "ExternalOutput")
        dbg_outs[name] = d
        return d

    constF = k.sb("constF", [128, 768])
    constB = k.sb("constB", [128, 512], BF16)
    strips = k.sb("strips", [128, 3, 896], BF16)
    k.dma("sp", constF[:], constF_d[:], writes=[constF])
    k.dma("sp", constB[:], constB_d[:], writes=[constB])
    k.dma("sp", strips[:], strips_d[:], writes=[strips])
    identF = constF[:, 0:128]
    triF = constF[:, 128:256]
    onesF = constF[:, 256:384]
    tribdF = constF[:, 384:512]
    trisufF = constF[:, 512:640]
    maskbdF = constF[:, 640:768]
    identB = constB[:, 0:128]
    onesB = constB[:, 128:256]
    negtriB = constB[:, 256:384]
    neglowB = constB[:, 384:512]

    hT = k.sb("hT", [128, 8, T_], BF16)
    ytok = k.sb("ytok", [128, 16, D_], BF16)
    normg = k.sb("normg", [128, 2, 8])
    fnormg = k.sb("fnormg", [128, 8])
    for l in range(2):
        k.dma("sp", normg[:, l, :], normg_d[l], writes=[normg])
    k.dma("sp", fnormg[:], fnormg_d[:], writes=[fnormg])

    PS = [k.ps(f"ps{i}", [128, 512]) for i in range(7)]
    PST = k.ps("pst", [128, 1024], BF16)
    ps_rr = {"s": [0, 1], "a": [2, 3], "o": [4, 5], "m": [6]}
    ps_ctr = {kk: 0 for kk in ps_rr}

    def psb(role):
        lst = ps_rr[role]
        i = lst[ps_ctr[role] % len(lst)]
        ps_ctr[role] += 1
        return PS[i]

    def mm(out, lhsT, rhs, start, stop, reads, writes, tp=None, ser=False):
        kw = {}
        if ser:
            k.pe_selfwait()
        if tp is not None:
            kw["tile_position"] = tp
        return k.op("pe", lambda: nc.tensor.matmul(out, lhsT=lhsT, rhs=rhs, start=start, stop=stop, **kw),
                    reads=reads, writes=writes)

    def act(out, in_, func, reads, writes, bias=None, scale=None):
        kw = {}
        if bias is not None:
            kw["bias"] = bias
        if scale is not None:
            kw["scale"] = scale
        return k.op("act", lambda: nc.scalar.activation(out=out, in_=in_, func=func, **kw),
                    reads=reads, writes=writes)

    def tt(eng, out, in0, in1, op, reads, writes):
        e = k.E[eng]
        return k.op(eng, lambda: e.tensor_tensor(out=out, in0=in0, in1=in1, op=op), reads=reads, writes=writes)

    def ts(eng, out, in0, s1, s2, op0, op1, reads, writes):
        e = k.E[eng]
        if s2 is None:
            return k.op(eng, lambda: e.tensor_scalar(out=out, in0=in0, scalar1=s1, scalar2=None, op0=op0),
                        reads=reads, writes=writes)
        return k.op(eng, lambda: e.tensor_scalar(out=out, in0=in0, scalar1=s1, scalar2=s2, op0=op0, op1=op1),
                    reads=reads, writes=writes)

    def stt(eng, out, in0, scalar, in1, op0, op1, reads, writes):
        e = k.E[eng]
        return k.op(eng, lambda: e.scalar_tensor_tensor(out=out, in0=in0, scalar=scalar, in1=in1, op0=op0, op1=op1),
                    reads=reads, writes=writes)

    def cp(eng, out, in_, reads, writes):
        if eng == "act":
            return k.op("act", lambda: nc.scalar.copy(out=out, in_=in_), reads=reads, writes=writes)
        e = k.E[eng]
        return k.op(eng, lambda: e.tensor_copy(out=out, in_=in_), reads=reads, writes=writes)

    def memset(eng, out, val, writes):
        e = k.E[eng]
        return k.op(eng, lambda: e.memset(out, val), writes=writes)

    def recip(out, in_, reads, writes):
        return k.op("dve", lambda: nc.vector.reciprocal(out=out, in_=in_), reads=reads, writes=writes)

    wslots = [k.sb("wslot0", [128, 8, 1024], BF16), k.sb("wslot1", [128, 8, 1024], BF16)]
    wcols = {"hg": [(C_HGF, 512), (C_HGQ, 512)], "fx": [(C_FXQ, 256), (C_FXK, 256), (C_FXV, 260)],
             "sb": [(C_SBQ, 256), (C_SBK, 256), (C_SBV, 256)], "ns": [(C_NSQ, 652)], "out": [(C_GATE, 1024)]}
    wplan = [(s_, l_, ph) for s_ in range(nseq) for l_ in range(nlayer)
             for ph in ("hg", "fx", "sb", "ns", "out") if ph in mixers]
    wissued = set()

    def w_issue(idx):
        if idx >= len(wplan) or idx in wissued:
            return
        wissued.add(idx)
        _, l_, ph = wplan[idx]
        slot = wslots[idx % 2]
        base = 0
        for (c0, n) in wcols[ph]:
            src = w_in_d[l_][:, c0:c0 + n].rearrange("(j p) c -> p j c", p=128)
            k.dma("pool", slot[:, 0:4, base:base + n], src[:, 0:4, :], writes=[slot])
            k.dma("pool", slot[:, 4:8, base:base + n], src[:, 4:8, :], writes=[slot])
            base += n

    def get_w(s_, l_, ph):
        idx = wplan.index((s_, l_, ph))
        w_issue(idx)
        views, base = [], 0
        for (c0, n) in wcols[ph]:
            views.append(WV(wslots[idx % 2], base))
            base += n
        w_issue(idx + 1)
        return views

    def load_w(l, c0, ncols, name="wblk"):
        w = k.sb(name, [128, 8, ncols], BF16)
        src = w_in_d[l][:, c0:c0 + ncols].rearrange("(j p) c -> p j c", p=128)
        half = 4
        k.dma("pool", w[:, 0:half, :], src[:, 0:half, :], writes=[w])
        k.dma("pool", w[:, half:8, :], src[:, half:8, :], writes=[w])
        return w

    def proj_fm(w, off, M, tb, ps, extra_reads=()):
        for j in range(8):
            mm(ps[0:M, :], w[:, j, off:off + M], hT[:, j, tb * 512:(tb + 1) * 512], j == 0, j == 7,
               reads=[w, hT, *extra_reads], writes=[ps])

    def proj_tm(w, off, N, tB, ps):
        for j in range(8):
            mm(ps[:, 0:N], hT[:, j, tB * 128:(tB + 1) * 128], w[:, j, off:off + N], j == 0, j == 7,
               reads=[w, hT], writes=[ps])

    def finalize(o, o_tok, out_ap, tmp, n=4):
        sq, ss, sd = tmp
        tt("dve", sq[:, 0:n, :], o, o, ALU.mult, reads=[o_tok], writes=[sq])
        k.op("dve", lambda: nc.vector.tensor_reduce(out=ss[:, 0:n], in_=sq[:, 0:n, :], axis=AX.X, op=ALU.add),
             reads=[sq], writes=[ss])
        act(sd[:, 0:n], ss[:, 0:n], AF.Ln, reads=[ss], writes=[sd], bias=EPS, scale=1.0 / 64.0)
        act(sd[:, 0:n], sd[:, 0:n], AF.Exp, reads=[sd], writes=[sd], scale=-0.5)
        tt("dve", out_ap, o, sd[:, 0:n].unsqueeze(2).to_broadcast([128, n, 64]), ALU.mult,
           reads=[o_tok, sd], writes=[ytok])

    def fin_tmp(n=4):
        return (k.sb("sq", [128, n, 64]), k.sb("ss", [128, n]), k.sb("sd", [128, n]))


    def run_streams(gens):
        gens = list(gens)
        while gens:
            for g in list(gens):
                try:
                    next(g)
                except StopIteration:
                    gens.remove(g)

    def attn_stream(jobs, sbank, obanks, pTs, K_ap, Q_ap, kq_toks, masks, bias_ap, bias_toks, V_ap, v_toks,
                    kb_range, qs_range, ncol, fin, post_mask=None):
        if not isinstance(obanks, (list, tuple)):
            obanks = [obanks]
        tiles = []
        for ji, (h, qb) in enumerate(jobs):
            kbs = list(kb_range(qb))
            for kb in kbs:
                tiles.append((h, qb, kb, kb == kbs[0], kb == kbs[-1], ji))
        nsb = len(sbank)
        L = max(nsb - 1, 1)

        def emit_qk(i):
            h, qb, kb = tiles[i][0:3]
            ps = sbank[i % nsb]
            ml = masks(h, qb, kb)
            mm(ps[:], K_ap(h, kb), Q_ap(h, qb), True, len(ml) == 0, reads=kq_toks, writes=[ps])
            for mi, (lt, rh, rd) in enumerate(ml):
                mm(ps[:], lt, rh, False, mi == len(ml) - 1, reads=rd, writes=[ps])

        if nsb > 1:
            for i in range(min(L, len(tiles))):
                emit_qk(i)
        else:
            emit_qk(0)
        first = True
        for i, (h, qb, kb, isfirst, islast, ji) in enumerate(tiles):
            if nsb > 1 and i + L < len(tiles):
                emit_qk(i + L)
            yield
            obank = obanks[ji % len(obanks)]
            accv = obank[:, 0:4 * ncol].rearrange("p (q d) -> p q d", q=4)
            ps = sbank[i % nsb]
            p = pTs[i % len(pTs)]
            if isfirst:
                first = True
            b = bias_ap(h, kb) if bias_ap is not None else None
            act(p[:], ps[:], AF.Exp, reads=[ps, *bias_toks], writes=[p], bias=b)
            pm = post_mask(h, qb, kb) if post_mask is not None else None
            if pm is not None:
                tt("dve", p[:], p[:], pm[0], ALU.mult, reads=[p, *pm[1]], writes=[p])
            for qs in range(4):
                lo, hi = qs_range(qb, qs)
                if kb < lo or kb > hi:
                    continue
                mm(accv[:, qs, :], p[:, qs * 128:(qs + 1) * 128], V_ap(h, kb), first, kb == hi,
                   reads=[p, *v_toks], writes=[obank])
                first = False
            if islast:
                fin(h, qb, obank, accv)
            if nsb == 1 and i + 1 < len(tiles):
                emit_qk(i + 1)
            yield

    def phase_norm(s, l):
        xsrc = xT_d if l == 0 else xres_d
        with k.scope():
            xin = [k.sb(f"xin{i}", [128, 8, 512]) for i in range(2)]
            sq = [k.sb(f"sqn{i}", [128, 8, 512], BF16) for i in range(2)]
            rs = [k.sb(f"rs{i}", [128, 512]) for i in range(2)]
            for tb in range(4):
                xi, sqi, rsi = xin[tb % 2], sq[tb % 2], rs[tb % 2]
                src = xsrc[s].rearrange("(j p) t -> p j t", p=128)[:, :, tb * 512:(tb + 1) * 512]
                k.dma("sp", xi[:], src, reads=[xsrc], writes=[xi])
                act(sqi[:], xi[:], AF.Square, reads=[xi], writes=[sqi])
                ps = psb("m")
                for j in range(8):
                    mm(ps[:], onesB, sqi[:, j, :], j == 0, j == 7, reads=[constB, sqi], writes=[ps])
                act(rsi[:], ps[:], AF.Ln, reads=[ps], writes=[rsi], bias=EPS, scale=1.0 / D_)
                act(rsi[:], rsi[:], AF.Exp, reads=[rsi], writes=[rsi], scale=-0.5)
                for j in range(8):
                    stt("dve", hT[:, j, tb * 512:(tb + 1) * 512], xi[:, j, :], normg[:, l, j:j + 1], rsi[:],
                        ALU.mult, ALU.mult, reads=[xi, rsi, normg], writes=[hT])

    def phase_fox(s, l):
        with k.scope():
            QT = k.sb("fxQT", [128, 4, T_], BF16)
            KT = k.sb("fxKT", [128, 4, T_], BF16)
            V = k.sb("fxV", [128, 16, 4, 65], BF16)
            ltok = k.sb("fxl", [128, 16, 4])
            negcum = k.sb("fxnc", [128, 16, 4])
            fbb = k.sb("fbb", [128, 4])
            k.dma("sp", fbb[:], fb_d[l:l + 1, :].to_broadcast([128, 4]), writes=[fbb])
            memset("pool", V[:, :, :, 64:65], 1.0, writes=[V])
            memset("pool", KT[64:67, :, :], 1.0, writes=[KT])
            wq_, wk_, wv_ = get_w(s, l, "fx")
            for (w, dst, scale) in ((wq_, QT, 0.125), (wk_, KT, None)):
                for pr in range(2):
                    for tb in range(4):
                        ps = psb("m")
                        proj_fm(w, pr * 128, 128, tb, ps)
                        sl = slice(tb * 512, (tb + 1) * 512)
                        for hh in range(2):
                            if scale is None:
                                cp("act", dst[0:64, 2 * pr + hh, sl], ps[64 * hh:64 * hh + 64, :], reads=[ps], writes=[dst])
                            else:
                                k.op("act", lambda: nc.scalar.mul(out=dst[0:64, 2 * pr + hh, sl],
                                                                  in_=ps[64 * hh:64 * hh + 64, :], mul=scale),
                                     reads=[ps], writes=[dst])
            if os.environ.get('FX_STOP') == '1':
                return
            w = wv_
            _skip = os.environ.get("FX_SKIP", "")
            for tB in range(int(os.environ.get("FX_NTB", "16"))):
                ps = psb("m")
                if "mm" not in _skip:
                    proj_tm(w, 0, 260, tB, ps)
                if "cp" not in _skip:
                    cp("act", V[:, tB, :, 0:64], ps[:, 0:256].rearrange("p (h d) -> p h d", h=4), reads=[ps], writes=[V])
                if "tt" not in _skip:
                    tt("dve", ltok[:, tB, :], ps[:, 256:260], fbb[:], ALU.add, reads=[ps, fbb], writes=[ltok])
            if os.environ.get('FX_STOP') == '2':
                return
            act(ltok[:], ltok[:], AF.Exp, reads=[ltok], writes=[ltok], scale=-1.0)
            act(ltok[:], ltok[:], AF.Ln, reads=[ltok], writes=[ltok], bias=1.0)
            if os.environ.get('FX_STOP') == '3':
                return
            ps = psb("m")
            lflat = ltok[:].rearrange("p b h -> p (b h)")
            mm(ps[:, 0:64], triF, lflat, True, True, reads=[constF, ltok], writes=[ps])
            mm(ps[:, 64:128], onesF, lflat, True, True, reads=[constF, ltok], writes=[ps])
            tot = k.sb("fxtot", [128, 16, 4])
            pre = k.sb("fxpre", [128, 16, 4])
            cp("dve", tot[:], ps[:, 64:128].rearrange("p (b h) -> p b h", h=4), reads=[ps], writes=[tot])
            memset("dve", pre[:, 0, :], 0.0, writes=[pre])
            for B in range(1, 16):
                tt("dve", pre[:, B, :], pre[:, B - 1, :], tot[:, B - 1, :], ALU.add, reads=[pre, tot], writes=[pre])
            tt("dve", negcum[:], ps[:, 0:64].rearrange("p (b h) -> p b h", h=4), pre[:], ALU.add,
               reads=[ps, pre], writes=[negcum])
            if os.environ.get('FX_STOP') == '4':
                return
            cumf = k.sb("cumf", [4, T_])
            c1f = k.sb("c1f", [4, T_])
            cb = [k.sb(f"cb{i}", [4, T_], BF16) for i in range(3)]
            for g in range(4):
                ps = psb("m")
                for bb in range(4):
                    B = 4 * g + bb
                    mm(ps[0:4, 128 * bb:128 * bb + 128], negcum[:, B, :], identF, True, True,
                       reads=[negcum, constF], writes=[ps])
                k.op("act", lambda: nc.scalar.mul(out=cumf[:, 512 * g:512 * g + 512], in_=ps[0:4, :], mul=-1.0),
                     reads=[ps], writes=[cumf])
            cp("dve", cb[0][:], cumf[:], reads=[cumf], writes=[cb[0]])
            cp("dve", c1f[:], cb[0][:], reads=[cb[0]], writes=[c1f])
            tt("dve", cumf[:], cumf[:], c1f[:], ALU.subtract, reads=[cumf, c1f], writes=[cumf])
            cp("dve", cb[1][:], cumf[:], reads=[cumf], writes=[cb[1]])
            cp("dve", c1f[:], cb[1][:], reads=[cb[1]], writes=[c1f])
            tt("dve", cumf[:], cumf[:], c1f[:], ALU.subtract, reads=[cumf, c1f], writes=[cumf])
            cp("dve", cb[2][:], cumf[:], reads=[cumf], writes=[cb[2]])
            for i in range(3):
                k.dma("sp", QT[64 + i:65 + i, :, :], cb[i][:], reads=[cb[i]], writes=[QT])
            if os.environ.get('FX_STOP') == '5':
                return
            def mk_stream(si, heads):
                pTs = [k.sb(f"fxpT{si}_{i}", [128, 512], BF16) for i in range(4)]
                o_sbs = [k.sb(f"fxo{si}_{i}", [128, 4, 64]) for i in range(2)]
                rds = [k.sb(f"fxrd{si}_{i}", [128, 4]) for i in range(2)]
                tmps = [fin_tmp() for i in range(2)]
                fc = [0]

                def masks(h, qb, kb):
                    if kb >= 4 * qb:
                        r = kb - 4 * qb
                        return [(identB, strips[:, 0, 384 - 128 * r:384 - 128 * r + 512], [constB, strips])]
                    return []

                def fin(h, qb, acc, accv):
                    o_sb, rd, tmp = o_sbs[fc[0] % 2], rds[fc[0] % 2], tmps[fc[0] % 2]
                    fc[0] += 1
                    recip(rd[:], accv[:, :, 64], reads=[acc], writes=[rd])
                    tt("dve", o_sb[:], accv[:, :, 0:64], rd[:].unsqueeze(2).to_broadcast([128, 4, 64]), ALU.mult,
                       reads=[acc, rd], writes=[o_sb])
                    finalize(o_sb[:], o_sb, ytok[:, 4 * qb:4 * qb + 4, 256 + 64 * h:256 + 64 * h + 64], tmp)

                return attn_stream(
                    [(h, qb) for h in heads for qb in range(4)],
                    [PS[0], PS[1], PS[2], PS[3]], [PS[4], PS[5]], pTs,
                    lambda h, kb: KT[0:67, h, kb * 128:(kb + 1) * 128],
                    lambda h, qb: QT[0:67, h, qb * 512:(qb + 1) * 512], [KT, QT],
                    masks, lambda h, kb: negcum[:, kb, h:h + 1], [negcum],
                    lambda h, kb: V[:, kb, h, :], [V],
                    lambda qb: range(0, 4 * qb + 4), lambda qb, qs: (0, 4 * qb + qs), 65, fin)

            run_streams([mk_stream(0, (0, 1, 2, 3))])


    def phase_sb(s, l):
        with k.scope():
            QT = k.sb("sbQT", [64, 4, T_], BF16)
            KT = k.sb("sbKT", [64, 4, T_], BF16)
            V = k.sb("sbV", [128, 16, 256], BF16)
            wq_, wk_, wv_ = get_w(s, l, "sb")
            for (w, dst, scale) in ((wq_, QT, 0.125), (wk_, KT, None)):
                for pr in range(2):
                    for tb in range(4):
                        ps = psb("m")
                        proj_fm(w, pr * 128, 128, tb, ps)
                        sl = slice(tb * 512, (tb + 1) * 512)
                        for hh in range(2):
                            if scale is None:
                                cp("act", dst[0:64, 2 * pr + hh, sl], ps[64 * hh:64 * hh + 64, :], reads=[ps], writes=[dst])
                            else:
                                k.op("act", lambda: nc.scalar.mul(out=dst[0:64, 2 * pr + hh, sl],
                                                                  in_=ps[64 * hh:64 * hh + 64, :], mul=scale),
                                     reads=[ps], writes=[dst])
            w = wv_
            for tB in range(16):
                ps = psb("m")
                proj_tm(w, 0, 256, tB, ps)
                cp("act", V[:, tB, :], ps[:, 0:256], reads=[ps], writes=[V])
            def sb_stream(si, heads):
                es = [k.sb(f"sbe{si}_{i}", [128, 512]) for i in range(2)]
                sps = [k.sb(f"sbsp{si}_{i}", [128, 512], BF16) for i in range(2)]
                er_ = k.sb(f"sber{si}", [128, 512])
                as_ = [k.sb(f"sbaT{si}_{i}", [128, 512], BF16) for i in range(2)]
                o = k.sb(f"sbo{si}", [128, 4, 64])
                tmp = fin_tmp()
                zb = [PS[3 * si], PS[3 * si + 1]]
                psR = PS[3 * si + 2]
                acc = PS[6]
                accv = acc[:, 256 * si:256 * si + 256].rearrange("p (q d) -> p q d", q=4)
                tiles = []
                for h in heads:
                    for qb in range(4):
                        nkb = 4 * qb + 4
                        for idx, kb in enumerate(reversed(range(nkb))):
                            tiles.append((h, qb, kb, idx, nkb))
                n = len(tiles)

                def stage1(i):
                    h, qb, kb, idx, nkb = tiles[i]
                    ps = zb[i % 2]
                    diag = kb >= 4 * qb
                    mm(ps[:], KT[0:64, h, kb * 128:(kb + 1) * 128], QT[0:64, h, qb * 512:(qb + 1) * 512],
                       True, not diag, reads=[KT, QT], writes=[ps])
                    if diag:
                        r = kb - 4 * qb
                        mm(ps[:], identB, strips[:, 1, 384 - 128 * r:384 - 128 * r + 512], False, True,
                           reads=[constB, strips], writes=[ps])

                def stage2(i):
                    ps, e, sp_ = zb[i % 2], es[i % 2], sps[i % 2]
                    act(e[:], ps[:], AF.Exp, reads=[ps], writes=[e])
                    act(sp_[:], e[:], AF.Ln, reads=[e], writes=[sp_], bias=1.0)

                stage1(0)
                stage2(0)
                for i, (h, qb, kb, idx, nkb) in enumerate(tiles):
                    e, sp_, a_ = es[i % 2], sps[i % 2], as_[i % 2]
                    if i + 1 < n:
                        stage1(i + 1)
                    mm(psR[:], negtriB, sp_[:], idx == 0, False, reads=[constB, sp_], writes=[psR])
                    if idx == 0:
                        memset("dve", accv, 0.0, writes=[acc])
                    yield
                    act(er_[:], psR[:], AF.Exp, reads=[psR], writes=[er_])
                    if i + 1 < n:
                        stage2(i + 1)
                    tt("dve", a_[:], e[:], er_[:], ALU.mult, reads=[e, er_], writes=[a_])
                    mm(psR[:], neglowB, sp_[:], False, idx == nkb - 1, reads=[constB, sp_], writes=[psR])
                    yield
                    for qs in range(4):
                        if kb > 4 * qb + qs:
                            continue
                        mm(accv[:, qs, :], a_[:, qs * 128:(qs + 1) * 128], V[:, kb, 64 * h:64 * h + 64],
                           False, kb == 0, reads=[a_, V], writes=[acc])
                    if idx == nkb - 1:
                        cp("dve", o[:], accv, reads=[acc], writes=[o])
                        finalize(o[:], o, ytok[:, 4 * qb:4 * qb + 4, 512 + 64 * h:512 + 64 * h + 64], tmp)
                    yield

            run_streams([sb_stream(0, (0, 1)), sb_stream(1, (2, 3))])


    def phase_hgrn(s, l):
        with k.scope():
            omlb = k.sb("omlb", [128, 256])
            omlT = k.sb("omlT", [128, 2])
            if l == 0:
                memset("pool", omlb[:], 1.0, writes=[omlb])
                memset("pool", omlT[:], 1.0, writes=[omlT])
            else:
                with k.scope():
                    lbb = k.sb("lbb", [128, 2, 256])
                    lblT = k.sb("lblT", [128, 2, 2])
                    k.dma("sp", lbb[:], lbl_d[:].unsqueeze(0).to_broadcast([128, 2, 256]), writes=[lbb])
                    k.dma("sp", lblT[:], lblT_d[:], writes=[lblT])
                    tt("dve", omlb[:], lbb[:, 0, :], lbb[:, 1, :], ALU.subtract, reads=[lbb], writes=[omlb])
                    act(omlb[:], omlb[:], AF.Sigmoid, reads=[omlb], writes=[omlb])
                    tt("dve", omlT[:], lblT[:, :, 0], lblT[:, :, 1], ALU.subtract, reads=[lblT], writes=[omlT])
                    act(omlT[:], omlT[:], AF.Sigmoid, reads=[omlT], writes=[omlT])
            big1 = k.sb("hgbig1", [128, 4096])
            gtok = k.sb("hggtok", [128, 16, 256])
            vtok = k.sb("hgvtok", [128, 16, 256], BF16)
            khat = k.sb("hgkhat", [128, 16, 256], BF16)
            ktok = big1[:].rearrange("p (b c) -> p b c", c=256)
            w, w2 = get_w(s, l, "hg")
            for tB in range(16):
                ps = psb("m")
                proj_tm(w, 0, 512, tB, ps)
                act(ktok[:, tB, :], ps[:, 0:256], AF.Sigmoid, reads=[ps], writes=[big1], scale=-1.0)
                cp("dve", vtok[:, tB, :], ps[:, 256:512], reads=[ps], writes=[vtok])
            for tB in range(16):
                tt("dve", ktok[:, tB, :], ktok[:, tB, :], omlb[:], ALU.mult, reads=[big1, omlb], writes=[big1])
            act(gtok[:], ktok, AF.Ln, reads=[big1], writes=[gtok], bias=1.0, scale=-1.0)
            ebs = [k.sb(f"hgebs{i}", [128, 256]) for i in range(1)]
            for tB in range(16):
                ps = psb("m")
                mm(ps[:, 0:256], trisufF, gtok[:, tB, :], True, True, reads=[constF, gtok], writes=[ps])
                eb = ebs[0]
                act(eb[:], ps[:, 0:256], AF.Exp, reads=[ps], writes=[eb])
                tt("dve", khat[:, tB, :], ktok[:, tB, :], eb[:], ALU.mult, reads=[big1, eb], writes=[khat])
            if os.environ.get('HG_STOP') == '1':
                return
            qsT = k.sb("hgqsT", [128, 2, T_])
            kT = big1[:].rearrange("p (a t) -> p a t", a=2)
            for pr in range(2):
                for tb in range(4):
                    sl = slice(tb * 512, (tb + 1) * 512)
                    ps = psb("m")
                    proj_fm(w2, pr * 128, 128, tb, ps)
                    act(qsT[:, pr, sl], ps[:], AF.Silu, reads=[ps], writes=[qsT])
                    ps = psb("m")
                    proj_fm(w2, 256 + pr * 128, 128, tb, ps, extra_reads=[khat])
                    act(kT[:, pr, sl], ps[:], AF.Sigmoid, reads=[ps, khat], writes=[big1], scale=-1.0)
                    ts("dve", kT[:, pr, sl], kT[:, pr, sl], omlT[:, pr:pr + 1], None, ALU.mult, None,
                       reads=[big1, omlT], writes=[big1])
            qtT = k.sb("hgqtT", [128, 2, T_], BF16)
            ktT = k.sb("hgktT", [128, 2, T_], BF16)
            dl = k.sb("hgdl", [128, 2, 64])
            e1s = [k.sb(f"hge1{i}", [128, 512]) for i in range(1)]
            e2s = [k.sb(f"hge2{i}", [128, 512]) for i in range(1)]
            it = 0
            for pr in range(2):
                for g4 in range(4):
                    sl = slice(g4 * 512, (g4 + 1) * 512)
                    ps = psb("a")
                    for bb in range(4):
                        tB = 4 * g4 + bb
                        mm(ps[:, 128 * bb:128 * bb + 128], gtok[:, tB, pr * 128:(pr + 1) * 128], tribdF, True, True,
                           reads=[gtok, constF], writes=[ps])
                    e1, e2 = e1s[0], e2s[0]
                    it += 1
                    act(e1[:], ps[:], AF.Exp, reads=[ps], writes=[e1])
                    act(e2[:], ps[:], AF.Exp, reads=[ps], writes=[e2], scale=-1.0)
                    tt("dve", qtT[:, pr, sl], qsT[:, pr, sl], e1[:], ALU.mult, reads=[qsT, e1], writes=[qtT])
                    tt("dve", ktT[:, pr, sl], kT[:, pr, sl], e2[:], ALU.mult, reads=[big1, e2], writes=[ktT])
                    cp("pool", dl[:, pr, 16 * g4:16 * g4 + 16], e1[:, 31:512:32], reads=[e1], writes=[dl])
            if os.environ.get('HG_STOP') == '2':
                return
            Srun = [k.sb(f"hgS{i}", [128, 5, 2, 64]) for i in range(2)]
            Sbf = [k.sb(f"hgSb{i}", [128, 4, 2, 64], BF16) for i in range(2)]
            Abd = [k.sb(f"hgA{i}", [128, 4, 128], BF16) for i in range(2)]
            vbd = [k.sb(f"hgvbd{i}", [128, 4, 4, 64], BF16) for i in range(2)]
            o_sb = [k.sb(f"hgo{i}", [128, 4, 64]) for i in range(1)]
            tmp = fin_tmp()
            memset("pool", Srun[1][:, 4, :, :], 0.0, writes=[Srun[1]])
            def hg_state(B):
                cur, prev = Srun[B % 2], Srun[(B + 1) % 2]
                vb = vbd[B % 2]
                tt("dve", vb[:], vtok[:, B, :].rearrange("p (h d) -> p h d", h=4).unsqueeze(2).to_broadcast([128, 4, 4, 64]),
                   maskbdF[:, 31:128:32].unsqueeze(1).unsqueeze(3).to_broadcast([128, 4, 4, 64]), ALU.mult,
                   reads=[vtok, constF], writes=[vb])
                psD = psb("a")
                for h in range(4):
                    pr, r0 = h // 2, 64 * (h % 2)
                    mm(psD[r0:r0 + 64, pr * 256:pr * 256 + 256], khat[:, B, 64 * h:64 * h + 64],
                       vb[:, h, :, :].rearrange("p c d -> p (c d)"), True, True,
                       reads=[khat, vb], writes=[psD], tp=(0, r0), ser=True)
                cp("pool", cur[:, 0, :, :], prev[:, 4, :, :], reads=[prev], writes=[cur])
                for c in range(4):
                    for pr in range(2):
                        col = (pr * 4 + c) * 64
                        stt("dve", cur[:, c + 1, pr, :], cur[:, c, pr, :], dl[:, pr, 4 * B + c:4 * B + c + 1],
                            psD[:, col:col + 64], ALU.mult, ALU.add, reads=[cur, dl, psD], writes=[cur])
                Sb = Sbf[B % 2]
                cp("act", Sb[:], cur[:, 0:4, :, :], reads=[cur], writes=[Sb])

            def hg_output(B):
                bsl = slice(B * 128, (B + 1) * 128)
                Sb = Sbf[B % 2]
                psA = psb("s")
                for h in (0, 2, 1, 3):
                    pr, r0 = h // 2, 64 * (h % 2)
                    mm(psA[:, 128 * h:128 * h + 128], ktT[r0:r0 + 64, pr, bsl], qtT[r0:r0 + 64, pr, bsl], True, True,
                       reads=[ktT, qtT], writes=[psA], ser=(h in (0, 1)))
                A = Abd[B % 2]
                tt("dve", A[:], psA[:].rearrange("p (h t) -> p h t", h=4),
                   maskbdF.unsqueeze(1).to_broadcast([128, 4, 128]), ALU.mult, reads=[psA, constF], writes=[A])
                psO = psb("o")
                for h in range(4):
                    mm(psO[:, 64 * h:64 * h + 64], A[:, h, :], vtok[:, B, 64 * h:64 * h + 64], h == 0, False,
                       reads=[A, vtok], writes=[psO], ser=(h == 0))
                for h in range(4):
                    pr, r0 = h // 2, 64 * (h % 2)
                    for c in range(4):
                        mm(psO[32 * c:32 * c + 32, 64 * h:64 * h + 64],
                           qtT[r0:r0 + 64, pr, B * 128 + 32 * c:B * 128 + 32 * c + 32], Sb[r0:r0 + 64, c, pr, :],
                           False, h == 3 and c == 3, reads=[qtT, Sb], writes=[psO], tp=(r0, 32 * c), ser=True)
                o = o_sb[0]
                cp("dve", o[:], psO[:, 0:256].rearrange("p (h d) -> p h d", h=4), reads=[psO], writes=[o])
                finalize(o[:], o, ytok[:, B, 0:256].rearrange("p (h d) -> p h d", h=4), tmp)

            hg_state(0)
            for B in range(16):
                if B + 1 < 16:
                    hg_state(B + 1)
                hg_output(B)

    def phase_nsa(s, l):
        with k.scope():
            ovl = k.sb("ovl", [128, 33], BF16)
            for dst, src in ((ovl, ovl_d),):
                k.dma("sp", dst[:], src[:], writes=[dst])
            qT = k.sb("nsqT", [96, 4, T_], BF16)
            ksT = k.sb("nsksT", [96, T_], BF16)
            k.dma("sp", ksT[64:96, :], eall_d[:], writes=[ksT])
            kwT = k.sb("nskwT", [64, T_], BF16)
            kcT = k.sb("nskcT", [64, T_], BF16)
            vcT = k.sb("nsvcT", [64, T_], BF16)
            Vs = k.sb("nsVs", [128, 16, 65], BF16)
            Vw = k.sb("nsVw", [128, 16, 65], BF16)
            gtok = k.sb("nsg", [128, 16, 12])
            memset("pool", Vs[:, :, 64:65], 1.0, writes=[Vs])
            memset("pool", Vw[:, :, 64:65], 1.0, writes=[Vw])
            kcmpT = k.sb("nskcmpT", [64, 128], BF16)
            rhsc = k.sb("nsrhsc", [128, 97], BF16)
            with k.scope():
                ropeC = k.sb("ropeC", [16, T_])
                ropeS = k.sb("ropeS", [16, T_])
                ropeCc = k.sb("ropeCc", [16, 128])
                ropeSc = k.sb("ropeSc", [16, 128])
                permF = k.sb("permF", [64, 16])
                for dst, src in ((ropeC, ropeC_d), (ropeS, ropeS_d), (ropeCc, ropeCc_d), (ropeSc, ropeSc_d),
                                 (permF, permF_d)):
                    k.dma("sp", dst[:], src[:], writes=[dst])
                q32s = [k.sb(f"nsq32{i}", [64, 512]) for i in range(2)]
                t1s = [k.sb(f"nst1{i}", [16, 512]) for i in range(2)]
                t2s = [k.sb(f"nst2{i}", [16, 512]) for i in range(2)]
                rc = [0]

                def rope_evac(src, dst, n, scale, Ct, St, extra_tok):
                    i = rc[0] % 2
                    rc[0] += 1
                    q32, t1, t2 = q32s[i], t1s[i], t2s[i]
                    k.op("act", lambda: nc.scalar.mul(out=q32[:, 0:n], in_=src, mul=scale),
                         reads=[extra_tok["src"]], writes=[q32])
                    cp("act", dst, q32[:, 0:n], reads=[q32], writes=[extra_tok["dst"]])
                    psw = psb("a")
                    mm(psw[0:16, 0:n], permF[:, :], q32[:, 0:n], True, True, reads=[permF, q32], writes=[psw])
                    tt("dve", t1[:, 0:n], q32[0:16, 0:n], Ct, ALU.mult, reads=[q32, extra_tok["tab"]], writes=[t1])
                    tt("dve", t2[:, 0:n], psw[0:16, 0:n], St, ALU.mult, reads=[psw, extra_tok["tab"]], writes=[t2])
                    tt("dve", extra_tok["dst16"], t1[:, 0:n], t2[:, 0:n], ALU.add,
                       reads=[t1, t2], writes=[extra_tok["dst"]])

                (w,) = get_w(s, l, "ns")
                cmpW = {}
                for kv in "kv":
                    W1 = k.sb(f"nsW1{kv}", [64, 32, 64], BF16)
                    W2 = k.sb(f"nsW2{kv}", [64, 64], BF16)
                    peT = k.sb(f"nspeT{kv}", [64, 32], BF16)
                    k.dma("pool", W1[:], w1_d[kv][l].rearrange("(lp d) j -> d lp j", d=64), writes=[W1])
                    k.dma("pool", W2[:], w2_d[kv][l], writes=[W2])
                    k.dma("pool", peT[:], peT_d[kv][l], writes=[peT])
                    cmpW[kv] = (W1, W2, peT)
                for pr in range(2):
                    for tb in range(4):
                        sl = slice(tb * 512, (tb + 1) * 512)
                        ps = psb("m")
                        proj_fm(w, pr * 128, 128, tb, ps)
                        for hh in range(2):
                            h = 2 * pr + hh
                            rope_evac(ps[64 * hh:64 * hh + 64, :], qT[0:64, h, sl], 512, 0.125, ropeC[:, sl], ropeS[:, sl],
                                      dict(src=ps, dst=qT, tab=ropeC, dst16=qT[0:16, h, sl]))
                for tb in range(4):
                    sl = slice(tb * 512, (tb + 1) * 512)
                    ps = psb("m")
                    proj_fm(w, 256, 128, tb, ps)
                    cp("act", kcT[:, sl], ps[0:64, :], reads=[ps], writes=[kcT])
                    cp("act", vcT[:, sl], ps[64:128, :], reads=[ps], writes=[vcT])
                for off, dst in ((384, ksT), (512, kwT)):
                    for tb in range(4):
                        sl = slice(tb * 512, (tb + 1) * 512)
                        ps = psb("m")
                        proj_fm(w, off, 64, tb, ps)
                        rope_evac(ps[0:64, :], dst[0:64, sl], 512, 1.0, ropeC[:, sl], ropeS[:, sl],
                                  dict(src=ps, dst=dst, tab=ropeC, dst16=dst[0:16, sl]))
                for tB in range(16):
                    ps = psb("m")
                    proj_tm(w, 448, 204, tB, ps)
                    cp("act", Vs[:, tB, 0:64], ps[:, 0:64], reads=[ps], writes=[Vs])
                    cp("act", Vw[:, tB, 0:64], ps[:, 128:192], reads=[ps], writes=[Vw])
                    act(gtok[:, tB, :], ps[:, 192:204], AF.Sigmoid, reads=[ps], writes=[gtok])
                memset("pool", kcmpT[:], 0.0, writes=[kcmpT])
                memset("pool", rhsc[:], 0.0, writes=[rhsc])
                cp("pool", rhsc[:, 64:97], ovl[:], reads=[ovl], writes=[rhsc])
                for kv, srcT in (("k", kcT), ("v", vcT)):
                    W1, W2, peT = cmpW[kv]
                    psH = psb("a")
                    for lp in range(32):
                        mm(psH[0:64, 0:127], W1[:, lp, :], srcT[:, lp:lp + 16 * 126 + 1:16], lp == 0, lp == 31,
                           reads=[W1, srcT], writes=[psH])
                    for lp in range(32):
                        mm(psH[0:64, 127:128], W1[:, lp, :], peT[:, lp:lp + 1], lp == 0, lp == 31,
                           reads=[W1, peT], writes=[psH])
                    bias = k.sb(f"nsbias{kv}", [64, 1])
                    hid = k.sb(f"nshid{kv}", [64, 128], BF16)
                    cp("dve", bias[:], psH[0:64, 127:128], reads=[psH], writes=[bias])
                    act(hid[:, 0:127], psH[0:64, 0:127], AF.Silu, reads=[psH, bias], writes=[hid], bias=bias[:, 0:1])
                    ps2 = psb("m")
                    if kv == "k":
                        mm(ps2[0:64, 0:127], W2[:, :], hid[:, 0:127], True, True, reads=[W2, hid], writes=[ps2])
                        rope_evac(ps2[0:64, 0:127], kcmpT[0:64, 0:127], 127, 1.0, ropeCc[:, 0:127], ropeSc[:, 0:127],
                                  dict(src=ps2, dst=kcmpT, tab=ropeCc, dst16=kcmpT[0:16, 0:127]))
                    else:
                        mm(ps2[0:127, 0:64], hid[:, 0:127], W2[:, :], True, True, reads=[W2, hid], writes=[ps2])
                        cp("act", rhsc[0:127, 0:64], ps2[0:127, 0:64], reads=[ps2], writes=[rhsc])
            cmask = k.sb("cmask", [128, T_], BF16)
            tkA = k.sb("tkA", [128, 16, 32])
            tkB = k.sb("tkB", [128, 16, 32])
            nacc = k.sb("nsacc", [128, 16, 256])
            imp = k.sb("nsimp", [128, 16, 32])
            s01 = k.sb("nss01", [128, 2, 896], BF16)
            for dst, src in ((cmask, cmask_d), (tkA, tkA_d), (tkB, tkB_d), (s01, strips01_d)):
                k.dma("sp", dst[:], src[:], writes=[dst])
            pT = [k.sb(f"nspT{i}", [128, 512], BF16) for i in range(3)]
            rden = [k.sb(f"nsrd{i}", [128, 4]) for i in range(2)]
            cf = [k.sb(f"nscf{i}", [128, 4]) for i in range(2)]
            tmpo = [k.sb(f"nstmpo{i}", [128, 4, 64]) for i in range(2)]
            tmpi = [k.sb(f"nstmpi{i}", [128, 4, 32]) for i in range(2)]
            it = 0
            fi = 0
            for h in range(4):
                for qb in range(4):
                    qsl = slice(qb * 512, (qb + 1) * 512)
                    ps = psb("s")
                    mm(ps[:], kcmpT[:, :], qT[0:64, h, qsl], True, False, reads=[kcmpT, qT], writes=[ps])
                    mm(ps[:], identB, cmask[:, qsl], False, True, reads=[constB, cmask], writes=[ps])
                    p = pT[it % 3]
                    it += 1
                    act(p[:], ps[:], AF.Exp, reads=[ps], writes=[p])
                    acc = psb("o")
                    accv = acc[:, 0:388].rearrange("p (q d) -> p q d", q=4)
                    for qs in range(4):
                        mm(accv[:, qs, :], p[:, qs * 128:(qs + 1) * 128], rhsc[:, :], qs == 0, True,
                           reads=[p, rhsc], writes=[acc])
                    rd, c_ = rden[fi % 2], cf[fi % 2]
                    to, ti = tmpo[fi % 2], tmpi[fi % 2]
                    fi += 1
                    ts("dve", rd[:], accv[:, :, 64], 1e-30, None, ALU.max, None, reads=[acc], writes=[rd])
                    recip(rd[:], rd[:], reads=[rd], writes=[rd])
                    tt("dve", c_[:], rd[:], gtok[:, 4 * qb:4 * qb + 4, h], ALU.mult, reads=[rd, gtok], writes=[c_])
                    tt("dve", nacc[:, 4 * qb:4 * qb + 4, 64 * h:64 * h + 64], accv[:, :, 0:64],
                       c_[:].unsqueeze(2).to_broadcast([128, 4, 64]), ALU.mult, reads=[acc, c_], writes=[nacc])
                    if h == 0:
                        tt("dve", imp[:, 4 * qb:4 * qb + 4, :], accv[:, :, 65:97],
                           rd[:].unsqueeze(2).to_broadcast([128, 4, 32]), ALU.mult, reads=[acc, rd], writes=[imp])
                    else:
                        tt("dve", ti[:], accv[:, :, 65:97], rd[:].unsqueeze(2).to_broadcast([128, 4, 32]), ALU.mult,
                           reads=[acc, rd], writes=[ti])
                        tt("pool", imp[:, 4 * qb:4 * qb + 4, :], imp[:, 4 * qb:4 * qb + 4, :], ti[:], ALU.add,
                           reads=[imp, ti], writes=[imp])
            score = k.sb("nsscore", [128, 16, 32])
            negm = k.sb("nsnegm", [128, 16, 32])
            mx = [k.sb(f"nsmx{i}", [128, 8]) for i in range(2)]
            sc2 = k.sb("nssc2", [128, 32])
            tt("dve", score[:], imp[:], tkA[:], ALU.mult, reads=[imp, tkA], writes=[score])
            tt("dve", score[:], score[:], tkB[:], ALU.add, reads=[score, tkB], writes=[score])
            for tB in range(16):
                k.op("dve", lambda: nc.vector.max(out=mx[0][:], in_=score[:, tB, :]), reads=[score], writes=[mx[0]])
                k.op("dve", lambda: nc.vector.match_replace(out=sc2[:], in_to_replace=mx[0][:], in_values=score[:, tB, :],
                                                            imm_value=-1.0e9), reads=[score, mx[0]], writes=[sc2])
                k.op("dve", lambda: nc.vector.max(out=mx[1][:], in_=sc2[:]), reads=[sc2], writes=[mx[1]])
                ts("dve", negm[:, tB, :], score[:, tB, :], mx[1][:, 7:8], NEG, ALU.is_lt, ALU.mult,
                   reads=[score, mx[1]], writes=[negm])
            for g4 in range(4):
                ps = psb("m")
                for bb in range(4):
                    mm(ps[0:32, 128 * bb:128 * bb + 128], negm[:, 4 * g4 + bb, :], identF, True, True,
                       reads=[negm, constF], writes=[ps])
                for h in range(4):
                    cp("act" if h % 2 == 0 else "dve", qT[64:96, h, 512 * g4:512 * g4 + 512], ps[0:32, :],
                       reads=[ps], writes=[qT])

            def caus_mask(qb, kb):
                if kb >= 4 * qb:
                    r = kb - 4 * qb
                    return s01[:, 0, 384 - 128 * r:384 - 128 * r + 512]
                return None

            def win_mask(qb, kb):
                r = kb - 4 * qb
                if r >= 0:
                    return s01[:, 0, 384 - 128 * r:384 - 128 * r + 512]
                return s01[:, 1, 384 - 128 * (r + 4):384 - 128 * (r + 4) + 512]

            pTs4 = pT + [k.sb("nspT3", [128, 512], BF16)]

            def br_stream(si, bi, KTt, Vt, kb_range, mask_fn, use_sel, qs_range):
                pTs = [k.sb(f"nsbp{si}_{i}", [128, 512], BF16) for i in range(2)] if False else pTs4
                fc = [0]
                kq = 96 if use_sel else 64

                def masks(h, qb, kb):
                    return []

                def post_mask(h, qb, kb):
                    mk = mask_fn(qb, kb)
                    return None if mk is None else (mk, [s01])

                def fin(h, qb, acc, accv):
                    rd, c_, to = rden[fc[0] % 2], cf[fc[0] % 2], tmpo[fc[0] % 2]
                    fc[0] += 1
                    recip(rd[:], accv[:, :, 64], reads=[acc], writes=[rd])
                    tt("dve", c_[:], rd[:], gtok[:, 4 * qb:4 * qb + 4, 4 * bi + h], ALU.mult, reads=[rd, gtok], writes=[c_])
                    tt("dve", to[:], accv[:, :, 0:64], c_[:].unsqueeze(2).to_broadcast([128, 4, 64]), ALU.mult,
                       reads=[acc, c_], writes=[to])
                    tt("pool", nacc[:, 4 * qb:4 * qb + 4, 64 * h:64 * h + 64],
                       nacc[:, 4 * qb:4 * qb + 4, 64 * h:64 * h + 64], to[:], ALU.add, reads=[nacc, to], writes=[nacc])

                return attn_stream(
                    [(h, qb) for h in range(4) for qb in range(4)],
                    [PS[0], PS[1], PS[2], PS[3]], [PS[4], PS[5]], pTs,
                    lambda h, kb: KTt[0:kq, kb * 128:(kb + 1) * 128],
                    lambda h, qb: qT[0:kq, h, qb * 512:(qb + 1) * 512], [KTt, qT],
                    masks, None, [],
                    lambda h, kb: Vt[:, kb, :], [Vt],
                    kb_range, qs_range, 65, fin, post_mask=post_mask)

            run_streams([
                br_stream(0, 1, ksT, Vs, lambda qb: range(0, 4 * qb + 4), caus_mask, True,
                          lambda qb, qs: (0, 4 * qb + qs))])
            run_streams([
                br_stream(1, 2, kwT, Vw, lambda qb: range(max(0, 4 * qb - 4), 4 * qb + 4), win_mask, False,
                          lambda qb, qs: (max(0, 4 * qb + qs - 4), 4 * qb + qs)),
            ])
            tmp = fin_tmp(4)
            for g4 in range(4):
                for bb in range(4):
                    finalize(nacc[:, 4 * g4 + bb, :].rearrange("p (h d) -> p h d", h=4), nacc,
                             ytok[:, 4 * g4 + bb, 768:1024].rearrange("p (h d) -> p h d", h=4), tmp, n=4)

    def phase_out(s, l, last):
        xsrc = xT_d if l == 0 else xres_d
        with k.scope():
            (wg,) = get_w(s, l, "out")
            yT = k.sb("yT", [128, 8, T_], BF16)
            wo = k.sb("wo", [128, 8, D_], BF16)
            wsrc = w_out_d[l].rearrange("(j p) c -> p j c", p=128)
            k.dma("pool", wo[:, 0:4, :], wsrc[:, 0:4, :], writes=[wo])
            k.dma("pool", wo[:, 4:8, :], wsrc[:, 4:8, :], writes=[wo])
            def gate_stream(si, tBs):
                sgi = k.sb(f"sg{si}", [128, D_])
                ygi = k.sb(f"yg{si}", [128, D_], BF16)
                pa, pb = PS[2 * si], PS[2 * si + 1]
                for tB in tBs:
                    proj_tm(wg, 0, 512, tB, pa)
                    proj_tm(wg, 512, 512, tB, pb)
                    yield
                    act(sgi[:, 0:512], pa[:], AF.Silu, reads=[pa], writes=[sgi])
                    act(sgi[:, 512:1024], pb[:], AF.Silu, reads=[pb], writes=[sgi])
                    tt("dve", sgi[:], sgi[:], ong[:], ALU.mult, reads=[sgi, ong], writes=[sgi])
                    yield
                    tt("dve", ygi[:], sgi[:], ytok[:, tB, :], ALU.mult, reads=[sgi, ytok], writes=[ygi])
                    for cc in range(8):
                        k.op("pe", lambda: nc.tensor.transpose(PST[:, cc * 128:(cc + 1) * 128],
                                                               ygi[:, cc * 128:(cc + 1) * 128], identB),
                             reads=[ygi, constB], writes=[PST])
                    cp("act", yT[:, :, tB * 128:(tB + 1) * 128], PST[:].rearrange("p (c t) -> p c t", c=8),
                       reads=[PST], writes=[yT])

            with k.scope():
                ong = k.sb("ong", [128, D_])
                k.dma("sp", ong[:], ong_d[l:l + 1, :].to_broadcast([128, D_]), writes=[ong])
                run_streams([gate_stream(0, range(0, 16, 3)), gate_stream(1, range(1, 16, 3)),
                             gate_stream(2, range(2, 16, 3))])
            xin = [k.sb(f"xo{i}", [128, 8, 512]) for i in range(2)]
            if last:
                sqf = k.sb("sqf", [128, 8, 512], BF16)
                rsf = k.sb("rsf", [128, 512])
            for tb in range(4):
                sl = slice(tb * 512, (tb + 1) * 512)
                xi = xin[tb % 2]
                src = xsrc[s].rearrange("(j p) t -> p j t", p=128)[:, :, sl]
                k.dma("sp", xi[:], src, reads=[xsrc], writes=[xi])
                for dj in range(8):
                    ps = PS[(tb * 8 + dj) % 6]
                    for cc in range(8):
                        mm(ps[:], wo[:, cc, dj * 128:(dj + 1) * 128], yT[:, cc, sl], cc == 0, cc == 7,
                           reads=[wo, yT], writes=[ps])
                    tt("dve", xi[:, dj, :], xi[:, dj, :], ps[:], ALU.add, reads=[xi, ps], writes=[xi])
                if not last:
                    dst = xres_d[s].rearrange("(j p) t -> p j t", p=128)[:, :, sl]
                    k.dma("sp", dst, xi[:], reads=[xi], writes=[xres_d])
                else:
                    act(sqf[:], xi[:], AF.Square, reads=[xi], writes=[sqf])
                    ps = psb("m")
                    for j in range(8):
                        mm(ps[:], onesB, sqf[:, j, :], j == 0, j == 7, reads=[constB, sqf], writes=[ps])
                    act(rsf[:], ps[:], AF.Ln, reads=[ps], writes=[rsf], bias=EPS, scale=1.0 / D_)
                    act(rsf[:], rsf[:], AF.Exp, reads=[rsf], writes=[rsf], scale=-0.5)
                    for j in range(8):
                        stt("dve", xi[:, j, :], xi[:, j, :], fnormg[:, j:j + 1], rsf[:], ALU.mult, ALU.mult,
                            reads=[xi, rsf, fnormg], writes=[xi])
                    dst = outT_d[s].rearrange("(j p) t -> p j t", p=128)[:, :, sl]
                    k.dma("sp", dst, xi[:], reads=[xi], writes=[outT_d])

    for s in range(nseq):
        for l in range(nlayer):
            phase_norm(s, l)
            if debug and s == 0 and l == 0 and "hT" in debug:
                d = dbg_out("dbg_hT", [128, 8, T_], BF16)
                k.dma("sp", d[:], hT[:], reads=[hT], writes=[d])
            if "hg" in mixers:
                phase_hgrn(s, l)
            if "fx" in mixers:
                phase_fox(s, l)
            if "sb" in mixers:
                phase_sb(s, l)
            if "ns" in mixers:
                phase_nsa(s, l)
            if debug and s == 0 and l == nlayer - 1 and "ytok" in debug:
                d = dbg_out("dbg_ytok", [128, 16, D_], BF16)
                k.dma("sp", d[:], ytok[:], reads=[ytok], writes=[d])
            if "out" in mixers:
                phase_out(s, l, l == nlayer - 1)

    k.finish(list(dbg_outs.values()) + [outT_d])
    k.barrier()
    k.close()
    return nc, k, list(dbg_outs.keys())


def host_inputs(inputs, core, consts, nseq=SEQ_PER_CORE):
    f32 = np.float32
    x = inputs["x"]
    b0 = core * nseq
    m = {}
    m["xT"] = np.ascontiguousarray(np.transpose(x[b0:b0 + nseq], (0, 2, 1))).astype(f32)
    m["w_in"] = np.ascontiguousarray(inputs["w_in"], dtype=f32)
    m["w_out"] = np.ascontiguousarray(inputs["w_out"], dtype=f32)
    m["norm_gT"] = np.ascontiguousarray(inputs["norm_g"].reshape(2, 8, 128).transpose(0, 2, 1), dtype=f32)
    m["fnorm_gT"] = np.ascontiguousarray(inputs["final_norm_g"].reshape(8, 128).T, dtype=f32)
    m["out_norm_g"] = np.ascontiguousarray(inputs["out_norm_g"], dtype=f32)
    lbl = np.asarray(inputs["hgrn_lb_logits"], dtype=f32)
    m["lb_logits"] = np.ascontiguousarray(lbl)
    m["lb_logitsT"] = np.ascontiguousarray(lbl.reshape(2, 2, 128).transpose(2, 1, 0))
    m["fox_fb"] = np.ascontiguousarray(inputs["fox_fb"], dtype=f32)
    for kv in "kv":
        m[f"peT_{kv}"] = np.ascontiguousarray(np.transpose(inputs[f"nsa_cmp_pe_{kv}"], (0, 2, 1)), dtype=f32)
        m[f"w1_{kv}"] = np.ascontiguousarray(inputs[f"nsa_cmp_w1_{kv}"], dtype=f32)
        m[f"w2_{kv}"] = np.ascontiguousarray(inputs[f"nsa_cmp_w2_{kv}"], dtype=f32)
    for nm in ["constF", "constB", "strips", "strips01", "cmask", "ovl", "eall", "ropeC", "ropeS", "ropeCc", "ropeSc",
               "permF", "tkA", "tkB"]:
        m[nm] = consts[nm]
    return m


def kernel(**inputs):
    inputs = {kk: np.asarray(v) for kk, v in inputs.items()}
    consts = make_consts()
    nc, kb, _ = build()
    in_maps = [host_inputs(inputs, c, consts) for c in range(NCORES)]
    res = run_bass_kernel_spmd(nc, in_maps, core_ids=list(range(NCORES)))
    outs = [np.asarray(r["outT"]) for r in res.results]
    full = np.concatenate(outs, axis=0)
    return np.ascontiguousarray(np.transpose(full, (0, 2, 1))).astype(np.float32)
```

```python
import os
import numpy as np
import ml_dtypes
from contextlib import ExitStack
import concourse.bass as bass
import concourse.mybir as mybir
from concourse.bass_utils import run_bass_kernel_spmd

F32 = mybir.dt.float32
BF16 = mybir.dt.bfloat16
AF = mybir.ActivationFunctionType
ALU = mybir.AluOpType
AX = mybir.AxisListType

T_ = 2048
D_ = 1024
NIN = 3984
NEG = -30000.0
EPS = 1e-6
NCORES = 8
SEQ_PER_CORE = 4

C_HGQ, C_HGF, C_HGI = 0, 256, 512
C_FXQ, C_FXK, C_FXV, C_FXF = 768, 1024, 1280, 1536
C_SBQ, C_SBK, C_SBV = 1540, 1796, 2052
C_NSQ = 2308
C_NKC, C_NVC, C_NKS, C_NVS, C_NKW, C_NVW = 2564, 2628, 2692, 2756, 2820, 2884
C_NSG = 2948
C_GATE = 2960


class Tok:
    __slots__ = ("w", "r", "name")

    def __init__(self, name=""):
        self.w = None
        self.r = {}
        self.name = name


class T:
    def __init__(self, h, name, excl=False):
        self.h = h
        self.tok = Tok(name)
        self.name = name
        self.excl = excl

    def __getitem__(self, k):
        return self.h[k]


class WV:
    def __init__(self, t, base):
        self.t = t
        self.base = base
        self.tok = t.tok

    def __getitem__(self, key):
        p, j, sl = key
        return self.t.h[p, j, self.base + sl.start:self.base + sl.stop]


class KB:
    NDQ = 8
    EPOCH_LIMIT = 30000

    def __init__(self, nc):
        self.nc = nc
        self.stack = [ExitStack()]
        self.E = {"pe": nc.tensor, "act": nc.scalar, "dve": nc.vector,
                  "pool": nc.gpsimd, "sp": nc.sync}
        self.semh = {}
        self.cur = {}
        self.cnt = {}
        self.epoch = {}
        for e in ["pe", "act", "dve", "pool"]:
            self.epoch[e] = -1
            self._new_epoch(e)
        self.dq = {}
        for q in ["sp", "pool", "act"]:
            sems = [self.stack[0].enter_context(nc.semaphore(f"d_{q}{i}")) for i in range(self.NDQ)]
            self.dq[q] = dict(n=0)
            for i, s in enumerate(sems):
                self.semh[("dma", q, i)] = s
        self.seen = {}
        self.nins = {e: 0 for e in self.E}
        self.uid = 0

    def _new_epoch(self, e):
        self.epoch[e] += 1
        key = (e, self.epoch[e])
        self.semh[key] = self.stack[0].enter_context(self.nc.semaphore(f"s_{e}{self.epoch[e]}"))
        self.cur[e] = key
        self.cnt[e] = 0

    def scope(self):
        kb = self

        class _S:
            def __enter__(s):
                kb.stack.append(ExitStack())

            def __exit__(s, *a):
                kb.barrier()
                kb.stack.pop().close()
                return False
        return _S()

    def _nm(self, name):
        self.uid += 1
        return f"{name}_{self.uid}"

    def sb(self, name, shape, dt=F32):
        h = self.stack[-1].enter_context(self.nc.sbuf_tensor(self._nm(name), list(shape), dt))
        return T(h, name)

    def ps(self, name, shape, dt=F32):
        h = self.stack[-1].enter_context(self.nc.psum_tensor(self._nm(name), list(shape), dt))
        return T(h, name, excl=True)

    def dram(self, name, shape, dt, kind):
        h = self.nc.dram_tensor(name, list(shape), dt, kind=kind)
        return T(h, name)

    def _wait(self, eng, deps):
        for key, val in deps.items():
            if key[0] == "pe" and eng == "pe":
                continue
            sk = (eng, key)
            if self.seen.get(sk, 0) >= val:
                continue
            self.seen[sk] = val
            self.E[eng].wait_ge(self.semh[key], val)
            self.nins[eng] += 1

    @staticmethod
    def _tok(x):
        return getattr(x, "tok", x)

    def _deps(self, reads, writes):
        deps = {}
        for t in reads:
            t = self._tok(t)
            if t.w and deps.get(t.w[0], 0) < t.w[1]:
                deps[t.w[0]] = t.w[1]
        for t in writes:
            t = self._tok(t)
            if t.w and deps.get(t.w[0], 0) < t.w[1]:
                deps[t.w[0]] = t.w[1]
            for kk, v in t.r.items():
                if deps.get(kk, 0) < v:
                    deps[kk] = v
        return deps

    def _mark(self, ev, reads, writes):
        kk, v = ev
        for t in reads:
            t = self._tok(t)
            if t.r.get(kk, 0) < v:
                t.r[kk] = v
        for t in writes:
            t = self._tok(t)
            t.w = ev
            t.r = {}

    def op(self, eng, fn, reads=(), writes=()):
        ex = [t for t in reads if isinstance(t, T) and t.excl]
        if ex:
            reads = [t for t in reads if not (isinstance(t, T) and t.excl)]
            writes = list(writes) + ex
        self._wait(eng, self._deps(reads, writes))
        if self.cnt[eng] >= self.EPOCH_LIMIT:
            self._new_epoch(eng)
        ins = fn()
        self.cnt[eng] += 1
        ins.then_inc(self.semh[self.cur[eng]], 1)
        self.nins[eng] += 1
        self._mark((self.cur[eng], self.cnt[eng]), reads, writes)
        return ins

    def pe_selfwait(self):
        key, val = self.cur["pe"], self.cnt["pe"]
        if val > 0 and self.seen.get(("pe", key), 0) < val:
            self.seen[("pe", key)] = val
            self.E["pe"].wait_ge(self.semh[key], val)
            self.nins["pe"] += 1

    def dma(self, q, out, in_, reads=(), writes=(), **kw):
        self._wait(q, self._deps(reads, writes))
        d = self.dq[q]
        slot = d["n"] % self.NDQ
        val = 16 * (d["n"] // self.NDQ + 1)
        d["n"] += 1
        ins = self.E[q].dma_start(out=out, in_=in_, **kw)
        ins.then_inc(self.semh[("dma", q, slot)], 16)
        self.nins[q] += 1
        self._mark((("dma", q, slot), val), reads, writes)
        return ins

    def _all_events(self):
        ev = {}
        for e in ["pe", "act", "dve", "pool"]:
            if self.cnt[e] > 0:
                ev[self.cur[e]] = self.cnt[e]
        for q, d in self.dq.items():
            n = d["n"]
            for slot in range(self.NDQ):
                c = (n - slot + self.NDQ - 1) // self.NDQ
                if c > 0:
                    ev[("dma", q, slot)] = 16 * c
        return ev

    def barrier(self):
        ev = self._all_events()
        for e in ["pe", "act", "dve", "pool", "sp"]:
            self._wait(e, {kk: v for kk, v in ev.items() if not (kk[0] == e)})

    def finish(self, toks, eng="sp"):
        deps = {}
        for t in toks:
            t = self._tok(t)
            if t.w and deps.get(t.w[0], 0) < t.w[1]:
                deps[t.w[0]] = t.w[1]
        self._wait(eng, deps)

    def close(self):
        while self.stack:
            self.stack.pop().close()


def _bf(a):
    return np.asarray(a, dtype=np.float32).astype(ml_dtypes.bfloat16)


def make_consts():
    c = {}
    i = np.arange(128)[:, None]
    j = np.arange(128)[None, :]
    c["identF"] = np.eye(128, dtype=np.float32)
    c["triF"] = (i <= j).astype(np.float32)
    c["onesF"] = np.ones((128, 128), np.float32)
    c["tribdF"] = ((i // 32 == j // 32) & (i <= j)).astype(np.float32)
    c["trisufF"] = ((i // 32 == j // 32) & (i > j)).astype(np.float32)
    c["maskbdF"] = ((i // 32 == j // 32) & (i <= j)).astype(np.float32)
    perm = np.zeros((64, 16), np.float32)
    for a in range(16):
        perm[a + 8 if a < 8 else a - 8, a] = 1.0
    c["permF"] = perm
    cf = np.concatenate([c["identF"], c["triF"], c["onesF"], c["tribdF"], c["trisufF"], c["maskbdF"]], axis=1)
    c["constF"] = cf
    negtri = -(i >= j).astype(np.float32)
    neglow = -(i < j).astype(np.float32)
    c["constB"] = _bf(np.concatenate([np.eye(128), np.ones((128, 128)), negtri, neglow], axis=1))
    jj = np.arange(896)[None, :] - 384
    caus = np.where(jj < 0, NEG, np.where(jj >= 128, 0.0, np.where(jj >= i, 0.0, NEG)))
    strict = np.where(jj < 0, NEG, np.where(jj >= 128, 0.0, np.where(jj > i, 0.0, NEG)))
    win = np.where(jj < 0, 0.0, np.where(jj >= 128, NEG, np.where(jj < i, 0.0, NEG)))
    c["strips"] = _bf(np.stack([caus, strict, win], axis=1))
    c["strips01"] = _bf(np.stack([(caus == 0.0), (win == 0.0)], axis=1).astype(np.float32))
    n = np.arange(128)[:, None]
    t = np.arange(T_)[None, :]
    c["cmask"] = _bf(np.where((16 * n + 31 <= t) & (n < 127), 0.0, NEG))
    cs = (np.arange(128) * 16)[:, None]
    ss = (np.arange(32) * 64)[None, :]
    ov = np.clip(np.minimum(cs + 32, ss + 64) - np.maximum(cs, ss), 0, None).astype(np.float32) / 32.0
    ovl = np.concatenate([np.ones((128, 1), np.float32), ov], axis=1)
    ovl[127] = 0.0
    c["ovl"] = _bf(ovl)
    c["eall"] = _bf((np.arange(T_)[None, :] // 64 == np.arange(32)[:, None]).astype(np.float32))
    half = 8
    inv = (500000.0 ** (-(np.arange(half, dtype=np.float32) * 2.0 / 16.0))).astype(np.float32)

    def tabs(pos):
        ang = pos.astype(np.float32)[None, :] * inv[:, None]
        cos, sin = np.cos(ang), np.sin(ang)
        return (np.concatenate([cos, cos], 0).astype(np.float32),
                np.concatenate([-sin, sin], 0).astype(np.float32))
    C, S = tabs(np.arange(T_))
    c["ropeC"], c["ropeS"] = C, S
    pc = np.arange(128) * 16 + 31
    Cc, Sc = tabs(pc)
    c["ropeCc"], c["ropeSc"] = Cc, Sc
    tt_ = np.arange(T_)
    qblk = tt_ // 64
    blk = np.arange(32)[None, :]
    forced = (blk == 0) | (blk == qblk[:, None]) | (blk == qblk[:, None] - 1)
    valid = blk <= qblk[:, None]
    A = (~forced & valid).astype(np.float32)
    Bc = np.where(forced, 1.0e4, np.where(valid, 0.0, -1.0e4)).astype(np.float32)
    c["tkA"] = A.reshape(16, 128, 32).transpose(1, 0, 2).copy()
    c["tkB"] = Bc.reshape(16, 128, 32).transpose(1, 0, 2).copy()
    return c


def build(nseq=SEQ_PER_CORE, nlayer=2, mixers=("hg", "fx", "sb", "ns", "out"), debug=None):
    nc = bass.Bass("TRN2", target_bir_lowering=False)
    k = KB(nc)
    dbg_outs = {}

    def din(name, shape, dt=F32):
        return k.dram(name, shape, dt, "ExternalInput")

    xT_d = din("xT", [nseq, D_, T_])
    w_in_d = din("w_in", [2, D_, NIN])
    w_out_d = din("w_out", [2, D_, D_])
    normg_d = din("norm_gT", [2, 128, 8])
    fnormg_d = din("fnorm_gT", [128, 8])
    ong_d = din("out_norm_g", [2, D_])
    lbl_d = din("lb_logits", [2, 256])
    lblT_d = din("lb_logitsT", [128, 2, 2])
    fb_d = din("fox_fb", [2, 4])
    peT_d = {kv: din(f"peT_{kv}", [2, 64, 32]) for kv in "kv"}
    w1_d = {kv: din(f"w1_{kv}", [2, 2048, 64]) for kv in "kv"}
    w2_d = {kv: din(f"w2_{kv}", [2, 64, 64]) for kv in "kv"}
    constF_d = din("constF", [128, 768])
    constB_d = din("constB", [128, 512], BF16)
    strips_d = din("strips", [128, 3, 896], BF16)
    strips01_d = din("strips01", [128, 2, 896], BF16)
    cmask_d = din("cmask", [128, T_], BF16)
    ovl_d = din("ovl", [128, 33], BF16)
    eall_d = din("eall", [32, T_], BF16)
    ropeC_d = din("ropeC", [16, T_])
    ropeS_d = din("ropeS", [16, T_])
    ropeCc_d = din("ropeCc", [16, 128])
    ropeSc_d = din("ropeSc", [16, 128])
    permF_d = din("permF", [64, 16])
    tkA_d = din("tkA", [128, 16, 32])
    tkB_d = din("tkB", [128, 16, 32])
    outT_d = k.dram("outT", [nseq, D_, T_], F32, "ExternalOutput")
    xres_d = k.dram("xres", [nseq, D_, T_], F32, "Internal")

    def dbg_out(name, shape, dt=F32):
        d = k.dram(name, shape, dt, "ExternalOutput")
        dbg_outs[name] = d
        return d

    constF = k.sb("constF", [128, 768])
    constB = k.sb("constB", [128, 512], BF16)
    strips = k.sb("strips", [128, 3, 896], BF16)
    k.dma("sp", constF[:], constF_d[:], writes=[constF])
    k.dma("sp", constB[:], constB_d[:], writes=[constB])
    k.dma("sp", strips[:], strips_d[:], writes=[strips])
    identF = constF[:, 0:128]
    triF = constF[:, 128:256]
    onesF = constF[:, 256:384]
    tribdF = constF[:, 384:512]
    trisufF = constF[:, 512:640]
    maskbdF = constF[:, 640:768]
    identB = constB[:, 0:128]
    onesB = constB[:, 128:256]
    negtriB = constB[:, 256:384]
    neglowB = constB[:, 384:512]

    hT = k.sb("hT", [128, 8, T_], BF16)
    ytok = k.sb("ytok", [128, 16, D_], BF16)
    normg = k.sb("normg", [128, 2, 8])
    fnormg = k.sb("fnormg", [128, 8])
    for l in range(2):
        k.dma("sp", normg[:, l, :], normg_d[l], writes=[normg])
    k.dma("sp", fnormg[:], fnormg_d[:], writes=[fnormg])

    PS = [k.ps(f"ps{i}", [128, 512]) for i in range(7)]
    PST = k.ps("pst", [128, 1024], BF16)
    ps_rr = {"s": [0, 1], "a": [2, 3], "o": [4, 5], "m": [6]}
    ps_ctr = {kk: 0 for kk in ps_rr}

    def psb(role):
        lst = ps_rr[role]
        i = lst[ps_ctr[role] % len(lst)]
        ps_ctr[role] += 1
        return PS[i]

    def mm(out, lhsT, rhs, start, stop, reads, writes, tp=None, ser=False):
        kw = {}
        if ser:
            k.pe_selfwait()
        if tp is not None:
            kw["tile_position"] = tp
        return k.op("pe", lambda: nc.tensor.matmul(out, lhsT=lhsT, rhs=rhs, start=start, stop=stop, **kw),
                    reads=reads, writes=writes)

    def act(out, in_, func, reads, writes, bias=None, scale=None):
        kw = {}
        if bias is not None:
            kw["bias"] = bias
        if scale is not None:
            kw["scale"] = scale
        return k.op("act", lambda: nc.scalar.activation(out=out, in_=in_, func=func, **kw),
                    reads=reads, writes=writes)

    def tt(eng, out, in0, in1, op, reads, writes):
        e = k.E[eng]
        return k.op(eng, lambda: e.tensor_tensor(out=out, in0=in0, in1=in1, op=op), reads=reads, writes=writes)

    def ts(eng, out, in0, s1, s2, op0, op1, reads, writes):
        e = k.E[eng]
        if s2 is None:
            return k.op(eng, lambda: e.tensor_scalar(out=out, in0=in0, scalar1=s1, scalar2=None, op0=op0),
                        reads=reads, writes=writes)
        return k.op(eng, lambda: e.tensor_scalar(out=out, in0=in0, scalar1=s1, scalar2=s2, op0=op0, op1=op1),
                    reads=reads, writes=writes)

    def stt(eng, out, in0, scalar, in1, op0, op1, reads, writes):
        e = k.E[eng]
        return k.op(eng, lambda: e.scalar_tensor_tensor(out=out, in0=in0, scalar=scalar, in1=in1, op0=op0, op1=op1),
                    reads=reads, writes=writes)

    def cp(eng, out, in_, reads, writes):
        if eng == "act":
            return k.op("act", lambda: nc.scalar.copy(out=out, in_=in_), reads=reads, writes=writes)
        e = k.E[eng]
        return k.op(eng, lambda: e.tensor_copy(out=out, in_=in_), reads=reads, writes=writes)

    def memset(eng, out, val, writes):
        e = k.E[eng]
        return k.op(eng, lambda: e.memset(out, val), writes=writes)

    def recip(out, in_, reads, writes):
        return k.op("dve", lambda: nc.vector.reciprocal(out=out, in_=in_), reads=reads, writes=writes)

    wslots = [k.sb("wslot0", [128, 8, 1024], BF16), k.sb("wslot1", [128, 8, 1024], BF16)]
    wcols = {"hg": [(C_HGF, 512), (C_HGQ, 512)], "fx": [(C_FXQ, 256), (C_FXK, 256), (C_FXV, 260)],
             "sb": [(C_SBQ, 256), (C_SBK, 256), (C_SBV, 256)], "ns": [(C_NSQ, 652)], "out": [(C_GATE, 1024)]}
    wplan = [(s_, l_, ph) for s_ in range(nseq) for l_ in range(nlayer)
             for ph in ("hg", "fx", "sb", "ns", "out") if ph in mixers]
    wissued = set()

    def w_issue(idx):
        if idx >= len(wplan) or idx in wissued:
            return
        wissued.add(idx)
        _, l_, ph = wplan[idx]
        slot = wslots[idx % 2]
        base = 0
        for (c0, n) in wcols[ph]:
            src = w_in_d[l_][:, c0:c0 + n].rearrange("(j p) c -> p j c", p=128)
            k.dma("pool", slot[:, 0:4, base:base + n], src[:, 0:4, :], writes=[slot])
            k.dma("pool", slot[:, 4:8, base:base + n], src[:, 4:8, :], writes=[slot])
            base += n

    def get_w(s_, l_, ph):
        idx = wplan.index((s_, l_, ph))
        w_issue(idx)
        views, base = [], 0
        for (c0, n) in wcols[ph]:
            views.append(WV(wslots[idx % 2], base))
            base += n
        w_issue(idx + 1)
        return views

    def load_w(l, c0, ncols, name="wblk"):
        w = k.sb(name, [128, 8, ncols], BF16)
        src = w_in_d[l][:, c0:c0 + ncols].rearrange("(j p) c -> p j c", p=128)
        half = 4
        k.dma("pool", w[:, 0:half, :], src[:, 0:half, :], writes=[w])
        k.dma("pool", w[:, half:8, :], src[:, half:8, :], writes=[w])
        return w

    def proj_fm(w, off, M, tb, ps, extra_reads=()):
        for j in range(8):
            mm(ps[0:M, :], w[:, j, off:off + M], hT[:, j, tb * 512:(tb + 1) * 512], j == 0, j == 7,
               reads=[w, hT, *extra_reads], writes=[ps])

    def proj_tm(w, off, N, tB, ps):
        for j in range(8):
            mm(ps[:, 0:N], hT[:, j, tB * 128:(tB + 1) * 128], w[:, j, off:off + N], j == 0, j == 7,
               reads=[w, hT], writes=[ps])

    def finalize(o, o_tok, out_ap, tmp, n=4):
        sq, ss, sd = tmp
        tt("dve", sq[:, 0:n, :], o, o, ALU.mult, reads=[o_tok], writes=[sq])
        k.op("dve", lambda: nc.vector.tensor_reduce(out=ss[:, 0:n], in_=sq[:, 0:n, :], axis=AX.X, op=ALU.add),
             reads=[sq], writes=[ss])
        act(sd[:, 0:n], ss[:, 0:n], AF.Ln, reads=[ss], writes=[sd], bias=EPS, scale=1.0 / 64.0)
        act(sd[:, 0:n], sd[:, 0:n], AF.Exp, reads=[sd], writes=[sd], scale=-0.5)
        tt("dve", out_ap, o, sd[:, 0:n].unsqueeze(2).to_broadcast([128, n, 64]), ALU.mult,
           reads=[o_tok, sd], writes=[ytok])

    def fin_tmp(n=4):
        return (k.sb("sq", [128, n, 64]), k.sb("ss", [128, n]), k.sb("sd", [128, n]))


    def run_streams(gens):
        gens = list(gens)
        while gens:
            for g in list(gens):
                try:
                    next(g)
                except StopIteration:
                    gens.remove(g)

    def attn_stream(jobs, sbank, obanks, pTs, K_ap, Q_ap, kq_toks, masks, bias_ap, bias_toks, V_ap, v_toks,
                    kb_range, qs_range, ncol, fin, post_mask=None):
        if not isinstance(obanks, (list, tuple)):
            obanks = [obanks]
        tiles = []
        for ji, (h, qb) in enumerate(jobs):
            kbs = list(kb_range(qb))
            for kb in kbs:
                tiles.append((h, qb, kb, kb == kbs[0], kb == kbs[-1], ji))
        nsb = len(sbank)
        L = max(nsb - 1, 1)

        def emit_qk(i):
            h, qb, kb = tiles[i][0:3]
            ps = sbank[i % nsb]
            ml = masks(h, qb, kb)
            mm(ps[:], K_ap(h, kb), Q_ap(h, qb), True, len(ml) == 0, reads=kq_toks, writes=[ps])
            for mi, (lt, rh, rd) in enumerate(ml):
                mm(ps[:], lt, rh, False, mi == len(ml) - 1, reads=rd, writes=[ps])

        if nsb > 1:
            for i in range(min(L, len(tiles))):
                emit_qk(i)
        else:
            emit_qk(0)
        first = True
        for i, (h, qb, kb, isfirst, islast, ji) in enumerate(tiles):
            if nsb > 1 and i + L < len(tiles):
                emit_qk(i + L)
            yield
            obank = obanks[ji % len(obanks)]
            accv = obank[:, 0:4 * ncol].rearrange("p (q d) -> p q d", q=4)
            ps = sbank[i % nsb]
            p = pTs[i % len(pTs)]
            if isfirst:
                first = True
            b = bias_ap(h, kb) if bias_ap is not None else None
            act(p[:], ps[:], AF.Exp, reads=[ps, *bias_toks], writes=[p], bias=b)
            pm = post_mask(h, qb, kb) if post_mask is not None else None
            if pm is not None:
                tt("dve", p[:], p[:], pm[0], ALU.mult, reads=[p, *pm[1]], writes=[p])
            for qs in range(4):
                lo, hi = qs_range(qb, qs)
                if kb < lo or kb > hi:
                    continue
                mm(accv[:, qs, :], p[:, qs * 128:(qs + 1) * 128], V_ap(h, kb), first, kb == hi,
                   reads=[p, *v_toks], writes=[obank])
                first = False
            if islast:
                fin(h, qb, obank, accv)
            if nsb == 1 and i + 1 < len(tiles):
                emit_qk(i + 1)
            yield

    def phase_norm(s, l):
        xsrc = xT_d if l == 0 else xres_d
        with k.scope():
            xin = [k.sb(f"xin{i}", [128, 8, 512]) for i in range(2)]
            sq = [k.sb(f"sqn{i}", [128, 8, 512], BF16) for i in range(2)]
            rs = [k.sb(f"rs{i}", [128, 512]) for i in range(2)]
            for tb in range(4):
                xi, sqi, rsi = xin[tb % 2], sq[tb % 2], rs[tb % 2]
                src = xsrc[s].rearrange("(j p) t -> p j t", p=128)[:, :, tb * 512:(tb + 1) * 512]
                k.dma("sp", xi[:], src, reads=[xsrc], writes=[xi])
                act(sqi[:], xi[:], AF.Square, reads=[xi], writes=[sqi])
                ps = psb("m")
                for j in range(8):
                    mm(ps[:], onesB, sqi[:, j, :], j == 0, j == 7, reads=[constB, sqi], writes=[ps])
                act(rsi[:], ps[:], AF.Ln, reads=[ps], writes=[rsi], bias=EPS, scale=1.0 / D_)
                act(rsi[:], rsi[:], AF.Exp, reads=[rsi], writes=[rsi], scale=-0.5)
                for j in range(8):
                    stt("dve", hT[:, j, tb * 512:(tb + 1) * 512], xi[:, j, :], normg[:, l, j:j + 1], rsi[:],
                        ALU.mult, ALU.mult, reads=[xi, rsi, normg], writes=[hT])

    def phase_fox(s, l):
        with k.scope():
            QT = k.sb("fxQT", [128, 4, T_], BF16)
            KT = k.sb("fxKT", [128, 4, T_], BF16)
            V = k.sb("fxV", [128, 16, 4, 65], BF16)
            ltok = k.sb("fxl", [128, 16, 4])
            negcum = k.sb("fxnc", [128, 16, 4])
            fbb = k.sb("fbb", [128, 4])
            k.dma("sp", fbb[:], fb_d[l:l + 1, :].to_broadcast([128, 4]), writes=[fbb])
            memset("pool", V[:, :, :, 64:65], 1.0, writes=[V])
            memset("pool", KT[64:67, :, :], 1.0, writes=[KT])
            wq_, wk_, wv_ = get_w(s, l, "fx")
            for (w, dst, scale) in ((wq_, QT, 0.125), (wk_, KT, None)):
                for pr in range(2):
                    for tb in range(4):
                        ps = psb("m")
                        proj_fm(w, pr * 128, 128, tb, ps)
                        sl = slice(tb * 512, (tb + 1) * 512)
                        for hh in range(2):
                            if scale is None:
                                cp("act", dst[0:64, 2 * pr + hh, sl], ps[64 * hh:64 * hh + 64, :], reads=[ps], writes=[dst])
                            else:
                                k.op("act", lambda: nc.scalar.mul(out=dst[0:64, 2 * pr + hh, sl],
                                                                  in_=ps[64 * hh:64 * hh + 64, :], mul=scale),
                                     reads=[ps], writes=[dst])
            if os.environ.get('FX_STOP') == '1':
                return
            w = wv_
            _skip = os.environ.get("FX_SKIP", "")
            for tB in range(int(os.environ.get("FX_NTB", "16"))):
                ps = psb("m")
                if "mm" not in _skip:
                    proj_tm(w, 0, 260, tB, ps)
                if "cp" not in _skip:
                    cp("act", V[:, tB, :, 0:64], ps[:, 0:256].rearrange("p (h d) -> p h d", h=4), reads=[ps], writes=[V])
                if "tt" not in _skip:
                    tt("dve", ltok[:, tB, :], ps[:, 256:260], fbb[:], ALU.add, reads=[ps, fbb], writes=[ltok])
            if os.environ.get('FX_STOP') == '2':
                return
            act(ltok[:], ltok[:], AF.Exp, reads=[ltok], writes=[ltok], scale=-1.0)
            act(ltok[:], ltok[:], AF.Ln, reads=[ltok], writes=[ltok], bias=1.0)
            if os.environ.get('FX_STOP') == '3':
                return
            ps = psb("m")
            lflat = ltok[:].rearrange("p b h -> p (b h)")
            mm(ps[:, 0:64], triF, lflat, True, True, reads=[constF, ltok], writes=[ps])
            mm(ps[:, 64:128], onesF, lflat, True, True, reads=[constF, ltok], writes=[ps])
            tot = k.sb("fxtot", [128, 16, 4])
            pre = k.sb("fxpre", [128, 16, 4])
            cp("dve", tot[:], ps[:, 64:128].rearrange("p (b h) -> p b h", h=4), reads=[ps], writes=[tot])
            memset("dve", pre[:, 0, :], 0.0, writes=[pre])
            for B in range(1, 16):
                tt("dve", pre[:, B, :], pre[:, B - 1, :], tot[:, B - 1, :], ALU.add, reads=[pre, tot], writes=[pre])
            tt("dve", negcum[:], ps[:, 0:64].rearrange("p (b h) -> p b h", h=4), pre[:], ALU.add,
               reads=[ps, pre], writes=[negcum])
            if os.environ.get('FX_STOP') == '4':
                return
            cumf = k.sb("cumf", [4, T_])
            c1f = k.sb("c1f", [4, T_])
            cb = [k.sb(f"cb{i}", [4, T_], BF16) for i in range(3)]
            for g in range(4):
                ps = psb("m")
                for bb in range(4):
                    B = 4 * g + bb
                    mm(ps[0:4, 128 * bb:128 * bb + 128], negcum[:, B, :], identF, True, True,
                       reads=[negcum, constF], writes=[ps])
                k.op("act", lambda: nc.scalar.mul(out=cumf[:, 512 * g:512 * g + 512], in_=ps[0:4, :], mul=-1.0),
                     reads=[ps], writes=[cumf])
            cp("dve", cb[0][:], cumf[:], reads=[cumf], writes=[cb[0]])
            cp("dve", c1f[:], cb[0][:], reads=[cb[0]], writes=[c1f])
            tt("dve", cumf[:], cumf[:], c1f[:], ALU.subtract, reads=[cumf, c1f], writes=[cumf])
            cp("dve", cb[1][:], cumf[:], reads=[cumf], writes=[cb[1]])
            cp("dve", c1f[:], cb[1][:], reads=[cb[1]], writes=[c1f])
            tt("dve", cumf[:], cumf[:], c1f[:], ALU.subtract, reads=[cumf, c1f], writes=[cumf])
            cp("dve", cb[2][:], cumf[:], reads=[cumf], writes=[cb[2]])
            for i in range(3):
                k.dma("sp", QT[64 + i:65 + i, :, :], cb[i][:], reads=[cb[i]], writes=[QT])
            if os.environ.get('FX_STOP') == '5':
                return
            def mk_stream(si, heads):
                pTs = [k.sb(f"fxpT{si}_{i}", [128, 512], BF16) for i in range(4)]
                o_sbs = [k.sb(f"fxo{si}_{i}", [128, 4, 64]) for i in range(2)]
                rds = [k.sb(f"fxrd{si}_{i}", [128, 4]) for i in range(2)]
                tmps = [fin_tmp() for i in range(2)]
                fc = [0]

                def masks(h, qb, kb):
                    if kb >= 4 * qb:
                        r = kb - 4 * qb
                        return [(identB, strips[:, 0, 384 - 128 * r:384 - 128 * r + 512], [constB, strips])]
                    return []

                def fin(h, qb, acc, accv):
                    o_sb, rd, tmp = o_sbs[fc[0] % 2], rds[fc[0] % 2], tmps[fc[0] % 2]
                    fc[0] += 1
                    recip(rd[:], accv[:, :, 64], reads=[acc], writes=[rd])
                    tt("dve", o_sb[:], accv[:, :, 0:64], rd[:].unsqueeze(2).to_broadcast([128, 4, 64]), ALU.mult,
                       reads=[acc, rd], writes=[o_sb])
                    finalize(o_sb[:], o_sb, ytok[:, 4 * qb:4 * qb + 4, 256 + 64 * h:256 + 64 * h + 64], tmp)

                return attn_stream(
                    [(h, qb) for h in heads for qb in range(4)],
                    [PS[0], PS[1], PS[2], PS[3]], [PS[4], PS[5]], pTs,
                    lambda h, kb: KT[0:67, h, kb * 128:(kb + 1) * 128],
                    lambda h, qb: QT[0:67, h, qb * 512:(qb + 1) * 512], [KT, QT],
                    masks, lambda h, kb: negcum[:, kb, h:h + 1], [negcum],
                    lambda h, kb: V[:, kb, h, :], [V],
                    lambda qb: range(0, 4 * qb + 4), lambda qb, qs: (0, 4 * qb + qs), 65, fin)

            run_streams([mk_stream(0, (0, 1, 2, 3))])


    def phase_sb(s, l):
        with k.scope():
            QT = k.sb("sbQT", [64, 4, T_], BF16)
            KT = k.sb("sbKT", [64, 4, T_], BF16)
            V = k.sb("sbV", [128, 16, 256], BF16)
            wq_, wk_, wv_ = get_w(s, l, "sb")
            for (w, dst, scale) in ((wq_, QT, 0.125), (wk_, KT, None)):
                for pr in range(2):
                    for tb in range(4):
                        ps = psb("m")
                        proj_fm(w, pr * 128, 128, tb, ps)
                        sl = slice(tb * 512, (tb + 1) * 512)
                        for hh in range(2):
                            if scale is None:
                                cp("act", dst[0:64, 2 * pr + hh, sl], ps[64 * hh:64 * hh + 64, :], reads=[ps], writes=[dst])
                            else:
                                k.op("act", lambda: nc.scalar.mul(out=dst[0:64, 2 * pr + hh, sl],
                                                                  in_=ps[64 * hh:64 * hh + 64, :], mul=scale),
                                     reads=[ps], writes=[dst])
            w = wv_
            for tB in range(16):
                ps = psb("m")
                proj_tm(w, 0, 256, tB, ps)
                cp("act", V[:, tB, :], ps[:, 0:256], reads=[ps], writes=[V])
            def sb_stream(si, heads):
                es = [k.sb(f"sbe{si}_{i}", [128, 512]) for i in range(2)]
                sps = [k.sb(f"sbsp{si}_{i}", [128, 512], BF16) for i in range(2)]
                er_ = k.sb(f"sber{si}", [128, 512])
                as_ = [k.sb(f"sbaT{si}_{i}", [128, 512], BF16) for i in range(2)]
                o = k.sb(f"sbo{si}", [128, 4, 64])
                tmp = fin_tmp()
                zb = [PS[3 * si], PS[3 * si + 1]]
                psR = PS[3 * si + 2]
                acc = PS[6]
                accv = acc[:, 256 * si:256 * si + 256].rearrange("p (q d) -> p q d", q=4)
                tiles = []
                for h in heads:
                    for qb in range(4):
                        nkb = 4 * qb + 4
                        for idx, kb in enumerate(reversed(range(nkb))):
                            tiles.append((h, qb, kb, idx, nkb))
                n = len(tiles)

                def stage1(i):
                    h, qb, kb, idx, nkb = tiles[i]
                    ps = zb[i % 2]
                    diag = kb >= 4 * qb
                    mm(ps[:], KT[0:64, h, kb * 128:(kb + 1) * 128], QT[0:64, h, qb * 512:(qb + 1) * 512],
                       True, not diag, reads=[KT, QT], writes=[ps])
                    if diag:
                        r = kb - 4 * qb
                        mm(ps[:], identB, strips[:, 1, 384 - 128 * r:384 - 128 * r + 512], False, True,
                           reads=[constB, strips], writes=[ps])

                def stage2(i):
                    ps, e, sp_ = zb[i % 2], es[i % 2], sps[i % 2]
                    act(e[:], ps[:], AF.Exp, reads=[ps], writes=[e])
                    act(sp_[:], e[:], AF.Ln, reads=[e], writes=[sp_], bias=1.0)

                stage1(0)
                stage2(0)
                for i, (h, qb, kb, idx, nkb) in enumerate(tiles):
                    e, sp_, a_ = es[i % 2], sps[i % 2], as_[i % 2]
                    if i + 1 < n:
                        stage1(i + 1)
                    mm(psR[:], negtriB, sp_[:], idx == 0, False, reads=[constB, sp_], writes=[psR])
                    if idx == 0:
                        memset("dve", accv, 0.0, writes=[acc])
                    yield
                    act(er_[:], psR[:], AF.Exp, reads=[psR], writes=[er_])
                    if i + 1 < n:
                        stage2(i + 1)
                    tt("dve", a_[:], e[:], er_[:], ALU.mult, reads=[e, er_], writes=[a_])
                    mm(psR[:], neglowB, sp_[:], False, idx == nkb - 1, reads=[constB, sp_], writes=[psR])
                    yield
                    for qs in range(4):
                        if kb > 4 * qb + qs:
                            continue
                        mm(accv[:, qs, :], a_[:, qs * 128:(qs + 1) * 128], V[:, kb, 64 * h:64 * h + 64],
                           False, kb == 0, reads=[a_, V], writes=[acc])
                    if idx == nkb - 1:
                        cp("dve", o[:], accv, reads=[acc], writes=[o])
                        finalize(o[:], o, ytok[:, 4 * qb:4 * qb + 4, 512 + 64 * h:512 + 64 * h + 64], tmp)
                    yield

            run_streams([sb_stream(0, (0, 1)), sb_stream(1, (2, 3))])


    def phase_hgrn(s, l):
        with k.scope():
            omlb = k.sb("omlb", [128, 256])
            omlT = k.sb("omlT", [128, 2])
            if l == 0:
                memset("pool", omlb[:], 1.0, writes=[omlb])
                memset("pool", omlT[:], 1.0, writes=[omlT])
            else:
                with k.scope():
                    lbb = k.sb("lbb", [128, 2, 256])
                    lblT = k.sb("lblT", [128, 2, 2])
                    k.dma("sp", lbb[:], lbl_d[:].unsqueeze(0).to_broadcast([128, 2, 256]), writes=[lbb])
                    k.dma("sp", lblT[:], lblT_d[:], writes=[lblT])
                    tt("dve", omlb[:], lbb[:, 0, :], lbb[:, 1, :], ALU.subtract, reads=[lbb], writes=[omlb])
                    act(omlb[:], omlb[:], AF.Sigmoid, reads=[omlb], writes=[omlb])
                    tt("dve", omlT[:], lblT[:, :, 0], lblT[:, :, 1], ALU.subtract, reads=[lblT], writes=[omlT])
                    act(omlT[:], omlT[:], AF.Sigmoid, reads=[omlT], writes=[omlT])
            big1 = k.sb("hgbig1", [128, 4096])
            gtok = k.sb("hggtok", [128, 16, 256])
            vtok = k.sb("hgvtok", [128, 16, 256], BF16)
            khat = k.sb("hgkhat", [128, 16, 256], BF16)
            ktok = big1[:].rearrange("p (b c) -> p b c", c=256)
            w, w2 = get_w(s, l, "hg")
            for tB in range(16):
                ps = psb("m")
                proj_tm(w, 0, 512, tB, ps)
                act(ktok[:, tB, :], ps[:, 0:256], AF.Sigmoid, reads=[ps], writes=[big1], scale=-1.0)
                cp("dve", vtok[:, tB, :], ps[:, 256:512], reads=[ps], writes=[vtok])
            for tB in range(16):
                tt("dve", ktok[:, tB, :], ktok[:, tB, :], omlb[:], ALU.mult, reads=[big1, omlb], writes=[big1])
            act(gtok[:], ktok, AF.Ln, reads=[big1], writes=[gtok], bias=1.0, scale=-1.0)
            ebs = [k.sb(f"hgebs{i}", [128, 256]) for i in range(1)]
            for tB in range(16):
                ps = psb("m")
                mm(ps[:, 0:256], trisufF, gtok[:, tB, :], True, True, reads=[constF, gtok], writes=[ps])
                eb = ebs[0]
                act(eb[:], ps[:, 0:256], AF.Exp, reads=[ps], writes=[eb])
                tt("dve", khat[:, tB, :], ktok[:, tB, :], eb[:], ALU.mult, reads=[big1, eb], writes=[khat])
            if os.environ.get('HG_STOP') == '1':
                return
            qsT = k.sb("hgqsT", [128, 2, T_])
            kT = big1[:].rearrange("p (a t) -> p a t", a=2)
            for pr in range(2):
                for tb in range(4):
                    sl = slice(tb * 512, (tb + 1) * 512)
                    ps = psb("m")
                    proj_fm(w2, pr * 128, 128, tb, ps)
                    act(qsT[:, pr, sl], ps[:], AF.Silu, reads=[ps], writes=[qsT])
                    ps = psb("m")
                    proj_fm(w2, 256 + pr * 128, 128, tb, ps, extra_reads=[khat])
                    act(kT[:, pr, sl], ps[:], AF.Sigmoid, reads=[ps, khat], writes=[big1], scale=-1.0)
                    ts("dve", kT[:, pr, sl], kT[:, pr, sl], omlT[:, pr:pr + 1], None, ALU.mult, None,
                       reads=[big1, omlT], writes=[big1])
            qtT = k.sb("hgqtT", [128, 2, T_], BF16)
            ktT = k.sb("hgktT", [128, 2, T_], BF16)
            dl = k.sb("hgdl", [128, 2, 64])
            e1s = [k.sb(f"hge1{i}", [128, 512]) for i in range(1)]
            e2s = [k.sb(f"hge2{i}", [128, 512]) for i in range(1)]
            it = 0
            for pr in range(2):
                for g4 in range(4):
                    sl = slice(g4 * 512, (g4 + 1) * 512)
                    ps = psb("a")
                    for bb in range(4):
                        tB = 4 * g4 + bb
                        mm(ps[:, 128 * bb:128 * bb + 128], gtok[:, tB, pr * 128:(pr + 1) * 128], tribdF, True, True,
                           reads=[gtok, constF], writes=[ps])
                    e1, e2 = e1s[0], e2s[0]
                    it += 1
                    act(e1[:], ps[:], AF.Exp, reads=[ps], writes=[e1])
                    act(e2[:], ps[:], AF.Exp, reads=[ps], writes=[e2], scale=-1.0)
                    tt("dve", qtT[:, pr, sl], qsT[:, pr, sl], e1[:], ALU.mult, reads=[qsT, e1], writes=[qtT])
                    tt("dve", ktT[:, pr, sl], kT[:, pr, sl], e2[:], ALU.mult, reads=[big1, e2], writes=[ktT])
                    cp("pool", dl[:, pr, 16 * g4:16 * g4 + 16], e1[:, 31:512:32], reads=[e1], writes=[dl])
            if os.environ.get('HG_STOP') == '2':
                return
            Srun = [k.sb(f"hgS{i}", [128, 5, 2, 64]) for i in range(2)]
            Sbf = [k.sb(f"hgSb{i}", [128, 4, 2, 64], BF16) for i in range(2)]
            Abd = [k.sb(f"hgA{i}", [128, 4, 128], BF16) for i in range(2)]
            vbd = [k.sb(f"hgvbd{i}", [128, 4, 4, 64], BF16) for i in range(2)]
            o_sb = [k.sb(f"hgo{i}", [128, 4, 64]) for i in range(1)]
            tmp = fin_tmp()
            memset("pool", Srun[1][:, 4, :, :], 0.0, writes=[Srun[1]])
            def hg_state(B):
                cur, prev = Srun[B % 2], Srun[(B + 1) % 2]
                vb = vbd[B % 2]
                tt("dve", vb[:], vtok[:, B, :].rearrange("p (h d) -> p h d", h=4).unsqueeze(2).to_broadcast([128, 4, 4, 64]),
                   maskbdF[:, 31:128:32].unsqueeze(1).unsqueeze(3).to_broadcast([128, 4, 4, 64]), ALU.mult,
                   reads=[vtok, constF], writes=[vb])
                psD = psb("a")
                for h in range(4):
                    pr, r0 = h // 2, 64 * (h % 2)
                    mm(psD[r0:r0 + 64, pr * 256:pr * 256 + 256], khat[:, B, 64 * h:64 * h + 64],
                       vb[:, h, :, :].rearrange("p c d -> p (c d)"), True, True,
                       reads=[khat, vb], writes=[psD], tp=(0, r0), ser=True)
                cp("pool", cur[:, 0, :, :], prev[:, 4, :, :], reads=[prev], writes=[cur])
                for c in range(4):
                    for pr in range(2):
                        col = (pr * 4 + c) * 64
                        stt("dve", cur[:, c + 1, pr, :], cur[:, c, pr, :], dl[:, pr, 4 * B + c:4 * B + c + 1],
                            psD[:, col:col + 64], ALU.mult, ALU.add, reads=[cur, dl, psD], writes=[cur])
                Sb = Sbf[B % 2]
                cp("act", Sb[:], cur[:, 0:4, :, :], reads=[cur], writes=[Sb])

            def hg_output(B):
                bsl = slice(B * 128, (B + 1) * 128)
                Sb = Sbf[B % 2]
                psA = psb("s")
                for h in (0, 2, 1, 3):
                    pr, r0 = h // 2, 64 * (h % 2)
                    mm(psA[:, 128 * h:128 * h + 128], ktT[r0:r0 + 64, pr, bsl], qtT[r0:r0 + 64, pr, bsl], True, True,
                       reads=[ktT, qtT], writes=[psA], ser=(h in (0, 1)))
                A = Abd[B % 2]
                tt("dve", A[:], psA[:].rearrange("p (h t) -> p h t", h=4),
                   maskbdF.unsqueeze(1).to_broadcast([128, 4, 128]), ALU.mult, reads=[psA, constF], writes=[A])
                psO = psb("o")
                for h in range(4):
                    mm(psO[:, 64 * h:64 * h + 64], A[:, h, :], vtok[:, B, 64 * h:64 * h + 64], h == 0, False,
                       reads=[A, vtok], writes=[psO], ser=(h == 0))
                for h in (0, 2, 1, 3):
                    pr, r0 = h // 2, 64 * (h % 2)
                    for c in range(4):
                        mm(psO[32 * c:32 * c + 32, 64 * h:64 * h + 64],
                           qtT[r0:r0 + 64, pr, B * 128 + 32 * c:B * 128 + 32 * c + 32], Sb[r0:r0 + 64, c, pr, :],
                           False, h == 3 and c == 3, reads=[qtT, Sb], writes=[psO], tp=(r0, 32 * c),
                           ser=(c == 0 and h in (0, 1)))
                o = o_sb[0]
                cp("dve", o[:], psO[:, 0:256].rearrange("p (h d) -> p h d", h=4), reads=[psO], writes=[o])
                finalize(o[:], o, ytok[:, B, 0:256].rearrange("p (h d) -> p h d", h=4), tmp)

            hg_state(0)
            for B in range(16):
                if B + 1 < 16:
                    hg_state(B + 1)
                hg_output(B)

    def phase_nsa(s, l):
        with k.scope():
            ovl = k.sb("ovl", [128, 33], BF16)
            for dst, src in ((ovl, ovl_d),):
                k.dma("sp", dst[:], src[:], writes=[dst])
            qT = k.sb("nsqT", [96, 4, T_], BF16)
            ksT = k.sb("nsksT", [96, T_], BF16)
            k.dma("sp", ksT[64:96, :], eall_d[:], writes=[ksT])
            kwT = k.sb("nskwT", [64, T_], BF16)
            kcT = k.sb("nskcT", [64, T_], BF16)
            vcT = k.sb("nsvcT", [64, T_], BF16)
            Vs = k.sb("nsVs", [128, 16, 65], BF16)
            Vw = k.sb("nsVw", [128, 16, 65], BF16)
            gtok = k.sb("nsg", [128, 16, 12])
            memset("pool", Vs[:, :, 64:65], 1.0, writes=[Vs])
            memset("pool", Vw[:, :, 64:65], 1.0, writes=[Vw])
            kcmpT = k.sb("nskcmpT", [64, 128], BF16)
            rhsc = k.sb("nsrhsc", [128, 97], BF16)
            with k.scope():
                ropeC = k.sb("ropeC", [16, T_])
                ropeS = k.sb("ropeS", [16, T_])
                ropeCc = k.sb("ropeCc", [16, 128])
                ropeSc = k.sb("ropeSc", [16, 128])
                permF = k.sb("permF", [64, 16])
                for dst, src in ((ropeC, ropeC_d), (ropeS, ropeS_d), (ropeCc, ropeCc_d), (ropeSc, ropeSc_d),
                                 (permF, permF_d)):
                    k.dma("sp", dst[:], src[:], writes=[dst])
                q32s = [k.sb(f"nsq32{i}", [64, 512]) for i in range(2)]
                t1s = [k.sb(f"nst1{i}", [16, 512]) for i in range(2)]
                t2s = [k.sb(f"nst2{i}", [16, 512]) for i in range(2)]
                rc = [0]

                def rope_evac(src, dst, n, scale, Ct, St, extra_tok):
                    i = rc[0] % 2
                    rc[0] += 1
                    q32, t1, t2 = q32s[i], t1s[i], t2s[i]
                    k.op("act", lambda: nc.scalar.mul(out=q32[:, 0:n], in_=src, mul=scale),
                         reads=[extra_tok["src"]], writes=[q32])
                    cp("act", dst, q32[:, 0:n], reads=[q32], writes=[extra_tok["dst"]])
                    psw = psb("a")
                    mm(psw[0:16, 0:n], permF[:, :], q32[:, 0:n], True, True, reads=[permF, q32], writes=[psw])
                    tt("dve", t1[:, 0:n], q32[0:16, 0:n], Ct, ALU.mult, reads=[q32, extra_tok["tab"]], writes=[t1])
                    tt("dve", t2[:, 0:n], psw[0:16, 0:n], St, ALU.mult, reads=[psw, extra_tok["tab"]], writes=[t2])
                    tt("dve", extra_tok["dst16"], t1[:, 0:n], t2[:, 0:n], ALU.add,
                       reads=[t1, t2], writes=[extra_tok["dst"]])

                (w,) = get_w(s, l, "ns")
                cmpW = {}
                for kv in "kv":
                    W1 = k.sb(f"nsW1{kv}", [64, 32, 64], BF16)
                    W2 = k.sb(f"nsW2{kv}", [64, 64], BF16)
                    peT = k.sb(f"nspeT{kv}", [64, 32], BF16)
                    k.dma("pool", W1[:], w1_d[kv][l].rearrange("(lp d) j -> d lp j", d=64), writes=[W1])
                    k.dma("pool", W2[:], w2_d[kv][l], writes=[W2])
                    k.dma("pool", peT[:], peT_d[kv][l], writes=[peT])
                    cmpW[kv] = (W1, W2, peT)
                for pr in range(2):
                    for tb in range(4):
                        sl = slice(tb * 512, (tb + 1) * 512)
                        ps = psb("m")
                        proj_fm(w, pr * 128, 128, tb, ps)
                        for hh in range(2):
                            h = 2 * pr + hh
                            rope_evac(ps[64 * hh:64 * hh + 64, :], qT[0:64, h, sl], 512, 0.125, ropeC[:, sl], ropeS[:, sl],
                                      dict(src=ps, dst=qT, tab=ropeC, dst16=qT[0:16, h, sl]))
                for tb in range(4):
                    sl = slice(tb * 512, (tb + 1) * 512)
                    ps = psb("m")
                    proj_fm(w, 256, 128, tb, ps)
                    cp("act", kcT[:, sl], ps[0:64, :], reads=[ps], writes=[kcT])
                    cp("act", vcT[:, sl], ps[64:128, :], reads=[ps], writes=[vcT])
                for off, dst in ((384, ksT), (512, kwT)):
                    for tb in range(4):
                        sl = slice(tb * 512, (tb + 1) * 512)
                        ps = psb("m")
                        proj_fm(w, off, 64, tb, ps)
                        rope_evac(ps[0:64, :], dst[0:64, sl], 512, 1.0, ropeC[:, sl], ropeS[:, sl],
                                  dict(src=ps, dst=dst, tab=ropeC, dst16=dst[0:16, sl]))
                for tB in range(16):
                    ps = psb("m")
                    proj_tm(w, 448, 204, tB, ps)
                    cp("act", Vs[:, tB, 0:64], ps[:, 0:64], reads=[ps], writes=[Vs])
                    cp("act", Vw[:, tB, 0:64], ps[:, 128:192], reads=[ps], writes=[Vw])
                    act(gtok[:, tB, :], ps[:, 192:204], AF.Sigmoid, reads=[ps], writes=[gtok])
                memset("pool", kcmpT[:], 0.0, writes=[kcmpT])
                memset("pool", rhsc[:], 0.0, writes=[rhsc])
                cp("pool", rhsc[:, 64:97], ovl[:], reads=[ovl], writes=[rhsc])
                for kv, srcT in (("k", kcT), ("v", vcT)):
                    W1, W2, peT = cmpW[kv]
                    psH = psb("a")
                    for lp in range(32):
                        mm(psH[0:64, 0:127], W1[:, lp, :], srcT[:, lp:lp + 16 * 126 + 1:16], lp == 0, lp == 31,
                           reads=[W1, srcT], writes=[psH])
                    for lp in range(32):
                        mm(psH[0:64, 127:128], W1[:, lp, :], peT[:, lp:lp + 1], lp == 0, lp == 31,
                           reads=[W1, peT], writes=[psH])
                    bias = k.sb(f"nsbias{kv}", [64, 1])
                    hid = k.sb(f"nshid{kv}", [64, 128], BF16)
                    cp("dve", bias[:], psH[0:64, 127:128], reads=[psH], writes=[bias])
                    act(hid[:, 0:127], psH[0:64, 0:127], AF.Silu, reads=[psH, bias], writes=[hid], bias=bias[:, 0:1])
                    ps2 = psb("m")
                    if kv == "k":
                        mm(ps2[0:64, 0:127], W2[:, :], hid[:, 0:127], True, True, reads=[W2, hid], writes=[ps2])
                        rope_evac(ps2[0:64, 0:127], kcmpT[0:64, 0:127], 127, 1.0, ropeCc[:, 0:127], ropeSc[:, 0:127],
                                  dict(src=ps2, dst=kcmpT, tab=ropeCc, dst16=kcmpT[0:16, 0:127]))
                    else:
                        mm(ps2[0:127, 0:64], hid[:, 0:127], W2[:, :], True, True, reads=[W2, hid], writes=[ps2])
                        cp("act", rhsc[0:127, 0:64], ps2[0:127, 0:64], reads=[ps2], writes=[rhsc])
            cmask = k.sb("cmask", [128, T_], BF16)
            tkA = k.sb("tkA", [128, 16, 32])
            tkB = k.sb("tkB", [128, 16, 32])
            nacc = k.sb("nsacc", [128, 16, 256])
            imp = k.sb("nsimp", [128, 16, 32])
            s01 = k.sb("nss01", [128, 2, 896], BF16)
            for dst, src in ((cmask, cmask_d), (tkA, tkA_d), (tkB, tkB_d), (s01, strips01_d)):
                k.dma("sp", dst[:], src[:], writes=[dst])
            pT = [k.sb(f"nspT{i}", [128, 512], BF16) for i in range(3)]
            rden = [k.sb(f"nsrd{i}", [128, 4]) for i in range(2)]
            cf = [k.sb(f"nscf{i}", [128, 4]) for i in range(2)]
            tmpo = [k.sb(f"nstmpo{i}", [128, 4, 64]) for i in range(2)]
            tmpi = [k.sb(f"nstmpi{i}", [128, 4, 32]) for i in range(2)]
            it = 0
            fi = 0
            for h in range(4):
                for qb in range(4):
                    qsl = slice(qb * 512, (qb + 1) * 512)
                    ps = psb("s")
                    mm(ps[:], kcmpT[:, :], qT[0:64, h, qsl], True, False, reads=[kcmpT, qT], writes=[ps])
                    mm(ps[:], identB, cmask[:, qsl], False, True, reads=[constB, cmask], writes=[ps])
                    p = pT[it % 3]
                    it += 1
                    act(p[:], ps[:], AF.Exp, reads=[ps], writes=[p])
                    acc = psb("o")
                    accv = acc[:, 0:388].rearrange("p (q d) -> p q d", q=4)
                    for qs in range(4):
                        mm(accv[:, qs, :], p[:, qs * 128:(qs + 1) * 128], rhsc[:, :], qs == 0, True,
                           reads=[p, rhsc], writes=[acc])
                    rd, c_ = rden[fi % 2], cf[fi % 2]
                    to, ti = tmpo[fi % 2], tmpi[fi % 2]
                    fi += 1
                    ts("dve", rd[:], accv[:, :, 64], 1e-30, None, ALU.max, None, reads=[acc], writes=[rd])
                    recip(rd[:], rd[:], reads=[rd], writes=[rd])
                    tt("dve", c_[:], rd[:], gtok[:, 4 * qb:4 * qb + 4, h], ALU.mult, reads=[rd, gtok], writes=[c_])
                    tt("dve", nacc[:, 4 * qb:4 * qb + 4, 64 * h:64 * h + 64], accv[:, :, 0:64],
                       c_[:].unsqueeze(2).to_broadcast([128, 4, 64]), ALU.mult, reads=[acc, c_], writes=[nacc])
                    if h == 0:
                        tt("dve", imp[:, 4 * qb:4 * qb + 4, :], accv[:, :, 65:97],
                           rd[:].unsqueeze(2).to_broadcast([128, 4, 32]), ALU.mult, reads=[acc, rd], writes=[imp])
                    else:
                        tt("dve", ti[:], accv[:, :, 65:97], rd[:].unsqueeze(2).to_broadcast([128, 4, 32]), ALU.mult,
                           reads=[acc, rd], writes=[ti])
                        tt("pool", imp[:, 4 * qb:4 * qb + 4, :], imp[:, 4 * qb:4 * qb + 4, :], ti[:], ALU.add,
                           reads=[imp, ti], writes=[imp])
            score = k.sb("nsscore", [128, 16, 32])
            negm = k.sb("nsnegm", [128, 16, 32])
            mx = [k.sb(f"nsmx{i}", [128, 8]) for i in range(2)]
            sc2 = k.sb("nssc2", [128, 32])
            tt("dve", score[:], imp[:], tkA[:], ALU.mult, reads=[imp, tkA], writes=[score])
            tt("dve", score[:], score[:], tkB[:], ALU.add, reads=[score, tkB], writes=[score])
            for tB in range(16):
                k.op("dve", lambda: nc.vector.max(out=mx[0][:], in_=score[:, tB, :]), reads=[score], writes=[mx[0]])
                k.op("dve", lambda: nc.vector.match_replace(out=sc2[:], in_to_replace=mx[0][:], in_values=score[:, tB, :],
                                                            imm_value=-1.0e9), reads=[score, mx[0]], writes=[sc2])
                k.op("dve", lambda: nc.vector.max(out=mx[1][:], in_=sc2[:]), reads=[sc2], writes=[mx[1]])
                ts("dve", negm[:, tB, :], score[:, tB, :], mx[1][:, 7:8], NEG, ALU.is_lt, ALU.mult,
                   reads=[score, mx[1]], writes=[negm])
            for g4 in range(4):
                ps = psb("m")
                for bb in range(4):
                    mm(ps[0:32, 128 * bb:128 * bb + 128], negm[:, 4 * g4 + bb, :], identF, True, True,
                       reads=[negm, constF], writes=[ps])
                for h in range(4):
                    cp("act" if h % 2 == 0 else "dve", qT[64:96, h, 512 * g4:512 * g4 + 512], ps[0:32, :],
                       reads=[ps], writes=[qT])

            def caus_mask(qb, kb):
                if kb >= 4 * qb:
                    r = kb - 4 * qb
                    return s01[:, 0, 384 - 128 * r:384 - 128 * r + 512]
                return None

            def win_mask(qb, kb):
                r = kb - 4 * qb
                if r >= 0:
                    return s01[:, 0, 384 - 128 * r:384 - 128 * r + 512]
                return s01[:, 1, 384 - 128 * (r + 4):384 - 128 * (r + 4) + 512]

            pTs4 = pT + [k.sb("nspT3", [128, 512], BF16)]

            def br_stream(si, bi, KTt, Vt, kb_range, mask_fn, use_sel, qs_range):
                pTs = [k.sb(f"nsbp{si}_{i}", [128, 512], BF16) for i in range(2)] if False else pTs4
                fc = [0]
                kq = 96 if use_sel else 64

                def masks(h, qb, kb):
                    return []

                def post_mask(h, qb, kb):
                    mk = mask_fn(qb, kb)
                    return None if mk is None else (mk, [s01])

                def fin(h, qb, acc, accv):
                    rd, c_, to = rden[fc[0] % 2], cf[fc[0] % 2], tmpo[fc[0] % 2]
                    fc[0] += 1
                    recip(rd[:], accv[:, :, 64], reads=[acc], writes=[rd])
                    tt("dve", c_[:], rd[:], gtok[:, 4 * qb:4 * qb + 4, 4 * bi + h], ALU.mult, reads=[rd, gtok], writes=[c_])
                    tt("dve", to[:], accv[:, :, 0:64], c_[:].unsqueeze(2).to_broadcast([128, 4, 64]), ALU.mult,
                       reads=[acc, c_], writes=[to])
                    tt("pool", nacc[:, 4 * qb:4 * qb + 4, 64 * h:64 * h + 64],
                       nacc[:, 4 * qb:4 * qb + 4, 64 * h:64 * h + 64], to[:], ALU.add, reads=[nacc, to], writes=[nacc])

                return attn_stream(
                    [(h, qb) for h in range(4) for qb in range(4)],
                    [PS[0], PS[1], PS[2], PS[3]], [PS[4], PS[5]], pTs,
                    lambda h, kb: KTt[0:kq, kb * 128:(kb + 1) * 128],
                    lambda h, qb: qT[0:kq, h, qb * 512:(qb + 1) * 512], [KTt, qT],
                    masks, None, [],
                    lambda h, kb: Vt[:, kb, :], [Vt],
                    kb_range, qs_range, 65, fin, post_mask=post_mask)

            run_streams([
                br_stream(0, 1, ksT, Vs, lambda qb: range(0, 4 * qb + 4), caus_mask, True,
                          lambda qb, qs: (0, 4 * qb + qs))])
            run_streams([
                br_stream(1, 2, kwT, Vw, lambda qb: range(max(0, 4 * qb - 4), 4 * qb + 4), win_mask, False,
                          lambda qb, qs: (max(0, 4 * qb + qs - 4), 4 * qb + qs)),
            ])
            tmp = fin_tmp(4)
            for g4 in range(4):
                for bb in range(4):
                    finalize(nacc[:, 4 * g4 + bb, :].rearrange("p (h d) -> p h d", h=4), nacc,
                             ytok[:, 4 * g4 + bb, 768:1024].rearrange("p (h d) -> p h d", h=4), tmp, n=4)

    def phase_out(s, l, last):
        xsrc = xT_d if l == 0 else xres_d
        with k.scope():
            (wg,) = get_w(s, l, "out")
            yT = k.sb("yT", [128, 8, T_], BF16)
            wo = k.sb("wo", [128, 8, D_], BF16)
            wsrc = w_out_d[l].rearrange("(j p) c -> p j c", p=128)
            k.dma("pool", wo[:, 0:4, :], wsrc[:, 0:4, :], writes=[wo])
            k.dma("pool", wo[:, 4:8, :], wsrc[:, 4:8, :], writes=[wo])
            def gate_stream(si, tBs):
                sgi = k.sb(f"sg{si}", [128, D_])
                ygi = k.sb(f"yg{si}", [128, D_], BF16)
                pa, pb = PS[2 * si], PS[2 * si + 1]
                for tB in tBs:
                    proj_tm(wg, 0, 512, tB, pa)
                    proj_tm(wg, 512, 512, tB, pb)
                    yield
                    act(sgi[:, 0:512], pa[:], AF.Silu, reads=[pa], writes=[sgi])
                    act(sgi[:, 512:1024], pb[:], AF.Silu, reads=[pb], writes=[sgi])
                    tt("dve", sgi[:], sgi[:], ong[:], ALU.mult, reads=[sgi, ong], writes=[sgi])
                    yield
                    tt("dve", ygi[:], sgi[:], ytok[:, tB, :], ALU.mult, reads=[sgi, ytok], writes=[ygi])
                    for cc in range(8):
                        k.op("pe", lambda: nc.tensor.transpose(PST[:, cc * 128:(cc + 1) * 128],
                                                               ygi[:, cc * 128:(cc + 1) * 128], identB),
                             reads=[ygi, constB], writes=[PST])
                    cp("act", yT[:, :, tB * 128:(tB + 1) * 128], PST[:].rearrange("p (c t) -> p c t", c=8),
                       reads=[PST], writes=[yT])

            with k.scope():
                ong = k.sb("ong", [128, D_])
                k.dma("sp", ong[:], ong_d[l:l + 1, :].to_broadcast([128, D_]), writes=[ong])
                run_streams([gate_stream(0, range(0, 16, 3)), gate_stream(1, range(1, 16, 3)),
                             gate_stream(2, range(2, 16, 3))])
            xin = [k.sb(f"xo{i}", [128, 8, 512]) for i in range(2)]
            if last:
                sqf = k.sb("sqf", [128, 8, 512], BF16)
                rsf = k.sb("rsf", [128, 512])
            for tb in range(4):
                sl = slice(tb * 512, (tb + 1) * 512)
                xi = xin[tb % 2]
                src = xsrc[s].rearrange("(j p) t -> p j t", p=128)[:, :, sl]
                k.dma("sp", xi[:], src, reads=[xsrc], writes=[xi])
                for dj in range(8):
                    ps = PS[(tb * 8 + dj) % 6]
                    for cc in range(8):
                        mm(ps[:], wo[:, cc, dj * 128:(dj + 1) * 128], yT[:, cc, sl], cc == 0, cc == 7,
                           reads=[wo, yT], writes=[ps])
                    tt("dve", xi[:, dj, :], xi[:, dj, :], ps[:], ALU.add, reads=[xi, ps], writes=[xi])
                if not last:
                    dst = xres_d[s].rearrange("(j p) t -> p j t", p=128)[:, :, sl]
                    k.dma("sp", dst, xi[:], reads=[xi], writes=[xres_d])
                else:
                    act(sqf[:], xi[:], AF.Square, reads=[xi], writes=[sqf])
                    ps = psb("m")
                    for j in range(8):
                        mm(ps[:], onesB, sqf[:, j, :], j == 0, j == 7, reads=[constB, sqf], writes=[ps])
                    act(rsf[:], ps[:], AF.Ln, reads=[ps], writes=[rsf], bias=EPS, scale=1.0 / D_)
                    act(rsf[:], rsf[:], AF.Exp, reads=[rsf], writes=[rsf], scale=-0.5)
                    for j in range(8):
                        stt("dve", xi[:, j, :], xi[:, j, :], fnormg[:, j:j + 1], rsf[:], ALU.mult, ALU.mult,
                            reads=[xi, rsf, fnormg], writes=[xi])
                    dst = outT_d[s].rearrange("(j p) t -> p j t", p=128)[:, :, sl]
                    k.dma("sp", dst, xi[:], reads=[xi], writes=[outT_d])

    for s in range(nseq):
        for l in range(nlayer):
            phase_norm(s, l)
            if debug and s == 0 and l == 0 and "hT" in debug:
                d = dbg_out("dbg_hT", [128, 8, T_], BF16)
                k.dma("sp", d[:], hT[:], reads=[hT], writes=[d])
            if "hg" in mixers:
                phase_hgrn(s, l)
            if "fx" in mixers:
                phase_fox(s, l)
            if "sb" in mixers:
                phase_sb(s, l)
            if "ns" in mixers:
                phase_nsa(s, l)
            if debug and s == 0 and l == nlayer - 1 and "ytok" in debug:
                d = dbg_out("dbg_ytok", [128, 16, D_], BF16)
                k.dma("sp", d[:], ytok[:], reads=[ytok], writes=[d])
            if "out" in mixers:
                phase_out(s, l, l == nlayer - 1)

    k.finish(list(dbg_outs.values()) + [outT_d])
    k.barrier()
    k.close()
    return nc, k, list(dbg_outs.keys())


def host_inputs(inputs, core, consts, nseq=SEQ_PER_CORE):
    f32 = np.float32
    x = inputs["x"]
    b0 = core * nseq
    m = {}
    m["xT"] = np.ascontiguousarray(np.transpose(x[b0:b0 + nseq], (0, 2, 1))).astype(f32)
    m["w_in"] = np.ascontiguousarray(inputs["w_in"], dtype=f32)
    m["w_out"] = np.ascontiguousarray(inputs["w_out"], dtype=f32)
    m["norm_gT"] = np.ascontiguousarray(inputs["norm_g"].reshape(2, 8, 128).transpose(0, 2, 1), dtype=f32)
    m["fnorm_gT"] = np.ascontiguousarray(inputs["final_norm_g"].reshape(8, 128).T, dtype=f32)
    m["out_norm_g"] = np.ascontiguousarray(inputs["out_norm_g"], dtype=f32)
    lbl = np.asarray(inputs["hgrn_lb_logits"], dtype=f32)
    m["lb_logits"] = np.ascontiguousarray(lbl)
    m["lb_logitsT"] = np.ascontiguousarray(lbl.reshape(2, 2, 128).transpose(2, 1, 0))
    m["fox_fb"] = np.ascontiguousarray(inputs["fox_fb"], dtype=f32)
    for kv in "kv":
        m[f"peT_{kv}"] = np.ascontiguousarray(np.transpose(inputs[f"nsa_cmp_pe_{kv}"], (0, 2, 1)), dtype=f32)
        m[f"w1_{kv}"] = np.ascontiguousarray(inputs[f"nsa_cmp_w1_{kv}"], dtype=f32)
        m[f"w2_{kv}"] = np.ascontiguousarray(inputs[f"nsa_cmp_w2_{kv}"], dtype=f32)
    for nm in ["constF", "constB", "strips", "strips01", "cmask", "ovl", "eall", "ropeC", "ropeS", "ropeCc", "ropeSc",
               "permF", "tkA", "tkB"]:
        m[nm] = consts[nm]
    return m


def kernel(**inputs):
    inputs = {kk: np.asarray(v) for kk, v in inputs.items()}
    consts = make_consts()
    nc, kb, _ = build()
    in_maps = [host_inputs(inputs, c, consts) for c in range(NCORES)]
    res = run_bass_kernel_spmd(nc, in_maps, core_ids=list(range(NCORES)))
    outs = [np.asarray(r["outT"]) for r in res.results]
    full = np.concatenate(outs, axis=0)
    return np.ascontiguousarray(np.transpose(full, (0, 2, 1))).astype(np.float32)
```

```python
import os
import numpy as np
import ml_dtypes
from contextlib import ExitStack
import concourse.bass as bass
import concourse.mybir as mybir
from concourse.bass_utils import run_bass_kernel_spmd

F32 = mybir.dt.float32
BF16 = mybir.dt.bfloat16
AF = mybir.ActivationFunctionType
ALU = mybir.AluOpType
AX = mybir.AxisListType

T_ = 2048
D_ = 1024
NIN = 3984
NEG = -30000.0
EPS = 1e-6
NCORES = 8
SEQ_PER_CORE = 4

C_HGQ, C_HGF, C_HGI = 0, 256, 512
C_FXQ, C_FXK, C_FXV, C_FXF = 768, 1024, 1280, 1536
C_SBQ, C_SBK, C_SBV = 1540, 1796, 2052
C_NSQ = 2308
C_NKC, C_NVC, C_NKS, C_NVS, C_NKW, C_NVW = 2564, 2628, 2692, 2756, 2820, 2884
C_NSG = 2948
C_GATE = 2960


class Tok:
    __slots__ = ("w", "r", "name")

    def __init__(self, name=""):
        self.w = None
        self.r = {}
        self.name = name


class T:
    def __init__(self, h, name, excl=False):
        self.h = h
        self.tok = Tok(name)
        self.name = name
        self.excl = excl

    def __getitem__(self, k):
        return self.h[k]


class WV:
    def __init__(self, t, base):
        self.t = t
        self.base = base
        self.tok = t.tok

    def __getitem__(self, key):
        p, j, sl = key
        return self.t.h[p, j, self.base + sl.start:self.base + sl.stop]


class KB:
    NDQ = 8
    EPOCH_LIMIT = 30000

    def __init__(self, nc):
        self.nc = nc
        self.stack = [ExitStack()]
        self.E = {"pe": nc.tensor, "act": nc.scalar, "dve": nc.vector,
                  "pool": nc.gpsimd, "sp": nc.sync}
        self.semh = {}
        self.cur = {}
        self.cnt = {}
        self.epoch = {}
        for e in ["pe", "act", "dve", "pool"]:
            self.epoch[e] = -1
            self._new_epoch(e)
        self.dq = {}
        for q in ["sp", "pool", "act"]:
            sems = [self.stack[0].enter_context(nc.semaphore(f"d_{q}{i}")) for i in range(self.NDQ)]
            self.dq[q] = dict(n=0)
            for i, s in enumerate(sems):
                self.semh[("dma", q, i)] = s
        self.seen = {}
        self.nins = {e: 0 for e in self.E}
        self.uid = 0

    def _new_epoch(self, e):
        self.epoch[e] += 1
        key = (e, self.epoch[e])
        self.semh[key] = self.stack[0].enter_context(self.nc.semaphore(f"s_{e}{self.epoch[e]}"))
        self.cur[e] = key
        self.cnt[e] = 0

    def scope(self):
        kb = self

        class _S:
            def __enter__(s):
                kb.stack.append(ExitStack())

            def __exit__(s, *a):
                kb.barrier()
                kb.stack.pop().close()
                return False
        return _S()

    def _nm(self, name):
        self.uid += 1
        return f"{name}_{self.uid}"

    def sb(self, name, shape, dt=F32):
        h = self.stack[-1].enter_context(self.nc.sbuf_tensor(self._nm(name), list(shape), dt))
        return T(h, name)

    def ps(self, name, shape, dt=F32):
        h = self.stack[-1].enter_context(self.nc.psum_tensor(self._nm(name), list(shape), dt))
        return T(h, name, excl=True)

    def dram(self, name, shape, dt, kind):
        h = self.nc.dram_tensor(name, list(shape), dt, kind=kind)
        return T(h, name)

    def _wait(self, eng, deps):
        for key, val in deps.items():
            if key[0] == "pe" and eng == "pe":
                continue
            sk = (eng, key)
            if self.seen.get(sk, 0) >= val:
                continue
            self.seen[sk] = val
            self.E[eng].wait_ge(self.semh[key], val)
            self.nins[eng] += 1

    @staticmethod
    def _tok(x):
        return getattr(x, "tok", x)

    def _deps(self, reads, writes):
        deps = {}
        for t in reads:
            t = self._tok(t)
            if t.w and deps.get(t.w[0], 0) < t.w[1]:
                deps[t.w[0]] = t.w[1]
        for t in writes:
            t = self._tok(t)
            if t.w and deps.get(t.w[0], 0) < t.w[1]:
                deps[t.w[0]] = t.w[1]
            for kk, v in t.r.items():
                if deps.get(kk, 0) < v:
                    deps[kk] = v
        return deps

    def _mark(self, ev, reads, writes):
        kk, v = ev
        for t in reads:
            t = self._tok(t)
            if t.r.get(kk, 0) < v:
                t.r[kk] = v
        for t in writes:
            t = self._tok(t)
            t.w = ev
            t.r = {}

    def op(self, eng, fn, reads=(), writes=()):
        ex = [t for t in reads if isinstance(t, T) and t.excl]
        if ex:
            reads = [t for t in reads if not (isinstance(t, T) and t.excl)]
            writes = list(writes) + ex
        self._wait(eng, self._deps(reads, writes))
        if self.cnt[eng] >= self.EPOCH_LIMIT:
            self._new_epoch(eng)
        ins = fn()
        self.cnt[eng] += 1
        ins.then_inc(self.semh[self.cur[eng]], 1)
        self.nins[eng] += 1
        self._mark((self.cur[eng], self.cnt[eng]), reads, writes)
        return ins

    def pe_selfwait(self):
        key, val = self.cur["pe"], self.cnt["pe"]
        if val > 0 and self.seen.get(("pe", key), 0) < val:
            self.seen[("pe", key)] = val
            self.E["pe"].wait_ge(self.semh[key], val)
            self.nins["pe"] += 1

    def dma(self, q, out, in_, reads=(), writes=(), **kw):
        self._wait(q, self._deps(reads, writes))
        d = self.dq[q]
        slot = d["n"] % self.NDQ
        val = 16 * (d["n"] // self.NDQ + 1)
        d["n"] += 1
        ins = self.E[q].dma_start(out=out, in_=in_, **kw)
        ins.then_inc(self.semh[("dma", q, slot)], 16)
        self.nins[q] += 1
        self._mark((("dma", q, slot), val), reads, writes)
        return ins

    def _all_events(self):
        ev = {}
        for e in ["pe", "act", "dve", "pool"]:
            if self.cnt[e] > 0:
                ev[self.cur[e]] = self.cnt[e]
        for q, d in self.dq.items():
            n = d["n"]
            for slot in range(self.NDQ):
                c = (n - slot + self.NDQ - 1) // self.NDQ
                if c > 0:
                    ev[("dma", q, slot)] = 16 * c
        return ev

    def barrier(self):
        ev = self._all_events()
        for e in ["pe", "act", "dve", "pool", "sp"]:
            self._wait(e, {kk: v for kk, v in ev.items() if not (kk[0] == e)})

    def finish(self, toks, eng="sp"):
        deps = {}
        for t in toks:
            t = self._tok(t)
            if t.w and deps.get(t.w[0], 0) < t.w[1]:
                deps[t.w[0]] = t.w[1]
        self._wait(eng, deps)

    def close(self):
        while self.stack:
            self.stack.pop().close()


def _bf(a):
    return np.asarray(a, dtype=np.float32).astype(ml_dtypes.bfloat16)


def make_consts():
    c = {}
    i = np.arange(128)[:, None]
    j = np.arange(128)[None, :]
    c["identF"] = np.eye(128, dtype=np.float32)
    c["triF"] = (i <= j).astype(np.float32)
    c["onesF"] = np.ones((128, 128), np.float32)
    c["tribdF"] = ((i // 32 == j // 32) & (i <= j)).astype(np.float32)
    c["trisufF"] = ((i // 32 == j // 32) & (i > j)).astype(np.float32)
    c["maskbdF"] = ((i // 32 == j // 32) & (i <= j)).astype(np.float32)
    perm = np.zeros((64, 16), np.float32)
    for a in range(16):
        perm[a + 8 if a < 8 else a - 8, a] = 1.0
    c["permF"] = perm
    cf = np.concatenate([c["identF"], c["triF"], c["onesF"], c["tribdF"], c["trisufF"], c["maskbdF"]], axis=1)
    c["constF"] = cf
    negtri = -(i >= j).astype(np.float32)
    neglow = -(i < j).astype(np.float32)
    c["constB"] = _bf(np.concatenate([np.eye(128), np.ones((128, 128)), negtri, neglow], axis=1))
    jj = np.arange(896)[None, :] - 384
    caus = np.where(jj < 0, NEG, np.where(jj >= 128, 0.0, np.where(jj >= i, 0.0, NEG)))
    strict = np.where(jj < 0, NEG, np.where(jj >= 128, 0.0, np.where(jj > i, 0.0, NEG)))
    win = np.where(jj < 0, 0.0, np.where(jj >= 128, NEG, np.where(jj < i, 0.0, NEG)))
    c["strips"] = _bf(np.stack([caus, strict, win], axis=1))
    c["strips01"] = _bf(np.stack([(caus == 0.0), (win == 0.0)], axis=1).astype(np.float32))
    n = np.arange(128)[:, None]
    t = np.arange(T_)[None, :]
    c["cmask"] = _bf(np.where((16 * n + 31 <= t) & (n < 127), 0.0, NEG))
    cs = (np.arange(128) * 16)[:, None]
    ss = (np.arange(32) * 64)[None, :]
    ov = np.clip(np.minimum(cs + 32, ss + 64) - np.maximum(cs, ss), 0, None).astype(np.float32) / 32.0
    ovl = np.concatenate([np.ones((128, 1), np.float32), ov], axis=1)
    ovl[127] = 0.0
    c["ovl"] = _bf(ovl)
    c["eall"] = _bf((np.arange(T_)[None, :] // 64 == np.arange(32)[:, None]).astype(np.float32))
    half = 8
    inv = (500000.0 ** (-(np.arange(half, dtype=np.float32) * 2.0 / 16.0))).astype(np.float32)

    def tabs(pos):
        ang = pos.astype(np.float32)[None, :] * inv[:, None]
        cos, sin = np.cos(ang), np.sin(ang)
        return (np.concatenate([cos, cos], 0).astype(np.float32),
                np.concatenate([-sin, sin], 0).astype(np.float32))
    C, S = tabs(np.arange(T_))
    c["ropeC"], c["ropeS"] = C, S
    pc = np.arange(128) * 16 + 31
    Cc, Sc = tabs(pc)
    c["ropeCc"], c["ropeSc"] = Cc, Sc
    tt_ = np.arange(T_)
    qblk = tt_ // 64
    blk = np.arange(32)[None, :]
    forced = (blk == 0) | (blk == qblk[:, None]) | (blk == qblk[:, None] - 1)
    valid = blk <= qblk[:, None]
    A = (~forced & valid).astype(np.float32)
    Bc = np.where(forced, 1.0e4, np.where(valid, 0.0, -1.0e4)).astype(np.float32)
    c["tkA"] = A.reshape(16, 128, 32).transpose(1, 0, 2).copy()
    c["tkB"] = Bc.reshape(16, 128, 32).transpose(1, 0, 2).copy()
    return c


def build(nseq=SEQ_PER_CORE, nlayer=2, mixers=("hg", "fx", "sb", "ns", "out"), debug=None):
    nc = bass.Bass("TRN2", target_bir_lowering=False)
    k = KB(nc)
    dbg_outs = {}

    def din(name, shape, dt=F32):
        return k.dram(name, shape, dt, "ExternalInput")

    xT_d = din("xT", [nseq, D_, T_])
    w_in_d = din("w_in", [2, D_, NIN])
    w_out_d = din("w_out", [2, D_, D_])
    normg_d = din("norm_gT", [2, 128, 8])
    fnormg_d = din("fnorm_gT", [128, 8])
    ong_d = din("out_norm_g", [2, D_])
    lbl_d = din("lb_logits", [2, 256])
    lblT_d = din("lb_logitsT", [128, 2, 2])
    fb_d = din("fox_fb", [2, 4])
    peT_d = {kv: din(f"peT_{kv}", [2, 64, 32]) for kv in "kv"}
    w1_d = {kv: din(f"w1_{kv}", [2, 2048, 64]) for kv in "kv"}
    w2_d = {kv: din(f"w2_{kv}", [2, 64, 64]) for kv in "kv"}
    constF_d = din("constF", [128, 768])
    constB_d = din("constB", [128, 512], BF16)
    strips_d = din("strips", [128, 3, 896], BF16)
    strips01_d = din("strips01", [128, 2, 896], BF16)
    cmask_d = din("cmask", [128, T_], BF16)
    ovl_d = din("ovl", [128, 33], BF16)
    eall_d = din("eall", [32, T_], BF16)
    ropeC_d = din("ropeC", [16, T_])
    ropeS_d = din("ropeS", [16, T_])
    ropeCc_d = din("ropeCc", [16, 128])
    ropeSc_d = din("ropeSc", [16, 128])
    permF_d = din("permF", [64, 16])
    tkA_d = din("tkA", [128, 16, 32])
    tkB_d = din("tkB", [128, 16, 32])
    outT_d = k.dram("outT", [nseq, D_, T_], F32, "ExternalOutput")
    xres_d = k.dram("xres", [nseq, D_, T_], F32, "Internal")

    def dbg_out(name, shape, dt=F32):
        d = k.dram(name, shape, dt, "ExternalOutput")
        dbg_outs[name] = d
        return d

    constF = k.sb("constF", [128, 768])
    constB = k.sb("constB", [128, 512], BF16)
    strips = k.sb("strips", [128, 3, 896], BF16)
    k.dma("sp", constF[:], constF_d[:], writes=[constF])
    k.dma("sp", constB[:], constB_d[:], writes=[constB])
    k.dma("sp", strips[:], strips_d[:], writes=[strips])
    identF = constF[:, 0:128]
    triF = constF[:, 128:256]
    onesF = constF[:, 256:384]
    tribdF = constF[:, 384:512]
    trisufF = constF[:, 512:640]
    maskbdF = constF[:, 640:768]
    identB = constB[:, 0:128]
    onesB = constB[:, 128:256]
    negtriB = constB[:, 256:384]
    neglowB = constB[:, 384:512]

    hT = k.sb("hT", [128, 8, T_], BF16)
    ytok = k.sb("ytok", [128, 16, D_], BF16)
    normg = k.sb("normg", [128, 2, 8])
    fnormg = k.sb("fnormg", [128, 8])
    for l in range(2):
        k.dma("sp", normg[:, l, :], normg_d[l], writes=[normg])
    k.dma("sp", fnormg[:], fnormg_d[:], writes=[fnormg])

    PS = [k.ps(f"ps{i}", [128, 512]) for i in range(7)]
    PST = k.ps("pst", [128, 1024], BF16)
    ps_rr = {"s": [0, 1], "a": [2, 3], "o": [4, 5], "m": [6]}
    ps_ctr = {kk: 0 for kk in ps_rr}

    def psb(role):
        lst = ps_rr[role]
        i = lst[ps_ctr[role] % len(lst)]
        ps_ctr[role] += 1
        return PS[i]

    def mm(out, lhsT, rhs, start, stop, reads, writes, tp=None, ser=False):
        kw = {}
        if ser:
            k.pe_selfwait()
        if tp is not None:
            kw["tile_position"] = tp
        return k.op("pe", lambda: nc.tensor.matmul(out, lhsT=lhsT, rhs=rhs, start=start, stop=stop, **kw),
                    reads=reads, writes=writes)

    def act(out, in_, func, reads, writes, bias=None, scale=None):
        kw = {}
        if bias is not None:
            kw["bias"] = bias
        if scale is not None:
            kw["scale"] = scale
        return k.op("act", lambda: nc.scalar.activation(out=out, in_=in_, func=func, **kw),
                    reads=reads, writes=writes)

    def tt(eng, out, in0, in1, op, reads, writes):
        e = k.E[eng]
        return k.op(eng, lambda: e.tensor_tensor(out=out, in0=in0, in1=in1, op=op), reads=reads, writes=writes)

    def ts(eng, out, in0, s1, s2, op0, op1, reads, writes):
        e = k.E[eng]
        if s2 is None:
            return k.op(eng, lambda: e.tensor_scalar(out=out, in0=in0, scalar1=s1, scalar2=None, op0=op0),
                        reads=reads, writes=writes)
        return k.op(eng, lambda: e.tensor_scalar(out=out, in0=in0, scalar1=s1, scalar2=s2, op0=op0, op1=op1),
                    reads=reads, writes=writes)

    def stt(eng, out, in0, scalar, in1, op0, op1, reads, writes):
        e = k.E[eng]
        return k.op(eng, lambda: e.scalar_tensor_tensor(out=out, in0=in0, scalar=scalar, in1=in1, op0=op0, op1=op1),
                    reads=reads, writes=writes)

    def cp(eng, out, in_, reads, writes):
        if eng == "act":
            return k.op("act", lambda: nc.scalar.copy(out=out, in_=in_), reads=reads, writes=writes)
        e = k.E[eng]
        return k.op(eng, lambda: e.tensor_copy(out=out, in_=in_), reads=reads, writes=writes)

    def memset(eng, out, val, writes):
        e = k.E[eng]
        return k.op(eng, lambda: e.memset(out, val), writes=writes)

    def recip(out, in_, reads, writes):
        return k.op("dve", lambda: nc.vector.reciprocal(out=out, in_=in_), reads=reads, writes=writes)

    wslots = [k.sb("wslot0", [128, 8, 1024], BF16), k.sb("wslot1", [128, 8, 1024], BF16)]
    wcols = {"hg": [(C_HGF, 512), (C_HGQ, 512)], "fx": [(C_FXQ, 256), (C_FXK, 256), (C_FXV, 260)],
             "sb": [(C_SBQ, 256), (C_SBK, 256), (C_SBV, 256)], "ns": [(C_NSQ, 652)], "out": [(C_GATE, 1024)]}
    wplan = [(s_, l_, ph) for s_ in range(nseq) for l_ in range(nlayer)
             for ph in ("hg", "fx", "sb", "ns", "out") if ph in mixers]
    wissued = set()

    def w_issue(idx):
        if idx >= len(wplan) or idx in wissued:
            return
        wissued.add(idx)
        _, l_, ph = wplan[idx]
        slot = wslots[idx % 2]
        base = 0
        for (c0, n) in wcols[ph]:
            src = w_in_d[l_][:, c0:c0 + n].rearrange("(j p) c -> p j c", p=128)
            k.dma("pool", slot[:, 0:4, base:base + n], src[:, 0:4, :], writes=[slot])
            k.dma("pool", slot[:, 4:8, base:base + n], src[:, 4:8, :], writes=[slot])
            base += n

    def get_w(s_, l_, ph):
        idx = wplan.index((s_, l_, ph))
        w_issue(idx)
        views, base = [], 0
        for (c0, n) in wcols[ph]:
            views.append(WV(wslots[idx % 2], base))
            base += n
        w_issue(idx + 1)
        return views

    def load_w(l, c0, ncols, name="wblk"):
        w = k.sb(name, [128, 8, ncols], BF16)
        src = w_in_d[l][:, c0:c0 + ncols].rearrange("(j p) c -> p j c", p=128)
        half = 4
        k.dma("pool", w[:, 0:half, :], src[:, 0:half, :], writes=[w])
        k.dma("pool", w[:, half:8, :], src[:, half:8, :], writes=[w])
        return w

    def proj_fm(w, off, M, tb, ps, extra_reads=()):
        for j in range(8):
            mm(ps[0:M, :], w[:, j, off:off + M], hT[:, j, tb * 512:(tb + 1) * 512], j == 0, j == 7,
               reads=[w, hT, *extra_reads], writes=[ps])

    def proj_tm(w, off, N, tB, ps):
        for j in range(8):
            mm(ps[:, 0:N], hT[:, j, tB * 128:(tB + 1) * 128], w[:, j, off:off + N], j == 0, j == 7,
               reads=[w, hT], writes=[ps])

    def finalize(o, o_tok, out_ap, tmp, n=4):
        sq, ss, sd = tmp
        tt("dve", sq[:, 0:n, :], o, o, ALU.mult, reads=[o_tok], writes=[sq])
        k.op("dve", lambda: nc.vector.tensor_reduce(out=ss[:, 0:n], in_=sq[:, 0:n, :], axis=AX.X, op=ALU.add),
             reads=[sq], writes=[ss])
        act(sd[:, 0:n], ss[:, 0:n], AF.Ln, reads=[ss], writes=[sd], bias=EPS, scale=1.0 / 64.0)
        act(sd[:, 0:n], sd[:, 0:n], AF.Exp, reads=[sd], writes=[sd], scale=-0.5)
        tt("dve", out_ap, o, sd[:, 0:n].unsqueeze(2).to_broadcast([128, n, 64]), ALU.mult,
           reads=[o_tok, sd], writes=[ytok])

    def fin_tmp(n=4):
        return (k.sb("sq", [128, n, 64]), k.sb("ss", [128, n]), k.sb("sd", [128, n]))


    def run_streams(gens):
        gens = list(gens)
        while gens:
            for g in list(gens):
                try:
                    next(g)
                except StopIteration:
                    gens.remove(g)

    def attn_stream(jobs, sbank, obanks, pTs, K_ap, Q_ap, kq_toks, masks, bias_ap, bias_toks, V_ap, v_toks,
                    kb_range, qs_range, ncol, fin, post_mask=None):
        if not isinstance(obanks, (list, tuple)):
            obanks = [obanks]
        tiles = []
        for ji, (h, qb) in enumerate(jobs):
            kbs = list(kb_range(qb))
            for kb in kbs:
                tiles.append((h, qb, kb, kb == kbs[0], kb == kbs[-1], ji))
        nsb = len(sbank)
        L = max(nsb - 1, 1)

        def emit_qk(i):
            h, qb, kb = tiles[i][0:3]
            ps = sbank[i % nsb]
            ml = masks(h, qb, kb)
            mm(ps[:], K_ap(h, kb), Q_ap(h, qb), True, len(ml) == 0, reads=kq_toks, writes=[ps])
            for mi, (lt, rh, rd) in enumerate(ml):
                mm(ps[:], lt, rh, False, mi == len(ml) - 1, reads=rd, writes=[ps])

        if nsb > 1:
            for i in range(min(L, len(tiles))):
                emit_qk(i)
        else:
            emit_qk(0)
        first = True
        for i, (h, qb, kb, isfirst, islast, ji) in enumerate(tiles):
            if nsb > 1 and i + L < len(tiles):
                emit_qk(i + L)
            yield
            obank = obanks[ji % len(obanks)]
            accv = obank[:, 0:4 * ncol].rearrange("p (q d) -> p q d", q=4)
            ps = sbank[i % nsb]
            p = pTs[i % len(pTs)]
            if isfirst:
                first = True
            b = bias_ap(h, kb) if bias_ap is not None else None
            act(p[:], ps[:], AF.Exp, reads=[ps, *bias_toks], writes=[p], bias=b)
            pm = post_mask(h, qb, kb) if post_mask is not None else None
            if pm is not None:
                tt("dve", p[:], p[:], pm[0], ALU.mult, reads=[p, *pm[1]], writes=[p])
            for qs in range(4):
                lo, hi = qs_range(qb, qs)
                if kb < lo or kb > hi:
                    continue
                mm(accv[:, qs, :], p[:, qs * 128:(qs + 1) * 128], V_ap(h, kb), first, kb == hi,
                   reads=[p, *v_toks], writes=[obank])
                first = False
            if islast:
                fin(h, qb, obank, accv)
            if nsb == 1 and i + 1 < len(tiles):
                emit_qk(i + 1)
            yield

    def phase_norm(s, l):
        xsrc = xT_d if l == 0 else xres_d
        with k.scope():
            xin = [k.sb(f"xin{i}", [128, 8, 512]) for i in range(2)]
            sq = [k.sb(f"sqn{i}", [128, 8, 512], BF16) for i in range(2)]
            rs = [k.sb(f"rs{i}", [128, 512]) for i in range(2)]
            for tb in range(4):
                xi, sqi, rsi = xin[tb % 2], sq[tb % 2], rs[tb % 2]
                src = xsrc[s].rearrange("(j p) t -> p j t", p=128)[:, :, tb * 512:(tb + 1) * 512]
                k.dma("sp", xi[:], src, reads=[xsrc], writes=[xi])
                act(sqi[:], xi[:], AF.Square, reads=[xi], writes=[sqi])
                ps = psb("m")
                for j in range(8):
                    mm(ps[:], onesB, sqi[:, j, :], j == 0, j == 7, reads=[constB, sqi], writes=[ps])
                act(rsi[:], ps[:], AF.Ln, reads=[ps], writes=[rsi], bias=EPS, scale=1.0 / D_)
                act(rsi[:], rsi[:], AF.Exp, reads=[rsi], writes=[rsi], scale=-0.5)
                for j in range(8):
                    stt("dve", hT[:, j, tb * 512:(tb + 1) * 512], xi[:, j, :], normg[:, l, j:j + 1], rsi[:],
                        ALU.mult, ALU.mult, reads=[xi, rsi, normg], writes=[hT])

    def phase_fox(s, l):
        with k.scope():
            QT = k.sb("fxQT", [128, 4, T_], BF16)
            KT = k.sb("fxKT", [128, 4, T_], BF16)
            V = k.sb("fxV", [128, 16, 4, 65], BF16)
            ltok = k.sb("fxl", [128, 16, 4])
            negcum = k.sb("fxnc", [128, 16, 4])
            fbb = k.sb("fbb", [128, 4])
            k.dma("sp", fbb[:], fb_d[l:l + 1, :].to_broadcast([128, 4]), writes=[fbb])
            memset("pool", V[:, :, :, 64:65], 1.0, writes=[V])
            memset("pool", KT[64:67, :, :], 1.0, writes=[KT])
            wq_, wk_, wv_ = get_w(s, l, "fx")
            for (w, dst, scale) in ((wq_, QT, 0.125), (wk_, KT, None)):
                for pr in range(2):
                    for tb in range(4):
                        ps = psb("m")
                        proj_fm(w, pr * 128, 128, tb, ps)
                        sl = slice(tb * 512, (tb + 1) * 512)
                        for hh in range(2):
                            if scale is None:
                                cp("act", dst[0:64, 2 * pr + hh, sl], ps[64 * hh:64 * hh + 64, :], reads=[ps], writes=[dst])
                            else:
                                k.op("act", lambda: nc.scalar.mul(out=dst[0:64, 2 * pr + hh, sl],
                                                                  in_=ps[64 * hh:64 * hh + 64, :], mul=scale),
                                     reads=[ps], writes=[dst])
            if os.environ.get('FX_STOP') == '1':
                return
            w = wv_
            _skip = os.environ.get("FX_SKIP", "")
            for tB in range(int(os.environ.get("FX_NTB", "16"))):
                ps = psb("m")
                if "mm" not in _skip:
                    proj_tm(w, 0, 260, tB, ps)
                if "cp" not in _skip:
                    cp("act", V[:, tB, :, 0:64], ps[:, 0:256].rearrange("p (h d) -> p h d", h=4), reads=[ps], writes=[V])
                if "tt" not in _skip:
                    tt("dve", ltok[:, tB, :], ps[:, 256:260], fbb[:], ALU.add, reads=[ps, fbb], writes=[ltok])
            if os.environ.get('FX_STOP') == '2':
                return
            act(ltok[:], ltok[:], AF.Exp, reads=[ltok], writes=[ltok], scale=-1.0)
            act(ltok[:], ltok[:], AF.Ln, reads=[ltok], writes=[ltok], bias=1.0)
            if os.environ.get('FX_STOP') == '3':
                return
            ps = psb("m")
            lflat = ltok[:].rearrange("p b h -> p (b h)")
            mm(ps[:, 0:64], triF, lflat, True, True, reads=[constF, ltok], writes=[ps])
            mm(ps[:, 64:128], onesF, lflat, True, True, reads=[constF, ltok], writes=[ps])
            tot = k.sb("fxtot", [128, 16, 4])
            pre = k.sb("fxpre", [128, 16, 4])
            cp("dve", tot[:], ps[:, 64:128].rearrange("p (b h) -> p b h", h=4), reads=[ps], writes=[tot])
            memset("dve", pre[:, 0, :], 0.0, writes=[pre])
            for B in range(1, 16):
                tt("dve", pre[:, B, :], pre[:, B - 1, :], tot[:, B - 1, :], ALU.add, reads=[pre, tot], writes=[pre])
            tt("dve", negcum[:], ps[:, 0:64].rearrange("p (b h) -> p b h", h=4), pre[:], ALU.add,
               reads=[ps, pre], writes=[negcum])
            if os.environ.get('FX_STOP') == '4':
                return
            cumf = k.sb("cumf", [4, T_])
            c1f = k.sb("c1f", [4, T_])
            cb = [k.sb(f"cb{i}", [4, T_], BF16) for i in range(3)]
            for g in range(4):
                ps = psb("m")
                for bb in range(4):
                    B = 4 * g + bb
                    mm(ps[0:4, 128 * bb:128 * bb + 128], negcum[:, B, :], identF, True, True,
                       reads=[negcum, constF], writes=[ps])
                k.op("act", lambda: nc.scalar.mul(out=cumf[:, 512 * g:512 * g + 512], in_=ps[0:4, :], mul=-1.0),
                     reads=[ps], writes=[cumf])
            cp("dve", cb[0][:], cumf[:], reads=[cumf], writes=[cb[0]])
            cp("dve", c1f[:], cb[0][:], reads=[cb[0]], writes=[c1f])
            tt("dve", cumf[:], cumf[:], c1f[:], ALU.subtract, reads=[cumf, c1f], writes=[cumf])
            cp("dve", cb[1][:], cumf[:], reads=[cumf], writes=[cb[1]])
            cp("dve", c1f[:], cb[1][:], reads=[cb[1]], writes=[c1f])
            tt("dve", cumf[:], cumf[:], c1f[:], ALU.subtract, reads=[cumf, c1f], writes=[cumf])
            cp("dve", cb[2][:], cumf[:], reads=[cumf], writes=[cb[2]])
            for i in range(3):
                k.dma("sp", QT[64 + i:65 + i, :, :], cb[i][:], reads=[cb[i]], writes=[QT])
            if os.environ.get('FX_STOP') == '5':
                return
            def mk_stream(si, heads):
                pTs = [k.sb(f"fxpT{si}_{i}", [128, 512], BF16) for i in range(4)]
                o_sbs = [k.sb(f"fxo{si}_{i}", [128, 4, 64]) for i in range(2)]
                rds = [k.sb(f"fxrd{si}_{i}", [128, 4]) for i in range(2)]
                tmps = [fin_tmp() for i in range(2)]
                fc = [0]

                def masks(h, qb, kb):
                    if kb >= 4 * qb:
                        r = kb - 4 * qb
                        return [(identB, strips[:, 0, 384 - 128 * r:384 - 128 * r + 512], [constB, strips])]
                    return []

                def fin(h, qb, acc, accv):
                    o_sb, rd, tmp = o_sbs[fc[0] % 2], rds[fc[0] % 2], tmps[fc[0] % 2]
                    fc[0] += 1
                    recip(rd[:], accv[:, :, 64], reads=[acc], writes=[rd])
                    tt("dve", o_sb[:], accv[:, :, 0:64], rd[:].unsqueeze(2).to_broadcast([128, 4, 64]), ALU.mult,
                       reads=[acc, rd], writes=[o_sb])
                    finalize(o_sb[:], o_sb, ytok[:, 4 * qb:4 * qb + 4, 256 + 64 * h:256 + 64 * h + 64], tmp)

                return attn_stream(
                    [(h, qb) for h in heads for qb in range(4)],
                    [PS[0], PS[1], PS[2], PS[3]], [PS[4], PS[5]], pTs,
                    lambda h, kb: KT[0:67, h, kb * 128:(kb + 1) * 128],
                    lambda h, qb: QT[0:67, h, qb * 512:(qb + 1) * 512], [KT, QT],
                    masks, lambda h, kb: negcum[:, kb, h:h + 1], [negcum],
                    lambda h, kb: V[:, kb, h, :], [V],
                    lambda qb: range(0, 4 * qb + 4), lambda qb, qs: (0, 4 * qb + qs), 65, fin)

            run_streams([mk_stream(0, (0, 1, 2, 3))])


    def phase_sb(s, l):
        with k.scope():
            QT = k.sb("sbQT", [64, 4, T_], BF16)
            KT = k.sb("sbKT", [64, 4, T_], BF16)
            V = k.sb("sbV", [128, 16, 256], BF16)
            wq_, wk_, wv_ = get_w(s, l, "sb")
            for (w, dst, scale) in ((wq_, QT, 0.125), (wk_, KT, None)):
                for pr in range(2):
                    for tb in range(4):
                        ps = psb("m")
                        proj_fm(w, pr * 128, 128, tb, ps)
                        sl = slice(tb * 512, (tb + 1) * 512)
                        for hh in range(2):
                            if scale is None:
                                cp("act", dst[0:64, 2 * pr + hh, sl], ps[64 * hh:64 * hh + 64, :], reads=[ps], writes=[dst])
                            else:
                                k.op("act", lambda: nc.scalar.mul(out=dst[0:64, 2 * pr + hh, sl],
                                                                  in_=ps[64 * hh:64 * hh + 64, :], mul=scale),
                                     reads=[ps], writes=[dst])
            w = wv_
            for tB in range(16):
                ps = psb("m")
                proj_tm(w, 0, 256, tB, ps)
                cp("act", V[:, tB, :], ps[:, 0:256], reads=[ps], writes=[V])
            def sb_stream(si, heads):
                es = [k.sb(f"sbe{si}_{i}", [128, 512]) for i in range(2)]
                sps = [k.sb(f"sbsp{si}_{i}", [128, 512], BF16) for i in range(2)]
                er_ = k.sb(f"sber{si}", [128, 512])
                as_ = [k.sb(f"sbaT{si}_{i}", [128, 512], BF16) for i in range(2)]
                o = k.sb(f"sbo{si}", [128, 4, 64])
                tmp = fin_tmp()
                zb = [PS[3 * si], PS[3 * si + 1]]
                psR = PS[3 * si + 2]
                acc = PS[6]
                accv = acc[:, 256 * si:256 * si + 256].rearrange("p (q d) -> p q d", q=4)
                tiles = []
                for h in heads:
                    for qb in range(4):
                        nkb = 4 * qb + 4
                        for idx, kb in enumerate(reversed(range(nkb))):
                            tiles.append((h, qb, kb, idx, nkb))
                n = len(tiles)

                def stage1(i):
                    h, qb, kb, idx, nkb = tiles[i]
                    ps = zb[i % 2]
                    diag = kb >= 4 * qb
                    mm(ps[:], KT[0:64, h, kb * 128:(kb + 1) * 128], QT[0:64, h, qb * 512:(qb + 1) * 512],
                       True, not diag, reads=[KT, QT], writes=[ps])
                    if diag:
                        r = kb - 4 * qb
                        mm(ps[:], identB, strips[:, 1, 384 - 128 * r:384 - 128 * r + 512], False, True,
                           reads=[constB, strips], writes=[ps])

                def stage2(i):
                    ps, e, sp_ = zb[i % 2], es[i % 2], sps[i % 2]
                    act(e[:], ps[:], AF.Exp, reads=[ps], writes=[e])
                    act(sp_[:], e[:], AF.Ln, reads=[e], writes=[sp_], bias=1.0)

                stage1(0)
                stage2(0)
                for i, (h, qb, kb, idx, nkb) in enumerate(tiles):
                    e, sp_, a_ = es[i % 2], sps[i % 2], as_[i % 2]
                    if i + 1 < n:
                        stage1(i + 1)
                    mm(psR[:], negtriB, sp_[:], idx == 0, False, reads=[constB, sp_], writes=[psR])
                    if idx == 0:
                        memset("dve", accv, 0.0, writes=[acc])
                    yield
                    act(er_[:], psR[:], AF.Exp, reads=[psR], writes=[er_])
                    if i + 1 < n:
                        stage2(i + 1)
                    tt("dve", a_[:], e[:], er_[:], ALU.mult, reads=[e, er_], writes=[a_])
                    mm(psR[:], neglowB, sp_[:], False, idx == nkb - 1, reads=[constB, sp_], writes=[psR])
                    yield
                    for qs in range(4):
                        if kb > 4 * qb + qs:
                            continue
                        mm(accv[:, qs, :], a_[:, qs * 128:(qs + 1) * 128], V[:, kb, 64 * h:64 * h + 64],
                           False, kb == 0, reads=[a_, V], writes=[acc])
                    if idx == nkb - 1:
                        cp("dve", o[:], accv, reads=[acc], writes=[o])
                        finalize(o[:], o, ytok[:, 4 * qb:4 * qb + 4, 512 + 64 * h:512 + 64 * h + 64], tmp)
                    yield

            run_streams([sb_stream(0, (0, 1)), sb_stream(1, (2, 3))])


    def phase_hgrn(s, l):
        with k.scope():
            omlb = k.sb("omlb", [128, 256])
            omlT = k.sb("omlT", [128, 2])
            if l == 0:
                memset("pool", omlb[:], 1.0, writes=[omlb])
                memset("pool", omlT[:], 1.0, writes=[omlT])
            else:
                with k.scope():
                    lbb = k.sb("lbb", [128, 2, 256])
                    lblT = k.sb("lblT", [128, 2, 2])
                    k.dma("sp", lbb[:], lbl_d[:].unsqueeze(0).to_broadcast([128, 2, 256]), writes=[lbb])
                    k.dma("sp", lblT[:], lblT_d[:], writes=[lblT])
                    tt("dve", omlb[:], lbb[:, 0, :], lbb[:, 1, :], ALU.subtract, reads=[lbb], writes=[omlb])
                    act(omlb[:], omlb[:], AF.Sigmoid, reads=[omlb], writes=[omlb])
                    tt("dve", omlT[:], lblT[:, :, 0], lblT[:, :, 1], ALU.subtract, reads=[lblT], writes=[omlT])
                    act(omlT[:], omlT[:], AF.Sigmoid, reads=[omlT], writes=[omlT])
            big1 = k.sb("hgbig1", [128, 4096])
            gtok = k.sb("hggtok", [128, 16, 256])
            vtok = k.sb("hgvtok", [128, 16, 256], BF16)
            khat = k.sb("hgkhat", [128, 16, 256], BF16)
            ktok = big1[:].rearrange("p (b c) -> p b c", c=256)
            w, w2 = get_w(s, l, "hg")
            for tB in range(16):
                ps = psb("m")
                proj_tm(w, 0, 512, tB, ps)
                act(ktok[:, tB, :], ps[:, 0:256], AF.Sigmoid, reads=[ps], writes=[big1], scale=-1.0)
                cp("dve", vtok[:, tB, :], ps[:, 256:512], reads=[ps], writes=[vtok])
            for tB in range(16):
                tt("dve", ktok[:, tB, :], ktok[:, tB, :], omlb[:], ALU.mult, reads=[big1, omlb], writes=[big1])
            act(gtok[:], ktok, AF.Ln, reads=[big1], writes=[gtok], bias=1.0, scale=-1.0)
            ebs = [k.sb(f"hgebs{i}", [128, 256]) for i in range(1)]
            for tB in range(16):
                ps = psb("m")
                mm(ps[:, 0:256], trisufF, gtok[:, tB, :], True, True, reads=[constF, gtok], writes=[ps])
                eb = ebs[0]
                act(eb[:], ps[:, 0:256], AF.Exp, reads=[ps], writes=[eb])
                tt("dve", khat[:, tB, :], ktok[:, tB, :], eb[:], ALU.mult, reads=[big1, eb], writes=[khat])
            if os.environ.get('HG_STOP') == '1':
                return
            qsT = k.sb("hgqsT", [128, 2, T_])
            kT = big1[:].rearrange("p (a t) -> p a t", a=2)
            for pr in range(2):
                for tb in range(4):
                    sl = slice(tb * 512, (tb + 1) * 512)
                    ps = psb("m")
                    proj_fm(w2, pr * 128, 128, tb, ps)
                    act(qsT[:, pr, sl], ps[:], AF.Silu, reads=[ps], writes=[qsT])
                    ps = psb("m")
                    proj_fm(w2, 256 + pr * 128, 128, tb, ps, extra_reads=[khat])
                    act(kT[:, pr, sl], ps[:], AF.Sigmoid, reads=[ps, khat], writes=[big1], scale=-1.0)
                    ts("dve", kT[:, pr, sl], kT[:, pr, sl], omlT[:, pr:pr + 1], None, ALU.mult, None,
                       reads=[big1, omlT], writes=[big1])
            qtT = k.sb("hgqtT", [128, 2, T_], BF16)
            ktT = k.sb("hgktT", [128, 2, T_], BF16)
            dl = k.sb("hgdl", [128, 2, 64])
            e1s = [k.sb(f"hge1{i}", [128, 512]) for i in range(1)]
            e2s = [k.sb(f"hge2{i}", [128, 512]) for i in range(1)]
            it = 0
            for pr in range(2):
                for g4 in range(4):
                    sl = slice(g4 * 512, (g4 + 1) * 512)
                    ps = psb("a")
                    for bb in range(4):
                        tB = 4 * g4 + bb
                        mm(ps[:, 128 * bb:128 * bb + 128], gtok[:, tB, pr * 128:(pr + 1) * 128], tribdF, True, True,
                           reads=[gtok, constF], writes=[ps])
                    e1, e2 = e1s[0], e2s[0]
                    it += 1
                    act(e1[:], ps[:], AF.Exp, reads=[ps], writes=[e1])
                    act(e2[:], ps[:], AF.Exp, reads=[ps], writes=[e2], scale=-1.0)
                    tt("dve", qtT[:, pr, sl], qsT[:, pr, sl], e1[:], ALU.mult, reads=[qsT, e1], writes=[qtT])
                    tt("dve", ktT[:, pr, sl], kT[:, pr, sl], e2[:], ALU.mult, reads=[big1, e2], writes=[ktT])
                    cp("pool", dl[:, pr, 16 * g4:16 * g4 + 16], e1[:, 31:512:32], reads=[e1], writes=[dl])
            if os.environ.get('HG_STOP') == '2':
                return
            Srun = [k.sb(f"hgS{i}", [128, 5, 2, 64]) for i in range(2)]
            Sbf = [k.sb(f"hgSb{i}", [128, 4, 2, 64], BF16) for i in range(2)]
            Abd = [k.sb(f"hgA{i}", [128, 4, 128], BF16) for i in range(2)]
            vbd = [k.sb(f"hgvbd{i}", [128, 4, 4, 64], BF16) for i in range(2)]
            o_sb = [k.sb(f"hgo{i}", [128, 4, 64]) for i in range(1)]
            tmp = fin_tmp()
            memset("pool", Srun[1][:, 4, :, :], 0.0, writes=[Srun[1]])
            def hg_state(B):
                cur, prev = Srun[B % 2], Srun[(B + 1) % 2]
                vb = vbd[B % 2]
                tt("dve", vb[:], vtok[:, B, :].rearrange("p (h d) -> p h d", h=4).unsqueeze(2).to_broadcast([128, 4, 4, 64]),
                   maskbdF[:, 31:128:32].unsqueeze(1).unsqueeze(3).to_broadcast([128, 4, 4, 64]), ALU.mult,
                   reads=[vtok, constF], writes=[vb])
                psD = psb("a")
                for h in range(4):
                    pr, r0 = h // 2, 64 * (h % 2)
                    mm(psD[r0:r0 + 64, pr * 256:pr * 256 + 256], khat[:, B, 64 * h:64 * h + 64],
                       vb[:, h, :, :].rearrange("p c d -> p (c d)"), True, True,
                       reads=[khat, vb], writes=[psD], tp=(0, r0))
                cp("pool", cur[:, 0, :, :], prev[:, 4, :, :], reads=[prev], writes=[cur])
                for c in range(4):
                    for pr in range(2):
                        col = (pr * 4 + c) * 64
                        stt("dve", cur[:, c + 1, pr, :], cur[:, c, pr, :], dl[:, pr, 4 * B + c:4 * B + c + 1],
                            psD[:, col:col + 64], ALU.mult, ALU.add, reads=[cur, dl, psD], writes=[cur])
                Sb = Sbf[B % 2]
                cp("act", Sb[:], cur[:, 0:4, :, :], reads=[cur], writes=[Sb])

            def hg_output(B):
                bsl = slice(B * 128, (B + 1) * 128)
                Sb = Sbf[B % 2]
                psA = psb("s")
                for h in (0, 2, 1, 3):
                    pr, r0 = h // 2, 64 * (h % 2)
                    mm(psA[:, 128 * h:128 * h + 128], ktT[r0:r0 + 64, pr, bsl], qtT[r0:r0 + 64, pr, bsl], True, True,
                       reads=[ktT, qtT], writes=[psA], ser=(h in (0, 1)))
                A = Abd[B % 2]
                tt("dve", A[:], psA[:].rearrange("p (h t) -> p h t", h=4),
                   maskbdF.unsqueeze(1).to_broadcast([128, 4, 128]), ALU.mult, reads=[psA, constF], writes=[A])
                psO = psb("o")
                for h in range(4):
                    mm(psO[:, 64 * h:64 * h + 64], A[:, h, :], vtok[:, B, 64 * h:64 * h + 64], h == 0, False,
                       reads=[A, vtok], writes=[psO])
                for h in (0, 2, 1, 3):
                    pr, r0 = h // 2, 64 * (h % 2)
                    for c in range(4):
                        mm(psO[32 * c:32 * c + 32, 64 * h:64 * h + 64],
                           qtT[r0:r0 + 64, pr, B * 128 + 32 * c:B * 128 + 32 * c + 32], Sb[r0:r0 + 64, c, pr, :],
                           False, h == 3 and c == 3, reads=[qtT, Sb], writes=[psO], tp=(r0, 32 * c),
                           ser=(c == 0 and h in (0, 1)))
                o = o_sb[0]
                cp("dve", o[:], psO[:, 0:256].rearrange("p (h d) -> p h d", h=4), reads=[psO], writes=[o])
                finalize(o[:], o, ytok[:, B, 0:256].rearrange("p (h d) -> p h d", h=4), tmp)

            hg_state(0)
            for B in range(16):
                if B + 1 < 16:
                    hg_state(B + 1)
                hg_output(B)

    def phase_nsa(s, l):
        with k.scope():
            ovl = k.sb("ovl", [128, 33], BF16)
            for dst, src in ((ovl, ovl_d),):
                k.dma("sp", dst[:], src[:], writes=[dst])
            qT = k.sb("nsqT", [96, 4, T_], BF16)
            ksT = k.sb("nsksT", [96, T_], BF16)
            k.dma("sp", ksT[64:96, :], eall_d[:], writes=[ksT])
            kwT = k.sb("nskwT", [64, T_], BF16)
            kcT = k.sb("nskcT", [64, T_], BF16)
            vcT = k.sb("nsvcT", [64, T_], BF16)
            Vs = k.sb("nsVs", [128, 16, 65], BF16)
            Vw = k.sb("nsVw", [128, 16, 65], BF16)
            gtok = k.sb("nsg", [128, 16, 12])
            memset("pool", Vs[:, :, 64:65], 1.0, writes=[Vs])
            memset("pool", Vw[:, :, 64:65], 1.0, writes=[Vw])
            kcmpT = k.sb("nskcmpT", [64, 128], BF16)
            rhsc = k.sb("nsrhsc", [128, 97], BF16)
            with k.scope():
                ropeC = k.sb("ropeC", [16, T_])
                ropeS = k.sb("ropeS", [16, T_])
                ropeCc = k.sb("ropeCc", [16, 128])
                ropeSc = k.sb("ropeSc", [16, 128])
                permF = k.sb("permF", [64, 16])
                for dst, src in ((ropeC, ropeC_d), (ropeS, ropeS_d), (ropeCc, ropeCc_d), (ropeSc, ropeSc_d),
                                 (permF, permF_d)):
                    k.dma("sp", dst[:], src[:], writes=[dst])
                q32s = [k.sb(f"nsq32{i}", [64, 512]) for i in range(2)]
                t1s = [k.sb(f"nst1{i}", [16, 512]) for i in range(2)]
                t2s = [k.sb(f"nst2{i}", [16, 512]) for i in range(2)]
                rc = [0]

                def rope_evac(src, dst, n, scale, Ct, St, extra_tok):
                    i = rc[0] % 2
                    rc[0] += 1
                    q32, t1, t2 = q32s[i], t1s[i], t2s[i]
                    k.op("act", lambda: nc.scalar.mul(out=q32[:, 0:n], in_=src, mul=scale),
                         reads=[extra_tok["src"]], writes=[q32])
                    cp("act", dst, q32[:, 0:n], reads=[q32], writes=[extra_tok["dst"]])
                    psw = psb("a")
                    mm(psw[0:16, 0:n], permF[:, :], q32[:, 0:n], True, True, reads=[permF, q32], writes=[psw])
                    tt("dve", t1[:, 0:n], q32[0:16, 0:n], Ct, ALU.mult, reads=[q32, extra_tok["tab"]], writes=[t1])
                    tt("dve", t2[:, 0:n], psw[0:16, 0:n], St, ALU.mult, reads=[psw, extra_tok["tab"]], writes=[t2])
                    tt("dve", extra_tok["dst16"], t1[:, 0:n], t2[:, 0:n], ALU.add,
                       reads=[t1, t2], writes=[extra_tok["dst"]])

                (w,) = get_w(s, l, "ns")
                cmpW = {}
                for kv in "kv":
                    W1 = k.sb(f"nsW1{kv}", [64, 32, 64], BF16)
                    W2 = k.sb(f"nsW2{kv}", [64, 64], BF16)
                    peT = k.sb(f"nspeT{kv}", [64, 32], BF16)
                    k.dma("pool", W1[:], w1_d[kv][l].rearrange("(lp d) j -> d lp j", d=64), writes=[W1])
                    k.dma("pool", W2[:], w2_d[kv][l], writes=[W2])
                    k.dma("pool", peT[:], peT_d[kv][l], writes=[peT])
                    cmpW[kv] = (W1, W2, peT)
                for pr in range(2):
                    for tb in range(4):
                        sl = slice(tb * 512, (tb + 1) * 512)
                        ps = psb("m")
                        proj_fm(w, pr * 128, 128, tb, ps)
                        for hh in range(2):
                            h = 2 * pr + hh
                            rope_evac(ps[64 * hh:64 * hh + 64, :], qT[0:64, h, sl], 512, 0.125, ropeC[:, sl], ropeS[:, sl],
                                      dict(src=ps, dst=qT, tab=ropeC, dst16=qT[0:16, h, sl]))
                for tb in range(4):
                    sl = slice(tb * 512, (tb + 1) * 512)
                    ps = psb("m")
                    proj_fm(w, 256, 128, tb, ps)
                    cp("act", kcT[:, sl], ps[0:64, :], reads=[ps], writes=[kcT])
                    cp("act", vcT[:, sl], ps[64:128, :], reads=[ps], writes=[vcT])
                for off, dst in ((384, ksT), (512, kwT)):
                    for tb in range(4):
                        sl = slice(tb * 512, (tb + 1) * 512)
                        ps = psb("m")
                        proj_fm(w, off, 64, tb, ps)
                        rope_evac(ps[0:64, :], dst[0:64, sl], 512, 1.0, ropeC[:, sl], ropeS[:, sl],
                                  dict(src=ps, dst=dst, tab=ropeC, dst16=dst[0:16, sl]))
                for tB in range(16):
                    ps = psb("m")
                    proj_tm(w, 448, 204, tB, ps)
                    cp("act", Vs[:, tB, 0:64], ps[:, 0:64], reads=[ps], writes=[Vs])
                    cp("act", Vw[:, tB, 0:64], ps[:, 128:192], reads=[ps], writes=[Vw])
                    act(gtok[:, tB, :], ps[:, 192:204], AF.Sigmoid, reads=[ps], writes=[gtok])
                memset("pool", kcmpT[:], 0.0, writes=[kcmpT])
                memset("pool", rhsc[:], 0.0, writes=[rhsc])
                cp("pool", rhsc[:, 64:97], ovl[:], reads=[ovl], writes=[rhsc])
                for kv, srcT in (("k", kcT), ("v", vcT)):
                    W1, W2, peT = cmpW[kv]
                    psH = psb("a")
                    for lp in range(32):
                        mm(psH[0:64, 0:127], W1[:, lp, :], srcT[:, lp:lp + 16 * 126 + 1:16], lp == 0, lp == 31,
                           reads=[W1, srcT], writes=[psH])
                    for lp in range(32):
                        mm(psH[0:64, 127:128], W1[:, lp, :], peT[:, lp:lp + 1], lp == 0, lp == 31,
                           reads=[W1, peT], writes=[psH])
                    bias = k.sb(f"nsbias{kv}", [64, 1])
                    hid = k.sb(f"nshid{kv}", [64, 128], BF16)
                    cp("dve", bias[:], psH[0:64, 127:128], reads=[psH], writes=[bias])
                    act(hid[:, 0:127], psH[0:64, 0:127], AF.Silu, reads=[psH, bias], writes=[hid], bias=bias[:, 0:1])
                    ps2 = psb("m")
                    if kv == "k":
                        mm(ps2[0:64, 0:127], W2[:, :], hid[:, 0:127], True, True, reads=[W2, hid], writes=[ps2])
                        rope_evac(ps2[0:64, 0:127], kcmpT[0:64, 0:127], 127, 1.0, ropeCc[:, 0:127], ropeSc[:, 0:127],
                                  dict(src=ps2, dst=kcmpT, tab=ropeCc, dst16=kcmpT[0:16, 0:127]))
                    else:
                        mm(ps2[0:127, 0:64], hid[:, 0:127], W2[:, :], True, True, reads=[W2, hid], writes=[ps2])
                        cp("act", rhsc[0:127, 0:64], ps2[0:127, 0:64], reads=[ps2], writes=[rhsc])
            cmask = k.sb("cmask", [128, T_], BF16)
            tkA = k.sb("tkA", [128, 16, 32])
            tkB = k.sb("tkB", [128, 16, 32])
            nacc = k.sb("nsacc", [128, 16, 256])
            imp = k.sb("nsimp", [128, 16, 32])
            s01 = k.sb("nss01", [128, 2, 896], BF16)
            for dst, src in ((cmask, cmask_d), (tkA, tkA_d), (tkB, tkB_d), (s01, strips01_d)):
                k.dma("sp", dst[:], src[:], writes=[dst])
            pT = [k.sb(f"nspT{i}", [128, 512], BF16) for i in range(3)]
            rden = [k.sb(f"nsrd{i}", [128, 4]) for i in range(2)]
            cf = [k.sb(f"nscf{i}", [128, 4]) for i in range(2)]
            tmpo = [k.sb(f"nstmpo{i}", [128, 4, 64]) for i in range(2)]
            tmpi = [k.sb(f"nstmpi{i}", [128, 4, 32]) for i in range(2)]
            it = 0
            fi = 0
            for h in range(4):
                for qb in range(4):
                    qsl = slice(qb * 512, (qb + 1) * 512)
                    ps = psb("s")
                    mm(ps[:], kcmpT[:, :], qT[0:64, h, qsl], True, False, reads=[kcmpT, qT], writes=[ps])
                    mm(ps[:], identB, cmask[:, qsl], False, True, reads=[constB, cmask], writes=[ps])
                    p = pT[it % 3]
                    it += 1
                    act(p[:], ps[:], AF.Exp, reads=[ps], writes=[p])
                    acc = psb("o")
                    accv = acc[:, 0:388].rearrange("p (q d) -> p q d", q=4)
                    for qs in range(4):
                        mm(accv[:, qs, :], p[:, qs * 128:(qs + 1) * 128], rhsc[:, :], qs == 0, True,
                           reads=[p, rhsc], writes=[acc])
                    rd, c_ = rden[fi % 2], cf[fi % 2]
                    to, ti = tmpo[fi % 2], tmpi[fi % 2]
                    fi += 1
                    ts("dve", rd[:], accv[:, :, 64], 1e-30, None, ALU.max, None, reads=[acc], writes=[rd])
                    recip(rd[:], rd[:], reads=[rd], writes=[rd])
                    tt("dve", c_[:], rd[:], gtok[:, 4 * qb:4 * qb + 4, h], ALU.mult, reads=[rd, gtok], writes=[c_])
                    tt("dve", nacc[:, 4 * qb:4 * qb + 4, 64 * h:64 * h + 64], accv[:, :, 0:64],
                       c_[:].unsqueeze(2).to_broadcast([128, 4, 64]), ALU.mult, reads=[acc, c_], writes=[nacc])
                    if h == 0:
                        tt("dve", imp[:, 4 * qb:4 * qb + 4, :], accv[:, :, 65:97],
                           rd[:].unsqueeze(2).to_broadcast([128, 4, 32]), ALU.mult, reads=[acc, rd], writes=[imp])
                    else:
                        tt("dve", ti[:], accv[:, :, 65:97], rd[:].unsqueeze(2).to_broadcast([128, 4, 32]), ALU.mult,
                           reads=[acc, rd], writes=[ti])
                        tt("pool", imp[:, 4 * qb:4 * qb + 4, :], imp[:, 4 * qb:4 * qb + 4, :], ti[:], ALU.add,
                           reads=[imp, ti], writes=[imp])
            score = k.sb("nsscore", [128, 16, 32])
            negm = k.sb("nsnegm", [128, 16, 32])
            mx = [k.sb(f"nsmx{i}", [128, 8]) for i in range(2)]
            sc2 = k.sb("nssc2", [128, 32])
            tt("dve", score[:], imp[:], tkA[:], ALU.mult, reads=[imp, tkA], writes=[score])
            tt("dve", score[:], score[:], tkB[:], ALU.add, reads=[score, tkB], writes=[score])
            for tB in range(16):
                k.op("dve", lambda: nc.vector.max(out=mx[0][:], in_=score[:, tB, :]), reads=[score], writes=[mx[0]])
                k.op("dve", lambda: nc.vector.match_replace(out=sc2[:], in_to_replace=mx[0][:], in_values=score[:, tB, :],
                                                            imm_value=-1.0e9), reads=[score, mx[0]], writes=[sc2])
                k.op("dve", lambda: nc.vector.max(out=mx[1][:], in_=sc2[:]), reads=[sc2], writes=[mx[1]])
                ts("dve", negm[:, tB, :], score[:, tB, :], mx[1][:, 7:8], NEG, ALU.is_lt, ALU.mult,
                   reads=[score, mx[1]], writes=[negm])
            for g4 in range(4):
                ps = psb("m")
                for bb in range(4):
                    mm(ps[0:32, 128 * bb:128 * bb + 128], negm[:, 4 * g4 + bb, :], identF, True, True,
                       reads=[negm, constF], writes=[ps])
                for h in range(4):
                    cp("act" if h % 2 == 0 else "dve", qT[64:96, h, 512 * g4:512 * g4 + 512], ps[0:32, :],
                       reads=[ps], writes=[qT])

            def caus_mask(qb, kb):
                if kb >= 4 * qb:
                    r = kb - 4 * qb
                    return s01[:, 0, 384 - 128 * r:384 - 128 * r + 512]
                return None

            def win_mask(qb, kb):
                r = kb - 4 * qb
                if r >= 0:
                    return s01[:, 0, 384 - 128 * r:384 - 128 * r + 512]
                return s01[:, 1, 384 - 128 * (r + 4):384 - 128 * (r + 4) + 512]

            pTs4 = pT + [k.sb("nspT3", [128, 512], BF16)]

            def br_stream(si, bi, KTt, Vt, kb_range, mask_fn, use_sel, qs_range):
                pTs = [k.sb(f"nsbp{si}_{i}", [128, 512], BF16) for i in range(2)] if False else pTs4
                fc = [0]
                kq = 96 if use_sel else 64

                def masks(h, qb, kb):
                    return []

                def post_mask(h, qb, kb):
                    mk = mask_fn(qb, kb)
                    return None if mk is None else (mk, [s01])

                def fin(h, qb, acc, accv):
                    rd, c_, to = rden[fc[0] % 2], cf[fc[0] % 2], tmpo[fc[0] % 2]
                    fc[0] += 1
                    recip(rd[:], accv[:, :, 64], reads=[acc], writes=[rd])
                    tt("dve", c_[:], rd[:], gtok[:, 4 * qb:4 * qb + 4, 4 * bi + h], ALU.mult, reads=[rd, gtok], writes=[c_])
                    tt("dve", to[:], accv[:, :, 0:64], c_[:].unsqueeze(2).to_broadcast([128, 4, 64]), ALU.mult,
                       reads=[acc, c_], writes=[to])
                    tt("pool", nacc[:, 4 * qb:4 * qb + 4, 64 * h:64 * h + 64],
                       nacc[:, 4 * qb:4 * qb + 4, 64 * h:64 * h + 64], to[:], ALU.add, reads=[nacc, to], writes=[nacc])

                return attn_stream(
                    [(h, qb) for h in range(4) for qb in range(4)],
                    [PS[0], PS[1], PS[2], PS[3]], [PS[4], PS[5]], pTs,
                    lambda h, kb: KTt[0:kq, kb * 128:(kb + 1) * 128],
                    lambda h, qb: qT[0:kq, h, qb * 512:(qb + 1) * 512], [KTt, qT],
                    masks, None, [],
                    lambda h, kb: Vt[:, kb, :], [Vt],
                    kb_range, qs_range, 65, fin, post_mask=post_mask)

            run_streams([
                br_stream(0, 1, ksT, Vs, lambda qb: range(0, 4 * qb + 4), caus_mask, True,
                          lambda qb, qs: (0, 4 * qb + qs))])
            run_streams([
                br_stream(1, 2, kwT, Vw, lambda qb: range(max(0, 4 * qb - 4), 4 * qb + 4), win_mask, False,
                          lambda qb, qs: (max(0, 4 * qb + qs - 4), 4 * qb + qs)),
            ])
            tmp = fin_tmp(4)
            for g4 in range(4):
                for bb in range(4):
                    finalize(nacc[:, 4 * g4 + bb, :].rearrange("p (h d) -> p h d", h=4), nacc,
                             ytok[:, 4 * g4 + bb, 768:1024].rearrange("p (h d) -> p h d", h=4), tmp, n=4)

    def phase_out(s, l, last):
        xsrc = xT_d if l == 0 else xres_d
        with k.scope():
            (wg,) = get_w(s, l, "out")
            yT = k.sb("yT", [128, 8, T_], BF16)
            wo = k.sb("wo", [128, 8, D_], BF16)
            wsrc = w_out_d[l].rearrange("(j p) c -> p j c", p=128)
            k.dma("pool", wo[:, 0:4, :], wsrc[:, 0:4, :], writes=[wo])
            k.dma("pool", wo[:, 4:8, :], wsrc[:, 4:8, :], writes=[wo])
            def gate_stream(si, tBs):
                sgi = k.sb(f"sg{si}", [128, D_])
                ygi = k.sb(f"yg{si}", [128, D_], BF16)
                pa, pb = PS[2 * si], PS[2 * si + 1]
                for tB in tBs:
                    proj_tm(wg, 0, 512, tB, pa)
                    proj_tm(wg, 512, 512, tB, pb)
                    yield
                    act(sgi[:, 0:512], pa[:], AF.Silu, reads=[pa], writes=[sgi])
                    act(sgi[:, 512:1024], pb[:], AF.Silu, reads=[pb], writes=[sgi])
                    tt("dve", sgi[:], sgi[:], ong[:], ALU.mult, reads=[sgi, ong], writes=[sgi])
                    yield
                    tt("dve", ygi[:], sgi[:], ytok[:, tB, :], ALU.mult, reads=[sgi, ytok], writes=[ygi])
                    for cc in range(8):
                        k.op("pe", lambda: nc.tensor.transpose(PST[:, cc * 128:(cc + 1) * 128],
                                                               ygi[:, cc * 128:(cc + 1) * 128], identB),
                             reads=[ygi, constB], writes=[PST])
                    cp("act", yT[:, :, tB * 128:(tB + 1) * 128], PST[:].rearrange("p (c t) -> p c t", c=8),
                       reads=[PST], writes=[yT])

            with k.scope():
                ong = k.sb("ong", [128, D_])
                k.dma("sp", ong[:], ong_d[l:l + 1, :].to_broadcast([128, D_]), writes=[ong])
                run_streams([gate_stream(0, range(0, 16, 3)), gate_stream(1, range(1, 16, 3)),
                             gate_stream(2, range(2, 16, 3))])
            xin = [k.sb(f"xo{i}", [128, 8, 512]) for i in range(2)]
            if last:
                sqf = k.sb("sqf", [128, 8, 512], BF16)
                rsf = k.sb("rsf", [128, 512])
            for tb in range(4):
                sl = slice(tb * 512, (tb + 1) * 512)
                xi = xin[tb % 2]
                src = xsrc[s].rearrange("(j p) t -> p j t", p=128)[:, :, sl]
                k.dma("sp", xi[:], src, reads=[xsrc], writes=[xi])
                for dj in range(8):
                    ps = PS[(tb * 8 + dj) % 6]
                    for cc in range(8):
                        mm(ps[:], wo[:, cc, dj * 128:(dj + 1) * 128], yT[:, cc, sl], cc == 0, cc == 7,
                           reads=[wo, yT], writes=[ps])
                    tt("dve", xi[:, dj, :], xi[:, dj, :], ps[:], ALU.add, reads=[xi, ps], writes=[xi])
                if not last:
                    dst = xres_d[s].rearrange("(j p) t -> p j t", p=128)[:, :, sl]
                    k.dma("sp", dst, xi[:], reads=[xi], writes=[xres_d])
                else:
                    act(sqf[:], xi[:], AF.Square, reads=[xi], writes=[sqf])
                    ps = psb("m")
                    for j in range(8):
                        mm(ps[:], onesB, sqf[:, j, :], j == 0, j == 7, reads=[constB, sqf], writes=[ps])
                    act(rsf[:], ps[:], AF.Ln, reads=[ps], writes=[rsf], bias=EPS, scale=1.0 / D_)
                    act(rsf[:], rsf[:], AF.Exp, reads=[rsf], writes=[rsf], scale=-0.5)
                    for j in range(8):
                        stt("dve", xi[:, j, :], xi[:, j, :], fnormg[:, j:j + 1], rsf[:], ALU.mult, ALU.mult,
                            reads=[xi, rsf, fnormg], writes=[xi])
                    dst = outT_d[s].rearrange("(j p) t -> p j t", p=128)[:, :, sl]
                    k.dma("sp", dst, xi[:], reads=[xi], writes=[outT_d])

    for s in range(nseq):
        for l in range(nlayer):
            phase_norm(s, l)
            if debug and s == 0 and l == 0 and "hT" in debug:
                d = dbg_out("dbg_hT", [128, 8, T_], BF16)
                k.dma("sp", d[:], hT[:], reads=[hT], writes=[d])
            if "hg" in mixers:
                phase_hgrn(s, l)
            if "fx" in mixers:
                phase_fox(s, l)
            if "sb" in mixers:
                phase_sb(s, l)
            if "ns" in mixers:
                phase_nsa(s, l)
            if debug and s == 0 and l == nlayer - 1 and "ytok" in debug:
                d = dbg_out("dbg_ytok", [128, 16, D_], BF16)
                k.dma("sp", d[:], ytok[:], reads=[ytok], writes=[d])
            if "out" in mixers:
                phase_out(s, l, l == nlayer - 1)

    k.finish(list(dbg_outs.values()) + [outT_d])
    k.barrier()
    k.close()
    return nc, k, list(dbg_outs.keys())


def host_inputs(inputs, core, consts, nseq=SEQ_PER_CORE):
    f32 = np.float32
    x = inputs["x"]
    b0 = core * nseq
    m = {}
    m["xT"] = np.ascontiguousarray(np.transpose(x[b0:b0 + nseq], (0, 2, 1))).astype(f32)
    m["w_in"] = np.ascontiguousarray(inputs["w_in"], dtype=f32)
    m["w_out"] = np.ascontiguousarray(inputs["w_out"], dtype=f32)
    m["norm_gT"] = np.ascontiguousarray(inputs["norm_g"].reshape(2, 8, 128).transpose(0, 2, 1), dtype=f32)
    m["fnorm_gT"] = np.ascontiguousarray(inputs["final_norm_g"].reshape(8, 128).T, dtype=f32)
    m["out_norm_g"] = np.ascontiguousarray(inputs["out_norm_g"], dtype=f32)
    lbl = np.asarray(inputs["hgrn_lb_logits"], dtype=f32)
    m["lb_logits"] = np.ascontiguousarray(lbl)
    m["lb_logitsT"] = np.ascontiguousarray(lbl.reshape(2, 2, 128).transpose(2, 1, 0))
    m["fox_fb"] = np.ascontiguousarray(inputs["fox_fb"], dtype=f32)
    for kv in "kv":
        m[f"peT_{kv}"] = np.ascontiguousarray(np.transpose(inputs[f"nsa_cmp_pe_{kv}"], (0, 2, 1)), dtype=f32)
        m[f"w1_{kv}"] = np.ascontiguousarray(inputs[f"nsa_cmp_w1_{kv}"], dtype=f32)
        m[f"w2_{kv}"] = np.ascontiguousarray(inputs[f"nsa_cmp_w2_{kv}"], dtype=f32)
    for nm in ["constF", "constB", "strips", "strips01", "cmask", "ovl", "eall", "ropeC", "ropeS", "ropeCc", "ropeSc",
               "permF", "tkA", "tkB"]:
        m[nm] = consts[nm]
    return m


def kernel(**inputs):
    inputs = {kk: np.asarray(v) for kk, v in inputs.items()}
    consts = make_consts()
    nc, kb, _ = build()
    in_maps = [host_inputs(inputs, c, consts) for c in range(NCORES)]
    res = run_bass_kernel_spmd(nc, in_maps, core_ids=list(range(NCORES)))
    outs = [np.asarray(r["outT"]) for r in res.results]
    full = np.concatenate(outs, axis=0)
    return np.ascontiguousarray(np.transpose(full, (0, 2, 1))).astype(np.float32)
```
